# Optimizing a Trainium2 kernel written in Bass

```python
import math
import jax
import jax.numpy as jnp
from jax import lax
import numpy as np

D_MODEL = 1024
BATCH = 8
SEQ = 2048
DEPTH = 2

HEAD_DIM = 64
RWKV_HEADS = 4
RWKV_N = 64
RWKV_W = RWKV_HEADS * RWKV_N
RWKV_DECAY_RANK = 64
RWKV_AAA_RANK = 64
RWKV_GATE_RANK = 128
RWKV_LN_EPS = 64e-5
MLSTM_HEADS = 4
MLSTM_DV = 64
MLSTM_DK = 32
MLSTM_W = MLSTM_HEADS * MLSTM_DV
MLSTM_CONV = 4
MLSTM_CHUNK = 64
GATE_SOFTCAP = 15.0
SWA_Q_HEADS = 4
SWA_KV_HEADS = 2
SWA_WINDOW = 128
SWA_W = SWA_Q_HEADS * HEAD_DIM
SWA_KV_W = SWA_KV_HEADS * HEAD_DIM
FOX_HEADS = 4
FOX_W = FOX_HEADS * HEAD_DIM
ATTN_BLOCK = 128
REL_BUCKETS = 32
REL_MAX_DIST = 128
D_FF = 2816
FFN_CONV = 3
NORM_EPS = 1e-6

RWKV_SPLITS = (RWKV_W, RWKV_W, RWKV_W, RWKV_DECAY_RANK, RWKV_AAA_RANK, RWKV_GATE_RANK)
MLSTM_SPLITS = (2 * MLSTM_HEADS * MLSTM_DK, MLSTM_W, MLSTM_W, MLSTM_HEADS, MLSTM_HEADS)
SWA_SPLITS = (SWA_W, SWA_KV_W, SWA_KV_W)
FOX_SPLITS = (FOX_W, FOX_W, FOX_W, FOX_HEADS)
GROUP_SPLITS = (sum(RWKV_SPLITS), sum(MLSTM_SPLITS), sum(SWA_SPLITS), sum(FOX_SPLITS))
N_IN = sum(GROUP_SPLITS)
D_MIX = RWKV_W + MLSTM_W + SWA_W + FOX_W

kernel_name = 'hybrid_parallel_heads_rwkv7_mlstm_swa_fox'


def _split(z, sizes):
    idx = np.cumsum(sizes)[:-1].tolist()
    return jnp.split(z, idx, axis=-1)


def _rmsnorm(x, g):
    xf = x.astype(jnp.float32)
    return xf * lax.rsqrt(jnp.mean(xf * xf, axis=-1, keepdims=True) + NORM_EPS) * g.astype(jnp.float32)


def _token_shift(u):
    return jnp.pad(u, ((0, 0), (1, 0), (0, 0)))[:, :-1]


def _causal_dwconv(u, w, b):
    k = w.shape[0]
    y = lax.conv_general_dilated(u, w[:, None, :].astype(u.dtype), window_strides=(1,),
                                 padding=[(k - 1, 0)], dimension_numbers=('NWC', 'WIO', 'NWC'),
                                 feature_group_count=u.shape[-1])
    return y + b.astype(u.dtype)


def _t5_bucket(dist):
    max_exact = REL_BUCKETS // 2
    d = np.maximum(dist, 1).astype(np.float32)
    large = max_exact + (np.log(d / max_exact) / math.log(REL_MAX_DIST / max_exact)
                         * (REL_BUCKETS - max_exact)).astype(np.int32)
    large = np.minimum(large, REL_BUCKETS - 1)
    return np.where(dist < max_exact, dist, large).astype(np.int32)


def _rwkv7(z, mu, w0, w_up, a0, a_up, g_up, k_k, k_a, r_k, ln_w, ln_b):
    z = z.astype(jnp.float32)
    z = z + mu * (_token_shift(z) - z)
    r, k, v, wd, ad, gd = _split(z, RWKV_SPLITS)
    bsz, t = r.shape[0], r.shape[1]
    log_decay = -jnp.exp(-jax.nn.softplus(-(w0 + jnp.tanh(wd) @ w_up)) - 0.5)
    decay = jnp.exp(log_decay)
    a = jax.nn.sigmoid(a0 + ad @ a_up)
    g = jax.nn.sigmoid(gd) @ g_up
    heads = lambda u: u.reshape(bsz, t, RWKV_HEADS, RWKV_N)
    kk = heads(k * k_k)
    kk = kk / jnp.maximum(jnp.sqrt(jnp.sum(kk * kk, axis=-1, keepdims=True)), 1e-12)
    k = k * (1.0 + (a - 1.0) * k_a)
    r, k, v, decay, a = heads(r), heads(k), heads(v), heads(decay), heads(a)

    def step(s, inp):
        r_t, w_t, k_t, v_t, kk_t, a_t = inp
        sa = jnp.einsum('bhvk,bhk->bhv', s, -kk_t)
        s = (s * w_t[:, :, None, :] + sa[..., None] * (kk_t * a_t)[:, :, None, :]
             + v_t[..., None] * k_t[:, :, None, :])
        return s, jnp.einsum('bhvk,bhk->bhv', s, r_t)

    xs = tuple(jnp.moveaxis(u, 1, 0) for u in (r, decay, k, v, kk, a))
    s0 = jnp.zeros((bsz, RWKV_HEADS, RWKV_N, RWKV_N), jnp.float32)
    _, y = lax.scan(step, s0, xs)
    y = jnp.moveaxis(y, 0, 1)
    mean = jnp.mean(y, axis=-1, keepdims=True)
    var = jnp.mean(jnp.square(y - mean), axis=-1, keepdims=True)
    y = ((y - mean) * lax.rsqrt(var + RWKV_LN_EPS)).reshape(bsz, t, RWKV_W) * ln_w + ln_b
    bonus = jnp.sum(r * k * r_k, axis=-1, keepdims=True) * v
    return (y + bonus.reshape(bsz, t, RWKV_W)) * g


def _mlstm(z, conv_w, conv_b, b_i, b_f, norm_g):
    z = z.astype(jnp.float32)
    qk, v, o, i_pre, f_pre = _split(z, MLSTM_SPLITS)
    qk = jax.nn.silu(_causal_dwconv(qk, conv_w, conv_b))
    q, k = jnp.split(qk, 2, axis=-1)
    cap = lambda u: GATE_SOFTCAP * jnp.tanh(u / GATE_SOFTCAP)
    li = cap(i_pre + b_i)
    lf = jax.nn.log_sigmoid(cap(f_pre + b_f))
    bsz, t = z.shape[0], z.shape[1]
    nc, L = t // MLSTM_CHUNK, MLSTM_CHUNK

    def chunks(u, d):
        return u.reshape(bsz, nc, L, MLSTM_HEADS, d).transpose(1, 0, 3, 2, 4)

    def gchunks(u):
        return u.reshape(bsz, nc, L, MLSTM_HEADS).transpose(1, 0, 3, 2)

    causal = np.tril(np.ones((L, L), dtype=bool))

    def step(carry, inp):
        c_st, n_st, m_st = carry
        qc, kc, vc, lic, lfc = inp
        b = jnp.cumsum(lfc, axis=-1)
        dmat = jnp.where(causal, b[..., :, None] - b[..., None, :] + lic[..., None, :], -jnp.inf)
        m_inter = b + m_st[..., None]
        m_t = jnp.maximum(m_inter, jnp.max(dmat, axis=-1))
        inter = jnp.exp(m_inter - m_t)
        s = jnp.einsum('bhtk,bhsk->bhts', qc, kc) * jnp.exp(dmat - m_t[..., None])
        num = jnp.einsum('bhts,bhsv->bhtv', s, vc) + inter[..., None] * jnp.einsum('bhvk,bhtk->bhtv', c_st, qc)
        den = jnp.sum(s, axis=-1) + inter * jnp.einsum('bhk,bhtk->bht', n_st, qc)
        h = num / jnp.maximum(jnp.abs(den), jnp.exp(-m_t))[..., None]
        b_last = b[..., -1]
        gexp = b_last[..., None] - b + lic
        m_new = jnp.maximum(b_last + m_st, jnp.max(gexp, axis=-1))
        wts = jnp.exp(gexp - m_new[..., None])
        dec = jnp.exp(b_last + m_st - m_new)
        c_new = dec[..., None, None] * c_st + jnp.einsum('bhs,bhsv,bhsk->bhvk', wts, vc, kc)
        n_new = dec[..., None] * n_st + jnp.einsum('bhs,bhsk->bhk', wts, kc)
        return (c_new, n_new, m_new), h

    xs = (chunks(q * MLSTM_DK ** -0.5, MLSTM_DK), chunks(k, MLSTM_DK), chunks(v, MLSTM_DV), gchunks(li), gchunks(lf))
    init = (jnp.zeros((bsz, MLSTM_HEADS, MLSTM_DV, MLSTM_DK), jnp.float32),
            jnp.zeros((bsz, MLSTM_HEADS, MLSTM_DK), jnp.float32),
            jnp.zeros((bsz, MLSTM_HEADS), jnp.float32))
    _, h = lax.scan(step, init, xs)
    h = h.transpose(1, 0, 3, 2, 4).reshape(bsz, t, MLSTM_HEADS, MLSTM_DV)
    h = h * lax.rsqrt(jnp.mean(h * h, axis=-1, keepdims=True) + NORM_EPS)
    return h.reshape(bsz, t, MLSTM_W) * norm_g * jax.nn.sigmoid(o)


def _swa(z, sinks, rel_bias):
    z = z.astype(jnp.float32)
    q, k, v = _split(z, SWA_SPLITS)
    bsz, t = z.shape[0], z.shape[1]
    nb, blk, grp = t // ATTN_BLOCK, ATTN_BLOCK, SWA_Q_HEADS // SWA_KV_HEADS
    qb = q.reshape(bsz, nb, blk, SWA_KV_HEADS, grp, HEAD_DIM)

    def kv_band(u):
        ub = u.reshape(bsz, nb, blk, SWA_KV_HEADS, HEAD_DIM)
        prev = jnp.pad(ub, ((0, 0), (1, 0), (0, 0), (0, 0), (0, 0)))[:, :-1]
        return jnp.concatenate([prev, ub], axis=2)

    kw, vw = kv_band(k), kv_band(v)
    logits = jnp.einsum('bnqhgd,bnkhd->bnhgqk', qb, kw) * HEAD_DIM ** -0.5
    tq = np.arange(blk)[:, None]
    sk = np.arange(2 * blk)[None, :]
    dist = tq + blk - sk
    in_window = (dist >= 0) & (dist < SWA_WINDOW)
    key_pos = np.arange(nb)[:, None, None] * blk - blk + sk[None]
    mask = in_window[None] & (key_pos >= 0)
    bias = rel_bias[_t5_bucket(np.clip(dist, 0, SWA_WINDOW - 1))].astype(jnp.float32)
    bias = jnp.transpose(bias, (2, 0, 1)).reshape(SWA_KV_HEADS, grp, blk, 2 * blk)
    logits = jnp.where(mask[None, :, None, None], logits + bias, -jnp.inf)
    sink = sinks.astype(jnp.float32).reshape(SWA_KV_HEADS, grp)[None, None, :, :, None, None]
    m = jnp.maximum(jnp.max(logits, axis=-1, keepdims=True), sink)
    p = jnp.exp(logits - m)
    denom = jnp.sum(p, axis=-1, keepdims=True) + jnp.exp(sink - m)
    out = jnp.einsum('bnhgqk,bnkhd->bnqhgd', p / denom, vw)
    return out.reshape(bsz, t, SWA_W)


def _fox(z, b_f):
    z = z.astype(jnp.float32)
    q, k, v, f_pre = _split(z, FOX_SPLITS)
    bsz, t = z.shape[0], z.shape[1]
    nb, blk = t // ATTN_BLOCK, ATTN_BLOCK
    k = k.reshape(bsz, t, FOX_HEADS, HEAD_DIM)
    v = v.reshape(bsz, t, FOX_HEADS, HEAD_DIM)
    c = jnp.cumsum(jax.nn.log_sigmoid(f_pre + b_f), axis=1)
    c_keys = c.transpose(0, 2, 1)
    qb = (q * HEAD_DIM ** -0.5).reshape(bsz, nb, blk, FOX_HEADS, HEAD_DIM).transpose(1, 0, 2, 3, 4)
    cqb = c.reshape(bsz, nb, blk, FOX_HEADS).transpose(1, 0, 3, 2)
    qpos = jnp.arange(t).reshape(nb, blk)
    kpos = jnp.arange(t)

    def block(args):
        qi, cqi, pi = args
        s = jnp.einsum('bqhd,bkhd->bhqk', qi, k) + cqi[..., :, None] - c_keys[:, :, None, :]
        s = jnp.where(kpos[None, :] <= pi[:, None], s, -jnp.inf)
        return jnp.einsum('bhqk,bkhd->bqhd', jax.nn.softmax(s, axis=-1), v)

    out = lax.map(block, (qb, cqb, qpos))
    return out.transpose(1, 0, 2, 3, 4).reshape(bsz, t, FOX_W)


def setup_inputs(seed: int = 0) -> dict:
    key = jax.random.key(seed)
    ks = iter(jax.random.split(key, 40))
    L = DEPTH
    nrm = lambda shape, scale: jax.random.normal(next(ks), shape, jnp.float32) * scale
    uni = lambda shape, lo, hi: jax.random.uniform(next(ks), shape, jnp.float32, lo, hi)
    gain = lambda shape: 1.0 + nrm(shape, 0.05)
    return {
        'x': nrm((BATCH, SEQ, D_MODEL), 1.0),
        'w_in': nrm((L, D_MODEL, N_IN), D_MODEL ** -0.5),
        'w_out': nrm((L, D_MIX, D_MODEL), D_MIX ** -0.5),
        'norm_mix_pre': gain((L, D_MODEL)),
        'norm_mix_post': gain((L, D_MODEL)),
        'norm_ffn_pre': gain((L, D_MODEL)),
        'norm_ffn_post': gain((L, D_MODEL)),
        'rwkv_mu': uni((L, GROUP_SPLITS[0]), 0.0, 1.0),
        'rwkv_w0': uni((L, RWKV_W), -6.0, 1.0),
        'rwkv_w_up': nrm((L, RWKV_DECAY_RANK, RWKV_W), 0.5 * RWKV_DECAY_RANK ** -0.5),
        'rwkv_a0': nrm((L, RWKV_W), 0.5),
        'rwkv_a_up': nrm((L, RWKV_AAA_RANK, RWKV_W), 0.5 * RWKV_AAA_RANK ** -0.5),
        'rwkv_g_up': nrm((L, RWKV_GATE_RANK, RWKV_W), RWKV_GATE_RANK ** -0.5),
        'rwkv_k_k': 0.85 + nrm((L, RWKV_W), 0.05),
        'rwkv_k_a': gain((L, RWKV_W)),
        'rwkv_r_k': nrm((L, RWKV_HEADS, RWKV_N), 0.1),
        'rwkv_ln_w': gain((L, RWKV_W)),
        'rwkv_ln_b': nrm((L, RWKV_W), 0.02),
        'mlstm_conv_w': nrm((L, MLSTM_CONV, 2 * MLSTM_HEADS * MLSTM_DK), MLSTM_CONV ** -0.5),
        'mlstm_conv_b': nrm((L, 2 * MLSTM_HEADS * MLSTM_DK), 0.02),
        'mlstm_b_i': nrm((L, MLSTM_HEADS), 0.5),
        'mlstm_b_f': uni((L, MLSTM_HEADS), 3.0, 6.0),
        'mlstm_norm': gain((L, MLSTM_W)),
        'swa_sinks': nrm((L, SWA_Q_HEADS), 0.5),
        'fox_b_f': uni((L, FOX_HEADS), 2.0, 4.0),
        'rel_bias': nrm((REL_BUCKETS, SWA_Q_HEADS), 0.5),
        'ffn_w_up': nrm((L, D_MODEL, 2 * D_FF), D_MODEL ** -0.5),
        'ffn_conv_w': nrm((L, FFN_CONV, D_FF), FFN_CONV ** -0.5),
        'ffn_conv_b': nrm((L, D_FF), 0.02),
        'ffn_w_down': nrm((L, D_FF, D_MODEL), D_FF ** -0.5),
    }


def reference(x, w_in, w_out, norm_mix_pre, norm_mix_post, norm_ffn_pre, norm_ffn_post,
              rwkv_mu, rwkv_w0, rwkv_w_up, rwkv_a0, rwkv_a_up, rwkv_g_up, rwkv_k_k, rwkv_k_a,
              rwkv_r_k, rwkv_ln_w, rwkv_ln_b, mlstm_conv_w, mlstm_conv_b, mlstm_b_i, mlstm_b_f,
              mlstm_norm, swa_sinks, fox_b_f, rel_bias, ffn_w_up, ffn_conv_w, ffn_conv_b, ffn_w_down):
    dt = x.dtype
    for l in range(DEPTH):
        h = _rmsnorm(x, norm_mix_pre[l]).astype(dt)
        z = h @ w_in[l]
        z_a, z_b, z_c, z_d = _split(z, GROUP_SPLITS)
        y_a = _rwkv7(z_a, rwkv_mu[l], rwkv_w0[l], rwkv_w_up[l], rwkv_a0[l], rwkv_a_up[l], rwkv_g_up[l],
                     rwkv_k_k[l], rwkv_k_a[l], rwkv_r_k[l], rwkv_ln_w[l], rwkv_ln_b[l])
        y_b = _mlstm(z_b, mlstm_conv_w[l], mlstm_conv_b[l], mlstm_b_i[l], mlstm_b_f[l], mlstm_norm[l])
        y_c = _swa(z_c, swa_sinks[l], rel_bias)
        y_d = _fox(z_d, fox_b_f[l])
        y = jnp.concatenate([y_a, y_b, y_c, y_d], axis=-1).astype(dt)
        x = x + _rmsnorm(y @ w_out[l], norm_mix_post[l]).astype(dt)
        h = _rmsnorm(x, norm_ffn_pre[l]).astype(dt)
        gate, up = jnp.split(h @ ffn_w_up[l], 2, axis=-1)
        gate = _causal_dwconv(gate, ffn_conv_w[l], ffn_conv_b[l])
        f = jax.nn.gelu(gate, approximate=True) * up
        x = x + _rmsnorm(f @ ffn_w_down[l], norm_ffn_post[l]).astype(dt)
    return x
```

```python
import contextlib
import math
import numpy as np
import concourse.bass as bass
import concourse.mybir as mybir
from concourse.bass_utils import run_bass_kernel_spmd

F32 = mybir.dt.float32
ALU = mybir.AluOpType
AF = mybir.ActivationFunctionType

ENGS = ("pe", "act", "dve", "pool", "sp")


def _flat(ks, out):
    for k in ks:
        if isinstance(k, (str, int)):
            out.append(k)
        elif isinstance(k, tuple) and (len(k) == 0 or isinstance(k[0], (str, int))):
            out.append(k)
        elif hasattr(k, "k"):
            _flat(k.k, out)
        else:
            _flat(k, out)
    return out


class Op:
    __slots__ = ("eng", "fn", "deps", "signals", "count", "pos", "dma", "dsem", "dval", "prewait")

    def __init__(self, eng, fn, dma=False):
        self.eng = eng
        self.fn = fn
        self.deps = []
        self.signals = False
        self.count = None
        self.pos = None
        self.dma = dma
        self.dsem = None
        self.dval = None
        self.prewait = None


class Buf:
    def __init__(self, t, off, ncols, keys):
        self.t, self.off, self.n, self.k = t, off, ncols, keys

    def __getitem__(self, idx):
        p, c = idx
        if isinstance(c, int):
            c = slice(c, c + 1)
        a = 0 if c.start is None else c.start
        b = self.n if c.stop is None else c.stop
        assert 0 <= a <= b <= self.n, (a, b, self.n)
        return self.t[p, self.off + a:self.off + b]

    def v(self, p, pat, **kw):
        return self.t[p, self.off:self.off + self.n].rearrange(pat, **kw)

    def sub(self, c0, n):
        ks = self.k
        if len(ks) > 1 and len(ks) * 512 >= self.n:
            ks = ks[c0 // 512:(c0 + n + 511) // 512]
        return Buf(self.t, self.off + c0, n, ks)


class Bank(Buf):
    def __init__(self, t, name):
        super().__init__(t, 0, 512, [name])
        self.opened = set()
        self.pinned = False


class Prog:
    NDMA = 32

    def __init__(self, nc):
        self.nc = nc
        self.ops = {e: [] for e in ENGS}
        self.lastw = {}
        self.readers = {}
        self.waited = {e: {} for e in ENGS}
        self.waited_dma = {e: set() for e in ENGS}
        self.dma_ops = []
        self.stack = contextlib.ExitStack()
        self.banks = []
        self.bank_i = 0
        self.ar = None
        self.ar_top = 0

    def sb(self, name, ncols, parts=128):
        t = self.stack.enter_context(self.nc.sbuf_tensor("sb_" + name, [parts, ncols], F32))
        return Buf(t, 0, ncols, [name])

    def sbs(self, name, nslots):
        t = self.stack.enter_context(self.nc.sbuf_tensor("sb_" + name, [128, nslots * 512], F32))
        return Buf(t, 0, nslots * 512, [(name, i) for i in range(nslots)])

    def make_banks(self):
        for i in range(8):
            t = self.stack.enter_context(self.nc.psum_tensor(f"bank{i}", [128, 512], F32))
            self.banks.append(Bank(t, f"bank{i}"))

    def bank(self, pin=False):
        for _ in range(16):
            b = self.banks[self.bank_i % 8]
            self.bank_i += 1
            if not b.pinned:
                b.opened = set()
                b.pinned = pin
                return b
        raise RuntimeError("no free psum bank")

    def make_arena(self, nslots):
        self.ar = self.stack.enter_context(self.nc.sbuf_tensor("arena", [128, nslots * 512], F32))
        self.ar_n = nslots

    def alloc(self, ncols=512):
        ns = (ncols + 511) // 512
        assert self.ar_top + ns <= self.ar_n, ("arena overflow", self.ar_top, ns, self.ar_n)
        b = Buf(self.ar, self.ar_top * 512, ncols, [("ar", s) for s in range(self.ar_top, self.ar_top + ns)])
        self.ar_top += ns
        return b

    def add(self, eng, fn, reads=(), writes=(), dma=False):
        op = Op(eng, fn, dma)
        op.pos = len(self.ops[eng])
        reads = _flat(reads, [])
        writes = _flat(writes, [])
        deps = []
        for k in reads:
            w = self.lastw.get(k)
            if w is not None:
                deps.append(w)
        for k in writes:
            w = self.lastw.get(k)
            if w is not None:
                deps.append(w)
            deps.extend(self.readers.get(k, ()))
        wt = self.waited[eng]
        wd = self.waited_dma[eng]
        best = {}
        dm = []
        for d in deps:
            if d is op:
                continue
            if d.dma:
                if id(d) not in wd:
                    wd.add(id(d))
                    dm.append(d)
                continue
            if d.eng == eng and eng in ("pe", "sp"):
                continue
            if wt.get(d.eng, -1) >= d.pos:
                continue
            if d.eng not in best or best[d.eng].pos < d.pos:
                best[d.eng] = d
        op.deps = list(best.values()) + dm
        for d in best.values():
            d.signals = True
            wt[d.eng] = max(wt.get(d.eng, -1), d.pos)
        for k in reads:
            self.readers.setdefault(k, []).append(op)
        for k in writes:
            self.lastw[k] = op
            self.readers[k] = []
        self.ops[eng].append(op)
        if dma:
            self.dma_ops.append(op)
        return op

    def emit(self, out_dma_ops=()):
        nc = self.nc
        nd = self.NDMA
        sems = {e: self.stack.enter_context(nc.semaphore(f"s_{e}")) for e in ENGS if e != "sp"}
        dsems = [self.stack.enter_context(nc.semaphore(f"s_dma{i}")) for i in range(nd)]
        for e in ENGS:
            c = 0
            for op in self.ops[e]:
                if not op.dma and op.signals:
                    c += 1
                    op.count = c
        per_eng = {}
        for op in self.dma_ops:
            per_eng.setdefault(op.eng, []).append(op)
        engs_with_dma = list(per_eng.keys())
        share = nd // max(1, len(engs_with_dma))
        for ei, e in enumerate(engs_with_dma):
            mysems = dsems[ei * share:(ei + 1) * share]
            for i, op in enumerate(per_eng[e]):
                op.dsem = mysems[i % share]
                op.dval = 16 * (i // share + 1)
                if i >= share:
                    op.prewait = (op.dsem, 16 * (i // share))
        final_waits = [(op.dsem, op.dval) for op in out_dma_ops]

        def run(e, eng):
            for op in self.ops[e]:
                if op.prewait is not None:
                    eng.wait_ge(op.prewait[0], op.prewait[1])
                for d in op.deps:
                    if d.dma:
                        eng.wait_ge(d.dsem, d.dval)
                    else:
                        eng.wait_ge(sems[d.eng], d.count)
                ins = op.fn(eng)
                if op.dma:
                    ins.then_inc(op.dsem, 16)
                elif op.signals:
                    ins.then_inc(sems[e], 1)
            if e == "sp":
                for s, v in final_waits:
                    eng.wait_ge(s, v)

        with nc.Block() as block:
            @block.tensor
            def _(eng):
                run("pe", eng)

            @block.scalar
            def _(eng):
                run("act", eng)

            @block.vector
            def _(eng):
                run("dve", eng)

            @block.gpsimd
            def _(eng):
                run("pool", eng)

            @block.sync
            def _(eng):
                run("sp", eng)

    def close(self):
        self.stack.close()

    def dma(self, out, in_, reads=(), writes=(), eng="sp"):
        return self.add(eng, lambda e: e.dma_start(out=out, in_=in_), reads, writes, dma=True)

    def mm(self, bank, out, lhsT, rhs, reads=()):
        p0 = out.start_partition()
        q = frozenset(range(p0 // 32, (p0 + out.partition_size() - 1) // 32 + 1))
        c0 = out.offset % 512 if hasattr(out, "offset") else 0
        c0 = self._ap_col0(out)
        c1 = c0 + self._ap_ncols(out)
        start = True
        for (qq, a, b) in bank.opened:
            if (qq & q) and a < c1 and c0 < b:
                assert q <= qq and a <= c0 and c1 <= b, ("partial psum overlap", q, qq, c0, c1, a, b)
                start = False
        if start:
            bank.opened.add((q, c0, c1))
        return self.add("pe", lambda e: e.matmul(out, lhsT, rhs, start=start, stop=True, skip_group_check=True),
                        reads, [bank])

    @staticmethod
    def _ap_col0(ap):
        pstride = ap.ap[0][0]
        return ap.offset % pstride

    @staticmethod
    def _ap_ncols(ap):
        span = 0
        for st, n in list(ap.ap)[1:]:
            span += st * (n - 1)
        return span + 1

    def tr(self, bank, out, in_, ident, reads=()):
        return self.add("pe", lambda e: e.transpose(out, in_, ident), reads, [bank])

    def act(self, out, in_, func, bias=None, scale=1.0, reads=(), writes=()):
        if bias is None:
            return self.add("act", lambda e: e.activation(out, in_, func, scale=scale), reads, writes)
        return self.add("act", lambda e: e.activation(out, in_, func, bias=bias, scale=scale), reads, writes)

    def tt(self, out, a, b, op, reads=(), writes=(), eng="dve"):
        return self.add(eng, lambda e: e.tensor_tensor(out, a, b, op), reads, writes)

    def ts(self, out, a, s1, op0, s2=None, op1=None, reads=(), writes=(), eng="dve"):
        if op1 is None:
            return self.add(eng, lambda e: e.tensor_scalar(out, a, s1, None, op0), reads, writes)
        return self.add(eng, lambda e: e.tensor_scalar(out, a, s1, s2, op0, op1), reads, writes)

    def stt(self, out, a, s, b, op0, op1, reads=(), writes=()):
        return self.add("dve", lambda e: e.scalar_tensor_tensor(out, a, s, b, op0, op1), reads, writes)

    def cp(self, out, a, reads=(), writes=(), eng="dve"):
        if eng == "act":
            return self.add(eng, lambda e: e.copy(out, a), reads, writes)
        return self.add(eng, lambda e: e.tensor_copy(out, a), reads, writes)

    def recip(self, out, a, reads=(), writes=()):
        return self.add("dve", lambda e: e.reciprocal(out, a), reads, writes)

    def scan(self, out, d0, d1, init, op0, op1, reads=(), writes=()):
        return self.add("dve", lambda e: e.tensor_tensor_scan(out, d0, d1, init, op0, op1), reads, writes)

    def memset(self, ap, v, writes=(), eng="dve"):
        return self.add(eng, lambda e: e.memset(ap, v), (), writes)


D = 1024
SEQ = 2048
DEPTH = 2
TB = 512
RW_STOP = 0
N_IN = 3084
DFF = 2816
NFC = DFF // 128

PC_GPRE, PC_GPOST, PC_FPRE, PC_FPOST, PC_MU = 0, 8, 16, 24, 32
PC_W0, PC_A0, PC_KK, PC_KA, PC_RK, PC_LNW, PC_LNB, PC_MNORM, PC_SINK = 40, 42, 44, 46, 48, 50, 52, 54, 56
PC_CW, PC_CB, PC_BI, PC_BF, PC_FOXB, PC_FCW, PC_FCB, NPC = 58, 74, 78, 79, 80, 81, 147, 169
DV_OMKA, DV_BI15, DV_BF15, DV_NFOXB, DV_ESINK, NDV = 0, 2, 3, 4, 5, 8

C_IDENT, C_ONESD, C_ALLONE, C_BLK1, C_BLKM, C_TRI = 0, 128, 256, 384, 512, 640
C_MA, C_MB, C_IDG, C_MM, C_SELF, C_SELK, C_SELV, C_HM, C_SWAM, NCST = 768, 1280, 1408, 1536, 1792, 2304, 2432, 2688, 2696, 3720


def _t5_bucket(dist):
    max_exact = 16
    d = np.maximum(dist, 1).astype(np.float32)
    large = max_exact + (np.log(d / max_exact) / math.log(128 / max_exact) * (32 - max_exact)).astype(np.int32)
    large = np.minimum(large, 31)
    return np.where(dist < max_exact, dist, large).astype(np.int32)


def _swa_dist():
    s = np.arange(128)[:, None, None]
    pcx = np.arange(2)[None, :, None]
    tq = np.arange(128)[None, None, :]
    sk = s + 128 * pcx
    dist = tq + 128 - sk
    vis = (dist >= 0) & (dist < 128)
    return dist, vis


def make_consts():
    c = np.zeros((128, NCST), np.float32)
    c[:, C_IDENT:C_IDENT + 128] = np.eye(128)
    c[:, C_ONESD:C_ONESD + 128] = 1.0 / 1024
    c[:, C_ALLONE:C_ALLONE + 128] = 1.0
    blk = np.zeros((128, 128), np.float32)
    blk[:64, :64] = 1
    blk[64:, 64:] = 1
    c[:, C_BLK1:C_BLK1 + 128] = blk
    c[:, C_BLKM:C_BLKM + 128] = blk / 64.0
    k = np.arange(128)
    c[:, C_TRI:C_TRI + 128] = (k[:, None] <= k[None, :])
    i = np.arange(64)[:, None]
    t = np.arange(64)[None, :]
    ma = np.zeros((64, 2, 2, 2, 64), np.float32)
    ma[:, :, :, 0, :] = (i < t)[:, None, None, :]
    ma[:, :, :, 1, :] = (i <= t)[:, None, None, :]
    c[:64, C_MA:C_MA + 512] = ma.reshape(64, 512)
    mb = np.zeros((64, 2, 64), np.float32)
    mb[:] = (i > t)[:, None, :]
    c[:64, C_MB:C_MB + 128] = mb.reshape(64, 128)
    idg = np.zeros((64, 2, 64), np.float32)
    idg[:] = np.eye(64)[:, None, :]
    c[:64, C_IDG:C_IDG + 128] = idg.reshape(64, 128)
    mm = np.zeros((64, 4, 64), np.float32)
    mm[:] = (i <= t)[:, None, :]
    c[:64, C_MM:C_MM + 256] = mm.reshape(64, 256)
    sf = np.zeros((4, 4, 128), np.float32)
    for h in range(4):
        sf[h, h, :] = 1
    c[:4, C_SELF:C_SELF + 512] = sf.reshape(4, 512)
    c[64:68, C_SELF:C_SELF + 512] = sf.reshape(4, 512)
    sk = np.zeros((4, 2, 64), np.float32)
    sv = np.zeros((4, 2, 128), np.float32)
    for h in range(4):
        for j in range(2):
            sk[h, j, :] = (h == 2 * j + np.arange(64) // 32)
            sv[h, j, :] = (h == 2 * j + np.arange(128) // 64)
    c[:4, C_SELK:C_SELK + 128] = sk.reshape(4, 128)
    c[:4, C_SELV:C_SELV + 256] = sv.reshape(4, 256)
    c[0:32, C_HM] = 1.0
    c[32:64, C_HM + 1] = 1.0
    _, vis = _swa_dist()
    m = np.where(vis, 0.0, -30000.0).astype(np.float32)
    m4 = np.broadcast_to(m[:, :, None, :], (128, 2, 4, 128))
    c[:, C_SWAM:C_SWAM + 1024] = m4.reshape(128, 1024)
    return c


def _cols(v, n):
    return np.ascontiguousarray(np.asarray(v, np.float32).reshape(n, 128).T)


def prep_inputs(inp):
    L = DEPTH
    f = lambda k: np.asarray(inp[k], np.float32)
    w_in_r = np.ascontiguousarray(f("w_in").reshape(L, 8, 128, N_IN).transpose(0, 2, 1, 3))
    w_out_r = np.ascontiguousarray(f("w_out").reshape(L, 8, 128, D).transpose(0, 2, 1, 3))
    w_up_r = np.ascontiguousarray(f("ffn_w_up").reshape(L, 8, 128, 2 * DFF).transpose(0, 2, 1, 3))
    w_dn_r = np.ascontiguousarray(f("ffn_w_down").reshape(L, NFC, 128, D).transpose(0, 2, 1, 3))
    lora = np.zeros((L, 128, 512), np.float32)
    lora[:, 0:64, 0:256] = f("rwkv_w_up")
    lora[:, 64:128, 0:256] = f("rwkv_a_up")
    lora[:, :, 256:512] = f("rwkv_g_up")
    pc = np.zeros((L, 128, NPC), np.float32)
    for l in range(L):
        pc[l, :, PC_GPRE:PC_GPRE + 8] = _cols(f("norm_mix_pre")[l], 8)
        pc[l, :, PC_GPOST:PC_GPOST + 8] = _cols(f("norm_mix_post")[l], 8)
        pc[l, :, PC_FPRE:PC_FPRE + 8] = _cols(f("norm_ffn_pre")[l], 8)
        pc[l, :, PC_FPOST:PC_FPOST + 8] = _cols(f("norm_ffn_post")[l], 8)
        pc[l, :, PC_MU:PC_MU + 8] = _cols(f("rwkv_mu")[l], 8)
        for col, key in ((PC_W0, "rwkv_w0"), (PC_A0, "rwkv_a0"), (PC_KK, "rwkv_k_k"), (PC_KA, "rwkv_k_a"),
                         (PC_LNW, "rwkv_ln_w"), (PC_LNB, "rwkv_ln_b"), (PC_MNORM, "mlstm_norm")):
            pc[l, :, col:col + 2] = _cols(f(key)[l], 2)
        pc[l, :, PC_RK:PC_RK + 2] = _cols(f("rwkv_r_k")[l].reshape(256), 2)
        pc[l, :, PC_SINK:PC_SINK + 2] = _cols(np.repeat(f("swa_sinks")[l], 64), 2)
        cw = f("mlstm_conv_w")[l]
        for i in range(4):
            for j in range(4):
                pc[l, 0:64, PC_CW + i * 4 + j] = cw[j, i * 64:(i + 1) * 64]
            pc[l, 0:64, PC_CB + i] = f("mlstm_conv_b")[l][i * 64:(i + 1) * 64]
        pc[l, 0:4, PC_BI] = f("mlstm_b_i")[l]
        pc[l, 0:4, PC_BF] = f("mlstm_b_f")[l]
        pc[l, 0:4, PC_FOXB] = f("fox_b_f")[l]
        pc[l, 64:68, PC_FOXB] = f("fox_b_f")[l]
        fw = f("ffn_conv_w")[l]
        for j in range(3):
            pc[l, :, PC_FCW + j:PC_FCW + 66:3] = _cols(fw[j], NFC)
        pc[l, :, PC_FCB:PC_FCB + NFC] = _cols(f("ffn_conv_b")[l], NFC)
    dist, _ = _swa_dist()
    bk = _t5_bucket(np.clip(dist, 0, 127))
    swab = f("rel_bias")[bk]
    swab = np.ascontiguousarray(swab.transpose(0, 1, 3, 2)).reshape(128, 1024)
    cst = make_consts()
    x = f("x")
    maps = []
    for b in range(x.shape[0]):
        xT = np.ascontiguousarray(x[b].T.reshape(8, 128, x.shape[1]).transpose(1, 0, 2))
        maps.append({"xT": xT, "w_in_r": w_in_r, "w_out_r": w_out_r, "w_up_r": w_up_r, "w_dn_r": w_dn_r,
                     "lora": lora, "pc": pc, "cst": cst, "swab": swab})
    return maps


ARENA_SLOTS = 26


def build(nlayer=DEPTH, nblk=SEQ // TB, dbg=None, phases="rmsfon"):
    nc = bass.Bass("TRN2", target_bir_lowering=False)
    T = nblk * TB
    NT = T // 128
    L = DEPTH
    dr = lambda n, s, kind="ExternalInput": nc.dram_tensor(n, list(s), F32, kind=kind).ap()
    xT_d = dr("xT", [128, 8, SEQ])
    w_in_d = dr("w_in_r", [L, 128, 8, N_IN])
    w_out_d = dr("w_out_r", [L, 128, 8, D])
    w_up_d = dr("w_up_r", [L, 128, 8, 2 * DFF])
    w_dn_d = dr("w_dn_r", [L, 128, NFC, D])
    lora_d = dr("lora", [L, 128, 512])
    pc_d = dr("pc", [L, 128, NPC])
    cst_d = dr("cst", [128, NCST])
    swab_d = dr("swab", [128, 1024])
    out_d = dr("outT", [128, 8, SEQ], "ExternalOutput")
    dbg_d = dr("dbg", [128, 8, SEQ], "ExternalOutput") if dbg else None

    P = Prog(nc)
    P.make_banks()
    xT = P.sb("xT", 8 * T)
    kTh = P.sb("kTh", 2 * T)
    Vh = P.sb("Vh", NT * 256)
    negc = P.sb("negc", NT * 4)
    yT = P.sbs("yT", 8)
    cst = P.sb("cst", C_SWAM)
    pc = P.sb("pc", NPC)
    lora = P.sb("lora", 512)
    swaB = P.sb("swaB", 1024)
    dv = P.sb("dv", NDV)
    rstm = P.sb("rstm", 4)
    Srw = P.sb("Srw", 2 * 2 * 2 * 64)
    pc1 = P.sb("pc1", 8)
    CN = P.sb("CN", 2 * 128)
    crw = P.sb("crw", 8)
    cml = P.sb("cml", 12)
    cffn = P.sb("cffn", NFC * 2)
    ccar = P.sb("ccar", 2)
    swaK = P.sb("swaK", 2 * 640)
    swaV = P.sb("swaV", 5 * 128)
    ebks = P.sb("ebks", 16)
    vones = P.sb("vones", 4 * 128)
    wbufs = [P.sb(f"wch{i}", 8 * 128) for i in range(3)]
    P.make_arena(ARENA_SLOTS)

    A = slice(0, 128)
    H0 = slice(0, 64)
    H1 = slice(64, 128)
    R4 = slice(0, 4)
    xT3 = xT.v(A, "p (c t) -> p c t", c=8)
    yT3 = yT.v(A, "p (c t) -> p c t", c=8)
    kTh3 = kTh.v(A, "p (j t) -> p j t", j=2)
    Vh3 = Vh.v(A, "p (n c) -> p n c", c=256)
    negc3 = negc.v(A, "p (n h) -> p n h", h=4)
    swaK3 = swaK.v(A, "p (j t) -> p j t", j=2)
    swaV3 = swaV.v(A, "p (n c) -> p n c", n=5)
    swaB4 = swaB.v(A, "p (c h q) -> p c h q", c=2, h=4)
    vo3 = vones.v(H0, "p (h c) -> p h c", h=4)
    CN3 = CN.v(H0, "p (j c) -> p j c", j=2)
    S5 = Srw.v(H0, "p (j h b v) -> p j h b v", j=2, h=2, b=2)

    def C(off, n, p=A):
        return cst[p, off:off + n]

    ident = C(C_IDENT, 128)
    evac_i = [0]

    def copy(out, in_, reads, writes, eng=None):
        if eng is None:
            evac_i[0] += 1
            eng = "act" if evac_i[0] % 2 else "dve"
        return P.cp(out, in_, reads, writes, eng=eng)

    wb_i = [0]

    def wchunk():
        b = wbufs[wb_i[0] % 3]
        wb_i[0] += 1
        return b

    P.dma(cst[A, :], cst_d[:, 0:C_SWAM], writes=[cst])
    P.dma(swaB[A, :], swab_d, writes=[swaB])
    mtmp = P.alloc(1024)
    P.dma(mtmp[A, :], cst_d[:, C_SWAM:C_SWAM + 1024], writes=[mtmp])
    for c in range(8):
        P.dma(xT3[:, c, :], xT_d[:, c, 0:T], writes=[xT])
    P.tt(swaB[A, :], swaB[A, :], mtmp[A, :], ALU.add, reads=[swaB, mtmp], writes=[swaB])
    P.memset(vones[H0, :], 1.0, writes=[vones])
    P.memset(yT[A, :], 0.0, writes=[yT])
    P.ar_top = 0

    def rms_rstd(src_fn, reads, eps=1e-6):
        ssb = P.bank(pin=True)
        rstd = P.alloc()
        sq = [P.alloc(), P.alloc()]
        for c in range(8):
            s = sq[c % 2]
            P.act(s[A, :], src_fn(c), AF.Square, reads=reads, writes=[s])
            P.mm(ssb, ssb[A, :], C(C_ONESD, 128), s[A, :], reads=[cst, s])
        P.ts(rstd[A, :], ssb[A, :], eps, ALU.add, reads=[ssb], writes=[rstd])
        ssb.pinned = False
        P.recip(rstd[A, :], rstd[A, :], reads=[rstd], writes=[rstd])
        P.act(rstd[A, :], rstd[A, :], AF.Sqrt, reads=[rstd], writes=[rstd])
        P.ar_top -= 2
        return rstd

    for l in range(nlayer):
        P.dma(pc[A, :], pc_d[l], writes=[pc])
        P.dma(lora[A, :], lora_d[l], writes=[lora])
        P.ts(dv[A, DV_OMKA:DV_OMKA + 2], pc[A, PC_KA:PC_KA + 2], -1.0, ALU.mult, 1.0, ALU.add, reads=[pc], writes=[dv])
        P.ts(dv[R4, DV_BI15:DV_BI15 + 2], pc[R4, PC_BI:PC_BI + 2], 1.0 / 15.0, ALU.mult, reads=[pc], writes=[dv])
        P.ts(dv[A, DV_NFOXB:DV_NFOXB + 1], pc[A, PC_FOXB:PC_FOXB + 1], -1.0, ALU.mult, reads=[pc], writes=[dv])
        P.act(dv[A, DV_ESINK:DV_ESINK + 2], pc[A, PC_SINK:PC_SINK + 2], AF.Exp, reads=[pc], writes=[dv])
        for b_ in (Srw, CN, crw, cml, cffn, ccar, swaK, swaV):
            P.memset(b_[A, :], 0.0, writes=[b_])

        for blk in range(nblk):
            t0 = blk * TB
            bs = slice(t0, t0 + TB)
            P.ar_top = 0
            rstd = rms_rstd(lambda c: xT3[:, c, bs], [xT])
            bk = P.bank()
            for tt_ in range(4):
                P.tr(bk, bk[A, tt_ * 128:(tt_ + 1) * 128], rstd[A, tt_ * 128:(tt_ + 1) * 128], ident, reads=[rstd, cst])
            P.cp(rstm[A, 0:4], bk.v(A, "p (n c) -> p n c", n=4)[:, :, 0], reads=[bk], writes=[rstm])
            MIX_BASE = P.ar_top
            gpre = pc[A, PC_GPRE:PC_GPRE + 8]

            def load_w(col0, M, dup=False):
                w = wchunk()
                w3 = w.v(A, "p (c m) -> p c m", c=8)
                if dup:
                    P.dma(w3[:, :, 0:64], w_in_d[l, :, :, col0:col0 + 64], writes=[w])
                    P.dma(w3[:, :, 64:128], w_in_d[l, :, :, col0:col0 + 64], writes=[w])
                    M = 128
                else:
                    P.dma(w3[:, :, 0:M], w_in_d[l, :, :, col0:col0 + M], writes=[w])
                P.tt(w3[:, :, 0:M], w3[:, :, 0:M], gpre.to_broadcast([128, 8, M]), ALU.mult, reads=[w, pc], writes=[w])
                return w, w3, M

            def proj_fm(col0, M, dst, dup=False, scale=None, rows=None, pbase=0):
                w, w3, M = load_w(col0, M, dup)
                bk = P.bank()
                r = slice(pbase, pbase + M)
                for c in range(8):
                    P.mm(bk, bk[r, :], w3[:, c, 0:M], xT3[:, c, bs], reads=[w, xT])
                if scale is None:
                    P.tt(dst, bk[r, :], rstd[r, :], ALU.mult, reads=[bk, rstd], writes=[rows])
                else:
                    P.stt(dst, bk[r, :], scale, rstd[r, :], ALU.mult, ALU.mult, reads=[bk, rstd], writes=[rows])

            def proj_tm(col0, ncols, dst_fn, wr):
                w, w3, _ = load_w(col0, ncols)
                for tt_ in range(4):
                    bk = P.bank()
                    for c in range(8):
                        P.mm(bk, bk[A, 0:ncols], xT3[:, c, t0 + tt_ * 128:t0 + (tt_ + 1) * 128], w3[:, c, 0:ncols], reads=[w, xT])
                    P.ts(dst_fn(tt_), bk[A, 0:ncols], rstm[A, tt_:tt_ + 1], ALU.mult, reads=[bk, rstm], writes=[wr])

            def shift_mix(z, ci, dst):
                d = P.alloc()
                P.tt(d[A, 1:512], z[A, 0:511], z[A, 1:512], ALU.subtract, reads=[z], writes=[d])
                P.tt(d[A, 0:1], crw[A, ci:ci + 1], z[A, 0:1], ALU.subtract, reads=[z, crw], writes=[d])
                P.cp(crw[A, ci:ci + 1], z[A, 511:512], reads=[z], writes=[crw], eng="pool")
                P.stt(dst[A, :], d[A, :], pc[A, PC_MU + ci:PC_MU + ci + 1], z[A, :], ALU.mult, ALU.add,
                      reads=[d, pc, z], writes=[dst])
                P.ar_top -= 1

            def rwkv():
                swa_ = P.alloc()
                sg = P.alloc()
                ztmp = P.alloc()
                proj_fm(768, 128, ztmp[A, :], rows=ztmp)
                shift_mix(ztmp, 6, swa_)
                proj_fm(896, 128, ztmp[A, :], rows=ztmp)
                shift_mix(ztmp, 7, sg)
                P.ar_top -= 1
                P.act(swa_[H0, :], swa_[H0, :], AF.Tanh, reads=[swa_], writes=[swa_])
                P.act(sg[A, :], sg[A, :], AF.Sigmoid, reads=[sg], writes=[sg])
                if RW_STOP == 1:
                    return
                base_top = P.ar_top
                for hp in range(2):
                    P.ar_top = base_top
                    z = [P.alloc() for _ in range(3)]
                    s = [P.alloc() for _ in range(3)]
                    for i in range(3):
                        proj_fm(i * 256 + hp * 128, 128, z[i][A, :], rows=z[i])
                    for i in range(3):
                        shift_mix(z[i], i * 2 + hp, s[i])
                    sr, sk, sv = s
                    cw = slice(hp * 128, (hp + 1) * 128)
                    pcc = lambda c0: pc[A, c0 + hp:c0 + hp + 1]
                    bk = P.bank()
                    P.mm(bk, bk[A, :], lora[H0, cw], swa_[H0, :], reads=[lora, swa_])
                    lw = z[0]
                    P.act(lw[A, :], bk[A, :], AF.Sigmoid, bias=pcc(PC_W0), reads=[bk, pc], writes=[lw])
                    P.ts(lw[A, :], lw[A, :], -math.exp(-0.5), ALU.mult, reads=[lw], writes=[lw])
                    bk = P.bank()
                    P.mm(bk, bk[A, :], lora[H1, cw], swa_[H1, :], reads=[lora, swa_])
                    a = z[1]
                    P.act(a[A, :], bk[A, :], AF.Sigmoid, bias=pcc(PC_A0), reads=[bk, pc], writes=[a])
                    bk = P.bank()
                    P.mm(bk, bk[A, :], lora[A, 256 + hp * 128:256 + (hp + 1) * 128], sg[A, :], reads=[lora, sg])
                    g = z[2]
                    copy(g[A, :], bk[A, :], [bk], [g])
                    kk = P.alloc()
                    tmp = P.alloc()
                    P.ts(kk[A, :], sk[A, :], pcc(PC_KK), ALU.mult, reads=[sk, pc], writes=[kk])
                    P.tt(tmp[A, :], kk[A, :], kk[A, :], ALU.mult, reads=[kk], writes=[tmp])
                    bk = P.bank()
                    P.mm(bk, bk[A, :], C(C_BLK1, 128), tmp[A, :], reads=[cst, tmp])
                    P.act(tmp[A, :], bk[A, :], AF.Sqrt, reads=[bk], writes=[tmp])
                    P.ts(tmp[A, :], tmp[A, :], 1e-12, ALU.max, reads=[tmp], writes=[tmp])
                    P.recip(tmp[A, :], tmp[A, :], reads=[tmp], writes=[tmp])
                    P.tt(kk[A, :], kk[A, :], tmp[A, :], ALU.mult, reads=[kk, tmp], writes=[kk])
                    kmod = P.alloc()
                    P.ts(kmod[A, :], a[A, :], pcc(PC_KA), ALU.mult, dv[A, DV_OMKA + hp:DV_OMKA + hp + 1], ALU.add,
                         reads=[a, pc, dv], writes=[kmod])
                    P.tt(kmod[A, :], kmod[A, :], sk[A, :], ALU.mult, reads=[kmod, sk], writes=[kmod])
                    bonus = sk
                    P.stt(tmp[A, :], sr[A, :], pcc(PC_RK), kmod[A, :], ALU.mult, ALU.mult, reads=[sr, pc, kmod], writes=[tmp])
                    bk = P.bank()
                    P.mm(bk, bk[A, :], C(C_BLK1, 128), tmp[A, :], reads=[cst, tmp])
                    P.tt(bonus[A, :], bk[A, :], sv[A, :], ALU.mult, reads=[bk, sv], writes=[bonus])
                    bb = a
                    P.tt(bb[A, :], kk[A, :], a[A, :], ALU.mult, reads=[kk, a], writes=[bb])
                    cum = P.alloc()
                    ones = C(C_ALLONE, 64)
                    for c in range(8):
                        cs = slice(c * 64, (c + 1) * 64)
                        P.scan(cum[A, cs], ones, lw[A, cs], 0.0, ALU.mult, ALU.add, reads=[cst, lw], writes=[cum])
                    cumx = lw
                    P.tt(cumx[A, :], cum[A, :], lw[A, :], ALU.subtract, reads=[cum, lw], writes=[cumx])
                    Ep = P.alloc()
                    Em = P.alloc()
                    Ee = P.alloc()
                    P.act(Ep[A, :], cum[A, :], AF.Exp, reads=[cum], writes=[Ep])
                    P.act(Em[A, :], cum[A, :], AF.Exp, scale=-1.0, reads=[cum], writes=[Em])
                    P.act(cumx[A, :], cumx[A, :], AF.Exp, reads=[cumx], writes=[cumx])
                    for c in range(8):
                        cs = slice(c * 64, (c + 1) * 64)
                        P.act(Ee[A, cs], cum[A, cs], AF.Exp, bias=cum[A, c * 64 + 63:c * 64 + 64], scale=-1.0,
                              reads=[cum], writes=[Ee])
                    AR = P.alloc(1024)
                    AR3 = AR.v(A, "p (a t) -> p a t", a=2)
                    P.stt(AR3[:, 0, :], kk[A, :], -1.0, cumx[A, :], ALU.mult, ALU.mult, reads=[kk, cumx], writes=[AR])
                    P.tt(AR3[:, 1, :], sr[A, :], Ep[A, :], ALU.mult, reads=[sr, Ep], writes=[AR])
                    Bt, Kt, Bte, Kte = cum, tmp, kk, kmod
                    P.tt(Bt[A, :], bb[A, :], Em[A, :], ALU.mult, reads=[bb, Em], writes=[Bt])
                    P.tt(Kt[A, :], kmod[A, :], Em[A, :], ALU.mult, reads=[kmod, Em], writes=[Kt])
                    P.tt(Bte[A, :], bb[A, :], Ee[A, :], ALU.mult, reads=[bb, Ee], writes=[Bte])
                    P.tt(Kte[A, :], kmod[A, :], Ee[A, :], ALU.mult, reads=[kmod, Ee], writes=[Kte])
                    yr = Em
                    AR1 = Buf(z[0].t, z[0].off, 1024, z[0].k + z[1].k)
                    AR13 = AR1.v(A, "p (a t) -> p a t", a=2)
                    Bt1, Kt1 = sr, P.alloc()
                    idn1 = cst[H1, C_IDENT + 64:C_IDENT + 128]
                    for src, dst, wr in ((AR3[H1, 0, :], AR13[H0, 0, :], AR1), (AR3[H1, 1, :], AR13[H0, 1, :], AR1),
                                         (Bt[H1, :], Bt1[H0, :], Bt1), (Kt[H1, :], Kt1[H0, :], Kt1)):
                        bk = P.bank()
                        P.mm(bk, bk[H0, :], idn1, src, reads=[cst, AR, Bt, Kt])
                        copy(dst, bk[H0, :], [bk], [wr])
                    bk = P.bank()
                    P.mm(bk, bk[H0, 0:8], idn1, Ep.v(H1, "p (c t) -> p c t", c=8)[:, :, 63], reads=[cst, Ep])
                    copy(pc1[H0, 0:8], bk[H0, 0:8], [bk], [pc1])
                    ARh = (AR3, AR13)
                    Bth = (Bt, Bt1)
                    Kth = (Kt, Kt1)
                    ARb = (AR, AR1)
                    if RW_STOP == 2:
                        continue
                    NA = [P.alloc() for _ in range(2)]
                    nmslot = P.alloc()
                    NMp = [nmslot.sub(0, 256), nmslot.sub(256, 256)]
                    gslot = P.alloc()
                    Gp = [gslot.sub(0, 128), gslot.sub(128, 128)]
                    RU = [gslot.sub(256, 256), Ee.sub(0, 256)]
                    TM = [P.alloc() for _ in range(2)]
                    for c in range(8):
                        if RW_STOP == 6:
                            continue
                        cs = slice(c * 64, (c + 1) * 64)
                        na = NA[c % 2]
                        na5 = na.v(H0, "p (h b a t) -> p h b a t", h=2, b=2, a=2)
                        bk = P.bank()
                        bk5 = bk.v(H0, "p (h b a t) -> p h b a t", h=2, b=2, a=2)
                        bkm = P.bank()
                        bkm3 = bkm.v(H0, "p (x h t) -> p x h t", x=4, h=2)
                        for h2 in range(1 if RW_STOP == 7 else 2):
                            ph = slice(h2 * 64, h2 * 64 + 64)
                            for a_ in range(2):
                                P.mm(bk, bk5[:, h2, 0, a_, :], Bth[h2][H0, cs], ARh[h2][H0, a_, cs], reads=[Bth[h2], ARb[h2]])
                                P.mm(bk, bk5[:, h2, 1, a_, :], Kth[h2][H0, cs], ARh[h2][H0, a_, cs], reads=[Kth[h2], ARb[h2]])
                            P.mm(bkm, bkm3[:, 0, h2, :], ARh[h2][H0, 0, cs], Bth[h2][H0, cs], reads=[Bth[h2], ARb[h2]])
                        P.tt(na[H0, :], bk[H0, :], C(C_MA, 512, H0), ALU.mult, reads=[bk, cst], writes=[na])
                        if RW_STOP in (3, 7):
                            continue
                        nm = NMp[0]
                        nm4 = nm.v(H0, "p (x h t) -> p x h t", x=2, h=2)
                        P.cp(nm4[:, 0, :, :], na5[:, :, 0, 0, :], reads=[na], writes=[nm], eng="pool")
                        P.tt(nm4[:, 1, :, :], bkm3[:, 0, :, :], C(C_MB, 128, H0).rearrange("p (h t) -> p h t", h=2),
                             ALU.mult, reads=[bkm, cst], writes=[nm])
                        G3_0 = Gp[0].v(H0, "p (h t) -> p h t", h=2)
                        P.tt(G3_0, na5[:, :, 0, 0, :], C(C_IDG, 128, H0).rearrange("p (h t) -> p h t", h=2), ALU.add,
                             reads=[na, cst], writes=[Gp[0]])
                        cur = 0
                        for lev in range(5):
                            nmc = NMp[cur]
                            nmc4 = nmc.v(H0, "p (x h t) -> p x h t", x=2, h=2)
                            nmn = NMp[1 - cur]
                            nmn4 = nmn.v(H0, "p (x h t) -> p x h t", x=2, h=2)
                            Gc = Gp[cur]
                            Gc3 = Gc.v(H0, "p (h t) -> p h t", h=2)
                            Gn = Gp[1 - cur]
                            bkp = P.bank()
                            bkp4 = bkp.v(H0, "p (x h t) -> p x h t", x=4, h=2)
                            for h2 in range(2):
                                P.mm(bkp, bkp4[:, 0, h2, :], nmc4[:, 1, h2, :], nmc4[:, 0, h2, :], reads=[nmc])
                                P.mm(bkp, bkp4[:, 1, h2, :], nmc4[:, 0, h2, :], nmc4[:, 1, h2, :], reads=[nmc])
                            copy(nmn[H0, :], bkp[H0, 0:256], [bkp], [nmn])
                            bkg = P.bank()
                            for h2 in range(2):
                                P.mm(bkg, bkg[H0, h2 * 64:(h2 + 1) * 64], nmn4[:, 1, h2, :], Gc3[:, h2, :], reads=[nmn, Gc])
                            P.tt(Gn[H0, :], bkg[H0, 0:128], Gc[H0, :], ALU.add, reads=[bkg, Gc], writes=[Gn])
                            cur = 1 - cur
                        Gf = Gp[cur]
                        Gf3 = Gf.v(H0, "p (h t) -> p h t", h=2)
                        if RW_STOP == 4:
                            continue
                        tm = TM[c % 2]
                        bkt = P.bank()
                        P.tr(bkt, bkt[H0, 0:128], Bte[A, cs], ident, reads=[Bte, cst])
                        P.tr(bkt, bkt[H0, 128:256], Kte[A, cs], ident, reads=[Kte, cst])
                        P.tr(bkt, bkt[H0, 256:384], sv[A, cs], ident, reads=[sv, cst])
                        copy(tm[H0, 0:384], bkt[H0, 0:384], [bkt], [tm])
                        if RW_STOP == 5:
                            continue
                        ru = RU[c % 2]
                        sb_old = c % 2
                        sb_new = 1 - sb_old
                        bkr = P.bank()
                        for h2 in range(2):
                            ph = slice(h2 * 64, h2 * 64 + 64)
                            o = bkr[H0, h2 * 64:(h2 + 1) * 64]
                            P.mm(bkr, o, ARh[h2][H0, 0, cs], S5[:, hp, h2, sb_old, :], reads=[ARb[h2], Srw])
                            P.mm(bkr, o, na5[:, h2, 1, 0, :], tm[H0, 256 + h2 * 64:256 + (h2 + 1) * 64], reads=[na, tm])
                        copy(ru[H0, 0:128], bkr[H0, 0:128], [bkr], [ru], eng="act")
                        bku = P.bank()
                        for h2 in range(2):
                            P.mm(bku, bku[H0, h2 * 64:(h2 + 1) * 64], Gf3[:, h2, :], ru[H0, h2 * 64:(h2 + 1) * 64], reads=[Gf, ru])
                        copy(ru[H0, 128:256], bku[H0, 0:128], [bku], [ru], eng="dve")
                        bky = P.bank()
                        for h2 in range(2):
                            ph = slice(h2 * 64, h2 * 64 + 64)
                            o = bky[ph, 0:64]
                            P.mm(bky, o, S5[:, hp, h2, sb_old, :], ARh[h2][H0, 1, cs], reads=[ARb[h2], Srw])
                            P.mm(bky, o, ru[H0, 128 + h2 * 64:128 + (h2 + 1) * 64], na5[:, h2, 0, 1, :], reads=[ru, na])
                            P.mm(bky, o, tm[H0, 256 + h2 * 64:256 + (h2 + 1) * 64], na5[:, h2, 1, 1, :], reads=[tm, na])
                        copy(yr[A, cs], bky[A, 0:64], [bky], [yr], eng="act")
                        bks = P.bank()
                        for h2 in range(2):
                            o = bks[H0, h2 * 64:(h2 + 1) * 64]
                            P.mm(bks, o, tm[H0, h2 * 64:(h2 + 1) * 64], ru[H0, 128 + h2 * 64:128 + (h2 + 1) * 64], reads=[tm, ru])
                            P.mm(bks, o, tm[H0, 128 + h2 * 64:128 + (h2 + 1) * 64], tm[H0, 256 + h2 * 64:256 + (h2 + 1) * 64], reads=[tm])
                        for h2 in range(2):
                            pcs = Ep[H0, c * 64 + 63:c * 64 + 64] if h2 == 0 else pc1[H0, c:c + 1]
                            P.stt(S5[:, hp, h2, sb_new, :], S5[:, hp, h2, sb_old, :], pcs, bks[H0, h2 * 64:(h2 + 1) * 64],
                                  ALU.mult, ALU.add, reads=[Srw, Ep, pc1, bks], writes=[Srw])
                    t1 = Ep
                    bk = P.bank()
                    P.mm(bk, bk[A, :], C(C_BLKM, 128), yr[A, :], reads=[cst, yr])
                    P.tt(yr[A, :], yr[A, :], bk[A, :], ALU.subtract, reads=[yr, bk], writes=[yr])
                    P.tt(t1[A, :], yr[A, :], yr[A, :], ALU.mult, reads=[yr], writes=[t1])
                    bk = P.bank()
                    P.mm(bk, bk[A, :], C(C_BLKM, 128), t1[A, :], reads=[cst, t1])
                    P.ts(t1[A, :], bk[A, :], 64e-5, ALU.add, reads=[bk], writes=[t1])
                    P.recip(t1[A, :], t1[A, :], reads=[t1], writes=[t1])
                    P.act(t1[A, :], t1[A, :], AF.Sqrt, reads=[t1], writes=[t1])
                    P.tt(yr[A, :], yr[A, :], t1[A, :], ALU.mult, reads=[yr, t1], writes=[yr])
                    P.ts(yr[A, :], yr[A, :], pcc(PC_LNW), ALU.mult, pcc(PC_LNB), ALU.add, reads=[yr, pc], writes=[yr])
                    P.tt(yr[A, :], yr[A, :], bonus[A, :], ALU.add, reads=[yr, bonus], writes=[yr])
                    P.tt(yT3[:, hp, :], yr[A, :], g[A, :], ALU.mult, reads=[yr, g], writes=[yT.sub(hp * 512, 512)])

            P.ar_top = MIX_BASE
            if "r" in phases:
                rwkv()

            def mlstm():
                qk = [P.alloc() for _ in range(4)]
                vT = [P.alloc(), P.alloc()]
                oT = [P.alloc(), P.alloc()]
                gi, gf, bcum, ge = (P.alloc() for _ in range(4))
                top = P.ar_top
                for i in range(4):
                    P.ar_top = top
                    uext = P.alloc(515)
                    acc = P.alloc()
                    proj_fm(1024 + i * 64, 64, uext[H0, 3:515], rows=uext)
                    P.cp(uext[H0, 0:3], cml[H0, i * 3:(i + 1) * 3], reads=[cml], writes=[uext], eng="pool")
                    wc = lambda j: pc[H0, PC_CW + i * 4 + j:PC_CW + i * 4 + j + 1]
                    P.ts(acc[H0, :], uext[H0, 0:512], wc(0), ALU.mult, reads=[uext, pc], writes=[acc])
                    for j in range(1, 4):
                        P.stt(acc[H0, :], uext[H0, j:j + 512], wc(j), acc[H0, :], ALU.mult, ALU.add, reads=[uext, pc, acc], writes=[acc])
                    P.cp(cml[H0, i * 3:(i + 1) * 3], uext[H0, 512:515], reads=[uext], writes=[cml], eng="pool")
                    P.act(qk[i][H0, :], acc[H0, :], AF.Silu, bias=pc[H0, PC_CB + i:PC_CB + i + 1], reads=[acc, pc], writes=[qk[i]])
                    if i < 2:
                        P.ts(qk[i][H0, :], qk[i][H0, :], 32.0 ** -0.5, ALU.mult, reads=[qk[i]], writes=[qk[i]])
                P.ar_top = top
                for hp in range(2):
                    proj_fm(1280 + hp * 128, 128, vT[hp][A, :], rows=vT[hp])
                    proj_fm(1536 + hp * 128, 128, oT[hp][A, :], rows=oT[hp])
                    P.act(oT[hp][A, :], oT[hp][A, :], AF.Sigmoid, reads=[oT[hp]], writes=[oT[hp]])
                proj_fm(1792, 4, gi[R4, :], rows=gi)
                proj_fm(1796, 4, gf[R4, :], rows=gf)
                P.act(gi[R4, :], gi[R4, :], AF.Tanh, bias=dv[R4, DV_BI15:DV_BI15 + 1], scale=1.0 / 15.0, reads=[gi, dv], writes=[gi])
                P.act(gf[R4, :], gf[R4, :], AF.Tanh, bias=dv[R4, DV_BF15:DV_BF15 + 1], scale=1.0 / 15.0, reads=[gf, dv], writes=[gf])
                P.ts(gi[R4, :], gi[R4, :], 15.0, ALU.mult, reads=[gi], writes=[gi])
                P.act(gf[R4, :], gf[R4, :], AF.Exp, scale=-15.0, reads=[gf], writes=[gf])
                P.act(gf[R4, :], gf[R4, :], AF.Ln, bias=1.0, reads=[gf], writes=[gf])
                ones4 = C(C_ALLONE, 64, R4)
                for c in range(8):
                    cs = slice(c * 64, (c + 1) * 64)
                    P.scan(bcum[R4, cs], ones4, gf[R4, cs], 0.0, ALU.mult, ALU.subtract, reads=[cst, gf], writes=[bcum])
                P.tt(gi[R4, :], gi[R4, :], bcum[R4, :], ALU.subtract, reads=[gi, bcum], writes=[gi])
                P.act(gi[R4, :], gi[R4, :], AF.Exp, reads=[gi], writes=[gi])
                P.act(bcum[R4, :], bcum[R4, :], AF.Exp, reads=[bcum], writes=[bcum])
                for c in range(8):
                    cs = slice(c * 64, (c + 1) * 64)
                    P.ts(ge[R4, cs], gi[R4, cs], bcum[R4, c * 64 + 63:c * 64 + 64], ALU.mult, reads=[gi, bcum], writes=[ge])
                kp = [P.alloc(), P.alloc()]
                kpe = [P.alloc(), P.alloc()]
                ebbc = [P.alloc(), P.alloc()]
                for j in range(2):
                    selk = C(C_SELK + j * 64, 64, R4)
                    bk = P.bank()
                    P.mm(bk, bk[H0, :], selk, gi[R4, :], reads=[cst, gi])
                    P.tt(kp[j][H0, :], qk[2 + j][H0, :], bk[H0, :], ALU.mult, reads=[qk[2 + j], bk], writes=[kp[j]])
                    bk = P.bank()
                    P.mm(bk, bk[H0, :], selk, ge[R4, :], reads=[cst, ge])
                    P.tt(kpe[j][H0, :], qk[2 + j][H0, :], bk[H0, :], ALU.mult, reads=[qk[2 + j], bk], writes=[kpe[j]])
                    bk = P.bank()
                    P.mm(bk, bk[H0, :], selk, bcum[R4, :], reads=[cst, bcum])
                    P.cp(ebks[H0, j * 8:(j + 1) * 8], bk.v(H0, "p (c t) -> p c t", c=8)[:, :, 63], reads=[bk], writes=[ebks])
                    bk = P.bank()
                    P.mm(bk, bk[A, :], C(C_SELV + j * 128, 128, R4), bcum[R4, :], reads=[cst, bcum])
                    copy(ebbc[j][A, :], bk[A, :], [bk], [ebbc[j]])
                NTs = P.alloc(1024)
                DNs = P.alloc(1024)
                NT3 = NTs.v(A, "p (j t) -> p j t", j=2)
                DN3 = DNs.v(A, "p (j t) -> p j t", j=2)
                sm = P.alloc()
                qmc = P.alloc(256)
                for c in range(8):
                    cs = slice(c * 64, (c + 1) * 64)
                    bkt = P.bank()
                    P.tr(bkt, bkt[H0, 0:128], vT[0][A, cs], ident, reads=[vT[0], cst])
                    P.tr(bkt, bkt[H0, 128:256], vT[1][A, cs], ident, reads=[vT[1], cst])
                    copy(vo3[:, :, 0:64], bkt.v(H0, "p (h c) -> p h c", h=8)[:, 0:4, :], [bkt], [vones])
                    bkt2 = P.bank()
                    P.tr(bkt2, bkt2[H0, 0:64], kpe[0][H0, cs], C(C_IDENT, 64, H0), reads=[kpe[0], cst])
                    P.tr(bkt2, bkt2[H0, 64:128], kpe[1][H0, cs], C(C_IDENT, 64, H0), reads=[kpe[1], cst])
                    copy(sm[H0, 256:384], bkt2[H0, 0:128], [bkt2], [sm])
                    for h in range(4):
                        P.ts(qmc[H0, h * 64:(h + 1) * 64], qk[h // 2][H0, cs], C(C_HM + h % 2, 1, H0), ALU.mult,
                             reads=[qk[h // 2], cst], writes=[qmc])
                    bka = P.bank()
                    for h in range(4):
                        j = h // 2
                        P.mm(bka, bka[H0, h * 64:(h + 1) * 64], kp[j][H0, cs], qmc[H0, h * 64:(h + 1) * 64], reads=[kp[j], qmc])
                    P.tt(sm[H0, 0:256], bka[H0, 0:256], C(C_MM, 256, H0), ALU.mult, reads=[bka, cst], writes=[sm])
                    bn = P.bank()
                    bd = P.bank()
                    for h in range(4):
                        j = h // 2
                        pq = slice((h % 2) * 32, (h % 2) * 32 + 32)
                        ph = slice((h % 2) * 64, (h % 2) * 64 + 64)
                        at = sm[H0, h * 64:(h + 1) * 64]
                        qm = qmc[H0, h * 64:(h + 1) * 64]
                        P.mm(bn, bn[ph, j * 64:(j + 1) * 64], vo3[:, h, 0:64], at, reads=[vones, sm])
                        P.mm(bn, bn[ph, j * 64:(j + 1) * 64], CN3[:, j, 0:64], qm, reads=[CN, qmc])
                        P.mm(bd, bd[ph, j * 64:(j + 1) * 64], vo3[:, h, 64:128], at, reads=[vones, sm])
                        P.mm(bd, bd[ph, j * 64:(j + 1) * 64], CN3[:, j, 64:128], qm, reads=[CN, qmc])
                    copy(NT3[:, :, cs], bn.v(A, "p (j t) -> p j t", j=8)[:, 0:2, :], [bn], [NTs], eng="act")
                    copy(DN3[:, :, cs], bd.v(A, "p (j t) -> p j t", j=8)[:, 0:2, :], [bd], [DNs], eng="act")
                    bs_ = P.bank()
                    for h in range(4):
                        j = h // 2
                        P.mm(bs_, bs_[H0, h * 128:(h + 1) * 128], sm[H0, 256 + j * 64:256 + (j + 1) * 64], vo3[:, h, :], reads=[sm, vones])
                    for j in range(2):
                        P.ts(CN3[:, j, :], CN3[:, j, :], ebks[H0, j * 8 + c:j * 8 + c + 1], ALU.mult, reads=[CN, ebks], writes=[CN])
                        for h2 in range(2):
                            h = 2 * j + h2
                            P.stt(CN3[:, j, :], bs_[H0, h * 128:(h + 1) * 128], C(C_HM + h2, 1, H0), CN3[:, j, :],
                                  ALU.mult, ALU.add, reads=[CN, bs_, cst], writes=[CN])
                for j in range(2):
                    d1 = DNs.sub(j * 512, 512)
                    n1 = NTs.sub(j * 512, 512)
                    P.tt(d1[A, :], d1[A, :], ebbc[j][A, :], ALU.mult, reads=[d1, ebbc[j]], writes=[d1])
                    P.stt(d1[A, :], d1[A, :], -1.0, d1[A, :], ALU.mult, ALU.max, reads=[d1], writes=[d1])
                    P.ts(d1[A, :], d1[A, :], 1.0, ALU.max, reads=[d1], writes=[d1])
                    P.recip(d1[A, :], d1[A, :], reads=[d1], writes=[d1])
                    P.tt(n1[A, :], n1[A, :], ebbc[j][A, :], ALU.mult, reads=[n1, ebbc[j]], writes=[n1])
                    P.tt(n1[A, :], n1[A, :], d1[A, :], ALU.mult, reads=[n1, d1], writes=[n1])
                    P.tt(d1[A, :], n1[A, :], n1[A, :], ALU.mult, reads=[n1], writes=[d1])
                    bk = P.bank()
                    P.mm(bk, bk[A, :], C(C_BLKM, 128), d1[A, :], reads=[cst, d1])
                    P.ts(d1[A, :], bk[A, :], 1e-6, ALU.add, reads=[bk], writes=[d1])
                    P.recip(d1[A, :], d1[A, :], reads=[d1], writes=[d1])
                    P.act(d1[A, :], d1[A, :], AF.Sqrt, reads=[d1], writes=[d1])
                    P.tt(n1[A, :], n1[A, :], d1[A, :], ALU.mult, reads=[n1, d1], writes=[n1])
                    P.stt(yT3[:, 2 + j, :], n1[A, :], pc[A, PC_MNORM + j:PC_MNORM + j + 1], oT[j][A, :], ALU.mult, ALU.mult,
                          reads=[n1, pc, oT[j]], writes=[yT.sub((2 + j) * 512, 512)])

            P.ar_top = MIX_BASE
            if "m" in phases:
                mlstm()

            def swa():
                qS = [P.alloc(), P.alloc()]
                for j in range(2):
                    proj_fm(1800 + j * 128, 128, qS[j][A, :], scale=0.125, rows=qS[j])
                    proj_fm(2056 + j * 64, 64, swaK3[:, j, 128:640], dup=True, rows=swaK)
                proj_tm(2184, 128, lambda tt_: swaV3[:, 1 + tt_, :], swaV)
                ETs = [P.alloc(1024), P.alloc(1024)]
                tmp = P.alloc()
                for i in range(4):
                    gi_ = blk * 4 + i
                    qc = slice(i * 128, (i + 1) * 128)
                    prev = slice(i * 128, (i + 1) * 128)
                    cur_ = slice((i + 1) * 128, (i + 2) * 128)
                    ET = ETs[i % 2]
                    bp = P.bank() if gi_ > 0 else None
                    bc = P.bank()
                    for hq in range(4):
                        j = hq // 2
                        ph = slice((hq % 2) * 64, (hq % 2) * 64 + 64)
                        o = slice(hq * 128, (hq + 1) * 128)
                        if gi_ > 0:
                            P.mm(bp, bp[A, o], swaK3[ph, j, prev], qS[j][ph, qc], reads=[swaK, qS[j]])
                            P.mm(bp, bp[A, o], ident, swaB4[:, 0, hq, :], reads=[cst, swaB])
                        P.mm(bc, bc[A, o], swaK3[ph, j, cur_], qS[j][ph, qc], reads=[swaK, qS[j]])
                        P.mm(bc, bc[A, o], ident, swaB4[:, 1, hq, :], reads=[cst, swaB])
                    if gi_ > 0:
                        P.act(ET[A, 0:512], bp[A, :], AF.Exp, reads=[bp], writes=[ET])
                    P.act(ET[A, 512:1024], bc[A, :], AF.Exp, reads=[bc], writes=[ET])
                    bo = P.bank()
                    bd = P.bank()
                    for hq in range(4):
                        j = hq // 2
                        ph = slice((hq % 2) * 64, (hq % 2) * 64 + 64)
                        o = slice(j * 128, (j + 1) * 128)
                        e = slice(hq * 128, (hq + 1) * 128)
                        if gi_ > 0:
                            P.mm(bo, bo[ph, o], swaV3[:, i, j * 64:(j + 1) * 64], ET[A, e.start:e.stop], reads=[swaV, ET])
                            P.mm(bd, bd[ph, o], C(C_ALLONE, 64), ET[A, e.start:e.stop], reads=[cst, ET])
                        P.mm(bo, bo[ph, o], swaV3[:, i + 1, j * 64:(j + 1) * 64], ET[A, 512 + e.start:512 + e.stop], reads=[swaV, ET])
                        P.mm(bd, bd[ph, o], C(C_ALLONE, 64), ET[A, 512 + e.start:512 + e.stop], reads=[cst, ET])
                    for j in range(2):
                        P.ts(tmp[A, j * 128:(j + 1) * 128], bd[A, j * 128:(j + 1) * 128], dv[A, DV_ESINK + j:DV_ESINK + j + 1], ALU.add,
                             reads=[bd, dv], writes=[tmp])
                    P.recip(tmp[A, 0:256], tmp[A, 0:256], reads=[tmp], writes=[tmp])
                    P.tt(yT3[:, 4:6, qc], bo.v(A, "p (j q) -> p j q", j=4)[:, 0:2, :], tmp.v(A, "p (j q) -> p j q", j=4)[:, 0:2, :],
                         ALU.mult, reads=[bo, tmp], writes=[yT.sub(4 * 512, 1024)])
                P.cp(swaK3[:, :, 0:128], swaK3[:, :, 512:640], reads=[swaK], writes=[swaK], eng="pool")
                P.cp(swaV3[:, 0, :], swaV3[:, 4, :], reads=[swaV], writes=[swaV], eng="pool")

            P.ar_top = MIX_BASE
            if "s" in phases:
                swa()

            def fox():
                qF = [P.alloc(), P.alloc()]
                for j in range(2):
                    proj_fm(2312 + j * 128, 128, qF[j][A, :], scale=0.125, rows=qF[j])
                    proj_fm(2568 + j * 128, 128, kTh3[:, j, bs], rows=kTh)
                for half in range(2):
                    proj_tm(2824 + half * 128, 128, lambda tt_: Vh3[:, blk * 4 + tt_, half * 128:(half + 1) * 128], Vh)
                fr = P.alloc()
                crow = P.alloc()
                for R_ in (slice(0, 4), slice(64, 68)):
                    proj_fm(3080, 4, fr[R_, :], rows=fr, pbase=R_.start)
                    P.act(fr[R_, :], fr[R_, :], AF.Exp, bias=dv[R_, DV_NFOXB:DV_NFOXB + 1], scale=-1.0, reads=[fr, dv], writes=[fr])
                    P.act(fr[R_, :], fr[R_, :], AF.Ln, bias=1.0, reads=[fr], writes=[fr])
                    for sg_ in range(4):
                        seg = slice(sg_ * 128, (sg_ + 1) * 128)
                        init = ccar[R_, 0:1] if sg_ == 0 else crow[R_, sg_ * 128 - 1:sg_ * 128]
                        P.scan(crow[R_, seg], C(C_ALLONE, 128, R_), fr[R_, seg], init, ALU.mult, ALU.subtract,
                               reads=[fr, ccar, cst, crow], writes=[crow])
                    P.cp(ccar[R_, 0:1], crow[R_, 511:512], reads=[crow], writes=[ccar], eng="pool")
                bk = P.bank()
                for tt_ in range(4):
                    P.mm(bk, bk[A, tt_ * 4:(tt_ + 1) * 4], crow[R4, tt_ * 128:(tt_ + 1) * 128], C(C_IDENT, 4, R4), reads=[crow, cst])
                P.ts(negc3[:, blk * 4:(blk + 1) * 4, :], bk.v(A, "p (n h) -> p n h", h=4)[:, 0:4, :], -1.0, ALU.mult,
                     reads=[bk], writes=[negc])
                ETs = [P.alloc() for _ in range(3)]
                rec = P.alloc()
                ei = 0
                for j in range(2):
                    bo = P.bank(pin=True)
                    bd = P.bank(pin=True)
                    for h2 in range(2):
                        h = 2 * j + h2
                        ph = slice(h2 * 64, h2 * 64 + 64)
                        for kb in range(blk * 4 + 4):
                            q0 = max(0, kb - blk * 4) * 128
                            nq = 512 - q0
                            bs_ = P.bank()
                            P.mm(bs_, bs_[A, 0:nq], kTh3[ph, j, kb * 128:(kb + 1) * 128], qF[j][ph, q0:512], reads=[kTh, qF[j]])
                            Rh = slice(h2 * 64, h2 * 64 + 4)
                            P.mm(bs_, bs_[A, 0:nq], C(C_SELF + h * 128, 128, Rh), crow[Rh, q0:512], reads=[cst, crow])
                            et = ETs[ei % 3]
                            ei += 1
                            P.act(et[A, 0:nq], bs_[A, 0:nq], AF.Exp, bias=negc3[:, kb, h:h + 1], reads=[bs_, negc], writes=[et])
                            if kb >= blk * 4:
                                P.tt(et[A, 0:128], et[A, 0:128], C(C_TRI, 128), ALU.mult, reads=[et, cst], writes=[et], eng="pool")
                            P.mm(bo, bo[ph, q0:512], Vh3[:, kb, h * 64:(h + 1) * 64], et[A, 0:nq], reads=[Vh, et])
                            P.mm(bd, bd[ph, q0:512], C(C_ALLONE, 64), et[A, 0:nq], reads=[cst, et])
                    P.recip(rec[A, :], bd[A, :], reads=[bd], writes=[rec])
                    P.tt(yT3[:, 6 + j, :], bo[A, :], rec[A, :], ALU.mult, reads=[bo, rec], writes=[yT.sub((6 + j) * 512, 512)])
                    bo.pinned = False
                    bd.pinned = False

            P.ar_top = MIX_BASE
            if "f" in phases:
                fox()

            if dbg == ("yT", l):
                for c in range(8):
                    P.dma(dbg_d[:, c, bs], yT3[:, c, :], reads=[yT])

            def post_residual(oT3, oT, gcol):
                rs = rms_rstd(lambda c: oT3[:, c, :], [oT])
                tmps = [P.alloc(), P.alloc()]
                for m in range(8):
                    t_ = tmps[m % 2]
                    P.stt(t_[A, :], oT3[:, m, :], pc[A, gcol + m:gcol + m + 1], rs[A, :], ALU.mult, ALU.mult,
                          reads=[oT, pc, rs], writes=[t_])
                    P.tt(xT3[:, m, bs], xT3[:, m, bs], t_[A, :], ALU.add, reads=[xT, t_], writes=[xT])

            if "o" not in phases:
                continue
            P.ar_top = 0
            oT = P.alloc(4096)
            oT3 = oT.v(A, "p (c t) -> p c t", c=8)
            for m in range(8):
                w = wchunk()
                w3 = w.v(A, "p (c m) -> p c m", c=8)
                P.dma(w3[:, :, :], w_out_d[l, :, :, m * 128:(m + 1) * 128], writes=[w])
                bk = P.bank()
                for j in range(8):
                    P.mm(bk, bk[A, :], w3[:, j, :], yT3[:, j, :], reads=[w, yT])
                copy(oT3[:, m, :], bk[A, :], [bk], [oT.sub(m * 512, 512)])
            post_residual(oT3, oT, PC_GPOST)

            if dbg == ("x1", l):
                for c in range(8):
                    P.dma(dbg_d[:, c, bs], xT3[:, c, bs], reads=[xT])

            if "n" not in phases:
                continue
            P.ar_top = 0
            rs3 = rms_rstd(lambda c: xT3[:, c, bs], [xT])
            fT = P.alloc(11 * 512)
            fT3 = fT.v(A, "p (c t) -> p c t", c=11)
            oT = P.alloc(4096)
            oT3 = oT.v(A, "p (c t) -> p c t", c=8)
            gbuf = P.alloc(514)
            acc = P.alloc()
            gfp = pc[A, PC_FPRE:PC_FPRE + 8]
            for half in range(2):
                for cc in range(11):
                    c = half * 11 + cc
                    ws = []
                    for col0 in (c * 128, DFF + c * 128):
                        w = wchunk()
                        w3 = w.v(A, "p (c m) -> p c m", c=8)
                        P.dma(w3[:, :, :], w_up_d[l, :, :, col0:col0 + 128], writes=[w])
                        P.tt(w3[:, :, :], w3[:, :, :], gfp.to_broadcast([128, 8, 128]), ALU.mult, reads=[w, pc], writes=[w])
                        ws.append((w, w3))
                    bg = P.bank()
                    for k in range(8):
                        P.mm(bg, bg[A, :], ws[0][1][:, k, :], xT3[:, k, bs], reads=[ws[0][0], xT])
                    bu = P.bank()
                    for k in range(8):
                        P.mm(bu, bu[A, :], ws[1][1][:, k, :], xT3[:, k, bs], reads=[ws[1][0], xT])
                    P.tt(gbuf[A, 2:514], bg[A, :], rs3[A, :], ALU.mult, reads=[bg, rs3], writes=[gbuf])
                    P.cp(gbuf[A, 0:2], cffn[A, c * 2:c * 2 + 2], reads=[cffn], writes=[gbuf], eng="pool")
                    fw = lambda j: pc[A, PC_FCW + c * 3 + j:PC_FCW + c * 3 + j + 1]
                    P.ts(acc[A, :], gbuf[A, 0:512], fw(0), ALU.mult, reads=[gbuf, pc], writes=[acc])
                    P.stt(acc[A, :], gbuf[A, 1:513], fw(1), acc[A, :], ALU.mult, ALU.add, reads=[gbuf, pc, acc], writes=[acc])
                    P.stt(acc[A, :], gbuf[A, 2:514], fw(2), acc[A, :], ALU.mult, ALU.add, reads=[gbuf, pc, acc], writes=[acc])
                    P.cp(cffn[A, c * 2:c * 2 + 2], gbuf[A, 512:514], reads=[gbuf], writes=[cffn], eng="pool")
                    P.act(acc[A, :], acc[A, :], AF.Gelu_apprx_tanh, bias=pc[A, PC_FCB + c:PC_FCB + c + 1], reads=[acc, pc], writes=[acc])
                    P.tt(acc[A, :], acc[A, :], rs3[A, :], ALU.mult, reads=[acc, rs3], writes=[acc])
                    P.tt(fT3[:, cc, :], acc[A, :], bu[A, :], ALU.mult, reads=[acc, bu], writes=[fT.sub(cc * 512, 512)])
                for m in range(8):
                    wa = wchunk()
                    wa3 = wa.v(A, "p (c m) -> p c m", c=8)
                    wb = wchunk()
                    wb3 = wb.v(A, "p (c m) -> p c m", c=8)
                    P.dma(wa3[:, 0:8, :], w_dn_d[l, :, half * 11:half * 11 + 8, m * 128:(m + 1) * 128], writes=[wa])
                    P.dma(wb3[:, 0:3, :], w_dn_d[l, :, half * 11 + 8:half * 11 + 11, m * 128:(m + 1) * 128], writes=[wb])
                    bk = P.bank()
                    for cc in range(11):
                        lhs = wa3[:, cc, :] if cc < 8 else wb3[:, cc - 8, :]
                        P.mm(bk, bk[A, :], lhs, fT3[:, cc, :], reads=[wa, wb, fT])
                    om = oT.sub(m * 512, 512)
                    if half == 0:
                        copy(oT3[:, m, :], bk[A, :], [bk], [om])
                    else:
                        P.tt(oT3[:, m, :], oT3[:, m, :], bk[A, :], ALU.add, reads=[om, bk], writes=[om])
            post_residual(oT3, oT, PC_FPOST)

    outops = []
    for c in range(8):
        outops.append(P.dma(out_d[:, c, 0:T], xT3[:, c, :], reads=[xT]))
    P.emit(outops + [op for op in P.dma_ops if False])
    P.close()
    return nc


_NC_CACHE = {}


def kernel(**inputs):
    maps = prep_inputs(inputs)
    if "nc" not in _NC_CACHE:
        _NC_CACHE["nc"] = build()
    nc = _NC_CACHE["nc"]
    res = run_bass_kernel_spmd(nc, maps, core_ids=list(range(len(maps))))
    x = np.asarray(inputs["x"])
    out = np.empty(x.shape, np.float32)
    for b in range(x.shape[0]):
        oT = np.asarray(res.results[b]["outT"])
        out[b] = oT.transpose(1, 0, 2).reshape(D, SEQ).T
    return out
```

```python
import contextlib
import math
import numpy as np
import concourse.bass as bass
import concourse.mybir as mybir
from concourse.bass_utils import run_bass_kernel_spmd

F32 = mybir.dt.float32
F32R = mybir.dt.float32r
ALU = mybir.AluOpType
AF = mybir.ActivationFunctionType

ENGS = ("pe", "act", "dve", "pool", "sp")


def _flat(ks, out):
    for k in ks:
        if isinstance(k, (str, int)):
            out.append(k)
        elif isinstance(k, tuple) and (len(k) == 0 or isinstance(k[0], (str, int))):
            out.append(k)
        elif hasattr(k, "k"):
            _flat(k.k, out)
        else:
            _flat(k, out)
    return out


class Op:
    __slots__ = ("eng", "fn", "deps", "signals", "count", "pos", "dma", "dsem", "dval", "prewait")

    def __init__(self, eng, fn, dma=False):
        self.eng = eng
        self.fn = fn
        self.deps = []
        self.signals = False
        self.count = None
        self.pos = None
        self.dma = dma
        self.dsem = None
        self.dval = None
        self.prewait = None


class Buf:
    def __init__(self, t, off, ncols, keys):
        self.t, self.off, self.n, self.k = t, off, ncols, keys

    def __getitem__(self, idx):
        p, c = idx
        if isinstance(c, int):
            c = slice(c, c + 1)
        a = 0 if c.start is None else c.start
        b = self.n if c.stop is None else c.stop
        assert 0 <= a <= b <= self.n, (a, b, self.n)
        return self.t[p, self.off + a:self.off + b]

    def v(self, p, pat, **kw):
        return self.t[p, self.off:self.off + self.n].rearrange(pat, **kw)

    def sub(self, c0, n):
        ks = self.k
        if len(ks) > 1 and len(ks) * 512 >= self.n:
            ks = ks[c0 // 512:(c0 + n + 511) // 512]
        return Buf(self.t, self.off + c0, n, ks)


class Bank(Buf):
    def __init__(self, t, name):
        super().__init__(t, 0, 512, [name])
        self.opened = set()
        self.pinned = False


class Prog:
    NDMA = 32

    def __init__(self, nc):
        self.nc = nc
        self.ops = {e: [] for e in ENGS}
        self.lastw = {}
        self.readers = {}
        self.waited = {e: {} for e in ENGS}
        self.waited_dma = {e: set() for e in ENGS}
        self.dma_ops = []
        self.stack = contextlib.ExitStack()
        self.banks = []
        self.bank_i = 0
        self.ar = None
        self.ar_top = 0

    def sb(self, name, ncols, parts=128):
        t = self.stack.enter_context(self.nc.sbuf_tensor("sb_" + name, [parts, ncols], F32))
        return Buf(t, 0, ncols, [name])

    def sbs(self, name, nslots):
        t = self.stack.enter_context(self.nc.sbuf_tensor("sb_" + name, [128, nslots * 512], F32))
        return Buf(t, 0, nslots * 512, [(name, i) for i in range(nslots)])

    def make_banks(self):
        for i in range(8):
            t = self.stack.enter_context(self.nc.psum_tensor(f"bank{i}", [128, 512], F32))
            self.banks.append(Bank(t, f"bank{i}"))

    def bank(self, pin=False):
        for _ in range(16):
            b = self.banks[self.bank_i % 8]
            self.bank_i += 1
            if not b.pinned:
                b.opened = set()
                b.pinned = pin
                return b
        raise RuntimeError("no free psum bank")

    def make_arena(self, nslots):
        self.ar = self.stack.enter_context(self.nc.sbuf_tensor("arena", [128, nslots * 512], F32))
        self.ar_n = nslots

    def alloc(self, ncols=512):
        ns = (ncols + 511) // 512
        assert self.ar_top + ns <= self.ar_n, ("arena overflow", self.ar_top, ns, self.ar_n)
        b = Buf(self.ar, self.ar_top * 512, ncols, [("ar", s) for s in range(self.ar_top, self.ar_top + ns)])
        self.ar_top += ns
        return b

    def add(self, eng, fn, reads=(), writes=(), dma=False):
        op = Op(eng, fn, dma)
        op.pos = len(self.ops[eng])
        reads = _flat(reads, [])
        writes = _flat(writes, [])
        deps = []
        for k in reads:
            w = self.lastw.get(k)
            if w is not None:
                deps.append(w)
        for k in writes:
            w = self.lastw.get(k)
            if w is not None:
                deps.append(w)
            deps.extend(self.readers.get(k, ()))
        wt = self.waited[eng]
        wd = self.waited_dma[eng]
        best = {}
        dm = []
        for d in deps:
            if d is op:
                continue
            if d.dma:
                if id(d) not in wd:
                    wd.add(id(d))
                    dm.append(d)
                continue
            if d.eng == eng and eng in ("pe", "sp"):
                continue
            if wt.get(d.eng, -1) >= d.pos:
                continue
            if d.eng not in best or best[d.eng].pos < d.pos:
                best[d.eng] = d
        op.deps = list(best.values()) + dm
        for d in best.values():
            d.signals = True
            wt[d.eng] = max(wt.get(d.eng, -1), d.pos)
        for k in reads:
            self.readers.setdefault(k, []).append(op)
        for k in writes:
            self.lastw[k] = op
            self.readers[k] = []
        self.ops[eng].append(op)
        if dma:
            self.dma_ops.append(op)
        return op

    def emit(self, out_dma_ops=()):
        nc = self.nc
        nd = self.NDMA
        sems = {e: self.stack.enter_context(nc.semaphore(f"s_{e}")) for e in ENGS if e != "sp"}
        dsems = [self.stack.enter_context(nc.semaphore(f"s_dma{i}")) for i in range(nd)]
        for e in ENGS:
            c = 0
            for op in self.ops[e]:
                if not op.dma and op.signals:
                    c += 1
                    op.count = c
        per_eng = {}
        for op in self.dma_ops:
            per_eng.setdefault(op.eng, []).append(op)
        engs_with_dma = list(per_eng.keys())
        share = nd // max(1, len(engs_with_dma))
        for ei, e in enumerate(engs_with_dma):
            mysems = dsems[ei * share:(ei + 1) * share]
            for i, op in enumerate(per_eng[e]):
                op.dsem = mysems[i % share]
                op.dval = 16 * (i // share + 1)
                if i >= share:
                    op.prewait = (op.dsem, 16 * (i // share))
        final_waits = [(op.dsem, op.dval) for op in out_dma_ops]

        def run(e, eng):
            for op in self.ops[e]:
                if op.prewait is not None:
                    eng.wait_ge(op.prewait[0], op.prewait[1])
                for d in op.deps:
                    if d.dma:
                        eng.wait_ge(d.dsem, d.dval)
                    else:
                        eng.wait_ge(sems[d.eng], d.count)
                ins = op.fn(eng)
                if op.dma:
                    ins.then_inc(op.dsem, 16)
                elif op.signals:
                    ins.then_inc(sems[e], 1)
            if e == "sp":
                for s, v in final_waits:
                    eng.wait_ge(s, v)

        with nc.Block() as block:
            @block.tensor
            def _(eng):
                run("pe", eng)

            @block.scalar
            def _(eng):
                run("act", eng)

            @block.vector
            def _(eng):
                run("dve", eng)

            @block.gpsimd
            def _(eng):
                run("pool", eng)

            @block.sync
            def _(eng):
                run("sp", eng)

    def close(self):
        self.stack.close()

    def dma(self, out, in_, reads=(), writes=(), eng="sp"):
        return self.add(eng, lambda e: e.dma_start(out=out, in_=in_), reads, writes, dma=True)

    def mm(self, bank, out, lhsT, rhs, reads=(), r=False):
        p0 = out.start_partition()
        q = frozenset(range(p0 // 32, (p0 + out.partition_size() - 1) // 32 + 1))
        c0 = out.offset % 512 if hasattr(out, "offset") else 0
        c0 = self._ap_col0(out)
        c1 = c0 + self._ap_ncols(out)
        start = True
        for (qq, a, b) in bank.opened:
            if (qq & q) and a < c1 and c0 < b:
                assert q <= qq and a <= c0 and c1 <= b, ("partial psum overlap", q, qq, c0, c1, a, b)
                start = False
        if start:
            bank.opened.add((q, c0, c1))
        if r and FAST_MM:
            lhsT = lhsT.bitcast(F32R)
            rhs = rhs.bitcast(F32R)
        return self.add("pe", lambda e: e.matmul(out, lhsT, rhs, start=start, stop=True, skip_group_check=True),
                        reads, [bank])

    @staticmethod
    def _ap_col0(ap):
        pstride = ap.ap[0][0]
        return ap.offset % pstride

    @staticmethod
    def _ap_ncols(ap):
        span = 0
        for st, n in list(ap.ap)[1:]:
            span += st * (n - 1)
        return span + 1

    def tr(self, bank, out, in_, ident, reads=()):
        return self.add("pe", lambda e: e.transpose(out, in_, ident), reads, [bank])

    def act(self, out, in_, func, bias=None, scale=1.0, reads=(), writes=()):
        if bias is None:
            return self.add("act", lambda e: e.activation(out, in_, func, scale=scale), reads, writes)
        return self.add("act", lambda e: e.activation(out, in_, func, bias=bias, scale=scale), reads, writes)

    def tt(self, out, a, b, op, reads=(), writes=(), eng="dve"):
        return self.add(eng, lambda e: e.tensor_tensor(out, a, b, op), reads, writes)

    def ts(self, out, a, s1, op0, s2=None, op1=None, reads=(), writes=(), eng="dve"):
        if op1 is None:
            return self.add(eng, lambda e: e.tensor_scalar(out, a, s1, None, op0), reads, writes)
        return self.add(eng, lambda e: e.tensor_scalar(out, a, s1, s2, op0, op1), reads, writes)

    def stt(self, out, a, s, b, op0, op1, reads=(), writes=()):
        return self.add("dve", lambda e: e.scalar_tensor_tensor(out, a, s, b, op0, op1), reads, writes)

    def cp(self, out, a, reads=(), writes=(), eng="dve"):
        if eng == "act":
            return self.add(eng, lambda e: e.copy(out, a), reads, writes)
        return self.add(eng, lambda e: e.tensor_copy(out, a), reads, writes)

    def recip(self, out, a, reads=(), writes=()):
        return self.add("dve", lambda e: e.reciprocal(out, a), reads, writes)

    def scan(self, out, d0, d1, init, op0, op1, reads=(), writes=()):
        return self.add("dve", lambda e: e.tensor_tensor_scan(out, d0, d1, init, op0, op1), reads, writes)

    def memset(self, ap, v, writes=(), eng="dve"):
        return self.add(eng, lambda e: e.memset(ap, v), (), writes)


D = 1024
SEQ = 2048
DEPTH = 2
TB = 512
RW_STOP = 0
FAST_MM = True
N_IN = 3084
DFF = 2816
NFC = DFF // 128

PC_GPRE, PC_GPOST, PC_FPRE, PC_FPOST, PC_MU = 0, 8, 16, 24, 32
PC_W0, PC_A0, PC_KK, PC_KA, PC_RK, PC_LNW, PC_LNB, PC_MNORM, PC_SINK = 40, 42, 44, 46, 48, 50, 52, 54, 56
PC_CW, PC_CB, PC_BI, PC_BF, PC_FOXB, PC_FCW, PC_FCB, NPC = 58, 74, 78, 79, 80, 81, 147, 169
DV_OMKA, DV_BI15, DV_BF15, DV_NFOXB, DV_ESINK, NDV = 0, 2, 3, 4, 5, 8

C_IDENT, C_ONESD, C_ALLONE, C_BLK1, C_BLKM, C_TRI = 0, 128, 256, 384, 512, 640
C_MA, C_MB, C_IDG, C_MM, C_SELF, C_SELK, C_SELV, C_HM, C_SWAM, NCST = 768, 1280, 1408, 1536, 1792, 2304, 2432, 2688, 2696, 3720


def _t5_bucket(dist):
    max_exact = 16
    d = np.maximum(dist, 1).astype(np.float32)
    large = max_exact + (np.log(d / max_exact) / math.log(128 / max_exact) * (32 - max_exact)).astype(np.int32)
    large = np.minimum(large, 31)
    return np.where(dist < max_exact, dist, large).astype(np.int32)


def _swa_dist():
    s = np.arange(128)[:, None, None]
    pcx = np.arange(2)[None, :, None]
    tq = np.arange(128)[None, None, :]
    sk = s + 128 * pcx
    dist = tq + 128 - sk
    vis = (dist >= 0) & (dist < 128)
    return dist, vis


def make_consts():
    c = np.zeros((128, NCST), np.float32)
    c[:, C_IDENT:C_IDENT + 128] = np.eye(128)
    c[:, C_ONESD:C_ONESD + 128] = 1.0 / 1024
    c[:, C_ALLONE:C_ALLONE + 128] = 1.0
    blk = np.zeros((128, 128), np.float32)
    blk[:64, :64] = 1
    blk[64:, 64:] = 1
    c[:, C_BLK1:C_BLK1 + 128] = blk
    c[:, C_BLKM:C_BLKM + 128] = blk / 64.0
    k = np.arange(128)
    c[:, C_TRI:C_TRI + 128] = (k[:, None] <= k[None, :])
    i = np.arange(64)[:, None]
    t = np.arange(64)[None, :]
    ma = np.zeros((64, 2, 2, 2, 64), np.float32)
    ma[:, :, :, 0, :] = (i < t)[:, None, None, :]
    ma[:, :, :, 1, :] = (i <= t)[:, None, None, :]
    c[:64, C_MA:C_MA + 512] = ma.reshape(64, 512)
    mb = np.zeros((64, 2, 64), np.float32)
    mb[:] = (i > t)[:, None, :]
    c[:64, C_MB:C_MB + 128] = mb.reshape(64, 128)
    idg = np.zeros((64, 2, 64), np.float32)
    idg[:] = np.eye(64)[:, None, :]
    c[:64, C_IDG:C_IDG + 128] = idg.reshape(64, 128)
    mm = np.zeros((64, 4, 64), np.float32)
    mm[:] = (i <= t)[:, None, :]
    c[:64, C_MM:C_MM + 256] = mm.reshape(64, 256)
    sf = np.zeros((4, 4, 128), np.float32)
    for h in range(4):
        sf[h, h, :] = 1
    c[:4, C_SELF:C_SELF + 512] = sf.reshape(4, 512)
    c[64:68, C_SELF:C_SELF + 512] = sf.reshape(4, 512)
    sk = np.zeros((4, 2, 64), np.float32)
    sv = np.zeros((4, 2, 128), np.float32)
    for h in range(4):
        for j in range(2):
            sk[h, j, :] = (h == 2 * j + np.arange(64) // 32)
            sv[h, j, :] = (h == 2 * j + np.arange(128) // 64)
    c[:4, C_SELK:C_SELK + 128] = sk.reshape(4, 128)
    c[:4, C_SELV:C_SELV + 256] = sv.reshape(4, 256)
    c[0:32, C_HM] = 1.0
    c[32:64, C_HM + 1] = 1.0
    _, vis = _swa_dist()
    m = np.where(vis, 0.0, -30000.0).astype(np.float32)
    m4 = np.broadcast_to(m[:, :, None, :], (128, 2, 4, 128))
    c[:, C_SWAM:C_SWAM + 1024] = m4.reshape(128, 1024)
    return c


def _cols(v, n):
    return np.ascontiguousarray(np.asarray(v, np.float32).reshape(n, 128).T)


def prep_inputs(inp):
    L = DEPTH
    f = lambda k: np.asarray(inp[k], np.float32)
    w_in_r = np.ascontiguousarray(f("w_in").reshape(L, 8, 128, N_IN).transpose(0, 2, 1, 3))
    w_out_r = np.ascontiguousarray(f("w_out").reshape(L, 8, 128, D).transpose(0, 2, 1, 3))
    w_up_r = np.ascontiguousarray(f("ffn_w_up").reshape(L, 8, 128, 2 * DFF).transpose(0, 2, 1, 3))
    w_dn_r = np.ascontiguousarray(f("ffn_w_down").reshape(L, NFC, 128, D).transpose(0, 2, 1, 3))
    lora = np.zeros((L, 128, 512), np.float32)
    lora[:, 0:64, 0:256] = f("rwkv_w_up")
    lora[:, 64:128, 0:256] = f("rwkv_a_up")
    lora[:, :, 256:512] = f("rwkv_g_up")
    pc = np.zeros((L, 128, NPC), np.float32)
    for l in range(L):
        pc[l, :, PC_GPRE:PC_GPRE + 8] = _cols(f("norm_mix_pre")[l], 8)
        pc[l, :, PC_GPOST:PC_GPOST + 8] = _cols(f("norm_mix_post")[l], 8)
        pc[l, :, PC_FPRE:PC_FPRE + 8] = _cols(f("norm_ffn_pre")[l], 8)
        pc[l, :, PC_FPOST:PC_FPOST + 8] = _cols(f("norm_ffn_post")[l], 8)
        pc[l, :, PC_MU:PC_MU + 8] = _cols(f("rwkv_mu")[l], 8)
        for col, key in ((PC_W0, "rwkv_w0"), (PC_A0, "rwkv_a0"), (PC_KK, "rwkv_k_k"), (PC_KA, "rwkv_k_a"),
                         (PC_LNW, "rwkv_ln_w"), (PC_LNB, "rwkv_ln_b"), (PC_MNORM, "mlstm_norm")):
            pc[l, :, col:col + 2] = _cols(f(key)[l], 2)
        pc[l, :, PC_RK:PC_RK + 2] = _cols(f("rwkv_r_k")[l].reshape(256), 2)
        pc[l, :, PC_SINK:PC_SINK + 2] = _cols(np.repeat(f("swa_sinks")[l], 64), 2)
        cw = f("mlstm_conv_w")[l]
        for i in range(4):
            for j in range(4):
                pc[l, 0:64, PC_CW + i * 4 + j] = cw[j, i * 64:(i + 1) * 64]
            pc[l, 0:64, PC_CB + i] = f("mlstm_conv_b")[l][i * 64:(i + 1) * 64]
        pc[l, 0:4, PC_BI] = f("mlstm_b_i")[l]
        pc[l, 0:4, PC_BF] = f("mlstm_b_f")[l]
        pc[l, 0:4, PC_FOXB] = f("fox_b_f")[l]
        pc[l, 64:68, PC_FOXB] = f("fox_b_f")[l]
        fw = f("ffn_conv_w")[l]
        for j in range(3):
            pc[l, :, PC_FCW + j:PC_FCW + 66:3] = _cols(fw[j], NFC)
        pc[l, :, PC_FCB:PC_FCB + NFC] = _cols(f("ffn_conv_b")[l], NFC)
    dist, _ = _swa_dist()
    bk = _t5_bucket(np.clip(dist, 0, 127))
    swab = f("rel_bias")[bk]
    swab = np.ascontiguousarray(swab.transpose(0, 1, 3, 2)).reshape(128, 1024)
    cst = make_consts()
    x = f("x")
    maps = []
    for b in range(x.shape[0]):
        xT = np.ascontiguousarray(x[b].T.reshape(8, 128, x.shape[1]).transpose(1, 0, 2))
        maps.append({"xT": xT, "w_in_r": w_in_r, "w_out_r": w_out_r, "w_up_r": w_up_r, "w_dn_r": w_dn_r,
                     "lora": lora, "pc": pc, "cst": cst, "swab": swab})
    return maps


ARENA_SLOTS = 26


def build(nlayer=DEPTH, nblk=SEQ // TB, dbg=None, phases="rmsfon"):
    nc = bass.Bass("TRN2", target_bir_lowering=False)
    T = nblk * TB
    NT = T // 128
    L = DEPTH
    dr = lambda n, s, kind="ExternalInput": nc.dram_tensor(n, list(s), F32, kind=kind).ap()
    xT_d = dr("xT", [128, 8, SEQ])
    w_in_d = dr("w_in_r", [L, 128, 8, N_IN])
    w_out_d = dr("w_out_r", [L, 128, 8, D])
    w_up_d = dr("w_up_r", [L, 128, 8, 2 * DFF])
    w_dn_d = dr("w_dn_r", [L, 128, NFC, D])
    lora_d = dr("lora", [L, 128, 512])
    pc_d = dr("pc", [L, 128, NPC])
    cst_d = dr("cst", [128, NCST])
    swab_d = dr("swab", [128, 1024])
    out_d = dr("outT", [128, 8, SEQ], "ExternalOutput")
    dbg_d = dr("dbg", [128, 8, SEQ], "ExternalOutput") if dbg else None

    P = Prog(nc)
    RR = lambda ap: ap.bitcast(F32R) if FAST_MM else ap
    RR0 = RR
    P.make_banks()
    xT = P.sb("xT", 8 * T)
    kTh = P.sb("kTh", 2 * T)
    Vh = P.sb("Vh", NT * 256)
    negc = P.sb("negc", NT * 4)
    yT = P.sbs("yT", 8)
    cst = P.sb("cst", C_SWAM)
    pc = P.sb("pc", NPC)
    dv = P.sb("dv", NDV)
    rstm = P.sb("rstm", 4)
    Srw = P.sb("Srw", 2 * 2 * 2 * 64)
    pc1 = P.sb("pc1", 8)
    CN = P.sb("CN", 2 * 128)
    crw = P.sb("crw", 8)
    cml = P.sb("cml", 12)
    cffn = P.sb("cffn", NFC * 2)
    ccar = P.sb("ccar", 2)
    swaKc = P.sb("swaKc", 2 * 128)
    swaVc = P.sb("swaVc", 128)
    hTr = P.sbs("hTr", 8)
    ebks = P.sb("ebks", 16)
    wbufs = [P.sb(f"wch{i}", 8 * 128) for i in range(2)]
    P.make_arena(ARENA_SLOTS)

    A = slice(0, 128)
    H0 = slice(0, 64)
    H1 = slice(64, 128)
    R4 = slice(0, 4)
    xT3 = xT.v(A, "p (c t) -> p c t", c=8)
    yT3 = yT.v(A, "p (c t) -> p c t", c=8)
    kTh3 = kTh.v(A, "p (j t) -> p j t", j=2)
    Vh3 = Vh.v(A, "p (n c) -> p n c", c=256)
    negc3 = negc.v(A, "p (n h) -> p n h", h=4)
    swaKc3 = swaKc.v(A, "p (j t) -> p j t", j=2)
    hTr3 = hTr.v(A, "p (c t) -> p c t", c=8)
    CN3 = CN.v(H0, "p (j c) -> p j c", j=2)
    S5 = Srw.v(H0, "p (j h b v) -> p j h b v", j=2, h=2, b=2)

    def C(off, n, p=A):
        return cst[p, off:off + n]

    ident = C(C_IDENT, 128)
    evac_i = [0]

    def copy(out, in_, reads, writes, eng=None):
        if eng is None:
            evac_i[0] += 1
            eng = "act" if evac_i[0] % 2 else "dve"
        return P.cp(out, in_, reads, writes, eng=eng)

    wb_i = [0]

    def wchunk():
        b = wbufs[wb_i[0] % 2]
        wb_i[0] += 1
        return b

    P.dma(RR0(cst[A, :]), cst_d[:, 0:C_SWAM], writes=[cst], eng="pool")
    for c in range(8):
        P.dma(xT3[:, c, :], xT_d[:, c, 0:T], writes=[xT])
    if phases != "rmsfon":
        P.ts(RR(yT[A, :]), cst[A, 0:4096 if C_SWAM >= 4096 else 2048].to_broadcast([128, 4096]) if False else xT[A, 0:4096], 0.0, ALU.mult, reads=[xT], writes=[yT])
    P.ar_top = 0

    def rms_rstd(src_fn, reads, eps=1e-6):
        ssb = P.bank(pin=True)
        rstd = P.alloc()
        sq = [P.alloc(), P.alloc()]
        for c in range(8):
            s = sq[c % 2]
            P.act(s[A, :], src_fn(c), AF.Square, reads=reads, writes=[s])
            P.mm(ssb, ssb[A, :], C(C_ONESD, 128), s[A, :], reads=[cst, s])
        P.ts(rstd[A, :], ssb[A, :], eps, ALU.add, reads=[ssb], writes=[rstd])
        ssb.pinned = False
        P.recip(rstd[A, :], rstd[A, :], reads=[rstd], writes=[rstd])
        P.act(rstd[A, :], rstd[A, :], AF.Sqrt, reads=[rstd], writes=[rstd])
        P.ar_top -= 2
        return rstd

    for l in range(nlayer):
        P.dma(pc[A, :], pc_d[l], writes=[pc])
        P.ts(dv[A, DV_OMKA:DV_OMKA + 2], pc[A, PC_KA:PC_KA + 2], -1.0, ALU.mult, 1.0, ALU.add, reads=[pc], writes=[dv])
        P.ts(dv[R4, DV_BI15:DV_BI15 + 2], pc[R4, PC_BI:PC_BI + 2], 1.0 / 15.0, ALU.mult, reads=[pc], writes=[dv])
        P.ts(dv[A, DV_NFOXB:DV_NFOXB + 1], pc[A, PC_FOXB:PC_FOXB + 1], -1.0, ALU.mult, reads=[pc], writes=[dv])
        P.act(dv[A, DV_ESINK:DV_ESINK + 2], pc[A, PC_SINK:PC_SINK + 2], AF.Exp, reads=[pc], writes=[dv])
        for b_ in (Srw, CN, crw, cml, cffn, ccar, swaKc, swaVc):
            P.memset(b_[A, :], 0.0, writes=[b_])

        for blk in range(nblk):
            t0 = blk * TB
            bs = slice(t0, t0 + TB)
            P.ar_top = 0
            rstd = rms_rstd(lambda c: xT3[:, c, bs], [xT])
            bk = P.bank()
            for tt_ in range(4):
                P.tr(bk, bk[A, tt_ * 128:(tt_ + 1) * 128], rstd[A, tt_ * 128:(tt_ + 1) * 128], ident, reads=[rstd, cst])
            P.cp(rstm[A, 0:4], bk.v(A, "p (n c) -> p n c", n=4)[:, :, 0], reads=[bk], writes=[rstm])
            MIX_BASE = P.ar_top

            def fill_hTr():
                for c in range(8):
                    P.cp(RR(hTr3[:, c, :]), xT3[:, c, bs], reads=[xT], writes=[hTr.sub(c * 512, 512)], eng=("pool", "act")[c % 2])

            fill_hTr()
            gpre = pc[A, PC_GPRE:PC_GPRE + 8]

            def load_w(col0, M, dup=False):
                w = wchunk()
                w3 = w.v(A, "p (c m) -> p c m", c=8)
                if dup:
                    P.dma(RR(w3[:, :, 0:64]), w_in_d[l, :, :, col0:col0 + 64], writes=[w], eng="pool")
                    P.dma(RR(w3[:, :, 64:128]), w_in_d[l, :, :, col0:col0 + 64], writes=[w], eng="pool")
                    M = 128
                else:
                    P.dma(RR(w3[:, :, 0:M]), w_in_d[l, :, :, col0:col0 + M], writes=[w], eng="pool")
                P.tt(RR(w3[:, :, 0:M]), w3[:, :, 0:M], gpre.to_broadcast([128, 8, M]), ALU.mult, reads=[w, pc], writes=[w])
                return w, w3, M

            def proj_fm(col0, M, dst, dup=False, scale=None, rows=None, pbase=0):
                w, w3, M = load_w(col0, M, dup)
                bk = P.bank()
                r = slice(pbase, pbase + M)
                for c in range(8):
                    P.mm(bk, bk[r, :], w3[:, c, 0:M], hTr3[:, c, :], reads=[w, hTr], r=(pbase == 0))
                if scale is None:
                    P.tt(dst, bk[r, :], rstd[r, :], ALU.mult, reads=[bk, rstd], writes=[rows])
                else:
                    P.stt(dst, bk[r, :], scale, rstd[r, :], ALU.mult, ALU.mult, reads=[bk, rstd], writes=[rows])

            def proj_tm(col0, ncols, dst_fn, wr):
                w, w3, _ = load_w(col0, ncols)
                for tt_ in range(4):
                    bk = P.bank()
                    for c in range(8):
                        P.mm(bk, bk[A, 0:ncols], hTr3[:, c, tt_ * 128:(tt_ + 1) * 128], w3[:, c, 0:ncols], reads=[w, hTr], r=True)
                    P.ts(dst_fn(tt_), bk[A, 0:ncols], rstm[A, tt_:tt_ + 1], ALU.mult, reads=[bk, rstm], writes=[wr])

            def shift_mix(z, ci, dst):
                d = P.alloc()
                P.tt(d[A, 1:512], z[A, 0:511], z[A, 1:512], ALU.subtract, reads=[z], writes=[d])
                P.tt(d[A, 0:1], crw[A, ci:ci + 1], z[A, 0:1], ALU.subtract, reads=[z, crw], writes=[d])
                P.cp(crw[A, ci:ci + 1], z[A, 511:512], reads=[z], writes=[crw], eng="pool")
                P.stt(dst[A, :], d[A, :], pc[A, PC_MU + ci:PC_MU + ci + 1], z[A, :], ALU.mult, ALU.add,
                      reads=[d, pc, z], writes=[dst])
                P.ar_top -= 1

            def rwkv():
                lora = P.alloc()
                P.dma(lora[A, :], lora_d[l], writes=[lora])
                swa_ = P.alloc()
                sg = P.alloc()
                ztmp = P.alloc()
                proj_fm(768, 128, ztmp[A, :], rows=ztmp)
                shift_mix(ztmp, 6, swa_)
                proj_fm(896, 128, ztmp[A, :], rows=ztmp)
                shift_mix(ztmp, 7, sg)
                P.ar_top -= 1
                P.act(swa_[H0, :], swa_[H0, :], AF.Tanh, reads=[swa_], writes=[swa_])
                P.act(sg[A, :], sg[A, :], AF.Sigmoid, reads=[sg], writes=[sg])
                if RW_STOP == 1:
                    return
                base_top = P.ar_top
                for hp in range(2):
                    P.ar_top = base_top
                    z = [P.alloc() for _ in range(3)]
                    s = [P.alloc() for _ in range(3)]
                    for i in range(3):
                        proj_fm(i * 256 + hp * 128, 128, z[i][A, :], rows=z[i])
                    for i in range(3):
                        shift_mix(z[i], i * 2 + hp, s[i])
                    sr, sk, sv = s
                    cw = slice(hp * 128, (hp + 1) * 128)
                    pcc = lambda c0: pc[A, c0 + hp:c0 + hp + 1]
                    bk = P.bank()
                    P.mm(bk, bk[A, :], lora[H0, cw], swa_[H0, :], reads=[lora, swa_])
                    lw = z[0]
                    P.act(lw[A, :], bk[A, :], AF.Sigmoid, bias=pcc(PC_W0), reads=[bk, pc], writes=[lw])
                    P.ts(lw[A, :], lw[A, :], -math.exp(-0.5), ALU.mult, reads=[lw], writes=[lw])
                    bk = P.bank()
                    P.mm(bk, bk[A, :], lora[H1, cw], swa_[H1, :], reads=[lora, swa_])
                    a = z[1]
                    P.act(a[A, :], bk[A, :], AF.Sigmoid, bias=pcc(PC_A0), reads=[bk, pc], writes=[a])
                    bk = P.bank()
                    P.mm(bk, bk[A, :], lora[A, 256 + hp * 128:256 + (hp + 1) * 128], sg[A, :], reads=[lora, sg])
                    g = z[2]
                    copy(g[A, :], bk[A, :], [bk], [g])
                    kk = P.alloc()
                    tmp = P.alloc()
                    P.ts(kk[A, :], sk[A, :], pcc(PC_KK), ALU.mult, reads=[sk, pc], writes=[kk])
                    P.tt(tmp[A, :], kk[A, :], kk[A, :], ALU.mult, reads=[kk], writes=[tmp])
                    bk = P.bank()
                    P.mm(bk, bk[A, :], C(C_BLK1, 128), tmp[A, :], reads=[cst, tmp])
                    P.act(tmp[A, :], bk[A, :], AF.Sqrt, reads=[bk], writes=[tmp])
                    P.ts(tmp[A, :], tmp[A, :], 1e-12, ALU.max, reads=[tmp], writes=[tmp])
                    P.recip(tmp[A, :], tmp[A, :], reads=[tmp], writes=[tmp])
                    P.tt(kk[A, :], kk[A, :], tmp[A, :], ALU.mult, reads=[kk, tmp], writes=[kk])
                    kmod = P.alloc()
                    P.ts(kmod[A, :], a[A, :], pcc(PC_KA), ALU.mult, dv[A, DV_OMKA + hp:DV_OMKA + hp + 1], ALU.add,
                         reads=[a, pc, dv], writes=[kmod])
                    P.tt(kmod[A, :], kmod[A, :], sk[A, :], ALU.mult, reads=[kmod, sk], writes=[kmod])
                    bonus = sk
                    P.stt(tmp[A, :], sr[A, :], pcc(PC_RK), kmod[A, :], ALU.mult, ALU.mult, reads=[sr, pc, kmod], writes=[tmp])
                    bk = P.bank()
                    P.mm(bk, bk[A, :], C(C_BLK1, 128), tmp[A, :], reads=[cst, tmp])
                    P.tt(bonus[A, :], bk[A, :], sv[A, :], ALU.mult, reads=[bk, sv], writes=[bonus])
                    bb = a
                    P.tt(bb[A, :], kk[A, :], a[A, :], ALU.mult, reads=[kk, a], writes=[bb])
                    cum = P.alloc()
                    ones = C(C_ALLONE, 64)
                    for c in range(8):
                        cs = slice(c * 64, (c + 1) * 64)
                        P.scan(cum[A, cs], ones, lw[A, cs], 0.0, ALU.mult, ALU.add, reads=[cst, lw], writes=[cum])
                    cumx = lw
                    P.tt(cumx[A, :], cum[A, :], lw[A, :], ALU.subtract, reads=[cum, lw], writes=[cumx])
                    Ep = P.alloc()
                    Em = P.alloc()
                    Ee = P.alloc()
                    P.act(Ep[A, :], cum[A, :], AF.Exp, reads=[cum], writes=[Ep])
                    P.act(Em[A, :], cum[A, :], AF.Exp, scale=-1.0, reads=[cum], writes=[Em])
                    P.act(cumx[A, :], cumx[A, :], AF.Exp, reads=[cumx], writes=[cumx])
                    for c in range(8):
                        cs = slice(c * 64, (c + 1) * 64)
                        P.act(Ee[A, cs], cum[A, cs], AF.Exp, bias=cum[A, c * 64 + 63:c * 64 + 64], scale=-1.0,
                              reads=[cum], writes=[Ee])
                    AR = P.alloc(1024)
                    AR3 = AR.v(A, "p (a t) -> p a t", a=2)
                    P.stt(AR3[:, 0, :], kk[A, :], -1.0, cumx[A, :], ALU.mult, ALU.mult, reads=[kk, cumx], writes=[AR])
                    P.tt(AR3[:, 1, :], sr[A, :], Ep[A, :], ALU.mult, reads=[sr, Ep], writes=[AR])
                    Bt, Kt, Bte, Kte = cum, tmp, kk, kmod
                    P.tt(Bt[A, :], bb[A, :], Em[A, :], ALU.mult, reads=[bb, Em], writes=[Bt])
                    P.tt(Kt[A, :], kmod[A, :], Em[A, :], ALU.mult, reads=[kmod, Em], writes=[Kt])
                    P.tt(Bte[A, :], bb[A, :], Ee[A, :], ALU.mult, reads=[bb, Ee], writes=[Bte])
                    P.tt(Kte[A, :], kmod[A, :], Ee[A, :], ALU.mult, reads=[kmod, Ee], writes=[Kte])
                    yr = Em
                    AR1 = Buf(z[0].t, z[0].off, 1024, z[0].k + z[1].k)
                    AR13 = AR1.v(A, "p (a t) -> p a t", a=2)
                    Bt1, Kt1 = sr, P.alloc()
                    idn1 = cst[H1, C_IDENT + 64:C_IDENT + 128]
                    for src, dst, wr in ((AR3[H1, 0, :], AR13[H0, 0, :], AR1), (AR3[H1, 1, :], AR13[H0, 1, :], AR1),
                                         (Bt[H1, :], Bt1[H0, :], Bt1), (Kt[H1, :], Kt1[H0, :], Kt1)):
                        bk = P.bank()
                        P.mm(bk, bk[H0, :], idn1, src, reads=[cst, AR, Bt, Kt])
                        copy(dst, bk[H0, :], [bk], [wr])
                    bk = P.bank()
                    P.mm(bk, bk[H0, 0:8], idn1, Ep.v(H1, "p (c t) -> p c t", c=8)[:, :, 63], reads=[cst, Ep])
                    copy(pc1[H0, 0:8], bk[H0, 0:8], [bk], [pc1])
                    ARh = (AR3, AR13)
                    Bth = (Bt, Bt1)
                    Kth = (Kt, Kt1)
                    ARb = (AR, AR1)
                    if RW_STOP == 2:
                        continue
                    NA = [P.alloc() for _ in range(2)]
                    nmslot = P.alloc()
                    NMp = [nmslot.sub(0, 256), nmslot.sub(256, 256)]
                    gslot = P.alloc()
                    Gp = [gslot.sub(0, 128), gslot.sub(128, 128)]
                    RU = [gslot.sub(256, 256), Ee.sub(0, 256)]
                    TM = [P.alloc() for _ in range(2)]
                    for c in range(8):
                        if RW_STOP == 6:
                            continue
                        cs = slice(c * 64, (c + 1) * 64)
                        na = NA[c % 2]
                        na5 = na.v(H0, "p (h b a t) -> p h b a t", h=2, b=2, a=2)
                        bk = P.bank()
                        bk5 = bk.v(H0, "p (h b a t) -> p h b a t", h=2, b=2, a=2)
                        bkm = P.bank()
                        bkm3 = bkm.v(H0, "p (x h t) -> p x h t", x=4, h=2)
                        for h2 in range(1 if RW_STOP == 7 else 2):
                            ph = slice(h2 * 64, h2 * 64 + 64)
                            for a_ in range(2):
                                P.mm(bk, bk5[:, h2, 0, a_, :], Bth[h2][H0, cs], ARh[h2][H0, a_, cs], reads=[Bth[h2], ARb[h2]])
                                P.mm(bk, bk5[:, h2, 1, a_, :], Kth[h2][H0, cs], ARh[h2][H0, a_, cs], reads=[Kth[h2], ARb[h2]])
                            P.mm(bkm, bkm3[:, 0, h2, :], ARh[h2][H0, 0, cs], Bth[h2][H0, cs], reads=[Bth[h2], ARb[h2]])
                        P.tt(na[H0, :], bk[H0, :], C(C_MA, 512, H0), ALU.mult, reads=[bk, cst], writes=[na])
                        if RW_STOP in (3, 7):
                            continue
                        nm = NMp[0]
                        nm4 = nm.v(H0, "p (x h t) -> p x h t", x=2, h=2)
                        P.cp(nm4[:, 0, :, :], na5[:, :, 0, 0, :], reads=[na], writes=[nm], eng="pool")
                        P.tt(nm4[:, 1, :, :], bkm3[:, 0, :, :], C(C_MB, 128, H0).rearrange("p (h t) -> p h t", h=2),
                             ALU.mult, reads=[bkm, cst], writes=[nm])
                        G3_0 = Gp[0].v(H0, "p (h t) -> p h t", h=2)
                        P.tt(G3_0, na5[:, :, 0, 0, :], C(C_IDG, 128, H0).rearrange("p (h t) -> p h t", h=2), ALU.add,
                             reads=[na, cst], writes=[Gp[0]])
                        cur = 0
                        for lev in range(5):
                            nmc = NMp[cur]
                            nmc4 = nmc.v(H0, "p (x h t) -> p x h t", x=2, h=2)
                            nmn = NMp[1 - cur]
                            nmn4 = nmn.v(H0, "p (x h t) -> p x h t", x=2, h=2)
                            Gc = Gp[cur]
                            Gc3 = Gc.v(H0, "p (h t) -> p h t", h=2)
                            Gn = Gp[1 - cur]
                            bkp = P.bank()
                            bkp4 = bkp.v(H0, "p (x h t) -> p x h t", x=4, h=2)
                            for h2 in range(2):
                                P.mm(bkp, bkp4[:, 0, h2, :], nmc4[:, 1, h2, :], nmc4[:, 0, h2, :], reads=[nmc])
                                P.mm(bkp, bkp4[:, 1, h2, :], nmc4[:, 0, h2, :], nmc4[:, 1, h2, :], reads=[nmc])
                            copy(nmn[H0, :], bkp[H0, 0:256], [bkp], [nmn])
                            bkg = P.bank()
                            for h2 in range(2):
                                P.mm(bkg, bkg[H0, h2 * 64:(h2 + 1) * 64], nmn4[:, 1, h2, :], Gc3[:, h2, :], reads=[nmn, Gc])
                            P.tt(Gn[H0, :], bkg[H0, 0:128], Gc[H0, :], ALU.add, reads=[bkg, Gc], writes=[Gn])
                            cur = 1 - cur
                        Gf = Gp[cur]
                        Gf3 = Gf.v(H0, "p (h t) -> p h t", h=2)
                        if RW_STOP == 4:
                            continue
                        tm = TM[c % 2]
                        bkt = P.bank()
                        P.tr(bkt, bkt[H0, 0:128], Bte[A, cs], ident, reads=[Bte, cst])
                        P.tr(bkt, bkt[H0, 128:256], Kte[A, cs], ident, reads=[Kte, cst])
                        P.tr(bkt, bkt[H0, 256:384], sv[A, cs], ident, reads=[sv, cst])
                        copy(tm[H0, 0:384], bkt[H0, 0:384], [bkt], [tm])
                        if RW_STOP == 5:
                            continue
                        ru = RU[c % 2]
                        sb_old = c % 2
                        sb_new = 1 - sb_old
                        bkr = P.bank()
                        for h2 in range(2):
                            ph = slice(h2 * 64, h2 * 64 + 64)
                            o = bkr[H0, h2 * 64:(h2 + 1) * 64]
                            P.mm(bkr, o, ARh[h2][H0, 0, cs], S5[:, hp, h2, sb_old, :], reads=[ARb[h2], Srw])
                            P.mm(bkr, o, na5[:, h2, 1, 0, :], tm[H0, 256 + h2 * 64:256 + (h2 + 1) * 64], reads=[na, tm])
                        copy(ru[H0, 0:128], bkr[H0, 0:128], [bkr], [ru], eng="act")
                        bku = P.bank()
                        for h2 in range(2):
                            P.mm(bku, bku[H0, h2 * 64:(h2 + 1) * 64], Gf3[:, h2, :], ru[H0, h2 * 64:(h2 + 1) * 64], reads=[Gf, ru])
                        copy(ru[H0, 128:256], bku[H0, 0:128], [bku], [ru], eng="dve")
                        bky = P.bank()
                        for h2 in range(2):
                            ph = slice(h2 * 64, h2 * 64 + 64)
                            o = bky[ph, 0:64]
                            P.mm(bky, o, S5[:, hp, h2, sb_old, :], ARh[h2][H0, 1, cs], reads=[ARb[h2], Srw])
                            P.mm(bky, o, ru[H0, 128 + h2 * 64:128 + (h2 + 1) * 64], na5[:, h2, 0, 1, :], reads=[ru, na])
                            P.mm(bky, o, tm[H0, 256 + h2 * 64:256 + (h2 + 1) * 64], na5[:, h2, 1, 1, :], reads=[tm, na])
                        copy(yr[A, cs], bky[A, 0:64], [bky], [yr], eng="act")
                        bks = P.bank()
                        for h2 in range(2):
                            o = bks[H0, h2 * 64:(h2 + 1) * 64]
                            P.mm(bks, o, tm[H0, h2 * 64:(h2 + 1) * 64], ru[H0, 128 + h2 * 64:128 + (h2 + 1) * 64], reads=[tm, ru])
                            P.mm(bks, o, tm[H0, 128 + h2 * 64:128 + (h2 + 1) * 64], tm[H0, 256 + h2 * 64:256 + (h2 + 1) * 64], reads=[tm])
                        for h2 in range(2):
                            pcs = Ep[H0, c * 64 + 63:c * 64 + 64] if h2 == 0 else pc1[H0, c:c + 1]
                            P.stt(S5[:, hp, h2, sb_new, :], S5[:, hp, h2, sb_old, :], pcs, bks[H0, h2 * 64:(h2 + 1) * 64],
                                  ALU.mult, ALU.add, reads=[Srw, Ep, pc1, bks], writes=[Srw])
                    t1 = Ep
                    bk = P.bank()
                    P.mm(bk, bk[A, :], C(C_BLKM, 128), yr[A, :], reads=[cst, yr])
                    P.tt(yr[A, :], yr[A, :], bk[A, :], ALU.subtract, reads=[yr, bk], writes=[yr])
                    P.tt(t1[A, :], yr[A, :], yr[A, :], ALU.mult, reads=[yr], writes=[t1])
                    bk = P.bank()
                    P.mm(bk, bk[A, :], C(C_BLKM, 128), t1[A, :], reads=[cst, t1])
                    P.ts(t1[A, :], bk[A, :], 64e-5, ALU.add, reads=[bk], writes=[t1])
                    P.recip(t1[A, :], t1[A, :], reads=[t1], writes=[t1])
                    P.act(t1[A, :], t1[A, :], AF.Sqrt, reads=[t1], writes=[t1])
                    P.tt(yr[A, :], yr[A, :], t1[A, :], ALU.mult, reads=[yr, t1], writes=[yr])
                    P.ts(yr[A, :], yr[A, :], pcc(PC_LNW), ALU.mult, pcc(PC_LNB), ALU.add, reads=[yr, pc], writes=[yr])
                    P.tt(yr[A, :], yr[A, :], bonus[A, :], ALU.add, reads=[yr, bonus], writes=[yr])
                    P.tt(RR(yT3[:, hp, :]), yr[A, :], g[A, :], ALU.mult, reads=[yr, g], writes=[yT.sub(hp * 512, 512)])

            P.ar_top = MIX_BASE
            if "r" in phases:
                rwkv()

            def mlstm():
                qk = [P.alloc() for _ in range(4)]
                vones = P.alloc()
                vo3 = vones.v(H0, "p (h c) -> p h c", h=4)
                P.memset(vones[H0, :], 1.0, writes=[vones])
                vT = [P.alloc(), P.alloc()]
                oT = [P.alloc(), P.alloc()]
                gi, gf, bcum, ge = (P.alloc() for _ in range(4))
                top = P.ar_top
                for i in range(4):
                    P.ar_top = top
                    uext = P.alloc(515)
                    acc = P.alloc()
                    proj_fm(1024 + i * 64, 64, uext[H0, 3:515], rows=uext)
                    P.cp(uext[H0, 0:3], cml[H0, i * 3:(i + 1) * 3], reads=[cml], writes=[uext], eng="pool")
                    wc = lambda j: pc[H0, PC_CW + i * 4 + j:PC_CW + i * 4 + j + 1]
                    P.ts(acc[H0, :], uext[H0, 0:512], wc(0), ALU.mult, reads=[uext, pc], writes=[acc])
                    for j in range(1, 4):
                        P.stt(acc[H0, :], uext[H0, j:j + 512], wc(j), acc[H0, :], ALU.mult, ALU.add, reads=[uext, pc, acc], writes=[acc])
                    P.cp(cml[H0, i * 3:(i + 1) * 3], uext[H0, 512:515], reads=[uext], writes=[cml], eng="pool")
                    P.act(qk[i][H0, :], acc[H0, :], AF.Silu, bias=pc[H0, PC_CB + i:PC_CB + i + 1], reads=[acc, pc], writes=[qk[i]])
                    if i < 2:
                        P.ts(qk[i][H0, :], qk[i][H0, :], 32.0 ** -0.5, ALU.mult, reads=[qk[i]], writes=[qk[i]])
                P.ar_top = top
                for hp in range(2):
                    proj_fm(1280 + hp * 128, 128, vT[hp][A, :], rows=vT[hp])
                    proj_fm(1536 + hp * 128, 128, oT[hp][A, :], rows=oT[hp])
                    P.act(oT[hp][A, :], oT[hp][A, :], AF.Sigmoid, reads=[oT[hp]], writes=[oT[hp]])
                proj_fm(1792, 4, gi[R4, :], rows=gi)
                proj_fm(1796, 4, gf[R4, :], rows=gf)
                P.act(gi[R4, :], gi[R4, :], AF.Tanh, bias=dv[R4, DV_BI15:DV_BI15 + 1], scale=1.0 / 15.0, reads=[gi, dv], writes=[gi])
                P.act(gf[R4, :], gf[R4, :], AF.Tanh, bias=dv[R4, DV_BF15:DV_BF15 + 1], scale=1.0 / 15.0, reads=[gf, dv], writes=[gf])
                P.ts(gi[R4, :], gi[R4, :], 15.0, ALU.mult, reads=[gi], writes=[gi])
                P.act(gf[R4, :], gf[R4, :], AF.Exp, scale=-15.0, reads=[gf], writes=[gf])
                P.act(gf[R4, :], gf[R4, :], AF.Ln, bias=1.0, reads=[gf], writes=[gf])
                ones4 = C(C_ALLONE, 64, R4)
                for c in range(8):
                    cs = slice(c * 64, (c + 1) * 64)
                    P.scan(bcum[R4, cs], ones4, gf[R4, cs], 0.0, ALU.mult, ALU.subtract, reads=[cst, gf], writes=[bcum])
                P.tt(gi[R4, :], gi[R4, :], bcum[R4, :], ALU.subtract, reads=[gi, bcum], writes=[gi])
                P.act(gi[R4, :], gi[R4, :], AF.Exp, reads=[gi], writes=[gi])
                P.act(bcum[R4, :], bcum[R4, :], AF.Exp, reads=[bcum], writes=[bcum])
                for c in range(8):
                    cs = slice(c * 64, (c + 1) * 64)
                    P.ts(ge[R4, cs], gi[R4, cs], bcum[R4, c * 64 + 63:c * 64 + 64], ALU.mult, reads=[gi, bcum], writes=[ge])
                kp = [P.alloc(), P.alloc()]
                kpe = [P.alloc(), P.alloc()]
                ebbc = [P.alloc(), P.alloc()]
                for j in range(2):
                    selk = C(C_SELK + j * 64, 64, R4)
                    bk = P.bank()
                    P.mm(bk, bk[H0, :], selk, gi[R4, :], reads=[cst, gi])
                    P.tt(kp[j][H0, :], qk[2 + j][H0, :], bk[H0, :], ALU.mult, reads=[qk[2 + j], bk], writes=[kp[j]])
                    bk = P.bank()
                    P.mm(bk, bk[H0, :], selk, ge[R4, :], reads=[cst, ge])
                    P.tt(kpe[j][H0, :], qk[2 + j][H0, :], bk[H0, :], ALU.mult, reads=[qk[2 + j], bk], writes=[kpe[j]])
                    bk = P.bank()
                    P.mm(bk, bk[H0, :], selk, bcum[R4, :], reads=[cst, bcum])
                    P.cp(ebks[H0, j * 8:(j + 1) * 8], bk.v(H0, "p (c t) -> p c t", c=8)[:, :, 63], reads=[bk], writes=[ebks])
                    bk = P.bank()
                    P.mm(bk, bk[A, :], C(C_SELV + j * 128, 128, R4), bcum[R4, :], reads=[cst, bcum])
                    copy(ebbc[j][A, :], bk[A, :], [bk], [ebbc[j]])
                NTs = P.alloc(1024)
                DNs = P.alloc(1024)
                NT3 = NTs.v(A, "p (j t) -> p j t", j=2)
                DN3 = DNs.v(A, "p (j t) -> p j t", j=2)
                sm = P.alloc()
                qmc = P.alloc(256)
                for c in range(8):
                    cs = slice(c * 64, (c + 1) * 64)
                    bkt = P.bank()
                    P.tr(bkt, bkt[H0, 0:128], vT[0][A, cs], ident, reads=[vT[0], cst])
                    P.tr(bkt, bkt[H0, 128:256], vT[1][A, cs], ident, reads=[vT[1], cst])
                    copy(vo3[:, :, 0:64], bkt.v(H0, "p (h c) -> p h c", h=8)[:, 0:4, :], [bkt], [vones])
                    bkt2 = P.bank()
                    P.tr(bkt2, bkt2[H0, 0:64], kpe[0][H0, cs], C(C_IDENT, 64, H0), reads=[kpe[0], cst])
                    P.tr(bkt2, bkt2[H0, 64:128], kpe[1][H0, cs], C(C_IDENT, 64, H0), reads=[kpe[1], cst])
                    copy(sm[H0, 256:384], bkt2[H0, 0:128], [bkt2], [sm])
                    for h in range(4):
                        P.ts(qmc[H0, h * 64:(h + 1) * 64], qk[h // 2][H0, cs], C(C_HM + h % 2, 1, H0), ALU.mult,
                             reads=[qk[h // 2], cst], writes=[qmc])
                    bka = P.bank()
                    for h in range(4):
                        j = h // 2
                        P.mm(bka, bka[H0, h * 64:(h + 1) * 64], kp[j][H0, cs], qmc[H0, h * 64:(h + 1) * 64], reads=[kp[j], qmc])
                    P.tt(sm[H0, 0:256], bka[H0, 0:256], C(C_MM, 256, H0), ALU.mult, reads=[bka, cst], writes=[sm])
                    bn = P.bank()
                    bd = P.bank()
                    for h in range(4):
                        j = h // 2
                        pq = slice((h % 2) * 32, (h % 2) * 32 + 32)
                        ph = slice((h % 2) * 64, (h % 2) * 64 + 64)
                        at = sm[H0, h * 64:(h + 1) * 64]
                        qm = qmc[H0, h * 64:(h + 1) * 64]
                        P.mm(bn, bn[ph, j * 64:(j + 1) * 64], vo3[:, h, 0:64], at, reads=[vones, sm])
                        P.mm(bn, bn[ph, j * 64:(j + 1) * 64], CN3[:, j, 0:64], qm, reads=[CN, qmc])
                        P.mm(bd, bd[ph, j * 64:(j + 1) * 64], vo3[:, h, 64:128], at, reads=[vones, sm])
                        P.mm(bd, bd[ph, j * 64:(j + 1) * 64], CN3[:, j, 64:128], qm, reads=[CN, qmc])
                    copy(NT3[:, :, cs], bn.v(A, "p (j t) -> p j t", j=8)[:, 0:2, :], [bn], [NTs], eng="act")
                    copy(DN3[:, :, cs], bd.v(A, "p (j t) -> p j t", j=8)[:, 0:2, :], [bd], [DNs], eng="act")
                    bs_ = P.bank()
                    for h in range(4):
                        j = h // 2
                        P.mm(bs_, bs_[H0, h * 128:(h + 1) * 128], sm[H0, 256 + j * 64:256 + (j + 1) * 64], vo3[:, h, :], reads=[sm, vones])
                    for j in range(2):
                        P.ts(CN3[:, j, :], CN3[:, j, :], ebks[H0, j * 8 + c:j * 8 + c + 1], ALU.mult, reads=[CN, ebks], writes=[CN])
                        for h2 in range(2):
                            h = 2 * j + h2
                            P.stt(CN3[:, j, :], bs_[H0, h * 128:(h + 1) * 128], C(C_HM + h2, 1, H0), CN3[:, j, :],
                                  ALU.mult, ALU.add, reads=[CN, bs_, cst], writes=[CN])
                for j in range(2):
                    d1 = DNs.sub(j * 512, 512)
                    n1 = NTs.sub(j * 512, 512)
                    P.tt(d1[A, :], d1[A, :], ebbc[j][A, :], ALU.mult, reads=[d1, ebbc[j]], writes=[d1])
                    P.stt(d1[A, :], d1[A, :], -1.0, d1[A, :], ALU.mult, ALU.max, reads=[d1], writes=[d1])
                    P.ts(d1[A, :], d1[A, :], 1.0, ALU.max, reads=[d1], writes=[d1])
                    P.recip(d1[A, :], d1[A, :], reads=[d1], writes=[d1])
                    P.tt(n1[A, :], n1[A, :], ebbc[j][A, :], ALU.mult, reads=[n1, ebbc[j]], writes=[n1])
                    P.tt(n1[A, :], n1[A, :], d1[A, :], ALU.mult, reads=[n1, d1], writes=[n1])
                    P.tt(d1[A, :], n1[A, :], n1[A, :], ALU.mult, reads=[n1], writes=[d1])
                    bk = P.bank()
                    P.mm(bk, bk[A, :], C(C_BLKM, 128), d1[A, :], reads=[cst, d1])
                    P.ts(d1[A, :], bk[A, :], 1e-6, ALU.add, reads=[bk], writes=[d1])
                    P.recip(d1[A, :], d1[A, :], reads=[d1], writes=[d1])
                    P.act(d1[A, :], d1[A, :], AF.Sqrt, reads=[d1], writes=[d1])
                    P.tt(n1[A, :], n1[A, :], d1[A, :], ALU.mult, reads=[n1, d1], writes=[n1])
                    P.stt(RR(yT3[:, 2 + j, :]), n1[A, :], pc[A, PC_MNORM + j:PC_MNORM + j + 1], oT[j][A, :], ALU.mult, ALU.mult,
                          reads=[n1, pc, oT[j]], writes=[yT.sub((2 + j) * 512, 512)])

            P.ar_top = MIX_BASE
            if "m" in phases:
                mlstm()

            def swa():
                swaB = P.alloc(1024)
                mtmp = P.alloc(1024)
                P.dma(swaB[A, :], swab_d, writes=[swaB])
                P.dma(mtmp[A, :], cst_d[:, C_SWAM:C_SWAM + 1024], writes=[mtmp])
                P.tt(swaB[A, :], swaB[A, :], mtmp[A, :], ALU.add, reads=[swaB, mtmp], writes=[swaB])
                P.ar_top -= 2
                swaB4 = swaB.v(A, "p (c h q) -> p c h q", c=2, h=4)
                swaK = P.alloc(1280)
                swaV = P.alloc(640)
                swaK3 = swaK.v(A, "p (j t) -> p j t", j=2)
                swaV3 = swaV.v(A, "p (n c) -> p n c", n=5)
                P.cp(swaK3[:, :, 0:128], swaKc3[:, :, :], reads=[swaKc], writes=[swaK], eng="pool")
                P.cp(swaV3[:, 0, :], swaVc[A, :], reads=[swaVc], writes=[swaV], eng="pool")
                qS = [P.alloc(), P.alloc()]
                for j in range(2):
                    proj_fm(1800 + j * 128, 128, qS[j][A, :], scale=0.125, rows=qS[j])
                    proj_fm(2056 + j * 64, 64, swaK3[:, j, 128:640], dup=True, rows=swaK)
                proj_tm(2184, 128, lambda tt_: swaV3[:, 1 + tt_, :], swaV)
                ETs = [P.alloc(1024), P.alloc(1024)]
                tmp = P.alloc()
                for i in range(4):
                    gi_ = blk * 4 + i
                    qc = slice(i * 128, (i + 1) * 128)
                    prev = slice(i * 128, (i + 1) * 128)
                    cur_ = slice((i + 1) * 128, (i + 2) * 128)
                    ET = ETs[i % 2]
                    bp = P.bank() if gi_ > 0 else None
                    bc = P.bank()
                    for hq in range(4):
                        j = hq // 2
                        ph = slice((hq % 2) * 64, (hq % 2) * 64 + 64)
                        o = slice(hq * 128, (hq + 1) * 128)
                        if gi_ > 0:
                            P.mm(bp, bp[A, o], swaK3[ph, j, prev], qS[j][ph, qc], reads=[swaK, qS[j]])
                            P.mm(bp, bp[A, o], ident, swaB4[:, 0, hq, :], reads=[cst, swaB])
                        P.mm(bc, bc[A, o], swaK3[ph, j, cur_], qS[j][ph, qc], reads=[swaK, qS[j]])
                        P.mm(bc, bc[A, o], ident, swaB4[:, 1, hq, :], reads=[cst, swaB])
                    if gi_ > 0:
                        P.act(ET[A, 0:512], bp[A, :], AF.Exp, reads=[bp], writes=[ET])
                    P.act(ET[A, 512:1024], bc[A, :], AF.Exp, reads=[bc], writes=[ET])
                    bo = P.bank()
                    bd = P.bank()
                    for hq in range(4):
                        j = hq // 2
                        ph = slice((hq % 2) * 64, (hq % 2) * 64 + 64)
                        o = slice(j * 128, (j + 1) * 128)
                        e = slice(hq * 128, (hq + 1) * 128)
                        if gi_ > 0:
                            P.mm(bo, bo[ph, o], swaV3[:, i, j * 64:(j + 1) * 64], ET[A, e.start:e.stop], reads=[swaV, ET])
                            P.mm(bd, bd[ph, o], C(C_ALLONE, 64), ET[A, e.start:e.stop], reads=[cst, ET])
                        P.mm(bo, bo[ph, o], swaV3[:, i + 1, j * 64:(j + 1) * 64], ET[A, 512 + e.start:512 + e.stop], reads=[swaV, ET])
                        P.mm(bd, bd[ph, o], C(C_ALLONE, 64), ET[A, 512 + e.start:512 + e.stop], reads=[cst, ET])
                    for j in range(2):
                        P.ts(tmp[A, j * 128:(j + 1) * 128], bd[A, j * 128:(j + 1) * 128], dv[A, DV_ESINK + j:DV_ESINK + j + 1], ALU.add,
                             reads=[bd, dv], writes=[tmp])
                    P.recip(tmp[A, 0:256], tmp[A, 0:256], reads=[tmp], writes=[tmp])
                    P.tt(RR(yT3[:, 4:6, qc]), bo.v(A, "p (j q) -> p j q", j=4)[:, 0:2, :], tmp.v(A, "p (j q) -> p j q", j=4)[:, 0:2, :],
                         ALU.mult, reads=[bo, tmp], writes=[yT.sub(4 * 512, 1024)])
                P.cp(swaKc3[:, :, :], swaK3[:, :, 512:640], reads=[swaK], writes=[swaKc], eng="pool")
                P.cp(swaVc[A, :], swaV3[:, 4, :], reads=[swaV], writes=[swaVc], eng="pool")

            P.ar_top = MIX_BASE
            if "s" in phases:
                swa()

            def fox():
                qtmp = [P.alloc(), P.alloc()]
                for j in range(2):
                    proj_fm(2312 + j * 128, 128, qtmp[j][A, :], scale=0.125, rows=qtmp[j])
                    proj_fm(2568 + j * 128, 128, RR(kTh3[:, j, bs]), rows=kTh)
                for half in range(2):
                    proj_tm(2824 + half * 128, 128, lambda tt_: RR(Vh3[:, blk * 4 + tt_, half * 128:(half + 1) * 128]), Vh)
                fr = P.alloc()
                crow = P.alloc()
                vtmp = P.alloc()
                chi = hTr.sub(2 * 512, 512)
                clo = hTr.sub(3 * 512, 512)
                qF = [hTr.sub(0, 512), hTr.sub(512, 512)]
                for R_ in (slice(0, 4), slice(64, 68)):
                    proj_fm(3080, 4, fr[R_, :], rows=fr, pbase=R_.start)
                for R_ in (slice(0, 4), slice(64, 68)):
                    P.act(fr[R_, :], fr[R_, :], AF.Exp, bias=dv[R_, DV_NFOXB:DV_NFOXB + 1], scale=-1.0, reads=[fr, dv], writes=[fr])
                    P.act(fr[R_, :], fr[R_, :], AF.Ln, bias=1.0, reads=[fr], writes=[fr])
                    for sg_ in range(4):
                        seg = slice(sg_ * 128, (sg_ + 1) * 128)
                        init = ccar[R_, 0:1] if sg_ == 0 else crow[R_, sg_ * 128 - 1:sg_ * 128]
                        P.scan(crow[R_, seg], C(C_ALLONE, 128, R_), fr[R_, seg], init, ALU.mult, ALU.subtract,
                               reads=[fr, ccar, cst, crow], writes=[crow])
                    P.cp(ccar[R_, 0:1], crow[R_, 511:512], reads=[crow], writes=[ccar], eng="pool")
                    P.ts(fr[R_, :], crow[R_, :], 4097.0, ALU.mult, reads=[crow], writes=[fr])
                    P.tt(vtmp[R_, :], fr[R_, :], crow[R_, :], ALU.subtract, reads=[fr, crow], writes=[vtmp])
                    P.tt(RR(chi[R_, :]), fr[R_, :], vtmp[R_, :], ALU.subtract, reads=[fr, vtmp], writes=[chi])
                    P.tt(RR(clo[R_, :]), crow[R_, :], chi[R_, :], ALU.subtract, reads=[crow, chi], writes=[clo])
                bk = P.bank()
                for tt_ in range(4):
                    P.mm(bk, bk[A, tt_ * 4:(tt_ + 1) * 4], crow[R4, tt_ * 128:(tt_ + 1) * 128], C(C_IDENT, 4, R4), reads=[crow, cst])
                P.ts(negc3[:, blk * 4:(blk + 1) * 4, :], bk.v(A, "p (n h) -> p n h", h=4)[:, 0:4, :], -1.0, ALU.mult,
                     reads=[bk], writes=[negc])
                for j in range(2):
                    P.cp(RR(qF[j][A, :]), qtmp[j][A, :], reads=[qtmp[j]], writes=[qF[j]], eng=("act", "pool")[j])
                ETs = [hTr.sub((4 + i_) * 512, 512) for i_ in range(3)]
                rec = P.alloc()
                ei = 0
                for j in range(2):
                    bo = P.bank(pin=True)
                    bd = P.bank(pin=True)
                    for h2 in range(2):
                        h = 2 * j + h2
                        ph = slice(h2 * 64, h2 * 64 + 64)
                        for kb in range(blk * 4 + 4):
                            q0 = max(0, kb - blk * 4) * 128
                            nq = 512 - q0
                            bs_ = P.bank()
                            P.mm(bs_, bs_[A, 0:nq], kTh3[ph, j, kb * 128:(kb + 1) * 128], qF[j][ph, q0:512], reads=[kTh, qF[j]], r=True)
                            Rh = slice(h2 * 64, h2 * 64 + 4)
                            P.mm(bs_, bs_[A, 0:nq], C(C_SELF + h * 128, 128, Rh), chi[Rh, q0:512], reads=[cst, chi], r=True)
                            P.mm(bs_, bs_[A, 0:nq], C(C_SELF + h * 128, 128, Rh), clo[Rh, q0:512], reads=[cst, clo], r=True)
                            et = ETs[ei % 3]
                            ei += 1
                            P.act(RR(et[A, 0:nq]), bs_[A, 0:nq], AF.Exp, bias=negc3[:, kb, h:h + 1], reads=[bs_, negc], writes=[et])
                            if kb >= blk * 4:
                                P.tt(RR(et[A, 0:128]), et[A, 0:128], C(C_TRI, 128), ALU.mult, reads=[et, cst], writes=[et], eng="pool")
                            P.mm(bo, bo[ph, q0:512], Vh3[:, kb, h * 64:(h + 1) * 64], et[A, 0:nq], reads=[Vh, et], r=(h2 == 0))
                            P.mm(bd, bd[ph, q0:512], C(C_ALLONE, 64), et[A, 0:nq], reads=[cst, et], r=(h2 == 0))
                    P.recip(rec[A, :], bd[A, :], reads=[bd], writes=[rec])
                    P.tt(RR(yT3[:, 6 + j, :]), bo[A, :], rec[A, :], ALU.mult, reads=[bo, rec], writes=[yT.sub((6 + j) * 512, 512)])
                    bo.pinned = False
                    bd.pinned = False

            P.ar_top = MIX_BASE
            if "f" in phases:
                fox()

            if dbg == ("yT", l):
                for c in range(8):
                    P.dma(dbg_d[:, c, bs], yT3[:, c, :], reads=[yT])

            def post_residual(oT3, oT, gcol):
                rs = rms_rstd(lambda c: oT3[:, c, :], [oT])
                tmps = [P.alloc(), P.alloc()]
                for m in range(8):
                    t_ = tmps[m % 2]
                    P.stt(t_[A, :], oT3[:, m, :], pc[A, gcol + m:gcol + m + 1], rs[A, :], ALU.mult, ALU.mult,
                          reads=[oT, pc, rs], writes=[t_])
                    P.tt(xT3[:, m, bs], xT3[:, m, bs], t_[A, :], ALU.add, reads=[xT, t_], writes=[xT])

            if "o" not in phases:
                continue
            P.ar_top = 0
            oT = P.alloc(4096)
            oT3 = oT.v(A, "p (c t) -> p c t", c=8)
            for m in range(8):
                w = wchunk()
                w3 = w.v(A, "p (c m) -> p c m", c=8)
                P.dma(RR(w3[:, :, :]), w_out_d[l, :, :, m * 128:(m + 1) * 128], writes=[w], eng="pool")
                bk = P.bank()
                for j in range(8):
                    P.mm(bk, bk[A, :], w3[:, j, :], yT3[:, j, :], reads=[w, yT], r=True)
                copy(oT3[:, m, :], bk[A, :], [bk], [oT.sub(m * 512, 512)])
            post_residual(oT3, oT, PC_GPOST)

            if dbg == ("x1", l):
                for c in range(8):
                    P.dma(dbg_d[:, c, bs], xT3[:, c, bs], reads=[xT])

            if "n" not in phases:
                continue
            P.ar_top = 0
            rs3 = rms_rstd(lambda c: xT3[:, c, bs], [xT])
            fill_hTr()
            fT = yT
            fT3 = yT3
            oT = P.alloc(4096)
            oT3 = oT.v(A, "p (c t) -> p c t", c=8)
            gbuf = P.alloc(514)
            acc = P.alloc()
            gfp = pc[A, PC_FPRE:PC_FPRE + 8]
            for (c_lo, c_hi) in ((0, 8), (8, 16), (16, 22)):
                ng = c_hi - c_lo
                for cc in range(ng):
                    c = c_lo + cc
                    ws = []
                    for col0 in (c * 128, DFF + c * 128):
                        w = wchunk()
                        w3 = w.v(A, "p (c m) -> p c m", c=8)
                        P.dma(RR(w3[:, :, :]), w_up_d[l, :, :, col0:col0 + 128], writes=[w], eng="pool")
                        P.tt(RR(w3[:, :, :]), w3[:, :, :], gfp.to_broadcast([128, 8, 128]), ALU.mult, reads=[w, pc], writes=[w])
                        ws.append((w, w3))
                    bg = P.bank()
                    for k in range(8):
                        P.mm(bg, bg[A, :], ws[0][1][:, k, :], hTr3[:, k, :], reads=[ws[0][0], hTr], r=True)
                    bu = P.bank()
                    for k in range(8):
                        P.mm(bu, bu[A, :], ws[1][1][:, k, :], hTr3[:, k, :], reads=[ws[1][0], hTr], r=True)
                    P.tt(gbuf[A, 2:514], bg[A, :], rs3[A, :], ALU.mult, reads=[bg, rs3], writes=[gbuf])
                    P.cp(gbuf[A, 0:2], cffn[A, c * 2:c * 2 + 2], reads=[cffn], writes=[gbuf], eng="pool")
                    fw = lambda j: pc[A, PC_FCW + c * 3 + j:PC_FCW + c * 3 + j + 1]
                    P.ts(acc[A, :], gbuf[A, 0:512], fw(0), ALU.mult, reads=[gbuf, pc], writes=[acc])
                    P.stt(acc[A, :], gbuf[A, 1:513], fw(1), acc[A, :], ALU.mult, ALU.add, reads=[gbuf, pc, acc], writes=[acc])
                    P.stt(acc[A, :], gbuf[A, 2:514], fw(2), acc[A, :], ALU.mult, ALU.add, reads=[gbuf, pc, acc], writes=[acc])
                    P.cp(cffn[A, c * 2:c * 2 + 2], gbuf[A, 512:514], reads=[gbuf], writes=[cffn], eng="pool")
                    P.act(acc[A, :], acc[A, :], AF.Gelu_apprx_tanh, bias=pc[A, PC_FCB + c:PC_FCB + c + 1], reads=[acc, pc], writes=[acc])
                    P.tt(acc[A, :], acc[A, :], rs3[A, :], ALU.mult, reads=[acc, rs3], writes=[acc])
                    P.tt(RR(fT3[:, cc, :]), acc[A, :], bu[A, :], ALU.mult, reads=[acc, bu], writes=[fT.sub(cc * 512, 512)])
                for m in range(8):
                    wa = wchunk()
                    wa3 = wa.v(A, "p (c m) -> p c m", c=8)
                    P.dma(RR(wa3[:, 0:ng, :]), w_dn_d[l, :, c_lo:c_hi, m * 128:(m + 1) * 128], writes=[wa], eng="pool")
                    bk = P.bank()
                    for cc in range(ng):
                        P.mm(bk, bk[A, :], wa3[:, cc, :], fT3[:, cc, :], reads=[wa, fT], r=True)
                    om = oT.sub(m * 512, 512)
                    if c_lo == 0:
                        copy(oT3[:, m, :], bk[A, :], [bk], [om])
                    else:
                        P.tt(oT3[:, m, :], oT3[:, m, :], bk[A, :], ALU.add, reads=[om, bk], writes=[om])
            post_residual(oT3, oT, PC_FPOST)

    outops = []
    for c in range(8):
        outops.append(P.dma(out_d[:, c, 0:T], xT3[:, c, :], reads=[xT]))
    P.emit(outops + [op for op in P.dma_ops if False])
    P.close()
    return nc


_NC_CACHE = {}


def kernel(**inputs):
    maps = prep_inputs(inputs)
    if "nc" not in _NC_CACHE:
        _NC_CACHE["nc"] = build()
    nc = _NC_CACHE["nc"]
    res = run_bass_kernel_spmd(nc, maps, core_ids=list(range(len(maps))))
    x = np.asarray(inputs["x"])
    out = np.empty(x.shape, np.float32)
    for b in range(x.shape[0]):
        oT = np.asarray(res.results[b]["outT"])
        out[b] = oT.transpose(1, 0, 2).reshape(D, SEQ).T
    return out
```

```python
import contextlib
import math
import numpy as np
import concourse.bass as bass
import concourse.mybir as mybir
from concourse.bass_utils import run_bass_kernel_spmd

F32 = mybir.dt.float32
F32R = mybir.dt.float32r
BF16 = mybir.dt.bfloat16
ALU = mybir.AluOpType
AF = mybir.ActivationFunctionType

ENGS = ("pe", "act", "dve", "pool", "sp")


def _flat(ks, out):
    for k in ks:
        if isinstance(k, (str, int)):
            out.append(k)
        elif isinstance(k, tuple) and (len(k) == 0 or isinstance(k[0], (str, int))):
            out.append(k)
        elif hasattr(k, "k"):
            _flat(k.k, out)
        else:
            _flat(k, out)
    return out


class Op:
    __slots__ = ("eng", "fn", "deps", "signals", "count", "pos", "dma", "dsem", "dval", "prewait")

    def __init__(self, eng, fn, dma=False):
        self.eng = eng
        self.fn = fn
        self.deps = []
        self.signals = False
        self.count = None
        self.pos = None
        self.dma = dma
        self.dsem = None
        self.dval = None
        self.prewait = None


class Buf:
    def __init__(self, t, off, ncols, keys):
        self.t, self.off, self.n, self.k = t, off, ncols, keys

    def __getitem__(self, idx):
        p, c = idx
        if isinstance(c, int):
            c = slice(c, c + 1)
        a = 0 if c.start is None else c.start
        b = self.n if c.stop is None else c.stop
        assert 0 <= a <= b <= self.n, (a, b, self.n)
        return self.t[p, self.off + a:self.off + b]

    def v(self, p, pat, **kw):
        return self.t[p, self.off:self.off + self.n].rearrange(pat, **kw)

    def sub(self, c0, n):
        ks = self.k
        if len(ks) > 1 and len(ks) * 512 >= self.n:
            ks = ks[c0 // 512:(c0 + n + 511) // 512]
        return Buf(self.t, self.off + c0, n, ks)


class Bank(Buf):
    def __init__(self, t, name):
        super().__init__(t, 0, 512, [name])
        self.opened = set()
        self.pinned = False


class Prog:
    NDMA = 32

    def __init__(self, nc):
        self.nc = nc
        self.ops = {e: [] for e in ENGS}
        self.lastw = {}
        self.readers = {}
        self.waited = {e: {} for e in ENGS}
        self.waited_dma = {e: set() for e in ENGS}
        self.dma_ops = []
        self.stack = contextlib.ExitStack()
        self.banks = []
        self.bank_i = 0
        self.ar = None
        self.ar_top = 0

    def sb(self, name, ncols, parts=128, dtype=F32):
        t = self.stack.enter_context(self.nc.sbuf_tensor("sb_" + name, [parts, ncols], dtype))
        return Buf(t, 0, ncols, [name])

    def sbs(self, name, nslots, dtype=F32):
        t = self.stack.enter_context(self.nc.sbuf_tensor("sb_" + name, [128, nslots * 512], dtype))
        return Buf(t, 0, nslots * 512, [(name, i) for i in range(nslots)])

    def make_banks(self):
        for i in range(8):
            t = self.stack.enter_context(self.nc.psum_tensor(f"bank{i}", [128, 512], F32))
            self.banks.append(Bank(t, f"bank{i}"))

    def bank(self, pin=False):
        for _ in range(16):
            b = self.banks[self.bank_i % 8]
            self.bank_i += 1
            if not b.pinned:
                b.opened = set()
                b.pinned = pin
                return b
        raise RuntimeError("no free psum bank")

    def make_arena(self, nslots):
        self.ar = self.stack.enter_context(self.nc.sbuf_tensor("arena", [128, nslots * 512], F32))
        self.ar_n = nslots

    def alloc(self, ncols=512):
        ns = (ncols + 511) // 512
        assert self.ar_top + ns <= self.ar_n, ("arena overflow", self.ar_top, ns, self.ar_n)
        b = Buf(self.ar, self.ar_top * 512, ncols, [("ar", s) for s in range(self.ar_top, self.ar_top + ns)])
        self.ar_top += ns
        return b

    def add(self, eng, fn, reads=(), writes=(), dma=False):
        op = Op(eng, fn, dma)
        op.pos = len(self.ops[eng])
        reads = _flat(reads, [])
        writes = _flat(writes, [])
        deps = []
        for k in reads:
            w = self.lastw.get(k)
            if w is not None:
                deps.append(w)
        for k in writes:
            w = self.lastw.get(k)
            if w is not None:
                deps.append(w)
            deps.extend(self.readers.get(k, ()))
        wt = self.waited[eng]
        wd = self.waited_dma[eng]
        best = {}
        dm = []
        for d in deps:
            if d is op:
                continue
            if d.dma:
                if id(d) not in wd:
                    wd.add(id(d))
                    dm.append(d)
                continue
            if d.eng == eng and eng in ("pe", "sp"):
                continue
            if wt.get(d.eng, -1) >= d.pos:
                continue
            if d.eng not in best or best[d.eng].pos < d.pos:
                best[d.eng] = d
        op.deps = list(best.values()) + dm
        for d in best.values():
            d.signals = True
            wt[d.eng] = max(wt.get(d.eng, -1), d.pos)
        for k in reads:
            self.readers.setdefault(k, []).append(op)
        for k in writes:
            self.lastw[k] = op
            self.readers[k] = []
        self.ops[eng].append(op)
        if dma:
            self.dma_ops.append(op)
        return op

    def emit(self, out_dma_ops=()):
        nc = self.nc
        nd = self.NDMA
        sems = {e: self.stack.enter_context(nc.semaphore(f"s_{e}")) for e in ENGS if e != "sp"}
        dsems = [self.stack.enter_context(nc.semaphore(f"s_dma{i}")) for i in range(nd)]
        for e in ENGS:
            c = 0
            for op in self.ops[e]:
                if not op.dma and op.signals:
                    c += 1
                    op.count = c
        per_eng = {}
        for op in self.dma_ops:
            per_eng.setdefault(op.eng, []).append(op)
        engs_with_dma = list(per_eng.keys())
        share = nd // max(1, len(engs_with_dma))
        for ei, e in enumerate(engs_with_dma):
            mysems = dsems[ei * share:(ei + 1) * share]
            for i, op in enumerate(per_eng[e]):
                op.dsem = mysems[i % share]
                op.dval = 16 * (i // share + 1)
                if i >= share:
                    op.prewait = (op.dsem, 16 * (i // share))
        final_waits = [(op.dsem, op.dval) for op in out_dma_ops]

        def run(e, eng):
            for op in self.ops[e]:
                if op.prewait is not None:
                    eng.wait_ge(op.prewait[0], op.prewait[1])
                for d in op.deps:
                    if d.dma:
                        eng.wait_ge(d.dsem, d.dval)
                    else:
                        eng.wait_ge(sems[d.eng], d.count)
                ins = op.fn(eng)
                if op.dma:
                    ins.then_inc(op.dsem, 16)
                elif op.signals:
                    ins.then_inc(sems[e], 1)
            if e == "sp":
                for s, v in final_waits:
                    eng.wait_ge(s, v)

        with nc.Block() as block:
            @block.tensor
            def _(eng):
                run("pe", eng)

            @block.scalar
            def _(eng):
                run("act", eng)

            @block.vector
            def _(eng):
                run("dve", eng)

            @block.gpsimd
            def _(eng):
                run("pool", eng)

            @block.sync
            def _(eng):
                run("sp", eng)

    def close(self):
        self.stack.close()

    def dma(self, out, in_, reads=(), writes=(), eng="sp"):
        return self.add(eng, lambda e: e.dma_start(out=out, in_=in_), reads, writes, dma=True)

    def mm(self, bank, out, lhsT, rhs, reads=(), r=False):
        p0 = out.start_partition()
        q = frozenset(range(p0 // 32, (p0 + out.partition_size() - 1) // 32 + 1))
        c0 = out.offset % 512 if hasattr(out, "offset") else 0
        c0 = self._ap_col0(out)
        c1 = c0 + self._ap_ncols(out)
        start = True
        for (qq, a, b) in bank.opened:
            if (qq & q) and a < c1 and c0 < b:
                assert q <= qq and a <= c0 and c1 <= b, ("partial psum overlap", q, qq, c0, c1, a, b)
                start = False
        if start:
            bank.opened.add((q, c0, c1))
        if r and FAST_MM and lhsT.dtype == F32:
            lhsT = lhsT.bitcast(F32R)
            rhs = rhs.bitcast(F32R)
        return self.add("pe", lambda e: e.matmul(out, lhsT, rhs, start=start, stop=True, skip_group_check=True),
                        reads, [bank])

    @staticmethod
    def _ap_col0(ap):
        pstride = ap.ap[0][0]
        return ap.offset % pstride

    @staticmethod
    def _ap_ncols(ap):
        span = 0
        for st, n in list(ap.ap)[1:]:
            span += st * (n - 1)
        return span + 1

    def tr(self, bank, out, in_, ident, reads=()):
        return self.add("pe", lambda e: e.transpose(out, in_, ident), reads, [bank])

    def act(self, out, in_, func, bias=None, scale=1.0, reads=(), writes=()):
        if bias is None:
            return self.add("act", lambda e: e.activation(out, in_, func, scale=scale), reads, writes)
        return self.add("act", lambda e: e.activation(out, in_, func, bias=bias, scale=scale), reads, writes)

    def tt(self, out, a, b, op, reads=(), writes=(), eng="dve"):
        return self.add(eng, lambda e: e.tensor_tensor(out, a, b, op), reads, writes)

    def ts(self, out, a, s1, op0, s2=None, op1=None, reads=(), writes=(), eng="dve"):
        if op1 is None:
            return self.add(eng, lambda e: e.tensor_scalar(out, a, s1, None, op0), reads, writes)
        return self.add(eng, lambda e: e.tensor_scalar(out, a, s1, s2, op0, op1), reads, writes)

    def stt(self, out, a, s, b, op0, op1, reads=(), writes=()):
        return self.add("dve", lambda e: e.scalar_tensor_tensor(out, a, s, b, op0, op1), reads, writes)

    def cp(self, out, a, reads=(), writes=(), eng="dve"):
        if eng == "act":
            return self.add(eng, lambda e: e.copy(out, a), reads, writes)
        return self.add(eng, lambda e: e.tensor_copy(out, a), reads, writes)

    def recip(self, out, a, reads=(), writes=()):
        return self.add("dve", lambda e: e.reciprocal(out, a), reads, writes)

    def scan(self, out, d0, d1, init, op0, op1, reads=(), writes=()):
        return self.add("dve", lambda e: e.tensor_tensor_scan(out, d0, d1, init, op0, op1), reads, writes)

    def memset(self, ap, v, writes=(), eng="dve"):
        return self.add(eng, lambda e: e.memset(ap, v), (), writes)


D = 1024
SEQ = 2048
DEPTH = 2
TB = 512
RW_STOP = 0
FAST_MM = True
N_IN = 3084
DFF = 2816
NFC = DFF // 128

PC_GPRE, PC_GPOST, PC_FPRE, PC_FPOST, PC_MU = 0, 8, 16, 24, 32
PC_W0, PC_A0, PC_KK, PC_KA, PC_RK, PC_LNW, PC_LNB, PC_MNORM, PC_SINK = 40, 42, 44, 46, 48, 50, 52, 54, 56
PC_CW, PC_CB, PC_BI, PC_BF, PC_FOXB, PC_FCW, PC_FCB, NPC = 58, 74, 78, 79, 80, 81, 147, 169
DV_OMKA, DV_BI15, DV_BF15, DV_NFOXB, DV_ESINK, NDV = 0, 2, 3, 4, 5, 8

C_IDENT, C_ONESD, C_ALLONE, C_BLK1, C_BLKM, C_TRI = 0, 128, 256, 384, 512, 640
C_MA, C_MB, C_IDG, C_MM, C_SELF, C_SELK, C_SELV, C_HM, C_SWAM, NCST = 768, 1280, 1408, 1536, 1792, 2304, 2432, 2688, 2696, 3720


W_IN_CHUNKS = ([(768, 128, False), (896, 128, False)]
               + [(i * 256 + hp * 128, 128, False) for hp in range(2) for i in range(3)]
               + [(1024 + i * 64, 64, False) for i in range(4)]
               + [(c0 + hp * 128, 128, False) for hp in range(2) for c0 in (1280, 1536)]
               + [(1792, 4, False), (1796, 4, False)]
               + [(1800, 128, False), (1928, 128, False), (2056, 64, True), (2120, 64, True), (2184, 128, False)]
               + [(2312, 128, False), (2440, 128, False), (2568, 128, False), (2696, 128, False)]
               + [(2824, 128, False), (2952, 128, False), (3080, 4, False)])
W_IN_OFF = {}
_o = 0
for (_c0, _m, _d) in W_IN_CHUNKS:
    W_IN_OFF[(_c0, _m)] = _o
    _o += 8 * (128 if _d else _m)
W_IN_TOT = _o


def _t5_bucket(dist):
    max_exact = 16
    d = np.maximum(dist, 1).astype(np.float32)
    large = max_exact + (np.log(d / max_exact) / math.log(128 / max_exact) * (32 - max_exact)).astype(np.int32)
    large = np.minimum(large, 31)
    return np.where(dist < max_exact, dist, large).astype(np.int32)


def _swa_dist():
    s = np.arange(128)[:, None, None]
    pcx = np.arange(2)[None, :, None]
    tq = np.arange(128)[None, None, :]
    sk = s + 128 * pcx
    dist = tq + 128 - sk
    vis = (dist >= 0) & (dist < 128)
    return dist, vis


def make_consts():
    c = np.zeros((128, NCST), np.float32)
    c[:, C_IDENT:C_IDENT + 128] = np.eye(128)
    c[:, C_ONESD:C_ONESD + 128] = 1.0 / 1024
    c[:, C_ALLONE:C_ALLONE + 128] = 1.0
    blk = np.zeros((128, 128), np.float32)
    blk[:64, :64] = 1
    blk[64:, 64:] = 1
    c[:, C_BLK1:C_BLK1 + 128] = blk
    c[:, C_BLKM:C_BLKM + 128] = blk / 64.0
    k = np.arange(128)
    c[:, C_TRI:C_TRI + 128] = (k[:, None] <= k[None, :])
    i = np.arange(64)[:, None]
    t = np.arange(64)[None, :]
    ma = np.zeros((64, 2, 2, 2, 64), np.float32)
    ma[:, :, :, 0, :] = (i < t)[:, None, None, :]
    ma[:, :, :, 1, :] = (i <= t)[:, None, None, :]
    c[:64, C_MA:C_MA + 512] = ma.reshape(64, 512)
    mb = np.zeros((64, 2, 64), np.float32)
    mb[:] = (i > t)[:, None, :]
    c[:64, C_MB:C_MB + 128] = mb.reshape(64, 128)
    idg = np.zeros((64, 2, 64), np.float32)
    idg[:] = np.eye(64)[:, None, :]
    c[:64, C_IDG:C_IDG + 128] = idg.reshape(64, 128)
    mm = np.zeros((64, 4, 64), np.float32)
    mm[:] = (i <= t)[:, None, :]
    c[:64, C_MM:C_MM + 256] = mm.reshape(64, 256)
    sf = np.zeros((4, 4, 128), np.float32)
    for h in range(4):
        sf[h, h, :] = 1
    c[:4, C_SELF:C_SELF + 512] = sf.reshape(4, 512)
    c[64:68, C_SELF:C_SELF + 512] = sf.reshape(4, 512)
    sk = np.zeros((4, 2, 64), np.float32)
    sv = np.zeros((4, 2, 128), np.float32)
    for h in range(4):
        for j in range(2):
            sk[h, j, :] = (h == 2 * j + np.arange(64) // 32)
            sv[h, j, :] = (h == 2 * j + np.arange(128) // 64)
    c[:4, C_SELK:C_SELK + 128] = sk.reshape(4, 128)
    c[:4, C_SELV:C_SELV + 256] = sv.reshape(4, 256)
    c[0:32, C_HM] = 1.0
    c[32:64, C_HM + 1] = 1.0
    _, vis = _swa_dist()
    m = np.where(vis, 0.0, -30000.0).astype(np.float32)
    m4 = np.broadcast_to(m[:, :, None, :], (128, 2, 4, 128))
    c[:, C_SWAM:C_SWAM + 1024] = m4.reshape(128, 1024)
    return c


def _cols(v, n):
    return np.ascontiguousarray(np.asarray(v, np.float32).reshape(n, 128).T)


def prep_inputs(inp):
    L = DEPTH
    f = lambda k: np.asarray(inp[k], np.float32)
    w_in4 = f("w_in").reshape(L, 8, 128, N_IN).transpose(0, 2, 1, 3)
    w_in_r = np.zeros((L, 128, W_IN_TOT), np.float32)
    for (c0, m, d) in W_IN_CHUNKS:
        blk = w_in4[:, :, :, c0:c0 + m]
        if d:
            blk = np.concatenate([blk, blk], axis=3)
        w_in_r[:, :, W_IN_OFF[(c0, m)]:W_IN_OFF[(c0, m)] + 8 * blk.shape[3]] = blk.reshape(L, 128, -1)
    w_out_r = np.ascontiguousarray(f("w_out").reshape(L, 8, 128, 8, 128).transpose(0, 3, 2, 1, 4)).reshape(L, 8, 128, 1024)
    w_up_r = np.ascontiguousarray(f("ffn_w_up").reshape(L, 8, 128, 2 * NFC, 128).transpose(0, 3, 2, 1, 4)).reshape(L, 2 * NFC, 128, 1024)
    w_dn_r = np.ascontiguousarray(f("ffn_w_down").reshape(L, NFC, 128, 8, 128).transpose(0, 3, 2, 1, 4)).reshape(L, 8, 128, NFC * 128)
    lora = np.zeros((L, 128, 512), np.float32)
    lora[:, 0:64, 0:256] = f("rwkv_w_up")
    lora[:, 64:128, 0:256] = f("rwkv_a_up")
    lora[:, :, 256:512] = f("rwkv_g_up")
    pc = np.zeros((L, 128, NPC), np.float32)
    for l in range(L):
        pc[l, :, PC_GPRE:PC_GPRE + 8] = _cols(f("norm_mix_pre")[l], 8)
        pc[l, :, PC_GPOST:PC_GPOST + 8] = _cols(f("norm_mix_post")[l], 8)
        pc[l, :, PC_FPRE:PC_FPRE + 8] = _cols(f("norm_ffn_pre")[l], 8)
        pc[l, :, PC_FPOST:PC_FPOST + 8] = _cols(f("norm_ffn_post")[l], 8)
        pc[l, :, PC_MU:PC_MU + 8] = _cols(f("rwkv_mu")[l], 8)
        for col, key in ((PC_W0, "rwkv_w0"), (PC_A0, "rwkv_a0"), (PC_KK, "rwkv_k_k"), (PC_KA, "rwkv_k_a"),
                         (PC_LNW, "rwkv_ln_w"), (PC_LNB, "rwkv_ln_b"), (PC_MNORM, "mlstm_norm")):
            pc[l, :, col:col + 2] = _cols(f(key)[l], 2)
        pc[l, :, PC_RK:PC_RK + 2] = _cols(f("rwkv_r_k")[l].reshape(256), 2)
        pc[l, :, PC_SINK:PC_SINK + 2] = _cols(np.repeat(f("swa_sinks")[l], 64), 2)
        cw = f("mlstm_conv_w")[l]
        for i in range(4):
            for j in range(4):
                pc[l, 0:64, PC_CW + i * 4 + j] = cw[j, i * 64:(i + 1) * 64]
            pc[l, 0:64, PC_CB + i] = f("mlstm_conv_b")[l][i * 64:(i + 1) * 64]
        pc[l, 0:4, PC_BI] = f("mlstm_b_i")[l]
        pc[l, 0:4, PC_BF] = f("mlstm_b_f")[l]
        pc[l, 0:4, PC_FOXB] = f("fox_b_f")[l]
        pc[l, 64:68, PC_FOXB] = f("fox_b_f")[l]
        fw = f("ffn_conv_w")[l]
        for j in range(3):
            pc[l, :, PC_FCW + j:PC_FCW + 66:3] = _cols(fw[j], NFC)
        pc[l, :, PC_FCB:PC_FCB + NFC] = _cols(f("ffn_conv_b")[l], NFC)
    dist, _ = _swa_dist()
    bk = _t5_bucket(np.clip(dist, 0, 127))
    swab = f("rel_bias")[bk]
    swab = np.ascontiguousarray(swab.transpose(0, 1, 3, 2)).reshape(128, 1024)
    cst = make_consts()
    x = f("x")
    maps = []
    for b in range(x.shape[0]):
        xT = np.ascontiguousarray(x[b].T.reshape(8, 128, x.shape[1]).transpose(1, 0, 2))
        maps.append({"xT": xT, "w_in_r": w_in_r, "w_out_r": w_out_r, "w_up_r": w_up_r, "w_dn_r": w_dn_r,
                     "lora": lora, "pc": pc, "cst": cst, "swab": swab})
    return maps


ARENA_SLOTS = 26


def build(nlayer=DEPTH, nblk=SEQ // TB, dbg=None, phases="rmsfon"):
    nc = bass.Bass("TRN2", target_bir_lowering=False)
    T = nblk * TB
    NT = T // 128
    L = DEPTH
    dr = lambda n, s, kind="ExternalInput": nc.dram_tensor(n, list(s), F32, kind=kind).ap()
    xT_d = dr("xT", [128, 8, SEQ])
    w_in_d = dr("w_in_r", [L, 128, W_IN_TOT])
    w_out_d = dr("w_out_r", [L, 8, 128, 1024])
    w_up_d = dr("w_up_r", [L, 2 * NFC, 128, 1024])
    w_dn_d = dr("w_dn_r", [L, 8, 128, NFC * 128])
    lora_d = dr("lora", [L, 128, 512])
    pc_d = dr("pc", [L, 128, NPC])
    cst_d = dr("cst", [128, NCST])
    swab_d = dr("swab", [128, 1024])
    out_d = dr("outT", [128, 8, SEQ], "ExternalOutput")
    dbg_d = dr("dbg", [128, 8, SEQ], "ExternalOutput") if dbg else None

    P = Prog(nc)
    RR = lambda ap: ap.bitcast(F32R) if (FAST_MM and ap.dtype == F32) else ap
    RR0 = RR
    P.make_banks()
    xT = P.sb("xT", 8 * T)
    kTh = P.sb("kTh", 2 * T)
    Vh = P.sb("Vh", NT * 256)
    negc = P.sb("negc", NT * 4)
    yT = P.sbs("yT", 8, dtype=BF16)
    foxR = P.sbs("foxR", 7)
    cst = P.sb("cst", C_SWAM)
    pc = P.sb("pc", NPC)
    dv = P.sb("dv", NDV)
    rstm = P.sb("rstm", 4)
    Srw = P.sb("Srw", 2 * 2 * 2 * 64)
    pc1 = P.sb("pc1", 8)
    CN = P.sb("CN", 2 * 128)
    crw = P.sb("crw", 8)
    cml = P.sb("cml", 12)
    cffn = P.sb("cffn", NFC * 2)
    ccar = P.sb("ccar", 2)
    swaKc = P.sb("swaKc", 2 * 128)
    swaVc = P.sb("swaVc", 128)
    hTr = P.sbs("hTr", 8, dtype=BF16)
    ebks = P.sb("ebks", 16)
    wbufs = [P.sb(f"wch{i}", 8 * 128, dtype=BF16) for i in range(4)]
    P.make_arena(ARENA_SLOTS)

    A = slice(0, 128)
    H0 = slice(0, 64)
    H1 = slice(64, 128)
    R4 = slice(0, 4)
    xT3 = xT.v(A, "p (c t) -> p c t", c=8)
    yT3 = yT.v(A, "p (c t) -> p c t", c=8)
    kTh3 = kTh.v(A, "p (j t) -> p j t", j=2)
    Vh3 = Vh.v(A, "p (n c) -> p n c", c=256)
    negc3 = negc.v(A, "p (n h) -> p n h", h=4)
    swaKc3 = swaKc.v(A, "p (j t) -> p j t", j=2)
    hTr3 = hTr.v(A, "p (c t) -> p c t", c=8)
    CN3 = CN.v(H0, "p (j c) -> p j c", j=2)
    S5 = Srw.v(H0, "p (j h b v) -> p j h b v", j=2, h=2, b=2)

    def C(off, n, p=A):
        return cst[p, off:off + n]

    ident = C(C_IDENT, 128)
    evac_i = [0]

    def copy(out, in_, reads, writes, eng=None):
        if eng is None:
            evac_i[0] += 1
            eng = "act" if evac_i[0] % 2 else "dve"
        return P.cp(out, in_, reads, writes, eng=eng)

    wb_i = [0]

    def wchunk():
        b = wbufs[wb_i[0] % 4]
        wb_i[0] += 1
        return b

    P.dma(RR0(cst[A, :]), cst_d[:, 0:C_SWAM], writes=[cst], eng="pool")
    for c in range(8):
        P.dma(xT3[:, c, :], xT_d[:, c, 0:T], writes=[xT])
    if phases != "rmsfon":
        P.ts(RR(yT[A, :]), cst[A, 0:4096 if C_SWAM >= 4096 else 2048].to_broadcast([128, 4096]) if False else xT[A, 0:4096], 0.0, ALU.mult, reads=[xT], writes=[yT])
    P.ar_top = 0

    def rms_rstd(src_fn, reads, eps=1e-6):
        ssb = P.bank(pin=True)
        rstd = P.alloc()
        sq = [P.alloc(), P.alloc()]
        for c in range(8):
            s = sq[c % 2]
            P.act(s[A, :], src_fn(c), AF.Square, reads=reads, writes=[s])
            P.mm(ssb, ssb[A, :], C(C_ONESD, 128), s[A, :], reads=[cst, s])
        P.ts(rstd[A, :], ssb[A, :], eps, ALU.add, reads=[ssb], writes=[rstd])
        ssb.pinned = False
        P.recip(rstd[A, :], rstd[A, :], reads=[rstd], writes=[rstd])
        P.act(rstd[A, :], rstd[A, :], AF.Sqrt, reads=[rstd], writes=[rstd])
        P.ar_top -= 2
        return rstd

    for l in range(nlayer):
        P.dma(pc[A, :], pc_d[l], writes=[pc])
        P.ts(dv[A, DV_OMKA:DV_OMKA + 2], pc[A, PC_KA:PC_KA + 2], -1.0, ALU.mult, 1.0, ALU.add, reads=[pc], writes=[dv])
        P.ts(dv[R4, DV_BI15:DV_BI15 + 2], pc[R4, PC_BI:PC_BI + 2], 1.0 / 15.0, ALU.mult, reads=[pc], writes=[dv])
        P.ts(dv[A, DV_NFOXB:DV_NFOXB + 1], pc[A, PC_FOXB:PC_FOXB + 1], -1.0, ALU.mult, reads=[pc], writes=[dv])
        P.act(dv[A, DV_ESINK:DV_ESINK + 2], pc[A, PC_SINK:PC_SINK + 2], AF.Exp, reads=[pc], writes=[dv])
        for b_ in (Srw, CN, crw, cml, cffn, ccar, swaKc, swaVc):
            P.memset(b_[A, :], 0.0, writes=[b_])

        for blk in range(nblk):
            t0 = blk * TB
            bs = slice(t0, t0 + TB)
            P.ar_top = 0
            rstd = rms_rstd(lambda c: xT3[:, c, bs], [xT])
            bk = P.bank()
            for tt_ in range(4):
                P.tr(bk, bk[A, tt_ * 128:(tt_ + 1) * 128], rstd[A, tt_ * 128:(tt_ + 1) * 128], ident, reads=[rstd, cst])
            P.cp(rstm[A, 0:4], bk.v(A, "p (n c) -> p n c", n=4)[:, :, 0], reads=[bk], writes=[rstm])
            MIX_BASE = P.ar_top

            def fill_hTr():
                for c in range(8):
                    P.cp(RR(hTr3[:, c, :]), xT3[:, c, bs], reads=[xT], writes=[hTr.sub(c * 512, 512)], eng=("pool", "act")[c % 2])

            fill_hTr()
            gpre = pc[A, PC_GPRE:PC_GPRE + 8]

            def load_w(col0, M, dup=False):
                w = wchunk()
                w3 = w.v(A, "p (c m) -> p c m", c=8)
                off = W_IN_OFF[(col0, M)]
                if dup:
                    M = 128
                if M == 128:
                    P.dma(RR(w[A, :]), w_in_d[l, :, off:off + 1024], writes=[w], eng="pool")
                else:
                    P.dma(RR(w3[:, :, 0:M]), w_in_d[l, :, off:off + 8 * M].rearrange("p (c m) -> p c m", c=8), writes=[w], eng="pool")
                P.tt(RR(w3[:, :, 0:M]), w3[:, :, 0:M], gpre.to_broadcast([128, 8, M]), ALU.mult, reads=[w, pc], writes=[w])
                return w, w3, M

            def proj_fm(col0, M, dst, dup=False, scale=None, rows=None, pbase=0):
                w, w3, M = load_w(col0, M, dup)
                bk = P.bank()
                r = slice(pbase, pbase + M)
                for c in range(8):
                    P.mm(bk, bk[r, :], w3[:, c, 0:M], hTr3[:, c, :], reads=[w, hTr], r=(pbase == 0))
                if scale is None:
                    P.tt(dst, bk[r, :], rstd[r, :], ALU.mult, reads=[bk, rstd], writes=[rows])
                else:
                    P.stt(dst, bk[r, :], scale, rstd[r, :], ALU.mult, ALU.mult, reads=[bk, rstd], writes=[rows])

            def proj_tm(col0, ncols, dst_fn, wr):
                w, w3, _ = load_w(col0, ncols)
                for tt_ in range(4):
                    bk = P.bank()
                    for c in range(8):
                        P.mm(bk, bk[A, 0:ncols], hTr3[:, c, tt_ * 128:(tt_ + 1) * 128], w3[:, c, 0:ncols], reads=[w, hTr], r=True)
                    P.ts(dst_fn(tt_), bk[A, 0:ncols], rstm[A, tt_:tt_ + 1], ALU.mult, reads=[bk, rstm], writes=[wr])

            def shift_mix(z, ci, dst):
                d = P.alloc()
                P.tt(d[A, 1:512], z[A, 0:511], z[A, 1:512], ALU.subtract, reads=[z], writes=[d])
                P.tt(d[A, 0:1], crw[A, ci:ci + 1], z[A, 0:1], ALU.subtract, reads=[z, crw], writes=[d])
                P.cp(crw[A, ci:ci + 1], z[A, 511:512], reads=[z], writes=[crw], eng="pool")
                P.stt(dst[A, :], d[A, :], pc[A, PC_MU + ci:PC_MU + ci + 1], z[A, :], ALU.mult, ALU.add,
                      reads=[d, pc, z], writes=[dst])
                P.ar_top -= 1

            def rwkv():
                lora = P.alloc()
                P.dma(lora[A, :], lora_d[l], writes=[lora])
                swa_ = P.alloc()
                sg = P.alloc()
                ztmp = P.alloc()
                proj_fm(768, 128, ztmp[A, :], rows=ztmp)
                shift_mix(ztmp, 6, swa_)
                proj_fm(896, 128, ztmp[A, :], rows=ztmp)
                shift_mix(ztmp, 7, sg)
                P.ar_top -= 1
                P.act(swa_[H0, :], swa_[H0, :], AF.Tanh, reads=[swa_], writes=[swa_])
                P.act(sg[A, :], sg[A, :], AF.Sigmoid, reads=[sg], writes=[sg])
                if RW_STOP == 1:
                    return
                base_top = P.ar_top
                for hp in range(2):
                    P.ar_top = base_top
                    z = [P.alloc() for _ in range(3)]
                    s = [P.alloc() for _ in range(3)]
                    for i in range(3):
                        proj_fm(i * 256 + hp * 128, 128, z[i][A, :], rows=z[i])
                    for i in range(3):
                        shift_mix(z[i], i * 2 + hp, s[i])
                    sr, sk, sv = s
                    cw = slice(hp * 128, (hp + 1) * 128)
                    pcc = lambda c0: pc[A, c0 + hp:c0 + hp + 1]
                    bk = P.bank()
                    P.mm(bk, bk[A, :], lora[H0, cw], swa_[H0, :], reads=[lora, swa_])
                    lw = z[0]
                    P.act(lw[A, :], bk[A, :], AF.Sigmoid, bias=pcc(PC_W0), reads=[bk, pc], writes=[lw])
                    P.ts(lw[A, :], lw[A, :], -math.exp(-0.5), ALU.mult, reads=[lw], writes=[lw])
                    bk = P.bank()
                    P.mm(bk, bk[A, :], lora[H1, cw], swa_[H1, :], reads=[lora, swa_])
                    a = z[1]
                    P.act(a[A, :], bk[A, :], AF.Sigmoid, bias=pcc(PC_A0), reads=[bk, pc], writes=[a])
                    bk = P.bank()
                    P.mm(bk, bk[A, :], lora[A, 256 + hp * 128:256 + (hp + 1) * 128], sg[A, :], reads=[lora, sg])
                    g = z[2]
                    copy(g[A, :], bk[A, :], [bk], [g])
                    kk = P.alloc()
                    tmp = P.alloc()
                    P.ts(kk[A, :], sk[A, :], pcc(PC_KK), ALU.mult, reads=[sk, pc], writes=[kk])
                    P.tt(tmp[A, :], kk[A, :], kk[A, :], ALU.mult, reads=[kk], writes=[tmp])
                    bk = P.bank()
                    P.mm(bk, bk[A, :], C(C_BLK1, 128), tmp[A, :], reads=[cst, tmp])
                    P.act(tmp[A, :], bk[A, :], AF.Sqrt, reads=[bk], writes=[tmp])
                    P.ts(tmp[A, :], tmp[A, :], 1e-12, ALU.max, reads=[tmp], writes=[tmp])
                    P.recip(tmp[A, :], tmp[A, :], reads=[tmp], writes=[tmp])
                    P.tt(kk[A, :], kk[A, :], tmp[A, :], ALU.mult, reads=[kk, tmp], writes=[kk])
                    kmod = P.alloc()
                    P.ts(kmod[A, :], a[A, :], pcc(PC_KA), ALU.mult, dv[A, DV_OMKA + hp:DV_OMKA + hp + 1], ALU.add,
                         reads=[a, pc, dv], writes=[kmod])
                    P.tt(kmod[A, :], kmod[A, :], sk[A, :], ALU.mult, reads=[kmod, sk], writes=[kmod])
                    bonus = sk
                    P.stt(tmp[A, :], sr[A, :], pcc(PC_RK), kmod[A, :], ALU.mult, ALU.mult, reads=[sr, pc, kmod], writes=[tmp])
                    bk = P.bank()
                    P.mm(bk, bk[A, :], C(C_BLK1, 128), tmp[A, :], reads=[cst, tmp])
                    P.tt(bonus[A, :], bk[A, :], sv[A, :], ALU.mult, reads=[bk, sv], writes=[bonus])
                    bb = a
                    P.tt(bb[A, :], kk[A, :], a[A, :], ALU.mult, reads=[kk, a], writes=[bb])
                    cum = P.alloc()
                    ones = C(C_ALLONE, 64)
                    for c in range(8):
                        cs = slice(c * 64, (c + 1) * 64)
                        P.scan(cum[A, cs], ones, lw[A, cs], 0.0, ALU.mult, ALU.add, reads=[cst, lw], writes=[cum])
                    cumx = lw
                    P.tt(cumx[A, :], cum[A, :], lw[A, :], ALU.subtract, reads=[cum, lw], writes=[cumx])
                    Ep = P.alloc()
                    Em = P.alloc()
                    Ee = P.alloc()
                    P.act(Ep[A, :], cum[A, :], AF.Exp, reads=[cum], writes=[Ep])
                    P.act(Em[A, :], cum[A, :], AF.Exp, scale=-1.0, reads=[cum], writes=[Em])
                    P.act(cumx[A, :], cumx[A, :], AF.Exp, reads=[cumx], writes=[cumx])
                    for c in range(8):
                        cs = slice(c * 64, (c + 1) * 64)
                        P.act(Ee[A, cs], cum[A, cs], AF.Exp, bias=cum[A, c * 64 + 63:c * 64 + 64], scale=-1.0,
                              reads=[cum], writes=[Ee])
                    AR = P.alloc(1024)
                    AR3 = AR.v(A, "p (a t) -> p a t", a=2)
                    P.stt(AR3[:, 0, :], kk[A, :], -1.0, cumx[A, :], ALU.mult, ALU.mult, reads=[kk, cumx], writes=[AR])
                    P.tt(AR3[:, 1, :], sr[A, :], Ep[A, :], ALU.mult, reads=[sr, Ep], writes=[AR])
                    Bt, Kt, Bte, Kte = cum, tmp, kk, kmod
                    P.tt(Bt[A, :], bb[A, :], Em[A, :], ALU.mult, reads=[bb, Em], writes=[Bt])
                    P.tt(Kt[A, :], kmod[A, :], Em[A, :], ALU.mult, reads=[kmod, Em], writes=[Kt])
                    P.tt(Bte[A, :], bb[A, :], Ee[A, :], ALU.mult, reads=[bb, Ee], writes=[Bte])
                    P.tt(Kte[A, :], kmod[A, :], Ee[A, :], ALU.mult, reads=[kmod, Ee], writes=[Kte])
                    yr = Em
                    AR1 = Buf(z[0].t, z[0].off, 1024, z[0].k + z[1].k)
                    AR13 = AR1.v(A, "p (a t) -> p a t", a=2)
                    Bt1, Kt1 = sr, P.alloc()
                    idn1 = cst[H1, C_IDENT + 64:C_IDENT + 128]
                    for src, dst, wr in ((AR3[H1, 0, :], AR13[H0, 0, :], AR1), (AR3[H1, 1, :], AR13[H0, 1, :], AR1),
                                         (Bt[H1, :], Bt1[H0, :], Bt1), (Kt[H1, :], Kt1[H0, :], Kt1)):
                        bk = P.bank()
                        P.mm(bk, bk[H0, :], idn1, src, reads=[cst, AR, Bt, Kt])
                        copy(dst, bk[H0, :], [bk], [wr])
                    bk = P.bank()
                    P.mm(bk, bk[H0, 0:8], idn1, Ep.v(H1, "p (c t) -> p c t", c=8)[:, :, 63], reads=[cst, Ep])
                    copy(pc1[H0, 0:8], bk[H0, 0:8], [bk], [pc1])
                    ARh = (AR3, AR13)
                    Bth = (Bt, Bt1)
                    Kth = (Kt, Kt1)
                    ARb = (AR, AR1)
                    if RW_STOP == 2:
                        continue
                    NA = [P.alloc() for _ in range(2)]
                    nmslot = P.alloc()
                    NMp = [nmslot.sub(0, 256), nmslot.sub(256, 256)]
                    gslot = P.alloc()
                    Gp = [gslot.sub(0, 128), gslot.sub(128, 128)]
                    RU = [gslot.sub(256, 256), Ee.sub(0, 256)]
                    TM = [P.alloc() for _ in range(2)]
                    for c in range(8):
                        if RW_STOP == 6:
                            continue
                        cs = slice(c * 64, (c + 1) * 64)
                        na = NA[c % 2]
                        na5 = na.v(H0, "p (h b a t) -> p h b a t", h=2, b=2, a=2)
                        bk = P.bank()
                        bk5 = bk.v(H0, "p (h b a t) -> p h b a t", h=2, b=2, a=2)
                        bkm = P.bank()
                        bkm3 = bkm.v(H0, "p (x h t) -> p x h t", x=4, h=2)
                        for h2 in range(1 if RW_STOP == 7 else 2):
                            ph = slice(h2 * 64, h2 * 64 + 64)
                            for a_ in range(2):
                                P.mm(bk, bk5[:, h2, 0, a_, :], Bth[h2][H0, cs], ARh[h2][H0, a_, cs], reads=[Bth[h2], ARb[h2]])
                                P.mm(bk, bk5[:, h2, 1, a_, :], Kth[h2][H0, cs], ARh[h2][H0, a_, cs], reads=[Kth[h2], ARb[h2]])
                            P.mm(bkm, bkm3[:, 0, h2, :], ARh[h2][H0, 0, cs], Bth[h2][H0, cs], reads=[Bth[h2], ARb[h2]])
                        P.tt(na[H0, :], bk[H0, :], C(C_MA, 512, H0), ALU.mult, reads=[bk, cst], writes=[na])
                        if RW_STOP in (3, 7):
                            continue
                        nm = NMp[0]
                        nm4 = nm.v(H0, "p (x h t) -> p x h t", x=2, h=2)
                        P.cp(nm4[:, 0, :, :], na5[:, :, 0, 0, :], reads=[na], writes=[nm], eng="pool")
                        P.tt(nm4[:, 1, :, :], bkm3[:, 0, :, :], C(C_MB, 128, H0).rearrange("p (h t) -> p h t", h=2),
                             ALU.mult, reads=[bkm, cst], writes=[nm])
                        G3_0 = Gp[0].v(H0, "p (h t) -> p h t", h=2)
                        P.tt(G3_0, na5[:, :, 0, 0, :], C(C_IDG, 128, H0).rearrange("p (h t) -> p h t", h=2), ALU.add,
                             reads=[na, cst], writes=[Gp[0]])
                        cur = 0
                        for lev in range(5):
                            nmc = NMp[cur]
                            nmc4 = nmc.v(H0, "p (x h t) -> p x h t", x=2, h=2)
                            nmn = NMp[1 - cur]
                            nmn4 = nmn.v(H0, "p (x h t) -> p x h t", x=2, h=2)
                            Gc = Gp[cur]
                            Gc3 = Gc.v(H0, "p (h t) -> p h t", h=2)
                            Gn = Gp[1 - cur]
                            bkp = P.bank()
                            bkp4 = bkp.v(H0, "p (x h t) -> p x h t", x=4, h=2)
                            for h2 in range(2):
                                P.mm(bkp, bkp4[:, 0, h2, :], nmc4[:, 1, h2, :], nmc4[:, 0, h2, :], reads=[nmc])
                                P.mm(bkp, bkp4[:, 1, h2, :], nmc4[:, 0, h2, :], nmc4[:, 1, h2, :], reads=[nmc])
                            copy(nmn[H0, :], bkp[H0, 0:256], [bkp], [nmn])
                            bkg = P.bank()
                            for h2 in range(2):
                                P.mm(bkg, bkg[H0, h2 * 64:(h2 + 1) * 64], nmn4[:, 1, h2, :], Gc3[:, h2, :], reads=[nmn, Gc])
                            P.tt(Gn[H0, :], bkg[H0, 0:128], Gc[H0, :], ALU.add, reads=[bkg, Gc], writes=[Gn])
                            cur = 1 - cur
                        Gf = Gp[cur]
                        Gf3 = Gf.v(H0, "p (h t) -> p h t", h=2)
                        if RW_STOP == 4:
                            continue
                        tm = TM[c % 2]
                        bkt = P.bank()
                        P.tr(bkt, bkt[H0, 0:128], Bte[A, cs], ident, reads=[Bte, cst])
                        P.tr(bkt, bkt[H0, 128:256], Kte[A, cs], ident, reads=[Kte, cst])
                        P.tr(bkt, bkt[H0, 256:384], sv[A, cs], ident, reads=[sv, cst])
                        copy(tm[H0, 0:384], bkt[H0, 0:384], [bkt], [tm])
                        if RW_STOP == 5:
                            continue
                        ru = RU[c % 2]
                        sb_old = c % 2
                        sb_new = 1 - sb_old
                        bkr = P.bank()
                        for h2 in range(2):
                            ph = slice(h2 * 64, h2 * 64 + 64)
                            o = bkr[H0, h2 * 64:(h2 + 1) * 64]
                            P.mm(bkr, o, ARh[h2][H0, 0, cs], S5[:, hp, h2, sb_old, :], reads=[ARb[h2], Srw])
                            P.mm(bkr, o, na5[:, h2, 1, 0, :], tm[H0, 256 + h2 * 64:256 + (h2 + 1) * 64], reads=[na, tm])
                        copy(ru[H0, 0:128], bkr[H0, 0:128], [bkr], [ru], eng="act")
                        bku = P.bank()
                        for h2 in range(2):
                            P.mm(bku, bku[H0, h2 * 64:(h2 + 1) * 64], Gf3[:, h2, :], ru[H0, h2 * 64:(h2 + 1) * 64], reads=[Gf, ru])
                        copy(ru[H0, 128:256], bku[H0, 0:128], [bku], [ru], eng="dve")
                        bky = P.bank()
                        for h2 in range(2):
                            ph = slice(h2 * 64, h2 * 64 + 64)
                            o = bky[ph, 0:64]
                            P.mm(bky, o, S5[:, hp, h2, sb_old, :], ARh[h2][H0, 1, cs], reads=[ARb[h2], Srw])
                            P.mm(bky, o, ru[H0, 128 + h2 * 64:128 + (h2 + 1) * 64], na5[:, h2, 0, 1, :], reads=[ru, na])
                            P.mm(bky, o, tm[H0, 256 + h2 * 64:256 + (h2 + 1) * 64], na5[:, h2, 1, 1, :], reads=[tm, na])
                        copy(yr[A, cs], bky[A, 0:64], [bky], [yr], eng="act")
                        bks = P.bank()
                        for h2 in range(2):
                            o = bks[H0, h2 * 64:(h2 + 1) * 64]
                            P.mm(bks, o, tm[H0, h2 * 64:(h2 + 1) * 64], ru[H0, 128 + h2 * 64:128 + (h2 + 1) * 64], reads=[tm, ru])
                            P.mm(bks, o, tm[H0, 128 + h2 * 64:128 + (h2 + 1) * 64], tm[H0, 256 + h2 * 64:256 + (h2 + 1) * 64], reads=[tm])
                        for h2 in range(2):
                            pcs = Ep[H0, c * 64 + 63:c * 64 + 64] if h2 == 0 else pc1[H0, c:c + 1]
                            P.stt(S5[:, hp, h2, sb_new, :], S5[:, hp, h2, sb_old, :], pcs, bks[H0, h2 * 64:(h2 + 1) * 64],
                                  ALU.mult, ALU.add, reads=[Srw, Ep, pc1, bks], writes=[Srw])
                    t1 = Ep
                    bk = P.bank()
                    P.mm(bk, bk[A, :], C(C_BLKM, 128), yr[A, :], reads=[cst, yr])
                    P.tt(yr[A, :], yr[A, :], bk[A, :], ALU.subtract, reads=[yr, bk], writes=[yr])
                    P.tt(t1[A, :], yr[A, :], yr[A, :], ALU.mult, reads=[yr], writes=[t1])
                    bk = P.bank()
                    P.mm(bk, bk[A, :], C(C_BLKM, 128), t1[A, :], reads=[cst, t1])
                    P.ts(t1[A, :], bk[A, :], 64e-5, ALU.add, reads=[bk], writes=[t1])
                    P.recip(t1[A, :], t1[A, :], reads=[t1], writes=[t1])
                    P.act(t1[A, :], t1[A, :], AF.Sqrt, reads=[t1], writes=[t1])
                    P.tt(yr[A, :], yr[A, :], t1[A, :], ALU.mult, reads=[yr, t1], writes=[yr])
                    P.ts(yr[A, :], yr[A, :], pcc(PC_LNW), ALU.mult, pcc(PC_LNB), ALU.add, reads=[yr, pc], writes=[yr])
                    P.tt(yr[A, :], yr[A, :], bonus[A, :], ALU.add, reads=[yr, bonus], writes=[yr])
                    P.tt(RR(yT3[:, hp, :]), yr[A, :], g[A, :], ALU.mult, reads=[yr, g], writes=[yT.sub(hp * 512, 512)])

            P.ar_top = MIX_BASE
            if "r" in phases:
                rwkv()

            def mlstm():
                qk = [P.alloc() for _ in range(4)]
                vones = P.alloc()
                vo3 = vones.v(H0, "p (h c) -> p h c", h=4)
                P.memset(vones[H0, :], 1.0, writes=[vones])
                vT = [P.alloc(), P.alloc()]
                oT = [P.alloc(), P.alloc()]
                gi, gf, bcum, ge = (P.alloc() for _ in range(4))
                top = P.ar_top
                for i in range(4):
                    P.ar_top = top
                    uext = P.alloc(515)
                    acc = P.alloc()
                    proj_fm(1024 + i * 64, 64, uext[H0, 3:515], rows=uext)
                    P.cp(uext[H0, 0:3], cml[H0, i * 3:(i + 1) * 3], reads=[cml], writes=[uext], eng="pool")
                    wc = lambda j: pc[H0, PC_CW + i * 4 + j:PC_CW + i * 4 + j + 1]
                    P.ts(acc[H0, :], uext[H0, 0:512], wc(0), ALU.mult, reads=[uext, pc], writes=[acc])
                    for j in range(1, 4):
                        P.stt(acc[H0, :], uext[H0, j:j + 512], wc(j), acc[H0, :], ALU.mult, ALU.add, reads=[uext, pc, acc], writes=[acc])
                    P.cp(cml[H0, i * 3:(i + 1) * 3], uext[H0, 512:515], reads=[uext], writes=[cml], eng="pool")
                    P.act(qk[i][H0, :], acc[H0, :], AF.Silu, bias=pc[H0, PC_CB + i:PC_CB + i + 1], reads=[acc, pc], writes=[qk[i]])
                    if i < 2:
                        P.ts(qk[i][H0, :], qk[i][H0, :], 32.0 ** -0.5, ALU.mult, reads=[qk[i]], writes=[qk[i]])
                P.ar_top = top
                for hp in range(2):
                    proj_fm(1280 + hp * 128, 128, vT[hp][A, :], rows=vT[hp])
                    proj_fm(1536 + hp * 128, 128, oT[hp][A, :], rows=oT[hp])
                    P.act(oT[hp][A, :], oT[hp][A, :], AF.Sigmoid, reads=[oT[hp]], writes=[oT[hp]])
                proj_fm(1792, 4, gi[R4, :], rows=gi)
                proj_fm(1796, 4, gf[R4, :], rows=gf)
                P.act(gi[R4, :], gi[R4, :], AF.Tanh, bias=dv[R4, DV_BI15:DV_BI15 + 1], scale=1.0 / 15.0, reads=[gi, dv], writes=[gi])
                P.act(gf[R4, :], gf[R4, :], AF.Tanh, bias=dv[R4, DV_BF15:DV_BF15 + 1], scale=1.0 / 15.0, reads=[gf, dv], writes=[gf])
                P.ts(gi[R4, :], gi[R4, :], 15.0, ALU.mult, reads=[gi], writes=[gi])
                P.act(gf[R4, :], gf[R4, :], AF.Exp, scale=-15.0, reads=[gf], writes=[gf])
                P.act(gf[R4, :], gf[R4, :], AF.Ln, bias=1.0, reads=[gf], writes=[gf])
                ones4 = C(C_ALLONE, 64, R4)
                for c in range(8):
                    cs = slice(c * 64, (c + 1) * 64)
                    P.scan(bcum[R4, cs], ones4, gf[R4, cs], 0.0, ALU.mult, ALU.subtract, reads=[cst, gf], writes=[bcum])
                P.tt(gi[R4, :], gi[R4, :], bcum[R4, :], ALU.subtract, reads=[gi, bcum], writes=[gi])
                P.act(gi[R4, :], gi[R4, :], AF.Exp, reads=[gi], writes=[gi])
                P.act(bcum[R4, :], bcum[R4, :], AF.Exp, reads=[bcum], writes=[bcum])
                for c in range(8):
                    cs = slice(c * 64, (c + 1) * 64)
                    P.ts(ge[R4, cs], gi[R4, cs], bcum[R4, c * 64 + 63:c * 64 + 64], ALU.mult, reads=[gi, bcum], writes=[ge])
                kp = [P.alloc(), P.alloc()]
                kpe = [P.alloc(), P.alloc()]
                ebbc = [P.alloc(), P.alloc()]
                for j in range(2):
                    selk = C(C_SELK + j * 64, 64, R4)
                    bk = P.bank()
                    P.mm(bk, bk[H0, :], selk, gi[R4, :], reads=[cst, gi])
                    P.tt(kp[j][H0, :], qk[2 + j][H0, :], bk[H0, :], ALU.mult, reads=[qk[2 + j], bk], writes=[kp[j]])
                    bk = P.bank()
                    P.mm(bk, bk[H0, :], selk, ge[R4, :], reads=[cst, ge])
                    P.tt(kpe[j][H0, :], qk[2 + j][H0, :], bk[H0, :], ALU.mult, reads=[qk[2 + j], bk], writes=[kpe[j]])
                    bk = P.bank()
                    P.mm(bk, bk[H0, :], selk, bcum[R4, :], reads=[cst, bcum])
                    P.cp(ebks[H0, j * 8:(j + 1) * 8], bk.v(H0, "p (c t) -> p c t", c=8)[:, :, 63], reads=[bk], writes=[ebks])
                    bk = P.bank()
                    P.mm(bk, bk[A, :], C(C_SELV + j * 128, 128, R4), bcum[R4, :], reads=[cst, bcum])
                    copy(ebbc[j][A, :], bk[A, :], [bk], [ebbc[j]])
                NTs = P.alloc(1024)
                DNs = P.alloc(1024)
                NT3 = NTs.v(A, "p (j t) -> p j t", j=2)
                DN3 = DNs.v(A, "p (j t) -> p j t", j=2)
                sm = P.alloc()
                qmc = P.alloc(256)
                for c in range(8):
                    cs = slice(c * 64, (c + 1) * 64)
                    bkt = P.bank()
                    P.tr(bkt, bkt[H0, 0:128], vT[0][A, cs], ident, reads=[vT[0], cst])
                    P.tr(bkt, bkt[H0, 128:256], vT[1][A, cs], ident, reads=[vT[1], cst])
                    copy(vo3[:, :, 0:64], bkt.v(H0, "p (h c) -> p h c", h=8)[:, 0:4, :], [bkt], [vones])
                    bkt2 = P.bank()
                    P.tr(bkt2, bkt2[H0, 0:64], kpe[0][H0, cs], C(C_IDENT, 64, H0), reads=[kpe[0], cst])
                    P.tr(bkt2, bkt2[H0, 64:128], kpe[1][H0, cs], C(C_IDENT, 64, H0), reads=[kpe[1], cst])
                    copy(sm[H0, 256:384], bkt2[H0, 0:128], [bkt2], [sm])
                    for h in range(4):
                        P.ts(qmc[H0, h * 64:(h + 1) * 64], qk[h // 2][H0, cs], C(C_HM + h % 2, 1, H0), ALU.mult,
                             reads=[qk[h // 2], cst], writes=[qmc])
                    bka = P.bank()
                    for h in range(4):
                        j = h // 2
                        P.mm(bka, bka[H0, h * 64:(h + 1) * 64], kp[j][H0, cs], qmc[H0, h * 64:(h + 1) * 64], reads=[kp[j], qmc])
                    P.tt(sm[H0, 0:256], bka[H0, 0:256], C(C_MM, 256, H0), ALU.mult, reads=[bka, cst], writes=[sm])
                    bn = P.bank()
                    bd = P.bank()
                    for h in range(4):
                        j = h // 2
                        pq = slice((h % 2) * 32, (h % 2) * 32 + 32)
                        ph = slice((h % 2) * 64, (h % 2) * 64 + 64)
                        at = sm[H0, h * 64:(h + 1) * 64]
                        qm = qmc[H0, h * 64:(h + 1) * 64]
                        P.mm(bn, bn[ph, j * 64:(j + 1) * 64], vo3[:, h, 0:64], at, reads=[vones, sm])
                        P.mm(bn, bn[ph, j * 64:(j + 1) * 64], CN3[:, j, 0:64], qm, reads=[CN, qmc])
                        P.mm(bd, bd[ph, j * 64:(j + 1) * 64], vo3[:, h, 64:128], at, reads=[vones, sm])
                        P.mm(bd, bd[ph, j * 64:(j + 1) * 64], CN3[:, j, 64:128], qm, reads=[CN, qmc])
                    copy(NT3[:, :, cs], bn.v(A, "p (j t) -> p j t", j=8)[:, 0:2, :], [bn], [NTs], eng="act")
                    copy(DN3[:, :, cs], bd.v(A, "p (j t) -> p j t", j=8)[:, 0:2, :], [bd], [DNs], eng="act")
                    bs_ = P.bank()
                    for h in range(4):
                        j = h // 2
                        P.mm(bs_, bs_[H0, h * 128:(h + 1) * 128], sm[H0, 256 + j * 64:256 + (j + 1) * 64], vo3[:, h, :], reads=[sm, vones])
                    for j in range(2):
                        P.ts(CN3[:, j, :], CN3[:, j, :], ebks[H0, j * 8 + c:j * 8 + c + 1], ALU.mult, reads=[CN, ebks], writes=[CN])
                        for h2 in range(2):
                            h = 2 * j + h2
                            P.stt(CN3[:, j, :], bs_[H0, h * 128:(h + 1) * 128], C(C_HM + h2, 1, H0), CN3[:, j, :],
                                  ALU.mult, ALU.add, reads=[CN, bs_, cst], writes=[CN])
                for j in range(2):
                    d1 = DNs.sub(j * 512, 512)
                    n1 = NTs.sub(j * 512, 512)
                    P.tt(d1[A, :], d1[A, :], ebbc[j][A, :], ALU.mult, reads=[d1, ebbc[j]], writes=[d1])
                    P.stt(d1[A, :], d1[A, :], -1.0, d1[A, :], ALU.mult, ALU.max, reads=[d1], writes=[d1])
                    P.ts(d1[A, :], d1[A, :], 1.0, ALU.max, reads=[d1], writes=[d1])
                    P.recip(d1[A, :], d1[A, :], reads=[d1], writes=[d1])
                    P.tt(n1[A, :], n1[A, :], ebbc[j][A, :], ALU.mult, reads=[n1, ebbc[j]], writes=[n1])
                    P.tt(n1[A, :], n1[A, :], d1[A, :], ALU.mult, reads=[n1, d1], writes=[n1])
                    P.tt(d1[A, :], n1[A, :], n1[A, :], ALU.mult, reads=[n1], writes=[d1])
                    bk = P.bank()
                    P.mm(bk, bk[A, :], C(C_BLKM, 128), d1[A, :], reads=[cst, d1])
                    P.ts(d1[A, :], bk[A, :], 1e-6, ALU.add, reads=[bk], writes=[d1])
                    P.recip(d1[A, :], d1[A, :], reads=[d1], writes=[d1])
                    P.act(d1[A, :], d1[A, :], AF.Sqrt, reads=[d1], writes=[d1])
                    P.tt(n1[A, :], n1[A, :], d1[A, :], ALU.mult, reads=[n1, d1], writes=[n1])
                    P.stt(RR(yT3[:, 2 + j, :]), n1[A, :], pc[A, PC_MNORM + j:PC_MNORM + j + 1], oT[j][A, :], ALU.mult, ALU.mult,
                          reads=[n1, pc, oT[j]], writes=[yT.sub((2 + j) * 512, 512)])

            P.ar_top = MIX_BASE
            if "m" in phases:
                mlstm()

            def swa():
                swaB = P.alloc(1024)
                mtmp = P.alloc(1024)
                P.dma(swaB[A, :], swab_d, writes=[swaB])
                P.dma(mtmp[A, :], cst_d[:, C_SWAM:C_SWAM + 1024], writes=[mtmp])
                P.tt(swaB[A, :], swaB[A, :], mtmp[A, :], ALU.add, reads=[swaB, mtmp], writes=[swaB])
                P.ar_top -= 2
                swaB4 = swaB.v(A, "p (c h q) -> p c h q", c=2, h=4)
                swaK = P.alloc(1280)
                swaV = P.alloc(640)
                swaK3 = swaK.v(A, "p (j t) -> p j t", j=2)
                swaV3 = swaV.v(A, "p (n c) -> p n c", n=5)
                P.cp(swaK3[:, :, 0:128], swaKc3[:, :, :], reads=[swaKc], writes=[swaK], eng="pool")
                P.cp(swaV3[:, 0, :], swaVc[A, :], reads=[swaVc], writes=[swaV], eng="pool")
                qS = [P.alloc(), P.alloc()]
                for j in range(2):
                    proj_fm(1800 + j * 128, 128, qS[j][A, :], scale=0.125, rows=qS[j])
                    proj_fm(2056 + j * 64, 64, swaK3[:, j, 128:640], dup=True, rows=swaK)
                proj_tm(2184, 128, lambda tt_: swaV3[:, 1 + tt_, :], swaV)
                ETs = [P.alloc(1024), P.alloc(1024)]
                tmp = P.alloc()
                for i in range(4):
                    gi_ = blk * 4 + i
                    qc = slice(i * 128, (i + 1) * 128)
                    prev = slice(i * 128, (i + 1) * 128)
                    cur_ = slice((i + 1) * 128, (i + 2) * 128)
                    ET = ETs[i % 2]
                    bp = P.bank() if gi_ > 0 else None
                    bc = P.bank()
                    for hq in range(4):
                        j = hq // 2
                        ph = slice((hq % 2) * 64, (hq % 2) * 64 + 64)
                        o = slice(hq * 128, (hq + 1) * 128)
                        if gi_ > 0:
                            P.mm(bp, bp[A, o], swaK3[ph, j, prev], qS[j][ph, qc], reads=[swaK, qS[j]])
                            P.mm(bp, bp[A, o], ident, swaB4[:, 0, hq, :], reads=[cst, swaB])
                        P.mm(bc, bc[A, o], swaK3[ph, j, cur_], qS[j][ph, qc], reads=[swaK, qS[j]])
                        P.mm(bc, bc[A, o], ident, swaB4[:, 1, hq, :], reads=[cst, swaB])
                    if gi_ > 0:
                        P.act(ET[A, 0:512], bp[A, :], AF.Exp, reads=[bp], writes=[ET])
                    P.act(ET[A, 512:1024], bc[A, :], AF.Exp, reads=[bc], writes=[ET])
                    bo = P.bank()
                    bd = P.bank()
                    for hq in range(4):
                        j = hq // 2
                        ph = slice((hq % 2) * 64, (hq % 2) * 64 + 64)
                        o = slice(j * 128, (j + 1) * 128)
                        e = slice(hq * 128, (hq + 1) * 128)
                        if gi_ > 0:
                            P.mm(bo, bo[ph, o], swaV3[:, i, j * 64:(j + 1) * 64], ET[A, e.start:e.stop], reads=[swaV, ET])
                            P.mm(bd, bd[ph, o], C(C_ALLONE, 64), ET[A, e.start:e.stop], reads=[cst, ET])
                        P.mm(bo, bo[ph, o], swaV3[:, i + 1, j * 64:(j + 1) * 64], ET[A, 512 + e.start:512 + e.stop], reads=[swaV, ET])
                        P.mm(bd, bd[ph, o], C(C_ALLONE, 64), ET[A, 512 + e.start:512 + e.stop], reads=[cst, ET])
                    for j in range(2):
                        P.ts(tmp[A, j * 128:(j + 1) * 128], bd[A, j * 128:(j + 1) * 128], dv[A, DV_ESINK + j:DV_ESINK + j + 1], ALU.add,
                             reads=[bd, dv], writes=[tmp])
                    P.recip(tmp[A, 0:256], tmp[A, 0:256], reads=[tmp], writes=[tmp])
                    P.tt(RR(yT3[:, 4:6, qc]), bo.v(A, "p (j q) -> p j q", j=4)[:, 0:2, :], tmp.v(A, "p (j q) -> p j q", j=4)[:, 0:2, :],
                         ALU.mult, reads=[bo, tmp], writes=[yT.sub(4 * 512, 1024)])
                P.cp(swaKc3[:, :, :], swaK3[:, :, 512:640], reads=[swaK], writes=[swaKc], eng="pool")
                P.cp(swaVc[A, :], swaV3[:, 4, :], reads=[swaV], writes=[swaVc], eng="pool")

            P.ar_top = MIX_BASE
            if "s" in phases:
                swa()

            def fox():
                qtmp = [P.alloc(), P.alloc()]
                for j in range(2):
                    proj_fm(2312 + j * 128, 128, qtmp[j][A, :], scale=0.125, rows=qtmp[j])
                    proj_fm(2568 + j * 128, 128, RR(kTh3[:, j, bs]), rows=kTh)
                for half in range(2):
                    proj_tm(2824 + half * 128, 128, lambda tt_: RR(Vh3[:, blk * 4 + tt_, half * 128:(half + 1) * 128]), Vh)
                fr = P.alloc()
                crow = P.alloc()
                vtmp = P.alloc()
                chi = foxR.sub(2 * 512, 512)
                clo = foxR.sub(3 * 512, 512)
                qF = [foxR.sub(0, 512), foxR.sub(512, 512)]
                for R_ in (slice(0, 4), slice(64, 68)):
                    proj_fm(3080, 4, fr[R_, :], rows=fr, pbase=R_.start)
                for R_ in (slice(0, 4), slice(64, 68)):
                    P.act(fr[R_, :], fr[R_, :], AF.Exp, bias=dv[R_, DV_NFOXB:DV_NFOXB + 1], scale=-1.0, reads=[fr, dv], writes=[fr])
                    P.act(fr[R_, :], fr[R_, :], AF.Ln, bias=1.0, reads=[fr], writes=[fr])
                    for sg_ in range(4):
                        seg = slice(sg_ * 128, (sg_ + 1) * 128)
                        init = ccar[R_, 0:1] if sg_ == 0 else crow[R_, sg_ * 128 - 1:sg_ * 128]
                        P.scan(crow[R_, seg], C(C_ALLONE, 128, R_), fr[R_, seg], init, ALU.mult, ALU.subtract,
                               reads=[fr, ccar, cst, crow], writes=[crow])
                    P.cp(ccar[R_, 0:1], crow[R_, 511:512], reads=[crow], writes=[ccar], eng="pool")
                    P.ts(fr[R_, :], crow[R_, :], 4097.0, ALU.mult, reads=[crow], writes=[fr])
                    P.tt(vtmp[R_, :], fr[R_, :], crow[R_, :], ALU.subtract, reads=[fr, crow], writes=[vtmp])
                    P.tt(RR(chi[R_, :]), fr[R_, :], vtmp[R_, :], ALU.subtract, reads=[fr, vtmp], writes=[chi])
                    P.tt(RR(clo[R_, :]), crow[R_, :], chi[R_, :], ALU.subtract, reads=[crow, chi], writes=[clo])
                bk = P.bank()
                for tt_ in range(4):
                    P.mm(bk, bk[A, tt_ * 4:(tt_ + 1) * 4], crow[R4, tt_ * 128:(tt_ + 1) * 128], C(C_IDENT, 4, R4), reads=[crow, cst])
                P.ts(negc3[:, blk * 4:(blk + 1) * 4, :], bk.v(A, "p (n h) -> p n h", h=4)[:, 0:4, :], -1.0, ALU.mult,
                     reads=[bk], writes=[negc])
                for j in range(2):
                    P.cp(RR(qF[j][A, :]), qtmp[j][A, :], reads=[qtmp[j]], writes=[qF[j]], eng=("act", "pool")[j])
                ETs = [foxR.sub((4 + i_) * 512, 512) for i_ in range(3)]
                rec = P.alloc()
                ei = 0
                for j in range(2):
                    bo = P.bank(pin=True)
                    bd = P.bank(pin=True)
                    for h2 in range(2):
                        h = 2 * j + h2
                        ph = slice(h2 * 64, h2 * 64 + 64)
                        for kb in range(blk * 4 + 4):
                            q0 = max(0, kb - blk * 4) * 128
                            nq = 512 - q0
                            bs_ = P.bank()
                            P.mm(bs_, bs_[A, 0:nq], kTh3[ph, j, kb * 128:(kb + 1) * 128], qF[j][ph, q0:512], reads=[kTh, qF[j]], r=True)
                            Rh = slice(h2 * 64, h2 * 64 + 4)
                            P.mm(bs_, bs_[A, 0:nq], C(C_SELF + h * 128, 128, Rh), chi[Rh, q0:512], reads=[cst, chi], r=True)
                            P.mm(bs_, bs_[A, 0:nq], C(C_SELF + h * 128, 128, Rh), clo[Rh, q0:512], reads=[cst, clo], r=True)
                            et = ETs[ei % 3]
                            ei += 1
                            P.act(RR(et[A, 0:nq]), bs_[A, 0:nq], AF.Exp, bias=negc3[:, kb, h:h + 1], reads=[bs_, negc], writes=[et])
                            if kb >= blk * 4:
                                P.tt(RR(et[A, 0:128]), et[A, 0:128], C(C_TRI, 128), ALU.mult, reads=[et, cst], writes=[et], eng="pool")
                            P.mm(bo, bo[ph, q0:512], Vh3[:, kb, h * 64:(h + 1) * 64], et[A, 0:nq], reads=[Vh, et], r=(h2 == 0))
                            P.mm(bd, bd[ph, q0:512], C(C_ALLONE, 64), et[A, 0:nq], reads=[cst, et], r=(h2 == 0))
                    P.recip(rec[A, :], bd[A, :], reads=[bd], writes=[rec])
                    P.tt(RR(yT3[:, 6 + j, :]), bo[A, :], rec[A, :], ALU.mult, reads=[bo, rec], writes=[yT.sub((6 + j) * 512, 512)])
                    bo.pinned = False
                    bd.pinned = False

            P.ar_top = MIX_BASE
            if "f" in phases:
                fox()

            if dbg == ("yT", l):
                for c in range(8):
                    P.dma(dbg_d[:, c, bs], yT3[:, c, :], reads=[yT], eng="pool")

            def post_residual(oT3, oT, gcol):
                rs = rms_rstd(lambda c: oT3[:, c, :], [oT])
                tmps = [P.alloc(), P.alloc()]
                for m in range(8):
                    t_ = tmps[m % 2]
                    P.stt(t_[A, :], oT3[:, m, :], pc[A, gcol + m:gcol + m + 1], rs[A, :], ALU.mult, ALU.mult,
                          reads=[oT, pc, rs], writes=[t_])
                    P.tt(xT3[:, m, bs], xT3[:, m, bs], t_[A, :], ALU.add, reads=[xT, t_], writes=[xT])

            if "o" not in phases:
                continue
            P.ar_top = 0
            oT = P.alloc(4096)
            oT3 = oT.v(A, "p (c t) -> p c t", c=8)
            for m in range(8):
                w = wchunk()
                w3 = w.v(A, "p (c m) -> p c m", c=8)
                P.dma(RR(w[A, :]), w_out_d[l, m], writes=[w], eng="pool")
                bk = P.bank()
                for j in range(8):
                    P.mm(bk, bk[A, :], w3[:, j, :], yT3[:, j, :], reads=[w, yT], r=True)
                copy(oT3[:, m, :], bk[A, :], [bk], [oT.sub(m * 512, 512)])
            post_residual(oT3, oT, PC_GPOST)

            if dbg == ("x1", l):
                for c in range(8):
                    P.dma(dbg_d[:, c, bs], xT3[:, c, bs], reads=[xT])

            if "n" not in phases:
                continue
            P.ar_top = 0
            rs3 = rms_rstd(lambda c: xT3[:, c, bs], [xT])
            fill_hTr()
            fT = yT
            fT3 = yT3
            oT = P.alloc(4096)
            oT3 = oT.v(A, "p (c t) -> p c t", c=8)
            gbuf = P.alloc(514)
            acc = P.alloc()
            gfp = pc[A, PC_FPRE:PC_FPRE + 8]
            for (c_lo, c_hi) in ((0, 8), (8, 16), (16, 22)):
                ng = c_hi - c_lo
                for cc in range(ng):
                    c = c_lo + cc
                    ws = []
                    for c2 in (c, NFC + c):
                        w = wchunk()
                        w3 = w.v(A, "p (c m) -> p c m", c=8)
                        P.dma(RR(w[A, :]), w_up_d[l, c2], writes=[w], eng="pool")
                        P.tt(RR(w3[:, :, :]), w3[:, :, :], gfp.to_broadcast([128, 8, 128]), ALU.mult, reads=[w, pc], writes=[w])
                        ws.append((w, w3))
                    bg = P.bank()
                    for k in range(8):
                        P.mm(bg, bg[A, :], ws[0][1][:, k, :], hTr3[:, k, :], reads=[ws[0][0], hTr], r=True)
                    bu = P.bank()
                    for k in range(8):
                        P.mm(bu, bu[A, :], ws[1][1][:, k, :], hTr3[:, k, :], reads=[ws[1][0], hTr], r=True)
                    P.tt(gbuf[A, 2:514], bg[A, :], rs3[A, :], ALU.mult, reads=[bg, rs3], writes=[gbuf])
                    P.cp(gbuf[A, 0:2], cffn[A, c * 2:c * 2 + 2], reads=[cffn], writes=[gbuf], eng="pool")
                    fw = lambda j: pc[A, PC_FCW + c * 3 + j:PC_FCW + c * 3 + j + 1]
                    P.ts(acc[A, :], gbuf[A, 0:512], fw(0), ALU.mult, reads=[gbuf, pc], writes=[acc])
                    P.stt(acc[A, :], gbuf[A, 1:513], fw(1), acc[A, :], ALU.mult, ALU.add, reads=[gbuf, pc, acc], writes=[acc])
                    P.stt(acc[A, :], gbuf[A, 2:514], fw(2), acc[A, :], ALU.mult, ALU.add, reads=[gbuf, pc, acc], writes=[acc])
                    P.cp(cffn[A, c * 2:c * 2 + 2], gbuf[A, 512:514], reads=[gbuf], writes=[cffn], eng="pool")
                    P.act(acc[A, :], acc[A, :], AF.Gelu_apprx_tanh, bias=pc[A, PC_FCB + c:PC_FCB + c + 1], reads=[acc, pc], writes=[acc])
                    P.tt(acc[A, :], acc[A, :], rs3[A, :], ALU.mult, reads=[acc, rs3], writes=[acc])
                    P.tt(RR(fT3[:, cc, :]), acc[A, :], bu[A, :], ALU.mult, reads=[acc, bu], writes=[fT.sub(cc * 512, 512)])
                for m in range(8):
                    wa = wchunk()
                    wa3 = wa.v(A, "p (c m) -> p c m", c=8)
                    P.dma(RR(wa[A, 0:ng * 128]), w_dn_d[l, m, :, c_lo * 128:c_hi * 128], writes=[wa], eng="pool")
                    bk = P.bank()
                    for cc in range(ng):
                        P.mm(bk, bk[A, :], wa3[:, cc, :], fT3[:, cc, :], reads=[wa, fT], r=True)
                    om = oT.sub(m * 512, 512)
                    if c_lo == 0:
                        copy(oT3[:, m, :], bk[A, :], [bk], [om])
                    else:
                        P.tt(oT3[:, m, :], oT3[:, m, :], bk[A, :], ALU.add, reads=[om, bk], writes=[om])
            post_residual(oT3, oT, PC_FPOST)

    outops = []
    for c in range(8):
        outops.append(P.dma(out_d[:, c, 0:T], xT3[:, c, :], reads=[xT]))
    P.emit(outops + [op for op in P.dma_ops if False])
    P.close()
    return nc


_NC_CACHE = {}


def kernel(**inputs):
    maps = prep_inputs(inputs)
    if "nc" not in _NC_CACHE:
        _NC_CACHE["nc"] = build()
    nc = _NC_CACHE["nc"]
    res = run_bass_kernel_spmd(nc, maps, core_ids=list(range(len(maps))))
    x = np.asarray(inputs["x"])
    out = np.empty(x.shape, np.float32)
    for b in range(x.shape[0]):
        oT = np.asarray(res.results[b]["outT"])
        out[b] = oT.transpose(1, 0, 2).reshape(D, SEQ).T
    return out
```

```python
import contextlib
import math
import numpy as np
import concourse.bass as bass
import concourse.mybir as mybir
from concourse.bass_utils import run_bass_kernel_spmd

F32 = mybir.dt.float32
F32R = mybir.dt.float32r
BF16 = mybir.dt.bfloat16
ALU = mybir.AluOpType
AF = mybir.ActivationFunctionType

ENGS = ("pe", "act", "dve", "pool", "sp")


def _flat(ks, out):
    for k in ks:
        if isinstance(k, (str, int)):
            out.append(k)
        elif isinstance(k, tuple) and (len(k) == 0 or isinstance(k[0], (str, int))):
            out.append(k)
        elif hasattr(k, "k"):
            _flat(k.k, out)
        else:
            _flat(k, out)
    return out


class Op:
    __slots__ = ("eng", "fn", "deps", "signals", "count", "pos", "dma", "dsem", "dval", "prewait")

    def __init__(self, eng, fn, dma=False):
        self.eng = eng
        self.fn = fn
        self.deps = []
        self.signals = False
        self.count = None
        self.pos = None
        self.dma = dma
        self.dsem = None
        self.dval = None
        self.prewait = None


class Buf:
    def __init__(self, t, off, ncols, keys):
        self.t, self.off, self.n, self.k = t, off, ncols, keys

    def __getitem__(self, idx):
        p, c = idx
        if isinstance(c, int):
            c = slice(c, c + 1)
        a = 0 if c.start is None else c.start
        b = self.n if c.stop is None else c.stop
        assert 0 <= a <= b <= self.n, (a, b, self.n)
        return self.t[p, self.off + a:self.off + b]

    def v(self, p, pat, **kw):
        return self.t[p, self.off:self.off + self.n].rearrange(pat, **kw)

    def sub(self, c0, n):
        ks = self.k
        if len(ks) > 1 and len(ks) * 512 >= self.n:
            ks = ks[c0 // 512:(c0 + n + 511) // 512]
        return Buf(self.t, self.off + c0, n, ks)


class Bank(Buf):
    def __init__(self, t, name):
        super().__init__(t, 0, 512, [name])
        self.opened = set()
        self.pinned = False


class Prog:
    NDMA = 32

    def __init__(self, nc):
        self.nc = nc
        self.ops = {e: [] for e in ENGS}
        self.lastw = {}
        self.readers = {}
        self.waited = {e: {} for e in ENGS}
        self.waited_dma = {e: set() for e in ENGS}
        self.dma_ops = []
        self.stack = contextlib.ExitStack()
        self.banks = []
        self.bank_i = 0
        self.ar = None
        self.ar_top = 0

    def sb(self, name, ncols, parts=128, dtype=F32):
        t = self.stack.enter_context(self.nc.sbuf_tensor("sb_" + name, [parts, ncols], dtype))
        return Buf(t, 0, ncols, [name])

    def sbs(self, name, nslots, dtype=F32):
        t = self.stack.enter_context(self.nc.sbuf_tensor("sb_" + name, [128, nslots * 512], dtype))
        return Buf(t, 0, nslots * 512, [(name, i) for i in range(nslots)])

    def make_banks(self):
        for i in range(8):
            t = self.stack.enter_context(self.nc.psum_tensor(f"bank{i}", [128, 512], F32))
            self.banks.append(Bank(t, f"bank{i}"))

    def bank(self, pin=False):
        for _ in range(16):
            b = self.banks[self.bank_i % 8]
            self.bank_i += 1
            if not b.pinned:
                b.opened = set()
                b.pinned = pin
                return b
        raise RuntimeError("no free psum bank")

    def make_arena(self, nslots):
        self.ar = self.stack.enter_context(self.nc.sbuf_tensor("arena", [128, nslots * 512], F32))
        self.ar_n = nslots

    def alloc(self, ncols=512):
        ns = (ncols + 511) // 512
        assert self.ar_top + ns <= self.ar_n, ("arena overflow", self.ar_top, ns, self.ar_n)
        b = Buf(self.ar, self.ar_top * 512, ncols, [("ar", s) for s in range(self.ar_top, self.ar_top + ns)])
        self.ar_top += ns
        return b

    def add(self, eng, fn, reads=(), writes=(), dma=False):
        op = Op(eng, fn, dma)
        op.pos = len(self.ops[eng])
        reads = _flat(reads, [])
        writes = _flat(writes, [])
        deps = []
        for k in reads:
            w = self.lastw.get(k)
            if w is not None:
                deps.append(w)
        for k in writes:
            w = self.lastw.get(k)
            if w is not None:
                deps.append(w)
            deps.extend(self.readers.get(k, ()))
        wt = self.waited[eng]
        wd = self.waited_dma[eng]
        best = {}
        dm = []
        for d in deps:
            if d is op:
                continue
            if d.dma:
                if id(d) not in wd:
                    wd.add(id(d))
                    dm.append(d)
                continue
            if d.eng == eng and eng in ("pe", "sp"):
                continue
            if wt.get(d.eng, -1) >= d.pos:
                continue
            if d.eng not in best or best[d.eng].pos < d.pos:
                best[d.eng] = d
        op.deps = list(best.values()) + dm
        for d in best.values():
            d.signals = True
            wt[d.eng] = max(wt.get(d.eng, -1), d.pos)
        for k in reads:
            self.readers.setdefault(k, []).append(op)
        for k in writes:
            self.lastw[k] = op
            self.readers[k] = []
        self.ops[eng].append(op)
        if dma:
            self.dma_ops.append(op)
        return op

    def emit(self, out_dma_ops=()):
        nc = self.nc
        nd = self.NDMA
        sems = {e: self.stack.enter_context(nc.semaphore(f"s_{e}")) for e in ENGS if e != "sp"}
        dsems = [self.stack.enter_context(nc.semaphore(f"s_dma{i}")) for i in range(nd)]
        for e in ENGS:
            c = 0
            for op in self.ops[e]:
                if not op.dma and op.signals:
                    c += 1
                    op.count = c
        per_eng = {}
        for op in self.dma_ops:
            per_eng.setdefault(op.eng, []).append(op)
        engs_with_dma = list(per_eng.keys())
        share = nd // max(1, len(engs_with_dma))
        for ei, e in enumerate(engs_with_dma):
            mysems = dsems[ei * share:(ei + 1) * share]
            for i, op in enumerate(per_eng[e]):
                op.dsem = mysems[i % share]
                op.dval = 16 * (i // share + 1)
                if i >= share:
                    op.prewait = (op.dsem, 16 * (i // share))
        final_waits = [(op.dsem, op.dval) for op in out_dma_ops]

        def run(e, eng):
            for op in self.ops[e]:
                if op.prewait is not None:
                    eng.wait_ge(op.prewait[0], op.prewait[1])
                for d in op.deps:
                    if d.dma:
                        eng.wait_ge(d.dsem, d.dval)
                    else:
                        eng.wait_ge(sems[d.eng], d.count)
                ins = op.fn(eng)
                if op.dma:
                    ins.then_inc(op.dsem, 16)
                elif op.signals:
                    ins.then_inc(sems[e], 1)
            if e == "sp":
                for s, v in final_waits:
                    eng.wait_ge(s, v)

        with nc.Block() as block:
            @block.tensor
            def _(eng):
                run("pe", eng)

            @block.scalar
            def _(eng):
                run("act", eng)

            @block.vector
            def _(eng):
                run("dve", eng)

            @block.gpsimd
            def _(eng):
                run("pool", eng)

            @block.sync
            def _(eng):
                run("sp", eng)

    def close(self):
        self.stack.close()

    def dma(self, out, in_, reads=(), writes=(), eng="sp"):
        return self.add(eng, lambda e: e.dma_start(out=out, in_=in_), reads, writes, dma=True)

    def mm(self, bank, out, lhsT, rhs, reads=(), r=False):
        p0 = out.start_partition()
        q = frozenset(range(p0 // 32, (p0 + out.partition_size() - 1) // 32 + 1))
        c0 = out.offset % 512 if hasattr(out, "offset") else 0
        c0 = self._ap_col0(out)
        c1 = c0 + self._ap_ncols(out)
        start = True
        for (qq, a, b) in bank.opened:
            if (qq & q) and a < c1 and c0 < b:
                assert q <= qq and a <= c0 and c1 <= b, ("partial psum overlap", q, qq, c0, c1, a, b)
                start = False
        if start:
            bank.opened.add((q, c0, c1))
        if r and FAST_MM and lhsT.dtype == F32:
            lhsT = lhsT.bitcast(F32R)
            rhs = rhs.bitcast(F32R)
        return self.add("pe", lambda e: e.matmul(out, lhsT, rhs, start=start, stop=True, skip_group_check=True),
                        reads, [bank])

    @staticmethod
    def _ap_col0(ap):
        pstride = ap.ap[0][0]
        return ap.offset % pstride

    @staticmethod
    def _ap_ncols(ap):
        span = 0
        for st, n in list(ap.ap)[1:]:
            span += st * (n - 1)
        return span + 1

    def tr(self, bank, out, in_, ident, reads=()):
        return self.add("pe", lambda e: e.transpose(out, in_, ident), reads, [bank])

    def act(self, out, in_, func, bias=None, scale=1.0, reads=(), writes=()):
        if bias is None:
            return self.add("act", lambda e: e.activation(out, in_, func, scale=scale), reads, writes)
        return self.add("act", lambda e: e.activation(out, in_, func, bias=bias, scale=scale), reads, writes)

    def tt(self, out, a, b, op, reads=(), writes=(), eng="dve"):
        return self.add(eng, lambda e: e.tensor_tensor(out, a, b, op), reads, writes)

    def ts(self, out, a, s1, op0, s2=None, op1=None, reads=(), writes=(), eng="dve"):
        if op1 is None:
            return self.add(eng, lambda e: e.tensor_scalar(out, a, s1, None, op0), reads, writes)
        return self.add(eng, lambda e: e.tensor_scalar(out, a, s1, s2, op0, op1), reads, writes)

    def stt(self, out, a, s, b, op0, op1, reads=(), writes=()):
        return self.add("dve", lambda e: e.scalar_tensor_tensor(out, a, s, b, op0, op1), reads, writes)

    def cp(self, out, a, reads=(), writes=(), eng="dve"):
        if eng == "act":
            return self.add(eng, lambda e: e.copy(out, a), reads, writes)
        return self.add(eng, lambda e: e.tensor_copy(out, a), reads, writes)

    def recip(self, out, a, reads=(), writes=()):
        return self.add("dve", lambda e: e.reciprocal(out, a), reads, writes)

    def scan(self, out, d0, d1, init, op0, op1, reads=(), writes=()):
        return self.add("dve", lambda e: e.tensor_tensor_scan(out, d0, d1, init, op0, op1), reads, writes)

    def memset(self, ap, v, writes=(), eng="dve"):
        return self.add(eng, lambda e: e.memset(ap, v), (), writes)


D = 1024
SEQ = 2048
DEPTH = 2
TB = 512
RW_STOP = 0
FAST_MM = True
N_IN = 3084
DFF = 2816
NFC = DFF // 128

PC_GPRE, PC_GPOST, PC_FPRE, PC_FPOST, PC_MU = 0, 8, 16, 24, 32
PC_W0, PC_A0, PC_KK, PC_KA, PC_RK, PC_LNW, PC_LNB, PC_MNORM, PC_SINK = 40, 42, 44, 46, 48, 50, 52, 54, 56
PC_CW, PC_CB, PC_BI, PC_BF, PC_FOXB, PC_FCW, PC_FCB, NPC = 58, 74, 78, 79, 80, 81, 147, 169
DV_OMKA, DV_BI15, DV_BF15, DV_NFOXB, DV_ESINK, NDV = 0, 2, 3, 4, 5, 8

C_IDENT, C_ONESD, C_ALLONE, C_BLK1, C_BLKM, C_TRI = 0, 128, 256, 384, 512, 640
C_MA, C_MB, C_IDG, C_MM, C_SELF, C_SELK, C_SELV, C_HM, C_SWAM, NCST = 768, 1280, 1408, 1536, 1792, 2304, 2432, 2688, 2696, 3720


W_IN_CHUNKS = ([(768, 128, False), (896, 128, False)]
               + [(i * 256 + hp * 128, 128, False) for hp in range(2) for i in range(3)]
               + [(1024 + i * 64, 64, False) for i in range(4)]
               + [(c0 + hp * 128, 128, False) for hp in range(2) for c0 in (1280, 1536)]
               + [(1792, 4, False), (1796, 4, False)]
               + [(1800, 128, False), (1928, 128, False), (2056, 64, True), (2120, 64, True), (2184, 128, False)]
               + [(2312, 128, False), (2440, 128, False), (2568, 128, False), (2696, 128, False)]
               + [(2824, 128, False), (2952, 128, False), (3080, 4, False)])
W_IN_OFF = {}
_o = 0
for (_c0, _m, _d) in W_IN_CHUNKS:
    W_IN_OFF[(_c0, _m)] = _o
    _o += 8 * (128 if _d else _m)
W_IN_TOT = _o


def _t5_bucket(dist):
    max_exact = 16
    d = np.maximum(dist, 1).astype(np.float32)
    large = max_exact + (np.log(d / max_exact) / math.log(128 / max_exact) * (32 - max_exact)).astype(np.int32)
    large = np.minimum(large, 31)
    return np.where(dist < max_exact, dist, large).astype(np.int32)


def _swa_dist():
    s = np.arange(128)[:, None, None]
    pcx = np.arange(2)[None, :, None]
    tq = np.arange(128)[None, None, :]
    sk = s + 128 * pcx
    dist = tq + 128 - sk
    vis = (dist >= 0) & (dist < 128)
    return dist, vis


def make_consts():
    c = np.zeros((128, NCST), np.float32)
    c[:, C_IDENT:C_IDENT + 128] = np.eye(128)
    c[:, C_ONESD:C_ONESD + 128] = 1.0 / 1024
    c[:, C_ALLONE:C_ALLONE + 128] = 1.0
    blk = np.zeros((128, 128), np.float32)
    blk[:64, :64] = 1
    blk[64:, 64:] = 1
    c[:, C_BLK1:C_BLK1 + 128] = blk
    c[:, C_BLKM:C_BLKM + 128] = blk / 64.0
    k = np.arange(128)
    c[:, C_TRI:C_TRI + 128] = (k[:, None] <= k[None, :])
    i = np.arange(64)[:, None]
    t = np.arange(64)[None, :]
    ma = np.zeros((64, 2, 2, 2, 64), np.float32)
    ma[:, :, :, 0, :] = (i < t)[:, None, None, :]
    ma[:, :, :, 1, :] = (i <= t)[:, None, None, :]
    c[:64, C_MA:C_MA + 512] = ma.reshape(64, 512)
    mb = np.zeros((64, 2, 64), np.float32)
    mb[:] = (i > t)[:, None, :]
    c[:64, C_MB:C_MB + 128] = mb.reshape(64, 128)
    idg = np.zeros((64, 2, 64), np.float32)
    idg[:] = np.eye(64)[:, None, :]
    c[:64, C_IDG:C_IDG + 128] = idg.reshape(64, 128)
    mm = np.zeros((64, 4, 64), np.float32)
    mm[:] = (i <= t)[:, None, :]
    c[:64, C_MM:C_MM + 256] = mm.reshape(64, 256)
    sf = np.zeros((4, 4, 128), np.float32)
    for h in range(4):
        sf[h, h, :] = 1
    c[:4, C_SELF:C_SELF + 512] = sf.reshape(4, 512)
    c[64:68, C_SELF:C_SELF + 512] = sf.reshape(4, 512)
    sk = np.zeros((4, 2, 64), np.float32)
    sv = np.zeros((4, 2, 128), np.float32)
    for h in range(4):
        for j in range(2):
            sk[h, j, :] = (h == 2 * j + np.arange(64) // 32)
            sv[h, j, :] = (h == 2 * j + np.arange(128) // 64)
    c[:4, C_SELK:C_SELK + 128] = sk.reshape(4, 128)
    c[:4, C_SELV:C_SELV + 256] = sv.reshape(4, 256)
    c[0:32, C_HM] = 1.0
    c[32:64, C_HM + 1] = 1.0
    _, vis = _swa_dist()
    m = np.where(vis, 0.0, -30000.0).astype(np.float32)
    m4 = np.broadcast_to(m[:, :, None, :], (128, 2, 4, 128))
    c[:, C_SWAM:C_SWAM + 1024] = m4.reshape(128, 1024)
    return c


def _cols(v, n):
    return np.ascontiguousarray(np.asarray(v, np.float32).reshape(n, 128).T)


def prep_inputs(inp):
    L = DEPTH
    f = lambda k: np.asarray(inp[k], np.float32)
    w_in4 = f("w_in").reshape(L, 8, 128, N_IN).transpose(0, 2, 1, 3)
    w_in_r = np.zeros((L, 128, W_IN_TOT), np.float32)
    for (c0, m, d) in W_IN_CHUNKS:
        blk = w_in4[:, :, :, c0:c0 + m]
        if d:
            blk = np.concatenate([blk, blk], axis=3)
        w_in_r[:, :, W_IN_OFF[(c0, m)]:W_IN_OFF[(c0, m)] + 8 * blk.shape[3]] = blk.reshape(L, 128, -1)
    w_out_r = np.ascontiguousarray(f("w_out").reshape(L, 8, 128, 8, 128).transpose(0, 3, 2, 1, 4)).reshape(L, 8, 128, 1024)
    w_up_r = np.ascontiguousarray(f("ffn_w_up").reshape(L, 8, 128, 2 * NFC, 128).transpose(0, 3, 2, 1, 4)).reshape(L, 2 * NFC, 128, 1024)
    w_dn_r = np.ascontiguousarray(f("ffn_w_down").reshape(L, NFC, 128, 8, 128).transpose(0, 3, 2, 1, 4)).reshape(L, 8, 128, NFC * 128)
    lora = np.zeros((L, 128, 512), np.float32)
    lora[:, 0:64, 0:256] = f("rwkv_w_up")
    lora[:, 64:128, 0:256] = f("rwkv_a_up")
    lora[:, :, 256:512] = f("rwkv_g_up")
    pc = np.zeros((L, 128, NPC), np.float32)
    for l in range(L):
        pc[l, :, PC_GPRE:PC_GPRE + 8] = _cols(f("norm_mix_pre")[l], 8)
        pc[l, :, PC_GPOST:PC_GPOST + 8] = _cols(f("norm_mix_post")[l], 8)
        pc[l, :, PC_FPRE:PC_FPRE + 8] = _cols(f("norm_ffn_pre")[l], 8)
        pc[l, :, PC_FPOST:PC_FPOST + 8] = _cols(f("norm_ffn_post")[l], 8)
        pc[l, :, PC_MU:PC_MU + 8] = _cols(f("rwkv_mu")[l], 8)
        for col, key in ((PC_W0, "rwkv_w0"), (PC_A0, "rwkv_a0"), (PC_KK, "rwkv_k_k"), (PC_KA, "rwkv_k_a"),
                         (PC_LNW, "rwkv_ln_w"), (PC_LNB, "rwkv_ln_b"), (PC_MNORM, "mlstm_norm")):
            pc[l, :, col:col + 2] = _cols(f(key)[l], 2)
        pc[l, :, PC_RK:PC_RK + 2] = _cols(f("rwkv_r_k")[l].reshape(256), 2)
        pc[l, :, PC_SINK:PC_SINK + 2] = _cols(np.repeat(f("swa_sinks")[l], 64), 2)
        cw = f("mlstm_conv_w")[l]
        for i in range(4):
            for j in range(4):
                pc[l, 0:64, PC_CW + i * 4 + j] = cw[j, i * 64:(i + 1) * 64]
            pc[l, 0:64, PC_CB + i] = f("mlstm_conv_b")[l][i * 64:(i + 1) * 64]
        pc[l, 0:4, PC_BI] = f("mlstm_b_i")[l]
        pc[l, 0:4, PC_BF] = f("mlstm_b_f")[l]
        pc[l, 0:4, PC_FOXB] = f("fox_b_f")[l]
        pc[l, 64:68, PC_FOXB] = f("fox_b_f")[l]
        fw = f("ffn_conv_w")[l]
        for j in range(3):
            pc[l, :, PC_FCW + j:PC_FCW + 66:3] = _cols(fw[j], NFC)
        pc[l, :, PC_FCB:PC_FCB + NFC] = _cols(f("ffn_conv_b")[l], NFC)
    dist, _ = _swa_dist()
    bk = _t5_bucket(np.clip(dist, 0, 127))
    swab = f("rel_bias")[bk]
    swab = np.ascontiguousarray(swab.transpose(0, 1, 3, 2)).reshape(128, 1024)
    cst = make_consts()
    x = f("x")
    maps = []
    for b in range(x.shape[0]):
        xT = np.ascontiguousarray(x[b].T.reshape(8, 128, x.shape[1]).transpose(1, 0, 2))
        maps.append({"xT": xT, "w_in_r": w_in_r, "w_out_r": w_out_r, "w_up_r": w_up_r, "w_dn_r": w_dn_r,
                     "lora": lora, "pc": pc, "cst": cst, "swab": swab})
    return maps


ARENA_SLOTS = 26


def build(nlayer=DEPTH, nblk=SEQ // TB, dbg=None, phases="rmsfon"):
    nc = bass.Bass("TRN2", target_bir_lowering=False)
    T = nblk * TB
    NT = T // 128
    L = DEPTH
    dr = lambda n, s, kind="ExternalInput": nc.dram_tensor(n, list(s), F32, kind=kind).ap()
    xT_d = dr("xT", [128, 8, SEQ])
    w_in_d = dr("w_in_r", [L, 128, W_IN_TOT])
    w_out_d = dr("w_out_r", [L, 8, 128, 1024])
    w_up_d = dr("w_up_r", [L, 2 * NFC, 128, 1024])
    w_dn_d = dr("w_dn_r", [L, 8, 128, NFC * 128])
    lora_d = dr("lora", [L, 128, 512])
    pc_d = dr("pc", [L, 128, NPC])
    cst_d = dr("cst", [128, NCST])
    swab_d = dr("swab", [128, 1024])
    out_d = dr("outT", [128, 8, SEQ], "ExternalOutput")
    dbg_d = dr("dbg", [128, 8, SEQ], "ExternalOutput") if dbg else None

    P = Prog(nc)
    RR = lambda ap: ap.bitcast(F32R) if (FAST_MM and ap.dtype == F32) else ap
    RR0 = RR
    P.make_banks()
    xT = P.sb("xT", 8 * T)
    kTh = P.sb("kTh", 2 * T)
    Vh = P.sb("Vh", NT * 256)
    negc = P.sb("negc", NT * 4)
    yT = P.sbs("yT", 8, dtype=BF16)
    foxR = P.sbs("foxR", 7)
    cst = P.sb("cst", C_SWAM)
    pc = P.sb("pc", NPC)
    dv = P.sb("dv", NDV)
    rstm = P.sb("rstm", 4)
    Srw = P.sb("Srw", 2 * 2 * 2 * 64)
    pc1 = P.sb("pc1", 8)
    CN = P.sb("CN", 2 * 128)
    crw = P.sb("crw", 8)
    cml = P.sb("cml", 12)
    cffn = P.sb("cffn", NFC * 2)
    ccar = P.sb("ccar", 2)
    swaKc = P.sb("swaKc", 2 * 128)
    swaVc = P.sb("swaVc", 128)
    hTr = P.sbs("hTr", 8, dtype=BF16)
    ebks = P.sb("ebks", 16)
    wbufs = [P.sb(f"wch{i}", 8 * 128, dtype=BF16) for i in range(4)]
    P.make_arena(ARENA_SLOTS)

    A = slice(0, 128)
    H0 = slice(0, 64)
    H1 = slice(64, 128)
    R4 = slice(0, 4)
    xT3 = xT.v(A, "p (c t) -> p c t", c=8)
    yT3 = yT.v(A, "p (c t) -> p c t", c=8)
    kTh3 = kTh.v(A, "p (j t) -> p j t", j=2)
    Vh3 = Vh.v(A, "p (n c) -> p n c", c=256)
    negc3 = negc.v(A, "p (n h) -> p n h", h=4)
    swaKc3 = swaKc.v(A, "p (j t) -> p j t", j=2)
    hTr3 = hTr.v(A, "p (c t) -> p c t", c=8)
    CN3 = CN.v(H0, "p (j c) -> p j c", j=2)
    S5 = Srw.v(H0, "p (j h b v) -> p j h b v", j=2, h=2, b=2)

    def C(off, n, p=A):
        return cst[p, off:off + n]

    ident = C(C_IDENT, 128)
    evac_i = [0]

    def copy(out, in_, reads, writes, eng=None):
        if eng is None:
            evac_i[0] += 1
            eng = "act" if evac_i[0] % 2 else "dve"
        return P.cp(out, in_, reads, writes, eng=eng)

    wb_i = [0]

    def wchunk():
        b = wbufs[wb_i[0] % 4]
        wb_i[0] += 1
        return b

    P.dma(RR0(cst[A, :]), cst_d[:, 0:C_SWAM], writes=[cst], eng="pool")
    for c in range(8):
        P.dma(xT3[:, c, :], xT_d[:, c, 0:T], writes=[xT])
    if phases != "rmsfon":
        P.ts(RR(yT[A, :]), cst[A, 0:4096 if C_SWAM >= 4096 else 2048].to_broadcast([128, 4096]) if False else xT[A, 0:4096], 0.0, ALU.mult, reads=[xT], writes=[yT])
    P.ar_top = 0

    def rms_rstd(src_fn, reads, eps=1e-6):
        ssb = P.bank(pin=True)
        rstd = P.alloc()
        sq = [P.alloc(), P.alloc()]
        for c in range(8):
            s = sq[c % 2]
            P.act(s[A, :], src_fn(c), AF.Square, reads=reads, writes=[s])
            P.mm(ssb, ssb[A, :], C(C_ONESD, 128), s[A, :], reads=[cst, s])
        P.ts(rstd[A, :], ssb[A, :], eps, ALU.add, reads=[ssb], writes=[rstd])
        ssb.pinned = False
        P.recip(rstd[A, :], rstd[A, :], reads=[rstd], writes=[rstd])
        P.act(rstd[A, :], rstd[A, :], AF.Sqrt, reads=[rstd], writes=[rstd])
        P.ar_top -= 2
        return rstd

    for l in range(nlayer):
        P.dma(pc[A, :], pc_d[l], writes=[pc])
        P.ts(dv[A, DV_OMKA:DV_OMKA + 2], pc[A, PC_KA:PC_KA + 2], -1.0, ALU.mult, 1.0, ALU.add, reads=[pc], writes=[dv])
        P.ts(dv[R4, DV_BI15:DV_BI15 + 2], pc[R4, PC_BI:PC_BI + 2], 1.0 / 15.0, ALU.mult, reads=[pc], writes=[dv])
        P.ts(dv[A, DV_NFOXB:DV_NFOXB + 1], pc[A, PC_FOXB:PC_FOXB + 1], -1.0, ALU.mult, reads=[pc], writes=[dv])
        P.act(dv[A, DV_ESINK:DV_ESINK + 2], pc[A, PC_SINK:PC_SINK + 2], AF.Exp, reads=[pc], writes=[dv])
        for b_ in (Srw, CN, crw, cml, cffn, ccar, swaKc, swaVc):
            P.memset(b_[A, :], 0.0, writes=[b_])

        for blk in range(nblk):
            t0 = blk * TB
            bs = slice(t0, t0 + TB)
            P.ar_top = 0
            rstd = rms_rstd(lambda c: xT3[:, c, bs], [xT])
            bk = P.bank()
            for tt_ in range(4):
                P.tr(bk, bk[A, tt_ * 128:(tt_ + 1) * 128], rstd[A, tt_ * 128:(tt_ + 1) * 128], ident, reads=[rstd, cst])
            P.cp(rstm[A, 0:4], bk.v(A, "p (n c) -> p n c", n=4)[:, :, 0], reads=[bk], writes=[rstm])
            MIX_BASE = P.ar_top

            def fill_hTr(gcol):
                for c in range(8):
                    gs = pc[A, gcol + c:gcol + c + 1]
                    if c % 2:
                        P.act(hTr3[:, c, :], xT3[:, c, bs], AF.Copy, scale=gs, reads=[xT, pc], writes=[hTr.sub(c * 512, 512)])
                    else:
                        P.ts(hTr3[:, c, :], xT3[:, c, bs], gs, ALU.mult, reads=[xT, pc], writes=[hTr.sub(c * 512, 512)], eng="pool")

            fill_hTr(PC_GPRE)
            gpre = pc[A, PC_GPRE:PC_GPRE + 8]

            def load_w(col0, M, dup=False):
                w = wchunk()
                w3 = w.v(A, "p (c m) -> p c m", c=8)
                off = W_IN_OFF[(col0, M)]
                if dup:
                    M = 128
                if M == 128:
                    P.dma(RR(w[A, :]), w_in_d[l, :, off:off + 1024], writes=[w], eng="pool")
                else:
                    P.dma(RR(w3[:, :, 0:M]), w_in_d[l, :, off:off + 8 * M].rearrange("p (c m) -> p c m", c=8), writes=[w], eng="pool")
                return w, w3, M

            def proj_fm(col0, M, dst, dup=False, scale=None, rows=None, pbase=0):
                w, w3, M = load_w(col0, M, dup)
                bk = P.bank()
                r = slice(pbase, pbase + M)
                for c in range(8):
                    P.mm(bk, bk[r, :], w3[:, c, 0:M], hTr3[:, c, :], reads=[w, hTr], r=(pbase == 0))
                if scale is None:
                    P.tt(dst, bk[r, :], rstd[r, :], ALU.mult, reads=[bk, rstd], writes=[rows])
                else:
                    P.stt(dst, bk[r, :], scale, rstd[r, :], ALU.mult, ALU.mult, reads=[bk, rstd], writes=[rows])

            def proj_tm(col0, ncols, dst_fn, wr):
                w, w3, _ = load_w(col0, ncols)
                for tt_ in range(4):
                    bk = P.bank()
                    for c in range(8):
                        P.mm(bk, bk[A, 0:ncols], hTr3[:, c, tt_ * 128:(tt_ + 1) * 128], w3[:, c, 0:ncols], reads=[w, hTr], r=True)
                    P.ts(dst_fn(tt_), bk[A, 0:ncols], rstm[A, tt_:tt_ + 1], ALU.mult, reads=[bk, rstm], writes=[wr])

            def shift_mix(z, ci, dst):
                d = P.alloc()
                P.tt(d[A, 1:512], z[A, 0:511], z[A, 1:512], ALU.subtract, reads=[z], writes=[d])
                P.tt(d[A, 0:1], crw[A, ci:ci + 1], z[A, 0:1], ALU.subtract, reads=[z, crw], writes=[d])
                P.cp(crw[A, ci:ci + 1], z[A, 511:512], reads=[z], writes=[crw], eng="pool")
                P.stt(dst[A, :], d[A, :], pc[A, PC_MU + ci:PC_MU + ci + 1], z[A, :], ALU.mult, ALU.add,
                      reads=[d, pc, z], writes=[dst])
                P.ar_top -= 1

            def rwkv():
                lora = P.alloc()
                P.dma(lora[A, :], lora_d[l], writes=[lora])
                swa_ = P.alloc()
                sg = P.alloc()
                ztmp = P.alloc()
                proj_fm(768, 128, ztmp[A, :], rows=ztmp)
                shift_mix(ztmp, 6, swa_)
                proj_fm(896, 128, ztmp[A, :], rows=ztmp)
                shift_mix(ztmp, 7, sg)
                P.ar_top -= 1
                P.act(swa_[H0, :], swa_[H0, :], AF.Tanh, reads=[swa_], writes=[swa_])
                P.act(sg[A, :], sg[A, :], AF.Sigmoid, reads=[sg], writes=[sg])
                if RW_STOP == 1:
                    return
                base_top = P.ar_top
                for hp in range(2):
                    P.ar_top = base_top
                    z = [P.alloc() for _ in range(3)]
                    s = [P.alloc() for _ in range(3)]
                    for i in range(3):
                        proj_fm(i * 256 + hp * 128, 128, z[i][A, :], rows=z[i])
                    for i in range(3):
                        shift_mix(z[i], i * 2 + hp, s[i])
                    sr, sk, sv = s
                    cw = slice(hp * 128, (hp + 1) * 128)
                    pcc = lambda c0: pc[A, c0 + hp:c0 + hp + 1]
                    bk = P.bank()
                    P.mm(bk, bk[A, :], lora[H0, cw], swa_[H0, :], reads=[lora, swa_])
                    lw = z[0]
                    P.act(lw[A, :], bk[A, :], AF.Sigmoid, bias=pcc(PC_W0), reads=[bk, pc], writes=[lw])
                    P.ts(lw[A, :], lw[A, :], -math.exp(-0.5), ALU.mult, reads=[lw], writes=[lw])
                    bk = P.bank()
                    P.mm(bk, bk[A, :], lora[H1, cw], swa_[H1, :], reads=[lora, swa_])
                    a = z[1]
                    P.act(a[A, :], bk[A, :], AF.Sigmoid, bias=pcc(PC_A0), reads=[bk, pc], writes=[a])
                    bk = P.bank()
                    P.mm(bk, bk[A, :], lora[A, 256 + hp * 128:256 + (hp + 1) * 128], sg[A, :], reads=[lora, sg])
                    g = z[2]
                    copy(g[A, :], bk[A, :], [bk], [g])
                    kk = P.alloc()
                    tmp = P.alloc()
                    P.ts(kk[A, :], sk[A, :], pcc(PC_KK), ALU.mult, reads=[sk, pc], writes=[kk])
                    P.tt(tmp[A, :], kk[A, :], kk[A, :], ALU.mult, reads=[kk], writes=[tmp])
                    bk = P.bank()
                    P.mm(bk, bk[A, :], C(C_BLK1, 128), tmp[A, :], reads=[cst, tmp])
                    P.act(tmp[A, :], bk[A, :], AF.Sqrt, reads=[bk], writes=[tmp])
                    P.ts(tmp[A, :], tmp[A, :], 1e-12, ALU.max, reads=[tmp], writes=[tmp])
                    P.recip(tmp[A, :], tmp[A, :], reads=[tmp], writes=[tmp])
                    P.tt(kk[A, :], kk[A, :], tmp[A, :], ALU.mult, reads=[kk, tmp], writes=[kk])
                    kmod = P.alloc()
                    P.ts(kmod[A, :], a[A, :], pcc(PC_KA), ALU.mult, dv[A, DV_OMKA + hp:DV_OMKA + hp + 1], ALU.add,
                         reads=[a, pc, dv], writes=[kmod])
                    P.tt(kmod[A, :], kmod[A, :], sk[A, :], ALU.mult, reads=[kmod, sk], writes=[kmod])
                    bonus = sk
                    P.stt(tmp[A, :], sr[A, :], pcc(PC_RK), kmod[A, :], ALU.mult, ALU.mult, reads=[sr, pc, kmod], writes=[tmp])
                    bk = P.bank()
                    P.mm(bk, bk[A, :], C(C_BLK1, 128), tmp[A, :], reads=[cst, tmp])
                    P.tt(bonus[A, :], bk[A, :], sv[A, :], ALU.mult, reads=[bk, sv], writes=[bonus])
                    bb = a
                    P.tt(bb[A, :], kk[A, :], a[A, :], ALU.mult, reads=[kk, a], writes=[bb])
                    cum = P.alloc()
                    ones = C(C_ALLONE, 64)
                    for c in range(8):
                        cs = slice(c * 64, (c + 1) * 64)
                        P.scan(cum[A, cs], ones, lw[A, cs], 0.0, ALU.mult, ALU.add, reads=[cst, lw], writes=[cum])
                    cumx = lw
                    P.tt(cumx[A, :], cum[A, :], lw[A, :], ALU.subtract, reads=[cum, lw], writes=[cumx])
                    Ep = P.alloc()
                    Em = P.alloc()
                    Ee = P.alloc()
                    P.act(Ep[A, :], cum[A, :], AF.Exp, reads=[cum], writes=[Ep])
                    P.act(Em[A, :], cum[A, :], AF.Exp, scale=-1.0, reads=[cum], writes=[Em])
                    P.act(cumx[A, :], cumx[A, :], AF.Exp, reads=[cumx], writes=[cumx])
                    for c in range(8):
                        cs = slice(c * 64, (c + 1) * 64)
                        P.act(Ee[A, cs], cum[A, cs], AF.Exp, bias=cum[A, c * 64 + 63:c * 64 + 64], scale=-1.0,
                              reads=[cum], writes=[Ee])
                    AR = P.alloc(1024)
                    AR3 = AR.v(A, "p (a t) -> p a t", a=2)
                    P.stt(AR3[:, 0, :], kk[A, :], -1.0, cumx[A, :], ALU.mult, ALU.mult, reads=[kk, cumx], writes=[AR])
                    P.tt(AR3[:, 1, :], sr[A, :], Ep[A, :], ALU.mult, reads=[sr, Ep], writes=[AR])
                    Bt, Kt, Bte, Kte = cum, tmp, kk, kmod
                    P.tt(Bt[A, :], bb[A, :], Em[A, :], ALU.mult, reads=[bb, Em], writes=[Bt])
                    P.tt(Kt[A, :], kmod[A, :], Em[A, :], ALU.mult, reads=[kmod, Em], writes=[Kt])
                    P.tt(Bte[A, :], bb[A, :], Ee[A, :], ALU.mult, reads=[bb, Ee], writes=[Bte])
                    P.tt(Kte[A, :], kmod[A, :], Ee[A, :], ALU.mult, reads=[kmod, Ee], writes=[Kte])
                    yr = Em
                    AR1 = Buf(z[0].t, z[0].off, 1024, z[0].k + z[1].k)
                    AR13 = AR1.v(A, "p (a t) -> p a t", a=2)
                    Bt1, Kt1 = sr, P.alloc()
                    idn1 = cst[H1, C_IDENT + 64:C_IDENT + 128]
                    for src, dst, wr in ((AR3[H1, 0, :], AR13[H0, 0, :], AR1), (AR3[H1, 1, :], AR13[H0, 1, :], AR1),
                                         (Bt[H1, :], Bt1[H0, :], Bt1), (Kt[H1, :], Kt1[H0, :], Kt1)):
                        bk = P.bank()
                        P.mm(bk, bk[H0, :], idn1, src, reads=[cst, AR, Bt, Kt])
                        copy(dst, bk[H0, :], [bk], [wr])
                    bk = P.bank()
                    P.mm(bk, bk[H0, 0:8], idn1, Ep.v(H1, "p (c t) -> p c t", c=8)[:, :, 63], reads=[cst, Ep])
                    copy(pc1[H0, 0:8], bk[H0, 0:8], [bk], [pc1])
                    ARh = (AR3, AR13)
                    Bth = (Bt, Bt1)
                    Kth = (Kt, Kt1)
                    ARb = (AR, AR1)
                    if RW_STOP == 2:
                        continue
                    NA = [P.alloc() for _ in range(2)]
                    nmslot = P.alloc()
                    NMp = [nmslot.sub(0, 256), nmslot.sub(256, 256)]
                    gslot = P.alloc()
                    Gp = [gslot.sub(0, 128), gslot.sub(128, 128)]
                    RU = [gslot.sub(256, 256), Ee.sub(0, 256)]
                    TM = [P.alloc() for _ in range(2)]
                    for c in range(8):
                        if RW_STOP == 6:
                            continue
                        cs = slice(c * 64, (c + 1) * 64)
                        na = NA[c % 2]
                        na5 = na.v(H0, "p (h b a t) -> p h b a t", h=2, b=2, a=2)
                        bk = P.bank()
                        bk5 = bk.v(H0, "p (h b a t) -> p h b a t", h=2, b=2, a=2)
                        bkm = P.bank()
                        bkm3 = bkm.v(H0, "p (x h t) -> p x h t", x=4, h=2)
                        for h2 in range(1 if RW_STOP == 7 else 2):
                            ph = slice(h2 * 64, h2 * 64 + 64)
                            for a_ in range(2):
                                P.mm(bk, bk5[:, h2, 0, a_, :], Bth[h2][H0, cs], ARh[h2][H0, a_, cs], reads=[Bth[h2], ARb[h2]])
                                P.mm(bk, bk5[:, h2, 1, a_, :], Kth[h2][H0, cs], ARh[h2][H0, a_, cs], reads=[Kth[h2], ARb[h2]])
                            P.mm(bkm, bkm3[:, 0, h2, :], ARh[h2][H0, 0, cs], Bth[h2][H0, cs], reads=[Bth[h2], ARb[h2]])
                        P.tt(na[H0, :], bk[H0, :], C(C_MA, 512, H0), ALU.mult, reads=[bk, cst], writes=[na])
                        if RW_STOP in (3, 7):
                            continue
                        nm = NMp[0]
                        nm4 = nm.v(H0, "p (x h t) -> p x h t", x=2, h=2)
                        P.cp(nm4[:, 0, :, :], na5[:, :, 0, 0, :], reads=[na], writes=[nm], eng="pool")
                        P.tt(nm4[:, 1, :, :], bkm3[:, 0, :, :], C(C_MB, 128, H0).rearrange("p (h t) -> p h t", h=2),
                             ALU.mult, reads=[bkm, cst], writes=[nm])
                        G3_0 = Gp[0].v(H0, "p (h t) -> p h t", h=2)
                        P.tt(G3_0, na5[:, :, 0, 0, :], C(C_IDG, 128, H0).rearrange("p (h t) -> p h t", h=2), ALU.add,
                             reads=[na, cst], writes=[Gp[0]])
                        cur = 0
                        for lev in range(5):
                            nmc = NMp[cur]
                            nmc4 = nmc.v(H0, "p (x h t) -> p x h t", x=2, h=2)
                            nmn = NMp[1 - cur]
                            nmn4 = nmn.v(H0, "p (x h t) -> p x h t", x=2, h=2)
                            Gc = Gp[cur]
                            Gc3 = Gc.v(H0, "p (h t) -> p h t", h=2)
                            Gn = Gp[1 - cur]
                            bkp = P.bank()
                            bkp4 = bkp.v(H0, "p (x h t) -> p x h t", x=4, h=2)
                            for h2 in range(2):
                                P.mm(bkp, bkp4[:, 0, h2, :], nmc4[:, 1, h2, :], nmc4[:, 0, h2, :], reads=[nmc])
                                P.mm(bkp, bkp4[:, 1, h2, :], nmc4[:, 0, h2, :], nmc4[:, 1, h2, :], reads=[nmc])
                            copy(nmn[H0, :], bkp[H0, 0:256], [bkp], [nmn])
                            bkg = P.bank()
                            for h2 in range(2):
                                P.mm(bkg, bkg[H0, h2 * 64:(h2 + 1) * 64], nmn4[:, 1, h2, :], Gc3[:, h2, :], reads=[nmn, Gc])
                            P.tt(Gn[H0, :], bkg[H0, 0:128], Gc[H0, :], ALU.add, reads=[bkg, Gc], writes=[Gn])
                            cur = 1 - cur
                        Gf = Gp[cur]
                        Gf3 = Gf.v(H0, "p (h t) -> p h t", h=2)
                        if RW_STOP == 4:
                            continue
                        tm = TM[c % 2]
                        bkt = P.bank()
                        P.tr(bkt, bkt[H0, 0:128], Bte[A, cs], ident, reads=[Bte, cst])
                        P.tr(bkt, bkt[H0, 128:256], Kte[A, cs], ident, reads=[Kte, cst])
                        P.tr(bkt, bkt[H0, 256:384], sv[A, cs], ident, reads=[sv, cst])
                        copy(tm[H0, 0:384], bkt[H0, 0:384], [bkt], [tm])
                        if RW_STOP == 5:
                            continue
                        ru = RU[c % 2]
                        sb_old = c % 2
                        sb_new = 1 - sb_old
                        bkr = P.bank()
                        for h2 in range(2):
                            ph = slice(h2 * 64, h2 * 64 + 64)
                            o = bkr[H0, h2 * 64:(h2 + 1) * 64]
                            P.mm(bkr, o, ARh[h2][H0, 0, cs], S5[:, hp, h2, sb_old, :], reads=[ARb[h2], Srw])
                            P.mm(bkr, o, na5[:, h2, 1, 0, :], tm[H0, 256 + h2 * 64:256 + (h2 + 1) * 64], reads=[na, tm])
                        copy(ru[H0, 0:128], bkr[H0, 0:128], [bkr], [ru], eng="act")
                        bku = P.bank()
                        for h2 in range(2):
                            P.mm(bku, bku[H0, h2 * 64:(h2 + 1) * 64], Gf3[:, h2, :], ru[H0, h2 * 64:(h2 + 1) * 64], reads=[Gf, ru])
                        copy(ru[H0, 128:256], bku[H0, 0:128], [bku], [ru], eng="dve")
                        bky = P.bank()
                        for h2 in range(2):
                            ph = slice(h2 * 64, h2 * 64 + 64)
                            o = bky[ph, 0:64]
                            P.mm(bky, o, S5[:, hp, h2, sb_old, :], ARh[h2][H0, 1, cs], reads=[ARb[h2], Srw])
                            P.mm(bky, o, ru[H0, 128 + h2 * 64:128 + (h2 + 1) * 64], na5[:, h2, 0, 1, :], reads=[ru, na])
                            P.mm(bky, o, tm[H0, 256 + h2 * 64:256 + (h2 + 1) * 64], na5[:, h2, 1, 1, :], reads=[tm, na])
                        copy(yr[A, cs], bky[A, 0:64], [bky], [yr], eng="act")
                        bks = P.bank()
                        for h2 in range(2):
                            o = bks[H0, h2 * 64:(h2 + 1) * 64]
                            P.mm(bks, o, tm[H0, h2 * 64:(h2 + 1) * 64], ru[H0, 128 + h2 * 64:128 + (h2 + 1) * 64], reads=[tm, ru])
                            P.mm(bks, o, tm[H0, 128 + h2 * 64:128 + (h2 + 1) * 64], tm[H0, 256 + h2 * 64:256 + (h2 + 1) * 64], reads=[tm])
                        for h2 in range(2):
                            pcs = Ep[H0, c * 64 + 63:c * 64 + 64] if h2 == 0 else pc1[H0, c:c + 1]
                            P.stt(S5[:, hp, h2, sb_new, :], S5[:, hp, h2, sb_old, :], pcs, bks[H0, h2 * 64:(h2 + 1) * 64],
                                  ALU.mult, ALU.add, reads=[Srw, Ep, pc1, bks], writes=[Srw])
                    t1 = Ep
                    bk = P.bank()
                    P.mm(bk, bk[A, :], C(C_BLKM, 128), yr[A, :], reads=[cst, yr])
                    P.tt(yr[A, :], yr[A, :], bk[A, :], ALU.subtract, reads=[yr, bk], writes=[yr])
                    P.tt(t1[A, :], yr[A, :], yr[A, :], ALU.mult, reads=[yr], writes=[t1])
                    bk = P.bank()
                    P.mm(bk, bk[A, :], C(C_BLKM, 128), t1[A, :], reads=[cst, t1])
                    P.ts(t1[A, :], bk[A, :], 64e-5, ALU.add, reads=[bk], writes=[t1])
                    P.recip(t1[A, :], t1[A, :], reads=[t1], writes=[t1])
                    P.act(t1[A, :], t1[A, :], AF.Sqrt, reads=[t1], writes=[t1])
                    P.tt(yr[A, :], yr[A, :], t1[A, :], ALU.mult, reads=[yr, t1], writes=[yr])
                    P.ts(yr[A, :], yr[A, :], pcc(PC_LNW), ALU.mult, pcc(PC_LNB), ALU.add, reads=[yr, pc], writes=[yr])
                    P.tt(yr[A, :], yr[A, :], bonus[A, :], ALU.add, reads=[yr, bonus], writes=[yr])
                    P.tt(RR(yT3[:, hp, :]), yr[A, :], g[A, :], ALU.mult, reads=[yr, g], writes=[yT.sub(hp * 512, 512)])

            P.ar_top = MIX_BASE
            if "r" in phases:
                rwkv()

            def mlstm():
                qk = [P.alloc() for _ in range(4)]
                vones = P.alloc()
                vo3 = vones.v(H0, "p (h c) -> p h c", h=4)
                P.memset(vones[H0, :], 1.0, writes=[vones])
                vT = [P.alloc(), P.alloc()]
                oT = [P.alloc(), P.alloc()]
                gi, gf, bcum, ge = (P.alloc() for _ in range(4))
                top = P.ar_top
                for i in range(4):
                    P.ar_top = top
                    uext = P.alloc(515)
                    acc = P.alloc()
                    proj_fm(1024 + i * 64, 64, uext[H0, 3:515], rows=uext)
                    P.cp(uext[H0, 0:3], cml[H0, i * 3:(i + 1) * 3], reads=[cml], writes=[uext], eng="pool")
                    wc = lambda j: pc[H0, PC_CW + i * 4 + j:PC_CW + i * 4 + j + 1]
                    P.ts(acc[H0, :], uext[H0, 0:512], wc(0), ALU.mult, reads=[uext, pc], writes=[acc])
                    for j in range(1, 4):
                        P.stt(acc[H0, :], uext[H0, j:j + 512], wc(j), acc[H0, :], ALU.mult, ALU.add, reads=[uext, pc, acc], writes=[acc])
                    P.cp(cml[H0, i * 3:(i + 1) * 3], uext[H0, 512:515], reads=[uext], writes=[cml], eng="pool")
                    P.act(qk[i][H0, :], acc[H0, :], AF.Silu, bias=pc[H0, PC_CB + i:PC_CB + i + 1], reads=[acc, pc], writes=[qk[i]])
                    if i < 2:
                        P.ts(qk[i][H0, :], qk[i][H0, :], 32.0 ** -0.5, ALU.mult, reads=[qk[i]], writes=[qk[i]])
                P.ar_top = top
                for hp in range(2):
                    proj_fm(1280 + hp * 128, 128, vT[hp][A, :], rows=vT[hp])
                    proj_fm(1536 + hp * 128, 128, oT[hp][A, :], rows=oT[hp])
                    P.act(oT[hp][A, :], oT[hp][A, :], AF.Sigmoid, reads=[oT[hp]], writes=[oT[hp]])
                proj_fm(1792, 4, gi[R4, :], rows=gi)
                proj_fm(1796, 4, gf[R4, :], rows=gf)
                P.act(gi[R4, :], gi[R4, :], AF.Tanh, bias=dv[R4, DV_BI15:DV_BI15 + 1], scale=1.0 / 15.0, reads=[gi, dv], writes=[gi])
                P.act(gf[R4, :], gf[R4, :], AF.Tanh, bias=dv[R4, DV_BF15:DV_BF15 + 1], scale=1.0 / 15.0, reads=[gf, dv], writes=[gf])
                P.ts(gi[R4, :], gi[R4, :], 15.0, ALU.mult, reads=[gi], writes=[gi])
                P.act(gf[R4, :], gf[R4, :], AF.Exp, scale=-15.0, reads=[gf], writes=[gf])
                P.act(gf[R4, :], gf[R4, :], AF.Ln, bias=1.0, reads=[gf], writes=[gf])
                ones4 = C(C_ALLONE, 64, R4)
                for c in range(8):
                    cs = slice(c * 64, (c + 1) * 64)
                    P.scan(bcum[R4, cs], ones4, gf[R4, cs], 0.0, ALU.mult, ALU.subtract, reads=[cst, gf], writes=[bcum])
                P.tt(gi[R4, :], gi[R4, :], bcum[R4, :], ALU.subtract, reads=[gi, bcum], writes=[gi])
                P.act(gi[R4, :], gi[R4, :], AF.Exp, reads=[gi], writes=[gi])
                P.act(bcum[R4, :], bcum[R4, :], AF.Exp, reads=[bcum], writes=[bcum])
                for c in range(8):
                    cs = slice(c * 64, (c + 1) * 64)
                    P.ts(ge[R4, cs], gi[R4, cs], bcum[R4, c * 64 + 63:c * 64 + 64], ALU.mult, reads=[gi, bcum], writes=[ge])
                kp = [P.alloc(), P.alloc()]
                kpe = [P.alloc(), P.alloc()]
                ebbc = [P.alloc(), P.alloc()]
                for j in range(2):
                    selk = C(C_SELK + j * 64, 64, R4)
                    bk = P.bank()
                    P.mm(bk, bk[H0, :], selk, gi[R4, :], reads=[cst, gi])
                    P.tt(kp[j][H0, :], qk[2 + j][H0, :], bk[H0, :], ALU.mult, reads=[qk[2 + j], bk], writes=[kp[j]])
                    bk = P.bank()
                    P.mm(bk, bk[H0, :], selk, ge[R4, :], reads=[cst, ge])
                    P.tt(kpe[j][H0, :], qk[2 + j][H0, :], bk[H0, :], ALU.mult, reads=[qk[2 + j], bk], writes=[kpe[j]])
                    bk = P.bank()
                    P.mm(bk, bk[H0, :], selk, bcum[R4, :], reads=[cst, bcum])
                    P.cp(ebks[H0, j * 8:(j + 1) * 8], bk.v(H0, "p (c t) -> p c t", c=8)[:, :, 63], reads=[bk], writes=[ebks])
                    bk = P.bank()
                    P.mm(bk, bk[A, :], C(C_SELV + j * 128, 128, R4), bcum[R4, :], reads=[cst, bcum])
                    copy(ebbc[j][A, :], bk[A, :], [bk], [ebbc[j]])
                NTs = P.alloc(1024)
                DNs = P.alloc(1024)
                NT3 = NTs.v(A, "p (j t) -> p j t", j=2)
                DN3 = DNs.v(A, "p (j t) -> p j t", j=2)
                sm = P.alloc()
                qmc = P.alloc(256)
                for c in range(8):
                    cs = slice(c * 64, (c + 1) * 64)
                    bkt = P.bank()
                    P.tr(bkt, bkt[H0, 0:128], vT[0][A, cs], ident, reads=[vT[0], cst])
                    P.tr(bkt, bkt[H0, 128:256], vT[1][A, cs], ident, reads=[vT[1], cst])
                    copy(vo3[:, :, 0:64], bkt.v(H0, "p (h c) -> p h c", h=8)[:, 0:4, :], [bkt], [vones])
                    bkt2 = P.bank()
                    P.tr(bkt2, bkt2[H0, 0:64], kpe[0][H0, cs], C(C_IDENT, 64, H0), reads=[kpe[0], cst])
                    P.tr(bkt2, bkt2[H0, 64:128], kpe[1][H0, cs], C(C_IDENT, 64, H0), reads=[kpe[1], cst])
                    copy(sm[H0, 256:384], bkt2[H0, 0:128], [bkt2], [sm])
                    for h in range(4):
                        P.ts(qmc[H0, h * 64:(h + 1) * 64], qk[h // 2][H0, cs], C(C_HM + h % 2, 1, H0), ALU.mult,
                             reads=[qk[h // 2], cst], writes=[qmc])
                    bka = P.bank()
                    for h in range(4):
                        j = h // 2
                        P.mm(bka, bka[H0, h * 64:(h + 1) * 64], kp[j][H0, cs], qmc[H0, h * 64:(h + 1) * 64], reads=[kp[j], qmc])
                    P.tt(sm[H0, 0:256], bka[H0, 0:256], C(C_MM, 256, H0), ALU.mult, reads=[bka, cst], writes=[sm])
                    bn = P.bank()
                    bd = P.bank()
                    for h in range(4):
                        j = h // 2
                        pq = slice((h % 2) * 32, (h % 2) * 32 + 32)
                        ph = slice((h % 2) * 64, (h % 2) * 64 + 64)
                        at = sm[H0, h * 64:(h + 1) * 64]
                        qm = qmc[H0, h * 64:(h + 1) * 64]
                        P.mm(bn, bn[ph, j * 64:(j + 1) * 64], vo3[:, h, 0:64], at, reads=[vones, sm])
                        P.mm(bn, bn[ph, j * 64:(j + 1) * 64], CN3[:, j, 0:64], qm, reads=[CN, qmc])
                        P.mm(bd, bd[ph, j * 64:(j + 1) * 64], vo3[:, h, 64:128], at, reads=[vones, sm])
                        P.mm(bd, bd[ph, j * 64:(j + 1) * 64], CN3[:, j, 64:128], qm, reads=[CN, qmc])
                    copy(NT3[:, :, cs], bn.v(A, "p (j t) -> p j t", j=8)[:, 0:2, :], [bn], [NTs], eng="act")
                    copy(DN3[:, :, cs], bd.v(A, "p (j t) -> p j t", j=8)[:, 0:2, :], [bd], [DNs], eng="act")
                    bs_ = P.bank()
                    for h in range(4):
                        j = h // 2
                        P.mm(bs_, bs_[H0, h * 128:(h + 1) * 128], sm[H0, 256 + j * 64:256 + (j + 1) * 64], vo3[:, h, :], reads=[sm, vones])
                    for j in range(2):
                        P.ts(CN3[:, j, :], CN3[:, j, :], ebks[H0, j * 8 + c:j * 8 + c + 1], ALU.mult, reads=[CN, ebks], writes=[CN])
                        for h2 in range(2):
                            h = 2 * j + h2
                            P.stt(CN3[:, j, :], bs_[H0, h * 128:(h + 1) * 128], C(C_HM + h2, 1, H0), CN3[:, j, :],
                                  ALU.mult, ALU.add, reads=[CN, bs_, cst], writes=[CN])
                for j in range(2):
                    d1 = DNs.sub(j * 512, 512)
                    n1 = NTs.sub(j * 512, 512)
                    P.tt(d1[A, :], d1[A, :], ebbc[j][A, :], ALU.mult, reads=[d1, ebbc[j]], writes=[d1])
                    P.stt(d1[A, :], d1[A, :], -1.0, d1[A, :], ALU.mult, ALU.max, reads=[d1], writes=[d1])
                    P.ts(d1[A, :], d1[A, :], 1.0, ALU.max, reads=[d1], writes=[d1])
                    P.recip(d1[A, :], d1[A, :], reads=[d1], writes=[d1])
                    P.tt(n1[A, :], n1[A, :], ebbc[j][A, :], ALU.mult, reads=[n1, ebbc[j]], writes=[n1])
                    P.tt(n1[A, :], n1[A, :], d1[A, :], ALU.mult, reads=[n1, d1], writes=[n1])
                    P.tt(d1[A, :], n1[A, :], n1[A, :], ALU.mult, reads=[n1], writes=[d1])
                    bk = P.bank()
                    P.mm(bk, bk[A, :], C(C_BLKM, 128), d1[A, :], reads=[cst, d1])
                    P.ts(d1[A, :], bk[A, :], 1e-6, ALU.add, reads=[bk], writes=[d1])
                    P.recip(d1[A, :], d1[A, :], reads=[d1], writes=[d1])
                    P.act(d1[A, :], d1[A, :], AF.Sqrt, reads=[d1], writes=[d1])
                    P.tt(n1[A, :], n1[A, :], d1[A, :], ALU.mult, reads=[n1, d1], writes=[n1])
                    P.stt(RR(yT3[:, 2 + j, :]), n1[A, :], pc[A, PC_MNORM + j:PC_MNORM + j + 1], oT[j][A, :], ALU.mult, ALU.mult,
                          reads=[n1, pc, oT[j]], writes=[yT.sub((2 + j) * 512, 512)])

            P.ar_top = MIX_BASE
            if "m" in phases:
                mlstm()

            def swa():
                swaB = P.alloc(1024)
                mtmp = P.alloc(1024)
                P.dma(swaB[A, :], swab_d, writes=[swaB])
                P.dma(mtmp[A, :], cst_d[:, C_SWAM:C_SWAM + 1024], writes=[mtmp])
                P.tt(swaB[A, :], swaB[A, :], mtmp[A, :], ALU.add, reads=[swaB, mtmp], writes=[swaB])
                P.ar_top -= 2
                swaB4 = swaB.v(A, "p (c h q) -> p c h q", c=2, h=4)
                swaK = P.alloc(1280)
                swaV = P.alloc(640)
                swaK3 = swaK.v(A, "p (j t) -> p j t", j=2)
                swaV3 = swaV.v(A, "p (n c) -> p n c", n=5)
                P.cp(swaK3[:, :, 0:128], swaKc3[:, :, :], reads=[swaKc], writes=[swaK], eng="pool")
                P.cp(swaV3[:, 0, :], swaVc[A, :], reads=[swaVc], writes=[swaV], eng="pool")
                qS = [P.alloc(), P.alloc()]
                for j in range(2):
                    proj_fm(1800 + j * 128, 128, qS[j][A, :], scale=0.125, rows=qS[j])
                    proj_fm(2056 + j * 64, 64, swaK3[:, j, 128:640], dup=True, rows=swaK)
                proj_tm(2184, 128, lambda tt_: swaV3[:, 1 + tt_, :], swaV)
                ETs = [P.alloc(1024), P.alloc(1024)]
                tmp = P.alloc()
                for i in range(4):
                    gi_ = blk * 4 + i
                    qc = slice(i * 128, (i + 1) * 128)
                    prev = slice(i * 128, (i + 1) * 128)
                    cur_ = slice((i + 1) * 128, (i + 2) * 128)
                    ET = ETs[i % 2]
                    bp = P.bank() if gi_ > 0 else None
                    bc = P.bank()
                    for hq in range(4):
                        j = hq // 2
                        ph = slice((hq % 2) * 64, (hq % 2) * 64 + 64)
                        o = slice(hq * 128, (hq + 1) * 128)
                        if gi_ > 0:
                            P.mm(bp, bp[A, o], swaK3[ph, j, prev], qS[j][ph, qc], reads=[swaK, qS[j]])
                            P.mm(bp, bp[A, o], ident, swaB4[:, 0, hq, :], reads=[cst, swaB])
                        P.mm(bc, bc[A, o], swaK3[ph, j, cur_], qS[j][ph, qc], reads=[swaK, qS[j]])
                        P.mm(bc, bc[A, o], ident, swaB4[:, 1, hq, :], reads=[cst, swaB])
                    if gi_ > 0:
                        P.act(ET[A, 0:512], bp[A, :], AF.Exp, reads=[bp], writes=[ET])
                    P.act(ET[A, 512:1024], bc[A, :], AF.Exp, reads=[bc], writes=[ET])
                    bo = P.bank()
                    bd = P.bank()
                    for hq in range(4):
                        j = hq // 2
                        ph = slice((hq % 2) * 64, (hq % 2) * 64 + 64)
                        o = slice(j * 128, (j + 1) * 128)
                        e = slice(hq * 128, (hq + 1) * 128)
                        if gi_ > 0:
                            P.mm(bo, bo[ph, o], swaV3[:, i, j * 64:(j + 1) * 64], ET[A, e.start:e.stop], reads=[swaV, ET])
                            P.mm(bd, bd[ph, o], C(C_ALLONE, 64), ET[A, e.start:e.stop], reads=[cst, ET])
                        P.mm(bo, bo[ph, o], swaV3[:, i + 1, j * 64:(j + 1) * 64], ET[A, 512 + e.start:512 + e.stop], reads=[swaV, ET])
                        P.mm(bd, bd[ph, o], C(C_ALLONE, 64), ET[A, 512 + e.start:512 + e.stop], reads=[cst, ET])
                    for j in range(2):
                        P.ts(tmp[A, j * 128:(j + 1) * 128], bd[A, j * 128:(j + 1) * 128], dv[A, DV_ESINK + j:DV_ESINK + j + 1], ALU.add,
                             reads=[bd, dv], writes=[tmp])
                    P.recip(tmp[A, 0:256], tmp[A, 0:256], reads=[tmp], writes=[tmp])
                    P.tt(RR(yT3[:, 4:6, qc]), bo.v(A, "p (j q) -> p j q", j=4)[:, 0:2, :], tmp.v(A, "p (j q) -> p j q", j=4)[:, 0:2, :],
                         ALU.mult, reads=[bo, tmp], writes=[yT.sub(4 * 512, 1024)])
                P.cp(swaKc3[:, :, :], swaK3[:, :, 512:640], reads=[swaK], writes=[swaKc], eng="pool")
                P.cp(swaVc[A, :], swaV3[:, 4, :], reads=[swaV], writes=[swaVc], eng="pool")

            P.ar_top = MIX_BASE
            if "s" in phases:
                swa()

            def fox():
                qtmp = [P.alloc(), P.alloc()]
                for j in range(2):
                    proj_fm(2312 + j * 128, 128, qtmp[j][A, :], scale=0.125, rows=qtmp[j])
                    proj_fm(2568 + j * 128, 128, RR(kTh3[:, j, bs]), rows=kTh)
                for half in range(2):
                    proj_tm(2824 + half * 128, 128, lambda tt_: RR(Vh3[:, blk * 4 + tt_, half * 128:(half + 1) * 128]), Vh)
                fr = P.alloc()
                crow = P.alloc()
                vtmp = P.alloc()
                chi = foxR.sub(2 * 512, 512)
                clo = foxR.sub(3 * 512, 512)
                qF = [foxR.sub(0, 512), foxR.sub(512, 512)]
                for R_ in (slice(0, 4), slice(64, 68)):
                    proj_fm(3080, 4, fr[R_, :], rows=fr, pbase=R_.start)
                for R_ in (slice(0, 4), slice(64, 68)):
                    P.act(fr[R_, :], fr[R_, :], AF.Exp, bias=dv[R_, DV_NFOXB:DV_NFOXB + 1], scale=-1.0, reads=[fr, dv], writes=[fr])
                    P.act(fr[R_, :], fr[R_, :], AF.Ln, bias=1.0, reads=[fr], writes=[fr])
                    for sg_ in range(4):
                        seg = slice(sg_ * 128, (sg_ + 1) * 128)
                        init = ccar[R_, 0:1] if sg_ == 0 else crow[R_, sg_ * 128 - 1:sg_ * 128]
                        P.scan(crow[R_, seg], C(C_ALLONE, 128, R_), fr[R_, seg], init, ALU.mult, ALU.subtract,
                               reads=[fr, ccar, cst, crow], writes=[crow])
                    P.cp(ccar[R_, 0:1], crow[R_, 511:512], reads=[crow], writes=[ccar], eng="pool")
                    P.ts(fr[R_, :], crow[R_, :], 4097.0, ALU.mult, reads=[crow], writes=[fr])
                    P.tt(vtmp[R_, :], fr[R_, :], crow[R_, :], ALU.subtract, reads=[fr, crow], writes=[vtmp])
                    P.tt(RR(chi[R_, :]), fr[R_, :], vtmp[R_, :], ALU.subtract, reads=[fr, vtmp], writes=[chi])
                    P.tt(RR(clo[R_, :]), crow[R_, :], chi[R_, :], ALU.subtract, reads=[crow, chi], writes=[clo])
                bk = P.bank()
                for tt_ in range(4):
                    P.mm(bk, bk[A, tt_ * 4:(tt_ + 1) * 4], crow[R4, tt_ * 128:(tt_ + 1) * 128], C(C_IDENT, 4, R4), reads=[crow, cst])
                P.ts(negc3[:, blk * 4:(blk + 1) * 4, :], bk.v(A, "p (n h) -> p n h", h=4)[:, 0:4, :], -1.0, ALU.mult,
                     reads=[bk], writes=[negc])
                for j in range(2):
                    P.cp(RR(qF[j][A, :]), qtmp[j][A, :], reads=[qtmp[j]], writes=[qF[j]], eng=("act", "pool")[j])
                ETs = [foxR.sub((4 + i_) * 512, 512) for i_ in range(3)]
                rec = P.alloc()
                ei = 0
                for j in range(2):
                    bo = P.bank(pin=True)
                    bd = P.bank(pin=True)
                    for h2 in range(2):
                        h = 2 * j + h2
                        ph = slice(h2 * 64, h2 * 64 + 64)
                        for kb in range(blk * 4 + 4):
                            q0 = max(0, kb - blk * 4) * 128
                            nq = 512 - q0
                            bs_ = P.bank()
                            P.mm(bs_, bs_[A, 0:nq], kTh3[ph, j, kb * 128:(kb + 1) * 128], qF[j][ph, q0:512], reads=[kTh, qF[j]], r=True)
                            Rh = slice(h2 * 64, h2 * 64 + 4)
                            P.mm(bs_, bs_[A, 0:nq], C(C_SELF + h * 128, 128, Rh), chi[Rh, q0:512], reads=[cst, chi], r=True)
                            P.mm(bs_, bs_[A, 0:nq], C(C_SELF + h * 128, 128, Rh), clo[Rh, q0:512], reads=[cst, clo], r=True)
                            et = ETs[ei % 3]
                            ei += 1
                            P.act(RR(et[A, 0:nq]), bs_[A, 0:nq], AF.Exp, bias=negc3[:, kb, h:h + 1], reads=[bs_, negc], writes=[et])
                            if kb >= blk * 4:
                                P.tt(RR(et[A, 0:128]), et[A, 0:128], C(C_TRI, 128), ALU.mult, reads=[et, cst], writes=[et], eng="pool")
                            P.mm(bo, bo[ph, q0:512], Vh3[:, kb, h * 64:(h + 1) * 64], et[A, 0:nq], reads=[Vh, et], r=(h2 == 0))
                            P.mm(bd, bd[ph, q0:512], C(C_ALLONE, 64), et[A, 0:nq], reads=[cst, et], r=(h2 == 0))
                    P.recip(rec[A, :], bd[A, :], reads=[bd], writes=[rec])
                    P.tt(RR(yT3[:, 6 + j, :]), bo[A, :], rec[A, :], ALU.mult, reads=[bo, rec], writes=[yT.sub((6 + j) * 512, 512)])
                    bo.pinned = False
                    bd.pinned = False

            P.ar_top = MIX_BASE
            if "f" in phases:
                fox()

            if dbg == ("yT", l):
                for c in range(8):
                    P.dma(dbg_d[:, c, bs], yT3[:, c, :], reads=[yT], eng="pool")

            def post_residual(oT3, oT, gcol):
                rs = rms_rstd(lambda c: oT3[:, c, :], [oT])
                tmps = [P.alloc(), P.alloc()]
                for m in range(8):
                    t_ = tmps[m % 2]
                    P.stt(t_[A, :], oT3[:, m, :], pc[A, gcol + m:gcol + m + 1], rs[A, :], ALU.mult, ALU.mult,
                          reads=[oT, pc, rs], writes=[t_])
                    P.tt(xT3[:, m, bs], xT3[:, m, bs], t_[A, :], ALU.add, reads=[xT, t_], writes=[xT])

            if "o" not in phases:
                continue
            P.ar_top = 0
            oT = P.alloc(4096)
            oT3 = oT.v(A, "p (c t) -> p c t", c=8)
            for m in range(8):
                w = wchunk()
                w3 = w.v(A, "p (c m) -> p c m", c=8)
                P.dma(RR(w[A, :]), w_out_d[l, m], writes=[w], eng="pool")
                bk = P.bank()
                for j in range(8):
                    P.mm(bk, bk[A, :], w3[:, j, :], yT3[:, j, :], reads=[w, yT], r=True)
                copy(oT3[:, m, :], bk[A, :], [bk], [oT.sub(m * 512, 512)])
            post_residual(oT3, oT, PC_GPOST)

            if dbg == ("x1", l):
                for c in range(8):
                    P.dma(dbg_d[:, c, bs], xT3[:, c, bs], reads=[xT])

            if "n" not in phases:
                continue
            P.ar_top = 0
            rs3 = rms_rstd(lambda c: xT3[:, c, bs], [xT])
            fill_hTr(PC_FPRE)
            fT = yT
            fT3 = yT3
            oT = P.alloc(4096)
            oT3 = oT.v(A, "p (c t) -> p c t", c=8)
            gbuf = P.alloc(514)
            acc = P.alloc()
            gfp = pc[A, PC_FPRE:PC_FPRE + 8]
            for (c_lo, c_hi) in ((0, 8), (8, 16), (16, 22)):
                ng = c_hi - c_lo
                for cc in range(ng):
                    c = c_lo + cc
                    ws = []
                    for c2 in (c, NFC + c):
                        w = wchunk()
                        w3 = w.v(A, "p (c m) -> p c m", c=8)
                        P.dma(RR(w[A, :]), w_up_d[l, c2], writes=[w], eng="pool")
                        ws.append((w, w3))
                    bg = P.bank()
                    for k in range(8):
                        P.mm(bg, bg[A, :], ws[0][1][:, k, :], hTr3[:, k, :], reads=[ws[0][0], hTr], r=True)
                    bu = P.bank()
                    for k in range(8):
                        P.mm(bu, bu[A, :], ws[1][1][:, k, :], hTr3[:, k, :], reads=[ws[1][0], hTr], r=True)
                    P.tt(gbuf[A, 2:514], bg[A, :], rs3[A, :], ALU.mult, reads=[bg, rs3], writes=[gbuf])
                    P.cp(gbuf[A, 0:2], cffn[A, c * 2:c * 2 + 2], reads=[cffn], writes=[gbuf], eng="pool")
                    fw = lambda j: pc[A, PC_FCW + c * 3 + j:PC_FCW + c * 3 + j + 1]
                    P.ts(acc[A, :], gbuf[A, 0:512], fw(0), ALU.mult, reads=[gbuf, pc], writes=[acc])
                    P.stt(acc[A, :], gbuf[A, 1:513], fw(1), acc[A, :], ALU.mult, ALU.add, reads=[gbuf, pc, acc], writes=[acc])
                    P.stt(acc[A, :], gbuf[A, 2:514], fw(2), acc[A, :], ALU.mult, ALU.add, reads=[gbuf, pc, acc], writes=[acc])
                    P.cp(cffn[A, c * 2:c * 2 + 2], gbuf[A, 512:514], reads=[gbuf], writes=[cffn], eng="pool")
                    P.act(acc[A, :], acc[A, :], AF.Gelu_apprx_tanh, bias=pc[A, PC_FCB + c:PC_FCB + c + 1], reads=[acc, pc], writes=[acc])
                    P.tt(acc[A, :], acc[A, :], rs3[A, :], ALU.mult, reads=[acc, rs3], writes=[acc])
                    P.tt(RR(fT3[:, cc, :]), acc[A, :], bu[A, :], ALU.mult, reads=[acc, bu], writes=[fT.sub(cc * 512, 512)])
                for m in range(8):
                    wa = wchunk()
                    wa3 = wa.v(A, "p (c m) -> p c m", c=8)
                    P.dma(RR(wa[A, 0:ng * 128]), w_dn_d[l, m, :, c_lo * 128:c_hi * 128], writes=[wa], eng="pool")
                    bk = P.bank()
                    for cc in range(ng):
                        P.mm(bk, bk[A, :], wa3[:, cc, :], fT3[:, cc, :], reads=[wa, fT], r=True)
                    om = oT.sub(m * 512, 512)
                    if c_lo == 0:
                        copy(oT3[:, m, :], bk[A, :], [bk], [om])
                    else:
                        P.tt(oT3[:, m, :], oT3[:, m, :], bk[A, :], ALU.add, reads=[om, bk], writes=[om])
            post_residual(oT3, oT, PC_FPOST)

    outops = []
    for c in range(8):
        outops.append(P.dma(out_d[:, c, 0:T], xT3[:, c, :], reads=[xT]))
    P.emit(outops + [op for op in P.dma_ops if False])
    P.close()
    return nc


_NC_CACHE = {}


def kernel(**inputs):
    maps = prep_inputs(inputs)
    if "nc" not in _NC_CACHE:
        _NC_CACHE["nc"] = build()
    nc = _NC_CACHE["nc"]
    res = run_bass_kernel_spmd(nc, maps, core_ids=list(range(len(maps))))
    x = np.asarray(inputs["x"])
    out = np.empty(x.shape, np.float32)
    for b in range(x.shape[0]):
        oT = np.asarray(res.results[b]["outT"])
        out[b] = oT.transpose(1, 0, 2).reshape(D, SEQ).T
    return out
```

```python
import contextlib
import math
import numpy as np
import concourse.bass as bass
import concourse.mybir as mybir
from concourse.bass_utils import run_bass_kernel_spmd

F32 = mybir.dt.float32
F32R = mybir.dt.float32r
BF16 = mybir.dt.bfloat16
ALU = mybir.AluOpType
AF = mybir.ActivationFunctionType

ENGS = ("pe", "act", "dve", "pool", "sp")


def _flat(ks, out):
    for k in ks:
        if isinstance(k, (str, int)):
            out.append(k)
        elif isinstance(k, tuple) and (len(k) == 0 or isinstance(k[0], (str, int))):
            out.append(k)
        elif hasattr(k, "k"):
            _flat(k.k, out)
        else:
            _flat(k, out)
    return out


class Op:
    __slots__ = ("eng", "fn", "deps", "signals", "count", "pos", "dma", "dsem", "dval", "prewait")

    def __init__(self, eng, fn, dma=False):
        self.eng = eng
        self.fn = fn
        self.deps = []
        self.signals = False
        self.count = None
        self.pos = None
        self.dma = dma
        self.dsem = None
        self.dval = None
        self.prewait = None


class Buf:
    def __init__(self, t, off, ncols, keys):
        self.t, self.off, self.n, self.k = t, off, ncols, keys

    def __getitem__(self, idx):
        p, c = idx
        if isinstance(c, int):
            c = slice(c, c + 1)
        a = 0 if c.start is None else c.start
        b = self.n if c.stop is None else c.stop
        assert 0 <= a <= b <= self.n, (a, b, self.n)
        return self.t[p, self.off + a:self.off + b]

    def v(self, p, pat, **kw):
        return self.t[p, self.off:self.off + self.n].rearrange(pat, **kw)

    def sub(self, c0, n):
        ks = self.k
        if len(ks) > 1 and len(ks) * 512 >= self.n:
            ks = ks[c0 // 512:(c0 + n + 511) // 512]
        return Buf(self.t, self.off + c0, n, ks)


class Bank(Buf):
    def __init__(self, t, name):
        super().__init__(t, 0, 512, [name])
        self.opened = set()
        self.pinned = False


class Prog:
    NDMA = 32

    def __init__(self, nc):
        self.nc = nc
        self.ops = {e: [] for e in ENGS}
        self.lastw = {}
        self.readers = {}
        self.waited = {e: {} for e in ENGS}
        self.waited_dma = {e: set() for e in ENGS}
        self.dma_ops = []
        self.stack = contextlib.ExitStack()
        self.banks = []
        self.bank_i = 0
        self.ar = None
        self.ar_top = 0

    def sb(self, name, ncols, parts=128, dtype=F32):
        t = self.stack.enter_context(self.nc.sbuf_tensor("sb_" + name, [parts, ncols], dtype))
        return Buf(t, 0, ncols, [name])

    def sbs(self, name, nslots, dtype=F32):
        t = self.stack.enter_context(self.nc.sbuf_tensor("sb_" + name, [128, nslots * 512], dtype))
        return Buf(t, 0, nslots * 512, [(name, i) for i in range(nslots)])

    def make_banks(self):
        for i in range(8):
            t = self.stack.enter_context(self.nc.psum_tensor(f"bank{i}", [128, 512], F32))
            self.banks.append(Bank(t, f"bank{i}"))

    def bank(self, pin=False):
        for _ in range(16):
            b = self.banks[self.bank_i % 8]
            self.bank_i += 1
            if not b.pinned:
                b.opened = set()
                b.pinned = pin
                return b
        raise RuntimeError("no free psum bank")

    def make_arena(self, nslots):
        self.ar = self.stack.enter_context(self.nc.sbuf_tensor("arena", [128, nslots * 512], F32))
        self.ar_n = nslots

    def alloc(self, ncols=512):
        ns = (ncols + 511) // 512
        assert self.ar_top + ns <= self.ar_n, ("arena overflow", self.ar_top, ns, self.ar_n)
        b = Buf(self.ar, self.ar_top * 512, ncols, [("ar", s) for s in range(self.ar_top, self.ar_top + ns)])
        self.ar_top += ns
        return b

    def add(self, eng, fn, reads=(), writes=(), dma=False):
        op = Op(eng, fn, dma)
        op.pos = len(self.ops[eng])
        reads = _flat(reads, [])
        writes = _flat(writes, [])
        deps = []
        for k in reads:
            w = self.lastw.get(k)
            if w is not None:
                deps.append(w)
        for k in writes:
            w = self.lastw.get(k)
            if w is not None:
                deps.append(w)
            deps.extend(self.readers.get(k, ()))
        wt = self.waited[eng]
        wd = self.waited_dma[eng]
        best = {}
        dm = []
        for d in deps:
            if d is op:
                continue
            if d.dma:
                if id(d) not in wd:
                    wd.add(id(d))
                    dm.append(d)
                continue
            if d.eng == eng and eng in ("pe", "sp"):
                continue
            if wt.get(d.eng, -1) >= d.pos:
                continue
            if d.eng not in best or best[d.eng].pos < d.pos:
                best[d.eng] = d
        op.deps = list(best.values()) + dm
        for d in best.values():
            d.signals = True
            wt[d.eng] = max(wt.get(d.eng, -1), d.pos)
        for k in reads:
            self.readers.setdefault(k, []).append(op)
        for k in writes:
            self.lastw[k] = op
            self.readers[k] = []
        self.ops[eng].append(op)
        if dma:
            self.dma_ops.append(op)
        return op

    def emit(self, out_dma_ops=()):
        nc = self.nc
        nd = self.NDMA
        sems = {e: self.stack.enter_context(nc.semaphore(f"s_{e}")) for e in ENGS if e != "sp"}
        dsems = [self.stack.enter_context(nc.semaphore(f"s_dma{i}")) for i in range(nd)]
        for e in ENGS:
            c = 0
            for op in self.ops[e]:
                if not op.dma and op.signals:
                    c += 1
                    op.count = c
        per_eng = {}
        for op in self.dma_ops:
            per_eng.setdefault(op.eng, []).append(op)
        engs_with_dma = list(per_eng.keys())
        share = nd // max(1, len(engs_with_dma))
        for ei, e in enumerate(engs_with_dma):
            mysems = dsems[ei * share:(ei + 1) * share]
            for i, op in enumerate(per_eng[e]):
                op.dsem = mysems[i % share]
                op.dval = 16 * (i // share + 1)
                if i >= share:
                    op.prewait = (op.dsem, 16 * (i // share))
        final_waits = [(op.dsem, op.dval) for op in out_dma_ops]

        def run(e, eng):
            for op in self.ops[e]:
                if op.prewait is not None:
                    eng.wait_ge(op.prewait[0], op.prewait[1])
                for d in op.deps:
                    if d.dma:
                        eng.wait_ge(d.dsem, d.dval)
                    else:
                        eng.wait_ge(sems[d.eng], d.count)
                ins = op.fn(eng)
                if op.dma:
                    ins.then_inc(op.dsem, 16)
                elif op.signals:
                    ins.then_inc(sems[e], 1)
            if e == "sp":
                for s, v in final_waits:
                    eng.wait_ge(s, v)

        with nc.Block() as block:
            @block.tensor
            def _(eng):
                run("pe", eng)

            @block.scalar
            def _(eng):
                run("act", eng)

            @block.vector
            def _(eng):
                run("dve", eng)

            @block.gpsimd
            def _(eng):
                run("pool", eng)

            @block.sync
            def _(eng):
                run("sp", eng)

    def close(self):
        self.stack.close()

    def dma(self, out, in_, reads=(), writes=(), eng="sp"):
        return self.add(eng, lambda e: e.dma_start(out=out, in_=in_), reads, writes, dma=True)

    def mm(self, bank, out, lhsT, rhs, reads=(), r=False):
        p0 = out.start_partition()
        q = frozenset(range(p0 // 32, (p0 + out.partition_size() - 1) // 32 + 1))
        c0 = out.offset % 512 if hasattr(out, "offset") else 0
        c0 = self._ap_col0(out)
        c1 = c0 + self._ap_ncols(out)
        start = True
        for (qq, a, b) in bank.opened:
            if (qq & q) and a < c1 and c0 < b:
                assert q <= qq and a <= c0 and c1 <= b, ("partial psum overlap", q, qq, c0, c1, a, b)
                start = False
        if start:
            bank.opened.add((q, c0, c1))
        if r and FAST_MM and lhsT.dtype == F32:
            lhsT = lhsT.bitcast(F32R)
            rhs = rhs.bitcast(F32R)
        return self.add("pe", lambda e: e.matmul(out, lhsT, rhs, start=start, stop=True, skip_group_check=True),
                        reads, [bank])

    @staticmethod
    def _ap_col0(ap):
        pstride = ap.ap[0][0]
        return ap.offset % pstride

    @staticmethod
    def _ap_ncols(ap):
        span = 0
        for st, n in list(ap.ap)[1:]:
            span += st * (n - 1)
        return span + 1

    def tr(self, bank, out, in_, ident, reads=()):
        return self.add("pe", lambda e: e.transpose(out, in_, ident), reads, [bank])

    def act(self, out, in_, func, bias=None, scale=1.0, reads=(), writes=()):
        if bias is None:
            return self.add("act", lambda e: e.activation(out, in_, func, scale=scale), reads, writes)
        return self.add("act", lambda e: e.activation(out, in_, func, bias=bias, scale=scale), reads, writes)

    def tt(self, out, a, b, op, reads=(), writes=(), eng="dve"):
        return self.add(eng, lambda e: e.tensor_tensor(out, a, b, op), reads, writes)

    def ts(self, out, a, s1, op0, s2=None, op1=None, reads=(), writes=(), eng="dve"):
        if op1 is None:
            return self.add(eng, lambda e: e.tensor_scalar(out, a, s1, None, op0), reads, writes)
        return self.add(eng, lambda e: e.tensor_scalar(out, a, s1, s2, op0, op1), reads, writes)

    def stt(self, out, a, s, b, op0, op1, reads=(), writes=()):
        return self.add("dve", lambda e: e.scalar_tensor_tensor(out, a, s, b, op0, op1), reads, writes)

    def cp(self, out, a, reads=(), writes=(), eng="dve"):
        if eng == "act":
            return self.add(eng, lambda e: e.copy(out, a), reads, writes)
        return self.add(eng, lambda e: e.tensor_copy(out, a), reads, writes)

    def recip(self, out, a, reads=(), writes=()):
        return self.add("dve", lambda e: e.reciprocal(out, a), reads, writes)

    def scan(self, out, d0, d1, init, op0, op1, reads=(), writes=()):
        return self.add("dve", lambda e: e.tensor_tensor_scan(out, d0, d1, init, op0, op1), reads, writes)

    def memset(self, ap, v, writes=(), eng="dve"):
        return self.add(eng, lambda e: e.memset(ap, v), (), writes)


D = 1024
SEQ = 2048
DEPTH = 2
TB = 512
RW_STOP = 0
FAST_MM = True
N_IN = 3084
DFF = 2816
NFC = DFF // 128

PC_GPRE, PC_GPOST, PC_FPRE, PC_FPOST, PC_MU = 0, 8, 16, 24, 32
PC_W0, PC_A0, PC_KK, PC_KA, PC_RK, PC_LNW, PC_LNB, PC_MNORM, PC_SINK = 40, 42, 44, 46, 48, 50, 52, 54, 56
PC_CW, PC_CB, PC_BI, PC_BF, PC_FOXB, PC_FCW, PC_FCB, NPC = 58, 74, 78, 79, 80, 81, 147, 169
DV_OMKA, DV_BI15, DV_BF15, DV_NFOXB, DV_ESINK, NDV = 0, 2, 3, 4, 5, 8

C_IDENT, C_ONESD, C_ALLONE, C_BLK1, C_BLKM, C_TRI = 0, 128, 256, 384, 512, 640
C_MA, C_MB, C_IDG, C_MM, C_SELF, C_SELK, C_SELV, C_HM, C_SWAM, NCST = 768, 1280, 1408, 1536, 1792, 2304, 2432, 2688, 2696, 3720


W_IN_CHUNKS = ([(768, 128, False), (896, 128, False)]
               + [(i * 256 + hp * 128, 128, False) for hp in range(2) for i in range(3)]
               + [(1024 + i * 64, 64, False) for i in range(4)]
               + [(c0 + hp * 128, 128, False) for hp in range(2) for c0 in (1280, 1536)]
               + [(1792, 4, False), (1796, 4, False)]
               + [(1800, 128, False), (1928, 128, False), (2056, 64, True), (2120, 64, True), (2184, 128, False)]
               + [(2312, 128, False), (2440, 128, False), (2568, 128, False), (2696, 128, False)]
               + [(2824, 128, False), (2952, 128, False), (3080, 4, False)])
W_IN_OFF = {}
_o = 0
for (_c0, _m, _d) in W_IN_CHUNKS:
    W_IN_OFF[(_c0, _m)] = _o
    _o += 8 * (128 if _d else _m)
W_IN_TOT = _o


def _t5_bucket(dist):
    max_exact = 16
    d = np.maximum(dist, 1).astype(np.float32)
    large = max_exact + (np.log(d / max_exact) / math.log(128 / max_exact) * (32 - max_exact)).astype(np.int32)
    large = np.minimum(large, 31)
    return np.where(dist < max_exact, dist, large).astype(np.int32)


def _swa_dist():
    s = np.arange(128)[:, None, None]
    pcx = np.arange(2)[None, :, None]
    tq = np.arange(128)[None, None, :]
    sk = s + 128 * pcx
    dist = tq + 128 - sk
    vis = (dist >= 0) & (dist < 128)
    return dist, vis


def make_consts():
    c = np.zeros((128, NCST), np.float32)
    c[:, C_IDENT:C_IDENT + 128] = np.eye(128)
    c[:, C_ONESD:C_ONESD + 128] = 1.0 / 1024
    c[:, C_ALLONE:C_ALLONE + 128] = 1.0
    blk = np.zeros((128, 128), np.float32)
    blk[:64, :64] = 1
    blk[64:, 64:] = 1
    c[:, C_BLK1:C_BLK1 + 128] = blk
    c[:, C_BLKM:C_BLKM + 128] = blk / 64.0
    k = np.arange(128)
    c[:, C_TRI:C_TRI + 128] = (k[:, None] <= k[None, :])
    i = np.arange(64)[:, None]
    t = np.arange(64)[None, :]
    ma = np.zeros((64, 2, 2, 2, 64), np.float32)
    ma[:, :, :, 0, :] = (i < t)[:, None, None, :]
    ma[:, :, :, 1, :] = (i <= t)[:, None, None, :]
    c[:64, C_MA:C_MA + 512] = ma.reshape(64, 512)
    mb = np.zeros((64, 2, 64), np.float32)
    mb[:] = (i > t)[:, None, :]
    c[:64, C_MB:C_MB + 128] = mb.reshape(64, 128)
    idg = np.zeros((64, 2, 64), np.float32)
    idg[:] = np.eye(64)[:, None, :]
    c[:64, C_IDG:C_IDG + 128] = idg.reshape(64, 128)
    mm = np.zeros((64, 4, 64), np.float32)
    mm[:] = (i <= t)[:, None, :]
    c[:64, C_MM:C_MM + 256] = mm.reshape(64, 256)
    sf = np.zeros((4, 4, 128), np.float32)
    for h in range(4):
        sf[h, h, :] = 1
    c[:4, C_SELF:C_SELF + 512] = sf.reshape(4, 512)
    c[64:68, C_SELF:C_SELF + 512] = sf.reshape(4, 512)
    sk = np.zeros((4, 2, 64), np.float32)
    sv = np.zeros((4, 2, 128), np.float32)
    for h in range(4):
        for j in range(2):
            sk[h, j, :] = (h == 2 * j + np.arange(64) // 32)
            sv[h, j, :] = (h == 2 * j + np.arange(128) // 64)
    c[:4, C_SELK:C_SELK + 128] = sk.reshape(4, 128)
    c[:4, C_SELV:C_SELV + 256] = sv.reshape(4, 256)
    c[0:32, C_HM] = 1.0
    c[32:64, C_HM + 1] = 1.0
    _, vis = _swa_dist()
    m = np.where(vis, 0.0, -30000.0).astype(np.float32)
    m4 = np.broadcast_to(m[:, :, None, :], (128, 2, 4, 128))
    c[:, C_SWAM:C_SWAM + 1024] = m4.reshape(128, 1024)
    return c


def _cols(v, n):
    return np.ascontiguousarray(np.asarray(v, np.float32).reshape(n, 128).T)


def prep_inputs(inp):
    L = DEPTH
    f = lambda k: np.asarray(inp[k], np.float32)
    w_in4 = f("w_in").reshape(L, 8, 128, N_IN).transpose(0, 2, 1, 3)
    w_in_r = np.zeros((L, 128, W_IN_TOT), np.float32)
    for (c0, m, d) in W_IN_CHUNKS:
        blk = w_in4[:, :, :, c0:c0 + m]
        if d:
            blk = np.concatenate([blk, blk], axis=3)
        w_in_r[:, :, W_IN_OFF[(c0, m)]:W_IN_OFF[(c0, m)] + 8 * blk.shape[3]] = blk.reshape(L, 128, -1)
    w_out_r = np.ascontiguousarray(f("w_out").reshape(L, 8, 128, 8, 128).transpose(0, 3, 2, 1, 4)).reshape(L, 8, 128, 1024)
    w_up_r = np.ascontiguousarray(f("ffn_w_up").reshape(L, 8, 128, 2 * NFC, 128).transpose(0, 3, 2, 1, 4)).reshape(L, 2 * NFC, 128, 1024)
    w_dn_r = np.ascontiguousarray(f("ffn_w_down").reshape(L, NFC, 128, 8, 128).transpose(0, 3, 2, 1, 4)).reshape(L, 8, 128, NFC * 128)
    lora = np.zeros((L, 128, 512), np.float32)
    lora[:, 0:64, 0:256] = f("rwkv_w_up")
    lora[:, 64:128, 0:256] = f("rwkv_a_up")
    lora[:, :, 256:512] = f("rwkv_g_up")
    pc = np.zeros((L, 128, NPC), np.float32)
    for l in range(L):
        pc[l, :, PC_GPRE:PC_GPRE + 8] = _cols(f("norm_mix_pre")[l], 8)
        pc[l, :, PC_GPOST:PC_GPOST + 8] = _cols(f("norm_mix_post")[l], 8)
        pc[l, :, PC_FPRE:PC_FPRE + 8] = _cols(f("norm_ffn_pre")[l], 8)
        pc[l, :, PC_FPOST:PC_FPOST + 8] = _cols(f("norm_ffn_post")[l], 8)
        pc[l, :, PC_MU:PC_MU + 8] = _cols(f("rwkv_mu")[l], 8)
        for col, key in ((PC_W0, "rwkv_w0"), (PC_A0, "rwkv_a0"), (PC_KK, "rwkv_k_k"), (PC_KA, "rwkv_k_a"),
                         (PC_LNW, "rwkv_ln_w"), (PC_LNB, "rwkv_ln_b"), (PC_MNORM, "mlstm_norm")):
            pc[l, :, col:col + 2] = _cols(f(key)[l], 2)
        pc[l, :, PC_RK:PC_RK + 2] = _cols(f("rwkv_r_k")[l].reshape(256), 2)
        pc[l, :, PC_SINK:PC_SINK + 2] = _cols(np.repeat(f("swa_sinks")[l], 64), 2)
        cw = f("mlstm_conv_w")[l]
        for i in range(4):
            for j in range(4):
                pc[l, 0:64, PC_CW + i * 4 + j] = cw[j, i * 64:(i + 1) * 64]
            pc[l, 0:64, PC_CB + i] = f("mlstm_conv_b")[l][i * 64:(i + 1) * 64]
        pc[l, 0:4, PC_BI] = f("mlstm_b_i")[l]
        pc[l, 0:4, PC_BF] = f("mlstm_b_f")[l]
        pc[l, 0:4, PC_FOXB] = f("fox_b_f")[l]
        pc[l, 64:68, PC_FOXB] = f("fox_b_f")[l]
        fw = f("ffn_conv_w")[l]
        for j in range(3):
            pc[l, :, PC_FCW + j:PC_FCW + 66:3] = _cols(fw[j], NFC)
        pc[l, :, PC_FCB:PC_FCB + NFC] = _cols(f("ffn_conv_b")[l], NFC)
    dist, _ = _swa_dist()
    bk = _t5_bucket(np.clip(dist, 0, 127))
    swab = f("rel_bias")[bk]
    swab = np.ascontiguousarray(swab.transpose(0, 1, 3, 2)).reshape(128, 1024)
    cst = make_consts()
    x = f("x")
    maps = []
    for b in range(x.shape[0]):
        xT = np.ascontiguousarray(x[b].T.reshape(8, 128, x.shape[1]).transpose(1, 0, 2))
        maps.append({"xT": xT, "w_in_r": w_in_r, "w_out_r": w_out_r, "w_up_r": w_up_r, "w_dn_r": w_dn_r,
                     "lora": lora, "pc": pc, "cst": cst, "swab": swab})
    return maps


ARENA_SLOTS = 26


def build(nlayer=DEPTH, nblk=SEQ // TB, dbg=None, phases="rmsfon"):
    nc = bass.Bass("TRN2", target_bir_lowering=False)
    T = nblk * TB
    NT = T // 128
    L = DEPTH
    dr = lambda n, s, kind="ExternalInput": nc.dram_tensor(n, list(s), F32, kind=kind).ap()
    xT_d = dr("xT", [128, 8, SEQ])
    w_in_d = dr("w_in_r", [L, 128, W_IN_TOT])
    w_out_d = dr("w_out_r", [L, 8, 128, 1024])
    w_up_d = dr("w_up_r", [L, 2 * NFC, 128, 1024])
    w_dn_d = dr("w_dn_r", [L, 8, 128, NFC * 128])
    lora_d = dr("lora", [L, 128, 512])
    pc_d = dr("pc", [L, 128, NPC])
    cst_d = dr("cst", [128, NCST])
    swab_d = dr("swab", [128, 1024])
    out_d = dr("outT", [128, 8, SEQ], "ExternalOutput")
    dbg_d = dr("dbg", [128, 8, SEQ], "ExternalOutput") if dbg else None

    P = Prog(nc)
    RR = lambda ap: ap.bitcast(F32R) if (FAST_MM and ap.dtype == F32) else ap
    RR0 = RR
    P.make_banks()
    xT = P.sb("xT", 8 * T)
    kTh = P.sb("kTh", 2 * T)
    Vh = P.sb("Vh", NT * 256)
    negc = P.sb("negc", NT * 4)
    yT = P.sbs("yT", 8, dtype=BF16)
    foxR = P.sbs("foxR", 7)
    cst = P.sb("cst", C_SWAM)
    pc = P.sb("pc", NPC)
    dv = P.sb("dv", NDV)
    rstm = P.sb("rstm", 4)
    Srw = P.sb("Srw", 2 * 2 * 2 * 64)
    pc1 = P.sb("pc1", 8)
    CN = P.sb("CN", 2 * 128)
    crw = P.sb("crw", 8)
    cml = P.sb("cml", 12)
    cffn = P.sb("cffn", NFC * 2)
    ccar = P.sb("ccar", 2)
    swaKc = P.sb("swaKc", 2 * 128)
    swaVc = P.sb("swaVc", 128)
    hTr = P.sbs("hTr", 8, dtype=BF16)
    ebks = P.sb("ebks", 16)
    wbufs = [P.sb(f"wch{i}", 8 * 128, dtype=BF16) for i in range(6)]
    P.make_arena(ARENA_SLOTS)

    A = slice(0, 128)
    H0 = slice(0, 64)
    H1 = slice(64, 128)
    R4 = slice(0, 4)
    xT3 = xT.v(A, "p (c t) -> p c t", c=8)
    yT3 = yT.v(A, "p (c t) -> p c t", c=8)
    kTh3 = kTh.v(A, "p (j t) -> p j t", j=2)
    Vh3 = Vh.v(A, "p (n c) -> p n c", c=256)
    negc3 = negc.v(A, "p (n h) -> p n h", h=4)
    swaKc3 = swaKc.v(A, "p (j t) -> p j t", j=2)
    hTr3 = hTr.v(A, "p (c t) -> p c t", c=8)
    CN3 = CN.v(H0, "p (j c) -> p j c", j=2)
    S5 = Srw.v(H0, "p (j h b v) -> p j h b v", j=2, h=2, b=2)

    def C(off, n, p=A):
        return cst[p, off:off + n]

    ident = C(C_IDENT, 128)
    evac_i = [0]

    def copy(out, in_, reads, writes, eng=None):
        if eng is None:
            evac_i[0] += 1
            eng = "act" if evac_i[0] % 2 else "dve"
        return P.cp(out, in_, reads, writes, eng=eng)

    wb_i = [0]

    def wchunk():
        b = wbufs[wb_i[0] % 6]
        wb_i[0] += 1
        return b

    P.dma(RR0(cst[A, :]), cst_d[:, 0:C_SWAM], writes=[cst], eng="pool")
    for c in range(8):
        P.dma(xT3[:, c, :], xT_d[:, c, 0:T], writes=[xT])
    if phases != "rmsfon":
        P.ts(RR(yT[A, :]), cst[A, 0:4096 if C_SWAM >= 4096 else 2048].to_broadcast([128, 4096]) if False else xT[A, 0:4096], 0.0, ALU.mult, reads=[xT], writes=[yT])
    P.ar_top = 0

    def rms_rstd(src_fn, reads, eps=1e-6):
        ssb = P.bank(pin=True)
        rstd = P.alloc()
        sq = [P.alloc(), P.alloc()]
        for c in range(8):
            s = sq[c % 2]
            P.act(s[A, :], src_fn(c), AF.Square, reads=reads, writes=[s])
            P.mm(ssb, ssb[A, :], C(C_ONESD, 128), s[A, :], reads=[cst, s])
        P.ts(rstd[A, :], ssb[A, :], eps, ALU.add, reads=[ssb], writes=[rstd])
        ssb.pinned = False
        P.recip(rstd[A, :], rstd[A, :], reads=[rstd], writes=[rstd])
        P.act(rstd[A, :], rstd[A, :], AF.Sqrt, reads=[rstd], writes=[rstd])
        P.ar_top -= 2
        return rstd

    for l in range(nlayer):
        P.dma(pc[A, :], pc_d[l], writes=[pc])
        P.ts(dv[A, DV_OMKA:DV_OMKA + 2], pc[A, PC_KA:PC_KA + 2], -1.0, ALU.mult, 1.0, ALU.add, reads=[pc], writes=[dv])
        P.ts(dv[R4, DV_BI15:DV_BI15 + 2], pc[R4, PC_BI:PC_BI + 2], 1.0 / 15.0, ALU.mult, reads=[pc], writes=[dv])
        P.ts(dv[A, DV_NFOXB:DV_NFOXB + 1], pc[A, PC_FOXB:PC_FOXB + 1], -1.0, ALU.mult, reads=[pc], writes=[dv])
        P.act(dv[A, DV_ESINK:DV_ESINK + 2], pc[A, PC_SINK:PC_SINK + 2], AF.Exp, reads=[pc], writes=[dv])
        for b_ in (Srw, CN, crw, cml, cffn, ccar, swaKc, swaVc):
            P.memset(b_[A, :], 0.0, writes=[b_])

        for blk in range(nblk):
            t0 = blk * TB
            bs = slice(t0, t0 + TB)
            P.ar_top = 0
            rstd = rms_rstd(lambda c: xT3[:, c, bs], [xT])
            bk = P.bank()
            for tt_ in range(4):
                P.tr(bk, bk[A, tt_ * 128:(tt_ + 1) * 128], rstd[A, tt_ * 128:(tt_ + 1) * 128], ident, reads=[rstd, cst])
            P.cp(rstm[A, 0:4], bk.v(A, "p (n c) -> p n c", n=4)[:, :, 0], reads=[bk], writes=[rstm])
            MIX_BASE = P.ar_top

            def fill_hTr(gcol):
                for c in range(8):
                    gs = pc[A, gcol + c:gcol + c + 1]
                    if c % 2:
                        P.act(hTr3[:, c, :], xT3[:, c, bs], AF.Copy, scale=gs, reads=[xT, pc], writes=[hTr.sub(c * 512, 512)])
                    else:
                        P.ts(hTr3[:, c, :], xT3[:, c, bs], gs, ALU.mult, reads=[xT, pc], writes=[hTr.sub(c * 512, 512)], eng="dve")

            fill_hTr(PC_GPRE)
            gpre = pc[A, PC_GPRE:PC_GPRE + 8]

            def load_w(col0, M, dup=False):
                w = wchunk()
                w3 = w.v(A, "p (c m) -> p c m", c=8)
                off = W_IN_OFF[(col0, M)]
                if dup:
                    M = 128
                if M == 128:
                    P.dma(RR(w[A, :]), w_in_d[l, :, off:off + 1024], writes=[w], eng="pool")
                else:
                    P.dma(RR(w3[:, :, 0:M]), w_in_d[l, :, off:off + 8 * M].rearrange("p (c m) -> p c m", c=8), writes=[w], eng="pool")
                return w, w3, M

            def proj_fm(col0, M, dst, dup=False, scale=None, rows=None, pbase=0):
                w, w3, M = load_w(col0, M, dup)
                bk = P.bank()
                r = slice(pbase, pbase + M)
                for c in range(8):
                    P.mm(bk, bk[r, :], w3[:, c, 0:M], hTr3[:, c, :], reads=[w, hTr], r=(pbase == 0))
                if scale is None:
                    P.tt(dst, bk[r, :], rstd[r, :], ALU.mult, reads=[bk, rstd], writes=[rows])
                else:
                    P.stt(dst, bk[r, :], scale, rstd[r, :], ALU.mult, ALU.mult, reads=[bk, rstd], writes=[rows])

            def proj_tm(col0, ncols, dst_fn, wr):
                w, w3, _ = load_w(col0, ncols)
                for tt_ in range(4):
                    bk = P.bank()
                    for c in range(8):
                        P.mm(bk, bk[A, 0:ncols], hTr3[:, c, tt_ * 128:(tt_ + 1) * 128], w3[:, c, 0:ncols], reads=[w, hTr], r=True)
                    P.ts(dst_fn(tt_), bk[A, 0:ncols], rstm[A, tt_:tt_ + 1], ALU.mult, reads=[bk, rstm], writes=[wr])

            def shift_mix(z, ci, dst):
                d = P.alloc()
                P.tt(d[A, 1:512], z[A, 0:511], z[A, 1:512], ALU.subtract, reads=[z], writes=[d])
                P.tt(d[A, 0:1], crw[A, ci:ci + 1], z[A, 0:1], ALU.subtract, reads=[z, crw], writes=[d])
                P.cp(crw[A, ci:ci + 1], z[A, 511:512], reads=[z], writes=[crw], eng="dve")
                P.stt(dst[A, :], d[A, :], pc[A, PC_MU + ci:PC_MU + ci + 1], z[A, :], ALU.mult, ALU.add,
                      reads=[d, pc, z], writes=[dst])
                P.ar_top -= 1

            def rwkv():
                lora = P.alloc()
                P.dma(lora[A, :], lora_d[l], writes=[lora])
                swa_ = P.alloc()
                sg = P.alloc()
                ztmp = P.alloc()
                proj_fm(768, 128, ztmp[A, :], rows=ztmp)
                shift_mix(ztmp, 6, swa_)
                proj_fm(896, 128, ztmp[A, :], rows=ztmp)
                shift_mix(ztmp, 7, sg)
                P.ar_top -= 1
                P.act(swa_[H0, :], swa_[H0, :], AF.Tanh, reads=[swa_], writes=[swa_])
                P.act(sg[A, :], sg[A, :], AF.Sigmoid, reads=[sg], writes=[sg])
                if RW_STOP == 1:
                    return
                base_top = P.ar_top
                for hp in range(2):
                    P.ar_top = base_top
                    z = [P.alloc() for _ in range(3)]
                    s = [P.alloc() for _ in range(3)]
                    for i in range(3):
                        proj_fm(i * 256 + hp * 128, 128, z[i][A, :], rows=z[i])
                    for i in range(3):
                        shift_mix(z[i], i * 2 + hp, s[i])
                    sr, sk, sv = s
                    cw = slice(hp * 128, (hp + 1) * 128)
                    pcc = lambda c0: pc[A, c0 + hp:c0 + hp + 1]
                    bk = P.bank()
                    P.mm(bk, bk[A, :], lora[H0, cw], swa_[H0, :], reads=[lora, swa_])
                    lw = z[0]
                    P.act(lw[A, :], bk[A, :], AF.Sigmoid, bias=pcc(PC_W0), reads=[bk, pc], writes=[lw])
                    P.ts(lw[A, :], lw[A, :], -math.exp(-0.5), ALU.mult, reads=[lw], writes=[lw])
                    bk = P.bank()
                    P.mm(bk, bk[A, :], lora[H1, cw], swa_[H1, :], reads=[lora, swa_])
                    a = z[1]
                    P.act(a[A, :], bk[A, :], AF.Sigmoid, bias=pcc(PC_A0), reads=[bk, pc], writes=[a])
                    bk = P.bank()
                    P.mm(bk, bk[A, :], lora[A, 256 + hp * 128:256 + (hp + 1) * 128], sg[A, :], reads=[lora, sg])
                    g = z[2]
                    copy(g[A, :], bk[A, :], [bk], [g])
                    kk = P.alloc()
                    tmp = P.alloc()
                    P.ts(kk[A, :], sk[A, :], pcc(PC_KK), ALU.mult, reads=[sk, pc], writes=[kk])
                    P.tt(tmp[A, :], kk[A, :], kk[A, :], ALU.mult, reads=[kk], writes=[tmp])
                    bk = P.bank()
                    P.mm(bk, bk[A, :], C(C_BLK1, 128), tmp[A, :], reads=[cst, tmp])
                    P.act(tmp[A, :], bk[A, :], AF.Sqrt, reads=[bk], writes=[tmp])
                    P.ts(tmp[A, :], tmp[A, :], 1e-12, ALU.max, reads=[tmp], writes=[tmp])
                    P.recip(tmp[A, :], tmp[A, :], reads=[tmp], writes=[tmp])
                    P.tt(kk[A, :], kk[A, :], tmp[A, :], ALU.mult, reads=[kk, tmp], writes=[kk])
                    kmod = P.alloc()
                    P.ts(kmod[A, :], a[A, :], pcc(PC_KA), ALU.mult, dv[A, DV_OMKA + hp:DV_OMKA + hp + 1], ALU.add,
                         reads=[a, pc, dv], writes=[kmod])
                    P.tt(kmod[A, :], kmod[A, :], sk[A, :], ALU.mult, reads=[kmod, sk], writes=[kmod])
                    bonus = sk
                    P.stt(tmp[A, :], sr[A, :], pcc(PC_RK), kmod[A, :], ALU.mult, ALU.mult, reads=[sr, pc, kmod], writes=[tmp])
                    bk = P.bank()
                    P.mm(bk, bk[A, :], C(C_BLK1, 128), tmp[A, :], reads=[cst, tmp])
                    P.tt(bonus[A, :], bk[A, :], sv[A, :], ALU.mult, reads=[bk, sv], writes=[bonus])
                    bb = a
                    P.tt(bb[A, :], kk[A, :], a[A, :], ALU.mult, reads=[kk, a], writes=[bb])
                    cum = P.alloc()
                    ones = C(C_ALLONE, 64)
                    for c in range(8):
                        cs = slice(c * 64, (c + 1) * 64)
                        P.scan(cum[A, cs], ones, lw[A, cs], 0.0, ALU.mult, ALU.add, reads=[cst, lw], writes=[cum])
                    cumx = lw
                    P.tt(cumx[A, :], cum[A, :], lw[A, :], ALU.subtract, reads=[cum, lw], writes=[cumx])
                    Ep = P.alloc()
                    Em = P.alloc()
                    Ee = P.alloc()
                    P.act(Ep[A, :], cum[A, :], AF.Exp, reads=[cum], writes=[Ep])
                    P.act(Em[A, :], cum[A, :], AF.Exp, scale=-1.0, reads=[cum], writes=[Em])
                    P.act(cumx[A, :], cumx[A, :], AF.Exp, reads=[cumx], writes=[cumx])
                    for c in range(8):
                        cs = slice(c * 64, (c + 1) * 64)
                        P.act(Ee[A, cs], cum[A, cs], AF.Exp, bias=cum[A, c * 64 + 63:c * 64 + 64], scale=-1.0,
                              reads=[cum], writes=[Ee])
                    AR = P.alloc(1024)
                    AR3 = AR.v(A, "p (a t) -> p a t", a=2)
                    P.stt(AR3[:, 0, :], kk[A, :], -1.0, cumx[A, :], ALU.mult, ALU.mult, reads=[kk, cumx], writes=[AR])
                    P.tt(AR3[:, 1, :], sr[A, :], Ep[A, :], ALU.mult, reads=[sr, Ep], writes=[AR])
                    Bt, Kt, Bte, Kte = cum, tmp, kk, kmod
                    P.tt(Bt[A, :], bb[A, :], Em[A, :], ALU.mult, reads=[bb, Em], writes=[Bt])
                    P.tt(Kt[A, :], kmod[A, :], Em[A, :], ALU.mult, reads=[kmod, Em], writes=[Kt])
                    P.tt(Bte[A, :], bb[A, :], Ee[A, :], ALU.mult, reads=[bb, Ee], writes=[Bte])
                    P.tt(Kte[A, :], kmod[A, :], Ee[A, :], ALU.mult, reads=[kmod, Ee], writes=[Kte])
                    yr = Em
                    AR1 = Buf(z[0].t, z[0].off, 1024, z[0].k + z[1].k)
                    AR13 = AR1.v(A, "p (a t) -> p a t", a=2)
                    Bt1, Kt1 = sr, P.alloc()
                    idn1 = cst[H1, C_IDENT + 64:C_IDENT + 128]
                    for src, dst, wr in ((AR3[H1, 0, :], AR13[H0, 0, :], AR1), (AR3[H1, 1, :], AR13[H0, 1, :], AR1),
                                         (Bt[H1, :], Bt1[H0, :], Bt1), (Kt[H1, :], Kt1[H0, :], Kt1)):
                        bk = P.bank()
                        P.mm(bk, bk[H0, :], idn1, src, reads=[cst, AR, Bt, Kt])
                        copy(dst, bk[H0, :], [bk], [wr])
                    bk = P.bank()
                    P.mm(bk, bk[H0, 0:8], idn1, Ep.v(H1, "p (c t) -> p c t", c=8)[:, :, 63], reads=[cst, Ep])
                    copy(pc1[H0, 0:8], bk[H0, 0:8], [bk], [pc1])
                    ARh = (AR3, AR13)
                    Bth = (Bt, Bt1)
                    Kth = (Kt, Kt1)
                    ARb = (AR, AR1)
                    if RW_STOP == 2:
                        continue
                    NA = [P.alloc() for _ in range(2)]
                    nmslot = P.alloc()
                    NMp = [nmslot.sub(0, 256), nmslot.sub(256, 256)]
                    gslot = P.alloc()
                    Gp = [gslot.sub(0, 128), gslot.sub(128, 128)]
                    RU = [gslot.sub(256, 256), Ee.sub(0, 256)]
                    TM = [P.alloc() for _ in range(2)]
                    for c in range(8):
                        if RW_STOP == 6:
                            continue
                        cs = slice(c * 64, (c + 1) * 64)
                        na = NA[c % 2]
                        na5 = na.v(H0, "p (h b a t) -> p h b a t", h=2, b=2, a=2)
                        bk = P.bank()
                        bk5 = bk.v(H0, "p (h b a t) -> p h b a t", h=2, b=2, a=2)
                        bkm = P.bank()
                        bkm3 = bkm.v(H0, "p (x h t) -> p x h t", x=4, h=2)
                        for h2 in range(1 if RW_STOP == 7 else 2):
                            ph = slice(h2 * 64, h2 * 64 + 64)
                            for a_ in range(2):
                                P.mm(bk, bk5[:, h2, 0, a_, :], Bth[h2][H0, cs], ARh[h2][H0, a_, cs], reads=[Bth[h2], ARb[h2]])
                                P.mm(bk, bk5[:, h2, 1, a_, :], Kth[h2][H0, cs], ARh[h2][H0, a_, cs], reads=[Kth[h2], ARb[h2]])
                            P.mm(bkm, bkm3[:, 0, h2, :], ARh[h2][H0, 0, cs], Bth[h2][H0, cs], reads=[Bth[h2], ARb[h2]])
                        P.tt(na[H0, :], bk[H0, :], C(C_MA, 512, H0), ALU.mult, reads=[bk, cst], writes=[na])
                        if RW_STOP in (3, 7):
                            continue
                        nm = NMp[0]
                        nm4 = nm.v(H0, "p (x h t) -> p x h t", x=2, h=2)
                        P.cp(nm4[:, 0, :, :], na5[:, :, 0, 0, :], reads=[na], writes=[nm], eng="dve")
                        P.tt(nm4[:, 1, :, :], bkm3[:, 0, :, :], C(C_MB, 128, H0).rearrange("p (h t) -> p h t", h=2),
                             ALU.mult, reads=[bkm, cst], writes=[nm])
                        G3_0 = Gp[0].v(H0, "p (h t) -> p h t", h=2)
                        P.tt(G3_0, na5[:, :, 0, 0, :], C(C_IDG, 128, H0).rearrange("p (h t) -> p h t", h=2), ALU.add,
                             reads=[na, cst], writes=[Gp[0]])
                        cur = 0
                        for lev in range(5):
                            nmc = NMp[cur]
                            nmc4 = nmc.v(H0, "p (x h t) -> p x h t", x=2, h=2)
                            nmn = NMp[1 - cur]
                            nmn4 = nmn.v(H0, "p (x h t) -> p x h t", x=2, h=2)
                            Gc = Gp[cur]
                            Gc3 = Gc.v(H0, "p (h t) -> p h t", h=2)
                            Gn = Gp[1 - cur]
                            bkp = P.bank()
                            bkp4 = bkp.v(H0, "p (x h t) -> p x h t", x=4, h=2)
                            for h2 in range(2):
                                P.mm(bkp, bkp4[:, 0, h2, :], nmc4[:, 1, h2, :], nmc4[:, 0, h2, :], reads=[nmc])
                                P.mm(bkp, bkp4[:, 1, h2, :], nmc4[:, 0, h2, :], nmc4[:, 1, h2, :], reads=[nmc])
                            copy(nmn[H0, :], bkp[H0, 0:256], [bkp], [nmn])
                            bkg = P.bank()
                            for h2 in range(2):
                                P.mm(bkg, bkg[H0, h2 * 64:(h2 + 1) * 64], nmn4[:, 1, h2, :], Gc3[:, h2, :], reads=[nmn, Gc])
                            P.tt(Gn[H0, :], bkg[H0, 0:128], Gc[H0, :], ALU.add, reads=[bkg, Gc], writes=[Gn])
                            cur = 1 - cur
                        Gf = Gp[cur]
                        Gf3 = Gf.v(H0, "p (h t) -> p h t", h=2)
                        if RW_STOP == 4:
                            continue
                        tm = TM[c % 2]
                        bkt = P.bank()
                        P.tr(bkt, bkt[H0, 0:128], Bte[A, cs], ident, reads=[Bte, cst])
                        P.tr(bkt, bkt[H0, 128:256], Kte[A, cs], ident, reads=[Kte, cst])
                        P.tr(bkt, bkt[H0, 256:384], sv[A, cs], ident, reads=[sv, cst])
                        copy(tm[H0, 0:384], bkt[H0, 0:384], [bkt], [tm])
                        if RW_STOP == 5:
                            continue
                        ru = RU[c % 2]
                        sb_old = c % 2
                        sb_new = 1 - sb_old
                        bkr = P.bank()
                        for h2 in range(2):
                            ph = slice(h2 * 64, h2 * 64 + 64)
                            o = bkr[H0, h2 * 64:(h2 + 1) * 64]
                            P.mm(bkr, o, ARh[h2][H0, 0, cs], S5[:, hp, h2, sb_old, :], reads=[ARb[h2], Srw])
                            P.mm(bkr, o, na5[:, h2, 1, 0, :], tm[H0, 256 + h2 * 64:256 + (h2 + 1) * 64], reads=[na, tm])
                        copy(ru[H0, 0:128], bkr[H0, 0:128], [bkr], [ru], eng="act")
                        bku = P.bank()
                        for h2 in range(2):
                            P.mm(bku, bku[H0, h2 * 64:(h2 + 1) * 64], Gf3[:, h2, :], ru[H0, h2 * 64:(h2 + 1) * 64], reads=[Gf, ru])
                        copy(ru[H0, 128:256], bku[H0, 0:128], [bku], [ru], eng="dve")
                        bky = P.bank()
                        for h2 in range(2):
                            ph = slice(h2 * 64, h2 * 64 + 64)
                            o = bky[ph, 0:64]
                            P.mm(bky, o, S5[:, hp, h2, sb_old, :], ARh[h2][H0, 1, cs], reads=[ARb[h2], Srw])
                            P.mm(bky, o, ru[H0, 128 + h2 * 64:128 + (h2 + 1) * 64], na5[:, h2, 0, 1, :], reads=[ru, na])
                            P.mm(bky, o, tm[H0, 256 + h2 * 64:256 + (h2 + 1) * 64], na5[:, h2, 1, 1, :], reads=[tm, na])
                        copy(yr[A, cs], bky[A, 0:64], [bky], [yr], eng="act")
                        bks = P.bank()
                        for h2 in range(2):
                            o = bks[H0, h2 * 64:(h2 + 1) * 64]
                            P.mm(bks, o, tm[H0, h2 * 64:(h2 + 1) * 64], ru[H0, 128 + h2 * 64:128 + (h2 + 1) * 64], reads=[tm, ru])
                            P.mm(bks, o, tm[H0, 128 + h2 * 64:128 + (h2 + 1) * 64], tm[H0, 256 + h2 * 64:256 + (h2 + 1) * 64], reads=[tm])
                        for h2 in range(2):
                            pcs = Ep[H0, c * 64 + 63:c * 64 + 64] if h2 == 0 else pc1[H0, c:c + 1]
                            P.stt(S5[:, hp, h2, sb_new, :], S5[:, hp, h2, sb_old, :], pcs, bks[H0, h2 * 64:(h2 + 1) * 64],
                                  ALU.mult, ALU.add, reads=[Srw, Ep, pc1, bks], writes=[Srw])
                    t1 = Ep
                    bk = P.bank()
                    P.mm(bk, bk[A, :], C(C_BLKM, 128), yr[A, :], reads=[cst, yr])
                    P.tt(yr[A, :], yr[A, :], bk[A, :], ALU.subtract, reads=[yr, bk], writes=[yr])
                    P.tt(t1[A, :], yr[A, :], yr[A, :], ALU.mult, reads=[yr], writes=[t1])
                    bk = P.bank()
                    P.mm(bk, bk[A, :], C(C_BLKM, 128), t1[A, :], reads=[cst, t1])
                    P.ts(t1[A, :], bk[A, :], 64e-5, ALU.add, reads=[bk], writes=[t1])
                    P.recip(t1[A, :], t1[A, :], reads=[t1], writes=[t1])
                    P.act(t1[A, :], t1[A, :], AF.Sqrt, reads=[t1], writes=[t1])
                    P.tt(yr[A, :], yr[A, :], t1[A, :], ALU.mult, reads=[yr, t1], writes=[yr])
                    P.ts(yr[A, :], yr[A, :], pcc(PC_LNW), ALU.mult, pcc(PC_LNB), ALU.add, reads=[yr, pc], writes=[yr])
                    P.tt(yr[A, :], yr[A, :], bonus[A, :], ALU.add, reads=[yr, bonus], writes=[yr])
                    P.tt(RR(yT3[:, hp, :]), yr[A, :], g[A, :], ALU.mult, reads=[yr, g], writes=[yT.sub(hp * 512, 512)])

            P.ar_top = MIX_BASE
            if "r" in phases:
                rwkv()

            def mlstm():
                qk = [P.alloc() for _ in range(4)]
                vones = P.alloc()
                vo3 = vones.v(H0, "p (h c) -> p h c", h=4)
                P.memset(vones[H0, :], 1.0, writes=[vones])
                vT = [P.alloc(), P.alloc()]
                oT = [P.alloc(), P.alloc()]
                gi, gf, bcum, ge = (P.alloc() for _ in range(4))
                top = P.ar_top
                for i in range(4):
                    P.ar_top = top
                    uext = P.alloc(515)
                    acc = P.alloc()
                    proj_fm(1024 + i * 64, 64, uext[H0, 3:515], rows=uext)
                    P.cp(uext[H0, 0:3], cml[H0, i * 3:(i + 1) * 3], reads=[cml], writes=[uext], eng="dve")
                    wc = lambda j: pc[H0, PC_CW + i * 4 + j:PC_CW + i * 4 + j + 1]
                    P.ts(acc[H0, :], uext[H0, 0:512], wc(0), ALU.mult, reads=[uext, pc], writes=[acc])
                    for j in range(1, 4):
                        P.stt(acc[H0, :], uext[H0, j:j + 512], wc(j), acc[H0, :], ALU.mult, ALU.add, reads=[uext, pc, acc], writes=[acc])
                    P.cp(cml[H0, i * 3:(i + 1) * 3], uext[H0, 512:515], reads=[uext], writes=[cml], eng="dve")
                    P.act(qk[i][H0, :], acc[H0, :], AF.Silu, bias=pc[H0, PC_CB + i:PC_CB + i + 1], reads=[acc, pc], writes=[qk[i]])
                    if i < 2:
                        P.ts(qk[i][H0, :], qk[i][H0, :], 32.0 ** -0.5, ALU.mult, reads=[qk[i]], writes=[qk[i]])
                P.ar_top = top
                for hp in range(2):
                    proj_fm(1280 + hp * 128, 128, vT[hp][A, :], rows=vT[hp])
                    proj_fm(1536 + hp * 128, 128, oT[hp][A, :], rows=oT[hp])
                    P.act(oT[hp][A, :], oT[hp][A, :], AF.Sigmoid, reads=[oT[hp]], writes=[oT[hp]])
                proj_fm(1792, 4, gi[R4, :], rows=gi)
                proj_fm(1796, 4, gf[R4, :], rows=gf)
                P.act(gi[R4, :], gi[R4, :], AF.Tanh, bias=dv[R4, DV_BI15:DV_BI15 + 1], scale=1.0 / 15.0, reads=[gi, dv], writes=[gi])
                P.act(gf[R4, :], gf[R4, :], AF.Tanh, bias=dv[R4, DV_BF15:DV_BF15 + 1], scale=1.0 / 15.0, reads=[gf, dv], writes=[gf])
                P.ts(gi[R4, :], gi[R4, :], 15.0, ALU.mult, reads=[gi], writes=[gi])
                P.act(gf[R4, :], gf[R4, :], AF.Exp, scale=-15.0, reads=[gf], writes=[gf])
                P.act(gf[R4, :], gf[R4, :], AF.Ln, bias=1.0, reads=[gf], writes=[gf])
                ones4 = C(C_ALLONE, 64, R4)
                for c in range(8):
                    cs = slice(c * 64, (c + 1) * 64)
                    P.scan(bcum[R4, cs], ones4, gf[R4, cs], 0.0, ALU.mult, ALU.subtract, reads=[cst, gf], writes=[bcum])
                P.tt(gi[R4, :], gi[R4, :], bcum[R4, :], ALU.subtract, reads=[gi, bcum], writes=[gi])
                P.act(gi[R4, :], gi[R4, :], AF.Exp, reads=[gi], writes=[gi])
                P.act(bcum[R4, :], bcum[R4, :], AF.Exp, reads=[bcum], writes=[bcum])
                for c in range(8):
                    cs = slice(c * 64, (c + 1) * 64)
                    P.ts(ge[R4, cs], gi[R4, cs], bcum[R4, c * 64 + 63:c * 64 + 64], ALU.mult, reads=[gi, bcum], writes=[ge])
                kp = [P.alloc(), P.alloc()]
                kpe = [P.alloc(), P.alloc()]
                ebbc = [P.alloc(), P.alloc()]
                for j in range(2):
                    selk = C(C_SELK + j * 64, 64, R4)
                    bk = P.bank()
                    P.mm(bk, bk[H0, :], selk, gi[R4, :], reads=[cst, gi])
                    P.tt(kp[j][H0, :], qk[2 + j][H0, :], bk[H0, :], ALU.mult, reads=[qk[2 + j], bk], writes=[kp[j]])
                    bk = P.bank()
                    P.mm(bk, bk[H0, :], selk, ge[R4, :], reads=[cst, ge])
                    P.tt(kpe[j][H0, :], qk[2 + j][H0, :], bk[H0, :], ALU.mult, reads=[qk[2 + j], bk], writes=[kpe[j]])
                    bk = P.bank()
                    P.mm(bk, bk[H0, :], selk, bcum[R4, :], reads=[cst, bcum])
                    P.cp(ebks[H0, j * 8:(j + 1) * 8], bk.v(H0, "p (c t) -> p c t", c=8)[:, :, 63], reads=[bk], writes=[ebks])
                    bk = P.bank()
                    P.mm(bk, bk[A, :], C(C_SELV + j * 128, 128, R4), bcum[R4, :], reads=[cst, bcum])
                    copy(ebbc[j][A, :], bk[A, :], [bk], [ebbc[j]])
                NTs = P.alloc(1024)
                DNs = P.alloc(1024)
                NT3 = NTs.v(A, "p (j t) -> p j t", j=2)
                DN3 = DNs.v(A, "p (j t) -> p j t", j=2)
                sm = P.alloc()
                qmc = P.alloc(256)
                for c in range(8):
                    cs = slice(c * 64, (c + 1) * 64)
                    bkt = P.bank()
                    P.tr(bkt, bkt[H0, 0:128], vT[0][A, cs], ident, reads=[vT[0], cst])
                    P.tr(bkt, bkt[H0, 128:256], vT[1][A, cs], ident, reads=[vT[1], cst])
                    copy(vo3[:, :, 0:64], bkt.v(H0, "p (h c) -> p h c", h=8)[:, 0:4, :], [bkt], [vones])
                    bkt2 = P.bank()
                    P.tr(bkt2, bkt2[H0, 0:64], kpe[0][H0, cs], C(C_IDENT, 64, H0), reads=[kpe[0], cst])
                    P.tr(bkt2, bkt2[H0, 64:128], kpe[1][H0, cs], C(C_IDENT, 64, H0), reads=[kpe[1], cst])
                    copy(sm[H0, 256:384], bkt2[H0, 0:128], [bkt2], [sm])
                    for h in range(4):
                        P.ts(qmc[H0, h * 64:(h + 1) * 64], qk[h // 2][H0, cs], C(C_HM + h % 2, 1, H0), ALU.mult,
                             reads=[qk[h // 2], cst], writes=[qmc])
                    bka = P.bank()
                    for h in range(4):
                        j = h // 2
                        P.mm(bka, bka[H0, h * 64:(h + 1) * 64], kp[j][H0, cs], qmc[H0, h * 64:(h + 1) * 64], reads=[kp[j], qmc])
                    P.tt(sm[H0, 0:256], bka[H0, 0:256], C(C_MM, 256, H0), ALU.mult, reads=[bka, cst], writes=[sm])
                    bn = P.bank()
                    bd = P.bank()
                    for h in range(4):
                        j = h // 2
                        pq = slice((h % 2) * 32, (h % 2) * 32 + 32)
                        ph = slice((h % 2) * 64, (h % 2) * 64 + 64)
                        at = sm[H0, h * 64:(h + 1) * 64]
                        qm = qmc[H0, h * 64:(h + 1) * 64]
                        P.mm(bn, bn[ph, j * 64:(j + 1) * 64], vo3[:, h, 0:64], at, reads=[vones, sm])
                        P.mm(bn, bn[ph, j * 64:(j + 1) * 64], CN3[:, j, 0:64], qm, reads=[CN, qmc])
                        P.mm(bd, bd[ph, j * 64:(j + 1) * 64], vo3[:, h, 64:128], at, reads=[vones, sm])
                        P.mm(bd, bd[ph, j * 64:(j + 1) * 64], CN3[:, j, 64:128], qm, reads=[CN, qmc])
                    copy(NT3[:, :, cs], bn.v(A, "p (j t) -> p j t", j=8)[:, 0:2, :], [bn], [NTs], eng="act")
                    copy(DN3[:, :, cs], bd.v(A, "p (j t) -> p j t", j=8)[:, 0:2, :], [bd], [DNs], eng="act")
                    bs_ = P.bank()
                    for h in range(4):
                        j = h // 2
                        P.mm(bs_, bs_[H0, h * 128:(h + 1) * 128], sm[H0, 256 + j * 64:256 + (j + 1) * 64], vo3[:, h, :], reads=[sm, vones])
                    for j in range(2):
                        P.ts(CN3[:, j, :], CN3[:, j, :], ebks[H0, j * 8 + c:j * 8 + c + 1], ALU.mult, reads=[CN, ebks], writes=[CN])
                        for h2 in range(2):
                            h = 2 * j + h2
                            P.stt(CN3[:, j, :], bs_[H0, h * 128:(h + 1) * 128], C(C_HM + h2, 1, H0), CN3[:, j, :],
                                  ALU.mult, ALU.add, reads=[CN, bs_, cst], writes=[CN])
                for j in range(2):
                    d1 = DNs.sub(j * 512, 512)
                    n1 = NTs.sub(j * 512, 512)
                    P.tt(d1[A, :], d1[A, :], ebbc[j][A, :], ALU.mult, reads=[d1, ebbc[j]], writes=[d1])
                    P.stt(d1[A, :], d1[A, :], -1.0, d1[A, :], ALU.mult, ALU.max, reads=[d1], writes=[d1])
                    P.ts(d1[A, :], d1[A, :], 1.0, ALU.max, reads=[d1], writes=[d1])
                    P.recip(d1[A, :], d1[A, :], reads=[d1], writes=[d1])
                    P.tt(n1[A, :], n1[A, :], ebbc[j][A, :], ALU.mult, reads=[n1, ebbc[j]], writes=[n1])
                    P.tt(n1[A, :], n1[A, :], d1[A, :], ALU.mult, reads=[n1, d1], writes=[n1])
                    P.tt(d1[A, :], n1[A, :], n1[A, :], ALU.mult, reads=[n1], writes=[d1])
                    bk = P.bank()
                    P.mm(bk, bk[A, :], C(C_BLKM, 128), d1[A, :], reads=[cst, d1])
                    P.ts(d1[A, :], bk[A, :], 1e-6, ALU.add, reads=[bk], writes=[d1])
                    P.recip(d1[A, :], d1[A, :], reads=[d1], writes=[d1])
                    P.act(d1[A, :], d1[A, :], AF.Sqrt, reads=[d1], writes=[d1])
                    P.tt(n1[A, :], n1[A, :], d1[A, :], ALU.mult, reads=[n1, d1], writes=[n1])
                    P.stt(RR(yT3[:, 2 + j, :]), n1[A, :], pc[A, PC_MNORM + j:PC_MNORM + j + 1], oT[j][A, :], ALU.mult, ALU.mult,
                          reads=[n1, pc, oT[j]], writes=[yT.sub((2 + j) * 512, 512)])

            P.ar_top = MIX_BASE
            if "m" in phases:
                mlstm()

            def swa():
                swaB = P.alloc(1024)
                mtmp = P.alloc(1024)
                P.dma(swaB[A, :], swab_d, writes=[swaB])
                P.dma(mtmp[A, :], cst_d[:, C_SWAM:C_SWAM + 1024], writes=[mtmp])
                P.tt(swaB[A, :], swaB[A, :], mtmp[A, :], ALU.add, reads=[swaB, mtmp], writes=[swaB])
                P.ar_top -= 2
                swaB4 = swaB.v(A, "p (c h q) -> p c h q", c=2, h=4)
                swaK = P.alloc(1280)
                swaV = P.alloc(640)
                swaK3 = swaK.v(A, "p (j t) -> p j t", j=2)
                swaV3 = swaV.v(A, "p (n c) -> p n c", n=5)
                P.cp(swaK3[:, :, 0:128], swaKc3[:, :, :], reads=[swaKc], writes=[swaK], eng="dve")
                P.cp(swaV3[:, 0, :], swaVc[A, :], reads=[swaVc], writes=[swaV], eng="dve")
                qS = [P.alloc(), P.alloc()]
                for j in range(2):
                    proj_fm(1800 + j * 128, 128, qS[j][A, :], scale=0.125, rows=qS[j])
                    proj_fm(2056 + j * 64, 64, swaK3[:, j, 128:640], dup=True, rows=swaK)
                proj_tm(2184, 128, lambda tt_: swaV3[:, 1 + tt_, :], swaV)
                ETs = [P.alloc(1024), P.alloc(1024)]
                tmp = P.alloc()
                for i in range(4):
                    gi_ = blk * 4 + i
                    qc = slice(i * 128, (i + 1) * 128)
                    prev = slice(i * 128, (i + 1) * 128)
                    cur_ = slice((i + 1) * 128, (i + 2) * 128)
                    ET = ETs[i % 2]
                    bp = P.bank() if gi_ > 0 else None
                    bc = P.bank()
                    for hq in range(4):
                        j = hq // 2
                        ph = slice((hq % 2) * 64, (hq % 2) * 64 + 64)
                        o = slice(hq * 128, (hq + 1) * 128)
                        if gi_ > 0:
                            P.mm(bp, bp[A, o], swaK3[ph, j, prev], qS[j][ph, qc], reads=[swaK, qS[j]])
                            P.mm(bp, bp[A, o], ident, swaB4[:, 0, hq, :], reads=[cst, swaB])
                        P.mm(bc, bc[A, o], swaK3[ph, j, cur_], qS[j][ph, qc], reads=[swaK, qS[j]])
                        P.mm(bc, bc[A, o], ident, swaB4[:, 1, hq, :], reads=[cst, swaB])
                    if gi_ > 0:
                        P.act(ET[A, 0:512], bp[A, :], AF.Exp, reads=[bp], writes=[ET])
                    P.act(ET[A, 512:1024], bc[A, :], AF.Exp, reads=[bc], writes=[ET])
                    bo = P.bank()
                    bd = P.bank()
                    for hq in range(4):
                        j = hq // 2
                        ph = slice((hq % 2) * 64, (hq % 2) * 64 + 64)
                        o = slice(j * 128, (j + 1) * 128)
                        e = slice(hq * 128, (hq + 1) * 128)
                        if gi_ > 0:
                            P.mm(bo, bo[ph, o], swaV3[:, i, j * 64:(j + 1) * 64], ET[A, e.start:e.stop], reads=[swaV, ET])
                            P.mm(bd, bd[ph, o], C(C_ALLONE, 64), ET[A, e.start:e.stop], reads=[cst, ET])
                        P.mm(bo, bo[ph, o], swaV3[:, i + 1, j * 64:(j + 1) * 64], ET[A, 512 + e.start:512 + e.stop], reads=[swaV, ET])
                        P.mm(bd, bd[ph, o], C(C_ALLONE, 64), ET[A, 512 + e.start:512 + e.stop], reads=[cst, ET])
                    for j in range(2):
                        P.ts(tmp[A, j * 128:(j + 1) * 128], bd[A, j * 128:(j + 1) * 128], dv[A, DV_ESINK + j:DV_ESINK + j + 1], ALU.add,
                             reads=[bd, dv], writes=[tmp])
                    P.recip(tmp[A, 0:256], tmp[A, 0:256], reads=[tmp], writes=[tmp])
                    P.tt(RR(yT3[:, 4:6, qc]), bo.v(A, "p (j q) -> p j q", j=4)[:, 0:2, :], tmp.v(A, "p (j q) -> p j q", j=4)[:, 0:2, :],
                         ALU.mult, reads=[bo, tmp], writes=[yT.sub(4 * 512, 1024)])
                P.cp(swaKc3[:, :, :], swaK3[:, :, 512:640], reads=[swaK], writes=[swaKc], eng="dve")
                P.cp(swaVc[A, :], swaV3[:, 4, :], reads=[swaV], writes=[swaVc], eng="dve")

            P.ar_top = MIX_BASE
            if "s" in phases:
                swa()

            def fox():
                qtmp = [P.alloc(), P.alloc()]
                for j in range(2):
                    proj_fm(2312 + j * 128, 128, qtmp[j][A, :], scale=0.125, rows=qtmp[j])
                    proj_fm(2568 + j * 128, 128, RR(kTh3[:, j, bs]), rows=kTh)
                for half in range(2):
                    proj_tm(2824 + half * 128, 128, lambda tt_: RR(Vh3[:, blk * 4 + tt_, half * 128:(half + 1) * 128]), Vh)
                fr = P.alloc()
                crow = P.alloc()
                vtmp = P.alloc()
                chi = foxR.sub(2 * 512, 512)
                clo = foxR.sub(3 * 512, 512)
                qF = [foxR.sub(0, 512), foxR.sub(512, 512)]
                for R_ in (slice(0, 4), slice(64, 68)):
                    proj_fm(3080, 4, fr[R_, :], rows=fr, pbase=R_.start)
                for R_ in (slice(0, 4), slice(64, 68)):
                    P.act(fr[R_, :], fr[R_, :], AF.Exp, bias=dv[R_, DV_NFOXB:DV_NFOXB + 1], scale=-1.0, reads=[fr, dv], writes=[fr])
                    P.act(fr[R_, :], fr[R_, :], AF.Ln, bias=1.0, reads=[fr], writes=[fr])
                    for sg_ in range(4):
                        seg = slice(sg_ * 128, (sg_ + 1) * 128)
                        init = ccar[R_, 0:1] if sg_ == 0 else crow[R_, sg_ * 128 - 1:sg_ * 128]
                        P.scan(crow[R_, seg], C(C_ALLONE, 128, R_), fr[R_, seg], init, ALU.mult, ALU.subtract,
                               reads=[fr, ccar, cst, crow], writes=[crow])
                    P.cp(ccar[R_, 0:1], crow[R_, 511:512], reads=[crow], writes=[ccar], eng="dve")
                    P.ts(fr[R_, :], crow[R_, :], 4097.0, ALU.mult, reads=[crow], writes=[fr])
                    P.tt(vtmp[R_, :], fr[R_, :], crow[R_, :], ALU.subtract, reads=[fr, crow], writes=[vtmp])
                    P.tt(RR(chi[R_, :]), fr[R_, :], vtmp[R_, :], ALU.subtract, reads=[fr, vtmp], writes=[chi])
                    P.tt(RR(clo[R_, :]), crow[R_, :], chi[R_, :], ALU.subtract, reads=[crow, chi], writes=[clo])
                bk = P.bank()
                for tt_ in range(4):
                    P.mm(bk, bk[A, tt_ * 4:(tt_ + 1) * 4], crow[R4, tt_ * 128:(tt_ + 1) * 128], C(C_IDENT, 4, R4), reads=[crow, cst])
                P.ts(negc3[:, blk * 4:(blk + 1) * 4, :], bk.v(A, "p (n h) -> p n h", h=4)[:, 0:4, :], -1.0, ALU.mult,
                     reads=[bk], writes=[negc])
                for j in range(2):
                    P.cp(RR(qF[j][A, :]), qtmp[j][A, :], reads=[qtmp[j]], writes=[qF[j]], eng=("act", "dve")[j])
                ETs = [foxR.sub((4 + i_) * 512, 512) for i_ in range(3)]
                rec = P.alloc()
                ei = 0
                for j in range(2):
                    bo = P.bank(pin=True)
                    bd = P.bank(pin=True)
                    for h2 in range(2):
                        h = 2 * j + h2
                        ph = slice(h2 * 64, h2 * 64 + 64)
                        for kb in range(blk * 4 + 4):
                            q0 = max(0, kb - blk * 4) * 128
                            nq = 512 - q0
                            bs_ = P.bank()
                            P.mm(bs_, bs_[A, 0:nq], kTh3[ph, j, kb * 128:(kb + 1) * 128], qF[j][ph, q0:512], reads=[kTh, qF[j]], r=True)
                            Rh = slice(h2 * 64, h2 * 64 + 4)
                            P.mm(bs_, bs_[A, 0:nq], C(C_SELF + h * 128, 128, Rh), chi[Rh, q0:512], reads=[cst, chi], r=True)
                            P.mm(bs_, bs_[A, 0:nq], C(C_SELF + h * 128, 128, Rh), clo[Rh, q0:512], reads=[cst, clo], r=True)
                            et = ETs[ei % 3]
                            ei += 1
                            P.act(RR(et[A, 0:nq]), bs_[A, 0:nq], AF.Exp, bias=negc3[:, kb, h:h + 1], reads=[bs_, negc], writes=[et])
                            if kb >= blk * 4:
                                P.tt(RR(et[A, 0:128]), et[A, 0:128], C(C_TRI, 128), ALU.mult, reads=[et, cst], writes=[et], eng="dve")
                            P.mm(bo, bo[ph, q0:512], Vh3[:, kb, h * 64:(h + 1) * 64], et[A, 0:nq], reads=[Vh, et], r=(h2 == 0))
                            P.mm(bd, bd[ph, q0:512], C(C_ALLONE, 64), et[A, 0:nq], reads=[cst, et], r=(h2 == 0))
                    P.recip(rec[A, :], bd[A, :], reads=[bd], writes=[rec])
                    P.tt(RR(yT3[:, 6 + j, :]), bo[A, :], rec[A, :], ALU.mult, reads=[bo, rec], writes=[yT.sub((6 + j) * 512, 512)])
                    bo.pinned = False
                    bd.pinned = False

            P.ar_top = MIX_BASE
            if "f" in phases:
                fox()

            if dbg == ("yT", l):
                for c in range(8):
                    P.dma(dbg_d[:, c, bs], yT3[:, c, :], reads=[yT], eng="pool")

            def post_residual(oT3, oT, gcol):
                rs = rms_rstd(lambda c: oT3[:, c, :], [oT])
                tmps = [P.alloc(), P.alloc()]
                for m in range(8):
                    t_ = tmps[m % 2]
                    P.stt(t_[A, :], oT3[:, m, :], pc[A, gcol + m:gcol + m + 1], rs[A, :], ALU.mult, ALU.mult,
                          reads=[oT, pc, rs], writes=[t_])
                    P.tt(xT3[:, m, bs], xT3[:, m, bs], t_[A, :], ALU.add, reads=[xT, t_], writes=[xT])

            if "o" not in phases:
                continue
            P.ar_top = 0
            oT = P.alloc(4096)
            oT3 = oT.v(A, "p (c t) -> p c t", c=8)
            for m in range(8):
                w = wchunk()
                w3 = w.v(A, "p (c m) -> p c m", c=8)
                P.dma(RR(w[A, :]), w_out_d[l, m], writes=[w], eng="pool")
                bk = P.bank()
                for j in range(8):
                    P.mm(bk, bk[A, :], w3[:, j, :], yT3[:, j, :], reads=[w, yT], r=True)
                copy(oT3[:, m, :], bk[A, :], [bk], [oT.sub(m * 512, 512)])
            post_residual(oT3, oT, PC_GPOST)

            if dbg == ("x1", l):
                for c in range(8):
                    P.dma(dbg_d[:, c, bs], xT3[:, c, bs], reads=[xT])

            if "n" not in phases:
                continue
            P.ar_top = 0
            rs3 = rms_rstd(lambda c: xT3[:, c, bs], [xT])
            fill_hTr(PC_FPRE)
            fT = yT
            fT3 = yT3
            oT = P.alloc(4096)
            oT3 = oT.v(A, "p (c t) -> p c t", c=8)
            gbuf = P.alloc(514)
            acc = P.alloc()
            gfp = pc[A, PC_FPRE:PC_FPRE + 8]
            for (c_lo, c_hi) in ((0, 8), (8, 16), (16, 22)):
                ng = c_hi - c_lo
                for cc in range(ng):
                    c = c_lo + cc
                    ws = []
                    for c2 in (c, NFC + c):
                        w = wchunk()
                        w3 = w.v(A, "p (c m) -> p c m", c=8)
                        P.dma(RR(w[A, :]), w_up_d[l, c2], writes=[w], eng="pool")
                        ws.append((w, w3))
                    bg = P.bank()
                    for k in range(8):
                        P.mm(bg, bg[A, :], ws[0][1][:, k, :], hTr3[:, k, :], reads=[ws[0][0], hTr], r=True)
                    bu = P.bank()
                    for k in range(8):
                        P.mm(bu, bu[A, :], ws[1][1][:, k, :], hTr3[:, k, :], reads=[ws[1][0], hTr], r=True)
                    P.tt(gbuf[A, 2:514], bg[A, :], rs3[A, :], ALU.mult, reads=[bg, rs3], writes=[gbuf])
                    P.cp(gbuf[A, 0:2], cffn[A, c * 2:c * 2 + 2], reads=[cffn], writes=[gbuf], eng="dve")
                    fw = lambda j: pc[A, PC_FCW + c * 3 + j:PC_FCW + c * 3 + j + 1]
                    P.ts(acc[A, :], gbuf[A, 0:512], fw(0), ALU.mult, reads=[gbuf, pc], writes=[acc])
                    P.stt(acc[A, :], gbuf[A, 1:513], fw(1), acc[A, :], ALU.mult, ALU.add, reads=[gbuf, pc, acc], writes=[acc])
                    P.stt(acc[A, :], gbuf[A, 2:514], fw(2), acc[A, :], ALU.mult, ALU.add, reads=[gbuf, pc, acc], writes=[acc])
                    P.cp(cffn[A, c * 2:c * 2 + 2], gbuf[A, 512:514], reads=[gbuf], writes=[cffn], eng="dve")
                    P.act(acc[A, :], acc[A, :], AF.Gelu_apprx_tanh, bias=pc[A, PC_FCB + c:PC_FCB + c + 1], reads=[acc, pc], writes=[acc])
                    P.tt(acc[A, :], acc[A, :], rs3[A, :], ALU.mult, reads=[acc, rs3], writes=[acc])
                    P.tt(RR(fT3[:, cc, :]), acc[A, :], bu[A, :], ALU.mult, reads=[acc, bu], writes=[fT.sub(cc * 512, 512)])
                for m in range(8):
                    wa = wchunk()
                    wa3 = wa.v(A, "p (c m) -> p c m", c=8)
                    P.dma(RR(wa[A, 0:ng * 128]), w_dn_d[l, m, :, c_lo * 128:c_hi * 128], writes=[wa], eng="pool")
                    bk = P.bank()
                    for cc in range(ng):
                        P.mm(bk, bk[A, :], wa3[:, cc, :], fT3[:, cc, :], reads=[wa, fT], r=True)
                    om = oT.sub(m * 512, 512)
                    if c_lo == 0:
                        copy(oT3[:, m, :], bk[A, :], [bk], [om])
                    else:
                        P.tt(oT3[:, m, :], oT3[:, m, :], bk[A, :], ALU.add, reads=[om, bk], writes=[om])
            post_residual(oT3, oT, PC_FPOST)

    outops = []
    for c in range(8):
        outops.append(P.dma(out_d[:, c, 0:T], xT3[:, c, :], reads=[xT]))
    P.emit(outops + [op for op in P.dma_ops if False])
    P.close()
    return nc


_NC_CACHE = {}


def kernel(**inputs):
    maps = prep_inputs(inputs)
    if "nc" not in _NC_CACHE:
        _NC_CACHE["nc"] = build()
    nc = _NC_CACHE["nc"]
    res = run_bass_kernel_spmd(nc, maps, core_ids=list(range(len(maps))))
    x = np.asarray(inputs["x"])
    out = np.empty(x.shape, np.float32)
    for b in range(x.shape[0]):
        oT = np.asarray(res.results[b]["outT"])
        out[b] = oT.transpose(1, 0, 2).reshape(D, SEQ).T
    return out
```

```python
import contextlib
import math
import numpy as np
import concourse.bass as bass
import concourse.mybir as mybir
from concourse.bass_utils import run_bass_kernel_spmd

F32 = mybir.dt.float32
F32R = mybir.dt.float32r
BF16 = mybir.dt.bfloat16
ALU = mybir.AluOpType
AF = mybir.ActivationFunctionType

ENGS = ("pe", "act", "dve", "pool", "sp")


def _flat(ks, out):
    for k in ks:
        if isinstance(k, (str, int)):
            out.append(k)
        elif isinstance(k, tuple) and (len(k) == 0 or isinstance(k[0], (str, int))):
            out.append(k)
        elif hasattr(k, "k"):
            _flat(k.k, out)
        else:
            _flat(k, out)
    return out


class Op:
    __slots__ = ("eng", "fn", "deps", "signals", "count", "pos", "dma", "dsem", "dval", "prewait")

    def __init__(self, eng, fn, dma=False):
        self.eng = eng
        self.fn = fn
        self.deps = []
        self.signals = False
        self.count = None
        self.pos = None
        self.dma = dma
        self.dsem = None
        self.dval = None
        self.prewait = None


class Buf:
    def __init__(self, t, off, ncols, keys):
        self.t, self.off, self.n, self.k = t, off, ncols, keys

    def __getitem__(self, idx):
        p, c = idx
        if isinstance(c, int):
            c = slice(c, c + 1)
        a = 0 if c.start is None else c.start
        b = self.n if c.stop is None else c.stop
        assert 0 <= a <= b <= self.n, (a, b, self.n)
        return self.t[p, self.off + a:self.off + b]

    def v(self, p, pat, **kw):
        return self.t[p, self.off:self.off + self.n].rearrange(pat, **kw)

    def sub(self, c0, n):
        ks = self.k
        if len(ks) > 1 and len(ks) * 512 >= self.n:
            ks = ks[c0 // 512:(c0 + n + 511) // 512]
        return Buf(self.t, self.off + c0, n, ks)


class Bank(Buf):
    def __init__(self, t, name):
        super().__init__(t, 0, 512, [name])
        self.opened = set()
        self.pinned = False


class Prog:
    NDMA = 32

    def __init__(self, nc):
        self.nc = nc
        self.ops = {e: [] for e in ENGS}
        self.lastw = {}
        self.readers = {}
        self.waited = {e: {} for e in ENGS}
        self.waited_dma = {e: set() for e in ENGS}
        self.dma_ops = []
        self.stack = contextlib.ExitStack()
        self.banks = []
        self.bank_i = 0
        self.ar = None
        self.ar_top = 0

    def sb(self, name, ncols, parts=128, dtype=F32):
        t = self.stack.enter_context(self.nc.sbuf_tensor("sb_" + name, [parts, ncols], dtype))
        return Buf(t, 0, ncols, [name])

    def sbs(self, name, nslots, dtype=F32):
        t = self.stack.enter_context(self.nc.sbuf_tensor("sb_" + name, [128, nslots * 512], dtype))
        return Buf(t, 0, nslots * 512, [(name, i) for i in range(nslots)])

    def make_banks(self):
        for i in range(8):
            t = self.stack.enter_context(self.nc.psum_tensor(f"bank{i}", [128, 512], F32))
            self.banks.append(Bank(t, f"bank{i}"))

    def bank(self, pin=False):
        for _ in range(16):
            b = self.banks[self.bank_i % 8]
            self.bank_i += 1
            if not b.pinned:
                b.opened = set()
                b.pinned = pin
                return b
        raise RuntimeError("no free psum bank")

    def make_arena(self, nslots):
        self.ar = self.stack.enter_context(self.nc.sbuf_tensor("arena", [128, nslots * 512], F32))
        self.ar_n = nslots

    def alloc(self, ncols=512):
        ns = (ncols + 511) // 512
        assert self.ar_top + ns <= self.ar_n, ("arena overflow", self.ar_top, ns, self.ar_n)
        b = Buf(self.ar, self.ar_top * 512, ncols, [("ar", s) for s in range(self.ar_top, self.ar_top + ns)])
        self.ar_top += ns
        return b

    def add(self, eng, fn, reads=(), writes=(), dma=False):
        op = Op(eng, fn, dma)
        op.pos = len(self.ops[eng])
        reads = _flat(reads, [])
        writes = _flat(writes, [])
        deps = []
        for k in reads:
            w = self.lastw.get(k)
            if w is not None:
                deps.append(w)
        for k in writes:
            w = self.lastw.get(k)
            if w is not None:
                deps.append(w)
            deps.extend(self.readers.get(k, ()))
        wt = self.waited[eng]
        wd = self.waited_dma[eng]
        best = {}
        dm = []
        for d in deps:
            if d is op:
                continue
            if d.dma:
                if id(d) not in wd:
                    wd.add(id(d))
                    dm.append(d)
                continue
            if d.eng == eng and eng in ("pe", "sp"):
                continue
            if wt.get(d.eng, -1) >= d.pos:
                continue
            if d.eng not in best or best[d.eng].pos < d.pos:
                best[d.eng] = d
        op.deps = list(best.values()) + dm
        for d in best.values():
            d.signals = True
            wt[d.eng] = max(wt.get(d.eng, -1), d.pos)
        for k in reads:
            self.readers.setdefault(k, []).append(op)
        for k in writes:
            self.lastw[k] = op
            self.readers[k] = []
        self.ops[eng].append(op)
        if dma:
            self.dma_ops.append(op)
        return op

    def emit(self, out_dma_ops=()):
        nc = self.nc
        nd = self.NDMA
        sems = {e: self.stack.enter_context(nc.semaphore(f"s_{e}")) for e in ENGS if e != "sp"}
        dsems = [self.stack.enter_context(nc.semaphore(f"s_dma{i}")) for i in range(nd)]
        for e in ENGS:
            c = 0
            for op in self.ops[e]:
                if not op.dma and op.signals:
                    c += 1
                    op.count = c
        per_eng = {}
        for op in self.dma_ops:
            per_eng.setdefault(op.eng, []).append(op)
        engs_with_dma = list(per_eng.keys())
        share = nd // max(1, len(engs_with_dma))
        for ei, e in enumerate(engs_with_dma):
            mysems = dsems[ei * share:(ei + 1) * share]
            for i, op in enumerate(per_eng[e]):
                op.dsem = mysems[i % share]
                op.dval = 16 * (i // share + 1)
                if i >= share:
                    op.prewait = (op.dsem, 16 * (i // share))
        final_waits = [(op.dsem, op.dval) for op in out_dma_ops]

        def run(e, eng):
            for op in self.ops[e]:
                if op.prewait is not None:
                    eng.wait_ge(op.prewait[0], op.prewait[1])
                for d in op.deps:
                    if d.dma:
                        eng.wait_ge(d.dsem, d.dval)
                    else:
                        eng.wait_ge(sems[d.eng], d.count)
                ins = op.fn(eng)
                if op.dma:
                    ins.then_inc(op.dsem, 16)
                elif op.signals:
                    ins.then_inc(sems[e], 1)
            if e == "sp":
                for s, v in final_waits:
                    eng.wait_ge(s, v)

        with nc.Block() as block:
            @block.tensor
            def _(eng):
                run("pe", eng)

            @block.scalar
            def _(eng):
                run("act", eng)

            @block.vector
            def _(eng):
                run("dve", eng)

            @block.gpsimd
            def _(eng):
                run("pool", eng)

            @block.sync
            def _(eng):
                run("sp", eng)

    def close(self):
        self.stack.close()

    def dma(self, out, in_, reads=(), writes=(), eng="sp"):
        return self.add(eng, lambda e: e.dma_start(out=out, in_=in_), reads, writes, dma=True)

    def mm(self, bank, out, lhsT, rhs, reads=(), r=False):
        p0 = out.start_partition()
        q = frozenset(range(p0 // 32, (p0 + out.partition_size() - 1) // 32 + 1))
        c0 = out.offset % 512 if hasattr(out, "offset") else 0
        c0 = self._ap_col0(out)
        c1 = c0 + self._ap_ncols(out)
        start = True
        for (qq, a, b) in bank.opened:
            if (qq & q) and a < c1 and c0 < b:
                assert q <= qq and a <= c0 and c1 <= b, ("partial psum overlap", q, qq, c0, c1, a, b)
                start = False
        if start:
            bank.opened.add((q, c0, c1))
        if r and FAST_MM and lhsT.dtype == F32:
            lhsT = lhsT.bitcast(F32R)
            rhs = rhs.bitcast(F32R)
        return self.add("pe", lambda e: e.matmul(out, lhsT, rhs, start=start, stop=True, skip_group_check=True),
                        reads, [bank])

    @staticmethod
    def _ap_col0(ap):
        pstride = ap.ap[0][0]
        return ap.offset % pstride

    @staticmethod
    def _ap_ncols(ap):
        span = 0
        for st, n in list(ap.ap)[1:]:
            span += st * (n - 1)
        return span + 1

    def tr(self, bank, out, in_, ident, reads=()):
        return self.add("pe", lambda e: e.transpose(out, in_, ident), reads, [bank])

    def act(self, out, in_, func, bias=None, scale=1.0, reads=(), writes=()):
        if bias is None:
            return self.add("act", lambda e: e.activation(out, in_, func, scale=scale), reads, writes)
        return self.add("act", lambda e: e.activation(out, in_, func, bias=bias, scale=scale), reads, writes)

    def tt(self, out, a, b, op, reads=(), writes=(), eng="dve"):
        return self.add(eng, lambda e: e.tensor_tensor(out, a, b, op), reads, writes)

    def ts(self, out, a, s1, op0, s2=None, op1=None, reads=(), writes=(), eng="dve"):
        if op1 is None:
            return self.add(eng, lambda e: e.tensor_scalar(out, a, s1, None, op0), reads, writes)
        return self.add(eng, lambda e: e.tensor_scalar(out, a, s1, s2, op0, op1), reads, writes)

    def stt(self, out, a, s, b, op0, op1, reads=(), writes=()):
        return self.add("dve", lambda e: e.scalar_tensor_tensor(out, a, s, b, op0, op1), reads, writes)

    def cp(self, out, a, reads=(), writes=(), eng="dve"):
        if eng == "act":
            return self.add(eng, lambda e: e.copy(out, a), reads, writes)
        return self.add(eng, lambda e: e.tensor_copy(out, a), reads, writes)

    def recip(self, out, a, reads=(), writes=()):
        return self.add("dve", lambda e: e.reciprocal(out, a), reads, writes)

    def scan(self, out, d0, d1, init, op0, op1, reads=(), writes=()):
        return self.add("dve", lambda e: e.tensor_tensor_scan(out, d0, d1, init, op0, op1), reads, writes)

    def memset(self, ap, v, writes=(), eng="dve"):
        return self.add(eng, lambda e: e.memset(ap, v), (), writes)


D = 1024
SEQ = 2048
DEPTH = 2
TB = 512
RW_STOP = 0
FAST_MM = True
N_IN = 3084
DFF = 2816
NFC = DFF // 128

PC_GPRE, PC_GPOST, PC_FPRE, PC_FPOST, PC_MU = 0, 8, 16, 24, 32
PC_W0, PC_A0, PC_KK, PC_KA, PC_RK, PC_LNW, PC_LNB, PC_MNORM, PC_SINK = 40, 42, 44, 46, 48, 50, 52, 54, 56
PC_CW, PC_CB, PC_BI, PC_BF, PC_FOXB, PC_FCW, PC_FCB, NPC = 58, 74, 78, 79, 80, 81, 147, 169
DV_OMKA, DV_BI15, DV_BF15, DV_NFOXB, DV_ESINK, NDV = 0, 2, 3, 4, 5, 8

C_IDENT, C_ONESD, C_ALLONE, C_BLK1, C_BLKM, C_TRI = 0, 128, 256, 384, 512, 640
C_MA, C_MB, C_IDG, C_MM, C_SELF, C_SELK, C_SELV, C_HM, C_SWAM, NCST = 768, 1280, 1408, 1536, 1792, 2304, 2432, 2688, 2696, 3720


W_IN_CHUNKS = ([(768, 128, False), (896, 128, False)]
               + [(i * 256 + hp * 128, 128, False) for hp in range(2) for i in range(3)]
               + [(1024 + i * 64, 64, False) for i in range(4)]
               + [(c0 + hp * 128, 128, False) for hp in range(2) for c0 in (1280, 1536)]
               + [(1792, 4, False), (1796, 4, False)]
               + [(1800, 128, False), (1928, 128, False), (2056, 64, True), (2120, 64, True), (2184, 128, False)]
               + [(2312, 128, False), (2440, 128, False), (2568, 128, False), (2696, 128, False)]
               + [(2824, 128, False), (2952, 128, False), (3080, 4, False)])
W_IN_OFF = {}
_o = 0
for (_c0, _m, _d) in W_IN_CHUNKS:
    W_IN_OFF[(_c0, _m)] = _o
    _o += 8 * (128 if _d else _m)
W_IN_TOT = _o


def _t5_bucket(dist):
    max_exact = 16
    d = np.maximum(dist, 1).astype(np.float32)
    large = max_exact + (np.log(d / max_exact) / math.log(128 / max_exact) * (32 - max_exact)).astype(np.int32)
    large = np.minimum(large, 31)
    return np.where(dist < max_exact, dist, large).astype(np.int32)


def _swa_dist():
    s = np.arange(128)[:, None, None]
    pcx = np.arange(2)[None, :, None]
    tq = np.arange(128)[None, None, :]
    sk = s + 128 * pcx
    dist = tq + 128 - sk
    vis = (dist >= 0) & (dist < 128)
    return dist, vis


def make_consts():
    c = np.zeros((128, NCST), np.float32)
    c[:, C_IDENT:C_IDENT + 128] = np.eye(128)
    c[:, C_ONESD:C_ONESD + 128] = 1.0 / 1024
    c[:, C_ALLONE:C_ALLONE + 128] = 1.0
    blk = np.zeros((128, 128), np.float32)
    blk[:64, :64] = 1
    blk[64:, 64:] = 1
    c[:, C_BLK1:C_BLK1 + 128] = blk
    c[:, C_BLKM:C_BLKM + 128] = blk / 64.0
    k = np.arange(128)
    c[:, C_TRI:C_TRI + 128] = (k[:, None] <= k[None, :])
    i = np.arange(64)[:, None]
    t = np.arange(64)[None, :]
    ma = np.zeros((64, 2, 2, 2, 64), np.float32)
    ma[:, :, :, 0, :] = (i < t)[:, None, None, :]
    ma[:, :, :, 1, :] = (i <= t)[:, None, None, :]
    c[:64, C_MA:C_MA + 512] = ma.reshape(64, 512)
    mb = np.zeros((64, 2, 64), np.float32)
    mb[:] = (i > t)[:, None, :]
    c[:64, C_MB:C_MB + 128] = mb.reshape(64, 128)
    idg = np.zeros((64, 2, 64), np.float32)
    idg[:] = np.eye(64)[:, None, :]
    c[:64, C_IDG:C_IDG + 128] = idg.reshape(64, 128)
    mm = np.zeros((64, 4, 64), np.float32)
    mm[:] = (i <= t)[:, None, :]
    c[:64, C_MM:C_MM + 256] = mm.reshape(64, 256)
    sf = np.zeros((4, 4, 128), np.float32)
    for h in range(4):
        sf[h, h, :] = 1
    c[:4, C_SELF:C_SELF + 512] = sf.reshape(4, 512)
    c[64:68, C_SELF:C_SELF + 512] = sf.reshape(4, 512)
    sk = np.zeros((4, 2, 64), np.float32)
    sv = np.zeros((4, 2, 128), np.float32)
    for h in range(4):
        for j in range(2):
            sk[h, j, :] = (h == 2 * j + np.arange(64) // 32)
            sv[h, j, :] = (h == 2 * j + np.arange(128) // 64)
    c[:4, C_SELK:C_SELK + 128] = sk.reshape(4, 128)
    c[:4, C_SELV:C_SELV + 256] = sv.reshape(4, 256)
    c[0:32, C_HM] = 1.0
    c[32:64, C_HM + 1] = 1.0
    _, vis = _swa_dist()
    m = np.where(vis, 0.0, -30000.0).astype(np.float32)
    m4 = np.broadcast_to(m[:, :, None, :], (128, 2, 4, 128))
    c[:, C_SWAM:C_SWAM + 1024] = m4.reshape(128, 1024)
    return c


def _cols(v, n):
    return np.ascontiguousarray(np.asarray(v, np.float32).reshape(n, 128).T)


def prep_inputs(inp):
    L = DEPTH
    f = lambda k: np.asarray(inp[k], np.float32)
    w_in4 = f("w_in").reshape(L, 8, 128, N_IN).transpose(0, 2, 1, 3)
    w_in_r = np.zeros((L, 128, W_IN_TOT), np.float32)
    for (c0, m, d) in W_IN_CHUNKS:
        blk = w_in4[:, :, :, c0:c0 + m]
        if d:
            blk = np.concatenate([blk, blk], axis=3)
        w_in_r[:, :, W_IN_OFF[(c0, m)]:W_IN_OFF[(c0, m)] + 8 * blk.shape[3]] = blk.reshape(L, 128, -1)
    w_out_r = np.ascontiguousarray(f("w_out").reshape(L, 8, 128, 8, 128).transpose(0, 3, 2, 1, 4)).reshape(L, 8, 128, 1024)
    w_up_r = np.ascontiguousarray(f("ffn_w_up").reshape(L, 8, 128, 2 * NFC, 128).transpose(0, 3, 2, 1, 4)).reshape(L, 2 * NFC, 128, 1024)
    w_dn_r = np.ascontiguousarray(f("ffn_w_down").reshape(L, NFC, 128, 8, 128).transpose(0, 3, 2, 1, 4)).reshape(L, 8, 128, NFC * 128)
    lora = np.zeros((L, 128, 512), np.float32)
    lora[:, 0:64, 0:256] = f("rwkv_w_up")
    lora[:, 64:128, 0:256] = f("rwkv_a_up")
    lora[:, :, 256:512] = f("rwkv_g_up")
    pc = np.zeros((L, 128, NPC), np.float32)
    for l in range(L):
        pc[l, :, PC_GPRE:PC_GPRE + 8] = _cols(f("norm_mix_pre")[l], 8)
        pc[l, :, PC_GPOST:PC_GPOST + 8] = _cols(f("norm_mix_post")[l], 8)
        pc[l, :, PC_FPRE:PC_FPRE + 8] = _cols(f("norm_ffn_pre")[l], 8)
        pc[l, :, PC_FPOST:PC_FPOST + 8] = _cols(f("norm_ffn_post")[l], 8)
        pc[l, :, PC_MU:PC_MU + 8] = _cols(f("rwkv_mu")[l], 8)
        for col, key in ((PC_W0, "rwkv_w0"), (PC_A0, "rwkv_a0"), (PC_KK, "rwkv_k_k"), (PC_KA, "rwkv_k_a"),
                         (PC_LNW, "rwkv_ln_w"), (PC_LNB, "rwkv_ln_b"), (PC_MNORM, "mlstm_norm")):
            pc[l, :, col:col + 2] = _cols(f(key)[l], 2)
        pc[l, :, PC_RK:PC_RK + 2] = _cols(f("rwkv_r_k")[l].reshape(256), 2)
        pc[l, :, PC_SINK:PC_SINK + 2] = _cols(np.repeat(f("swa_sinks")[l], 64), 2)
        cw = f("mlstm_conv_w")[l]
        for i in range(4):
            for j in range(4):
                pc[l, 0:64, PC_CW + i * 4 + j] = cw[j, i * 64:(i + 1) * 64]
            pc[l, 0:64, PC_CB + i] = f("mlstm_conv_b")[l][i * 64:(i + 1) * 64]
        pc[l, 0:4, PC_BI] = f("mlstm_b_i")[l]
        pc[l, 0:4, PC_BF] = f("mlstm_b_f")[l]
        pc[l, 0:4, PC_FOXB] = f("fox_b_f")[l]
        pc[l, 64:68, PC_FOXB] = f("fox_b_f")[l]
        fw = f("ffn_conv_w")[l]
        for j in range(3):
            pc[l, :, PC_FCW + j:PC_FCW + 66:3] = _cols(fw[j], NFC)
        pc[l, :, PC_FCB:PC_FCB + NFC] = _cols(f("ffn_conv_b")[l], NFC)
    dist, _ = _swa_dist()
    bk = _t5_bucket(np.clip(dist, 0, 127))
    swab = f("rel_bias")[bk]
    swab = np.ascontiguousarray(swab.transpose(0, 1, 3, 2)).reshape(128, 1024)
    cst = make_consts()
    x = f("x")
    maps = []
    for b in range(x.shape[0]):
        xT = np.ascontiguousarray(x[b].T.reshape(8, 128, x.shape[1]).transpose(1, 0, 2))
        maps.append({"xT": xT, "w_in_r": w_in_r, "w_out_r": w_out_r, "w_up_r": w_up_r, "w_dn_r": w_dn_r,
                     "lora": lora, "pc": pc, "cst": cst, "swab": swab})
    return maps


ARENA_SLOTS = 26


def build(nlayer=DEPTH, nblk=SEQ // TB, dbg=None, phases="rmsfon"):
    nc = bass.Bass("TRN2", target_bir_lowering=False)
    T = nblk * TB
    NT = T // 128
    L = DEPTH
    dr = lambda n, s, kind="ExternalInput": nc.dram_tensor(n, list(s), F32, kind=kind).ap()
    xT_d = dr("xT", [128, 8, SEQ])
    w_in_d = dr("w_in_r", [L, 128, W_IN_TOT])
    w_out_d = dr("w_out_r", [L, 8, 128, 1024])
    w_up_d = dr("w_up_r", [L, 2 * NFC, 128, 1024])
    w_dn_d = dr("w_dn_r", [L, 8, 128, NFC * 128])
    lora_d = dr("lora", [L, 128, 512])
    pc_d = dr("pc", [L, 128, NPC])
    cst_d = dr("cst", [128, NCST])
    swab_d = dr("swab", [128, 1024])
    out_d = dr("outT", [128, 8, SEQ], "ExternalOutput")
    dbg_d = dr("dbg", [128, 8, SEQ], "ExternalOutput") if dbg else None

    P = Prog(nc)
    RR = lambda ap: ap.bitcast(F32R) if (FAST_MM and ap.dtype == F32) else ap
    RR0 = RR
    P.make_banks()
    xT = P.sb("xT", 8 * T)
    kTh = P.sb("kTh", 2 * T)
    Vh = P.sb("Vh", NT * 256)
    negc = P.sb("negc", NT * 4)
    yT = P.sbs("yT", 8, dtype=BF16)
    foxR = P.sbs("foxR", 7)
    cst = P.sb("cst", C_SWAM)
    pc = P.sb("pc", NPC)
    dv = P.sb("dv", NDV)
    rstm = P.sb("rstm", 4)
    Srw = P.sb("Srw", 2 * 2 * 2 * 64)
    pc1 = P.sb("pc1", 8)
    CN = P.sb("CN", 2 * 128)
    crw = P.sb("crw", 8)
    cml = P.sb("cml", 12)
    cffn = P.sb("cffn", NFC * 2)
    ccar = P.sb("ccar", 2)
    swaKc = P.sb("swaKc", 2 * 128)
    swaVc = P.sb("swaVc", 128)
    hTr = P.sbs("hTr", 8, dtype=BF16)
    ebks = P.sb("ebks", 16)
    wbufs = [P.sb(f"wch{i}", 8 * 128, dtype=BF16) for i in range(6)]
    P.make_arena(ARENA_SLOTS)

    A = slice(0, 128)
    H0 = slice(0, 64)
    H1 = slice(64, 128)
    R4 = slice(0, 4)
    xT3 = xT.v(A, "p (c t) -> p c t", c=8)
    yT3 = yT.v(A, "p (c t) -> p c t", c=8)
    kTh3 = kTh.v(A, "p (j t) -> p j t", j=2)
    Vh3 = Vh.v(A, "p (n c) -> p n c", c=256)
    negc3 = negc.v(A, "p (n h) -> p n h", h=4)
    swaKc3 = swaKc.v(A, "p (j t) -> p j t", j=2)
    hTr3 = hTr.v(A, "p (c t) -> p c t", c=8)
    CN3 = CN.v(H0, "p (j c) -> p j c", j=2)
    S5 = Srw.v(H0, "p (j h b v) -> p j h b v", j=2, h=2, b=2)

    def C(off, n, p=A):
        return cst[p, off:off + n]

    ident = C(C_IDENT, 128)
    evac_i = [0]

    def copy(out, in_, reads, writes, eng=None):
        if eng is None:
            evac_i[0] += 1
            eng = "act" if evac_i[0] % 2 else "dve"
        return P.cp(out, in_, reads, writes, eng=eng)

    wb_i = [0]

    def wchunk():
        b = wbufs[wb_i[0] % 6]
        wb_i[0] += 1
        return b

    P.dma(RR0(cst[A, :]), cst_d[:, 0:C_SWAM], writes=[cst], eng="pool")
    for c in range(8):
        P.dma(xT3[:, c, :], xT_d[:, c, 0:T], writes=[xT])
    if phases != "rmsfon":
        P.ts(RR(yT[A, :]), cst[A, 0:4096 if C_SWAM >= 4096 else 2048].to_broadcast([128, 4096]) if False else xT[A, 0:4096], 0.0, ALU.mult, reads=[xT], writes=[yT])
    P.ar_top = 0

    def rms_rstd(src_fn, reads, eps=1e-6):
        ssb = P.bank(pin=True)
        rstd = P.alloc()
        sq = [P.alloc(), P.alloc()]
        for c in range(8):
            s = sq[c % 2]
            P.act(s[A, :], src_fn(c), AF.Square, reads=reads, writes=[s])
            P.mm(ssb, ssb[A, :], C(C_ONESD, 128), s[A, :], reads=[cst, s])
        P.ts(rstd[A, :], ssb[A, :], eps, ALU.add, reads=[ssb], writes=[rstd])
        ssb.pinned = False
        P.recip(rstd[A, :], rstd[A, :], reads=[rstd], writes=[rstd])
        P.act(rstd[A, :], rstd[A, :], AF.Sqrt, reads=[rstd], writes=[rstd])
        P.ar_top -= 2
        return rstd

    for l in range(nlayer):
        P.dma(pc[A, :], pc_d[l], writes=[pc])
        P.ts(dv[A, DV_OMKA:DV_OMKA + 2], pc[A, PC_KA:PC_KA + 2], -1.0, ALU.mult, 1.0, ALU.add, reads=[pc], writes=[dv])
        P.ts(dv[R4, DV_BI15:DV_BI15 + 2], pc[R4, PC_BI:PC_BI + 2], 1.0 / 15.0, ALU.mult, reads=[pc], writes=[dv])
        P.ts(dv[A, DV_NFOXB:DV_NFOXB + 1], pc[A, PC_FOXB:PC_FOXB + 1], -1.0, ALU.mult, reads=[pc], writes=[dv])
        P.act(dv[A, DV_ESINK:DV_ESINK + 2], pc[A, PC_SINK:PC_SINK + 2], AF.Exp, reads=[pc], writes=[dv])
        for b_ in (Srw, CN, crw, cml, cffn, ccar, swaKc, swaVc):
            P.memset(b_[A, :], 0.0, writes=[b_])

        for blk in range(nblk):
            t0 = blk * TB
            bs = slice(t0, t0 + TB)
            P.ar_top = 0
            rstd = rms_rstd(lambda c: xT3[:, c, bs], [xT])
            bk = P.bank()
            for tt_ in range(4):
                P.tr(bk, bk[A, tt_ * 128:(tt_ + 1) * 128], rstd[A, tt_ * 128:(tt_ + 1) * 128], ident, reads=[rstd, cst])
            P.cp(rstm[A, 0:4], bk.v(A, "p (n c) -> p n c", n=4)[:, :, 0], reads=[bk], writes=[rstm])
            MIX_BASE = P.ar_top

            def fill_hTr(gcol):
                for c in range(8):
                    gs = pc[A, gcol + c:gcol + c + 1]
                    if c % 2:
                        P.act(hTr3[:, c, :], xT3[:, c, bs], AF.Copy, scale=gs, reads=[xT, pc], writes=[hTr.sub(c * 512, 512)])
                    else:
                        P.ts(hTr3[:, c, :], xT3[:, c, bs], gs, ALU.mult, reads=[xT, pc], writes=[hTr.sub(c * 512, 512)], eng="dve")

            fill_hTr(PC_GPRE)
            gpre = pc[A, PC_GPRE:PC_GPRE + 8]

            def load_w(col0, M, dup=False):
                w = wchunk()
                w3 = w.v(A, "p (c m) -> p c m", c=8)
                off = W_IN_OFF[(col0, M)]
                if dup:
                    M = 128
                if M == 128:
                    P.dma(RR(w[A, :]), w_in_d[l, :, off:off + 1024], writes=[w], eng="pool")
                else:
                    P.dma(RR(w3[:, :, 0:M]), w_in_d[l, :, off:off + 8 * M].rearrange("p (c m) -> p c m", c=8), writes=[w], eng="pool")
                return w, w3, M

            def proj_fm(col0, M, dst, dup=False, scale=None, rows=None, pbase=0):
                w, w3, M = load_w(col0, M, dup)
                bk = P.bank()
                r = slice(pbase, pbase + M)
                for c in range(8):
                    P.mm(bk, bk[r, :], w3[:, c, 0:M], hTr3[:, c, :], reads=[w, hTr], r=(pbase == 0))
                if scale is None:
                    P.tt(dst, bk[r, :], rstd[r, :], ALU.mult, reads=[bk, rstd], writes=[rows])
                else:
                    P.stt(dst, bk[r, :], scale, rstd[r, :], ALU.mult, ALU.mult, reads=[bk, rstd], writes=[rows])

            def proj_tm(col0, ncols, dst_fn, wr):
                w, w3, _ = load_w(col0, ncols)
                for tt_ in range(4):
                    bk = P.bank()
                    for c in range(8):
                        P.mm(bk, bk[A, 0:ncols], hTr3[:, c, tt_ * 128:(tt_ + 1) * 128], w3[:, c, 0:ncols], reads=[w, hTr], r=True)
                    P.ts(dst_fn(tt_), bk[A, 0:ncols], rstm[A, tt_:tt_ + 1], ALU.mult, reads=[bk, rstm], writes=[wr])

            def shift_mix(z, ci, dst):
                d = P.alloc()
                P.tt(d[A, 1:512], z[A, 0:511], z[A, 1:512], ALU.subtract, reads=[z], writes=[d])
                P.tt(d[A, 0:1], crw[A, ci:ci + 1], z[A, 0:1], ALU.subtract, reads=[z, crw], writes=[d])
                P.cp(crw[A, ci:ci + 1], z[A, 511:512], reads=[z], writes=[crw], eng="dve")
                P.stt(dst[A, :], d[A, :], pc[A, PC_MU + ci:PC_MU + ci + 1], z[A, :], ALU.mult, ALU.add,
                      reads=[d, pc, z], writes=[dst])
                P.ar_top -= 1

            def rwkv():
                lora = P.alloc()
                P.dma(lora[A, :], lora_d[l], writes=[lora])
                swa_ = P.alloc()
                sg = P.alloc()
                ztmp = P.alloc()
                proj_fm(768, 128, ztmp[A, :], rows=ztmp)
                shift_mix(ztmp, 6, swa_)
                proj_fm(896, 128, ztmp[A, :], rows=ztmp)
                shift_mix(ztmp, 7, sg)
                P.ar_top -= 1
                P.act(swa_[H0, :], swa_[H0, :], AF.Tanh, reads=[swa_], writes=[swa_])
                P.act(sg[A, :], sg[A, :], AF.Sigmoid, reads=[sg], writes=[sg])
                if RW_STOP == 1:
                    return
                base_top = P.ar_top
                for hp in range(2):
                    P.ar_top = base_top
                    z = [P.alloc() for _ in range(3)]
                    s = [P.alloc() for _ in range(3)]
                    for i in range(3):
                        proj_fm(i * 256 + hp * 128, 128, z[i][A, :], rows=z[i])
                    for i in range(3):
                        shift_mix(z[i], i * 2 + hp, s[i])
                    sr, sk, sv = s
                    cw = slice(hp * 128, (hp + 1) * 128)
                    pcc = lambda c0: pc[A, c0 + hp:c0 + hp + 1]
                    bk = P.bank()
                    P.mm(bk, bk[A, :], lora[H0, cw], swa_[H0, :], reads=[lora, swa_])
                    lw = z[0]
                    P.act(lw[A, :], bk[A, :], AF.Sigmoid, bias=pcc(PC_W0), reads=[bk, pc], writes=[lw])
                    P.ts(lw[A, :], lw[A, :], -math.exp(-0.5), ALU.mult, reads=[lw], writes=[lw])
                    bk = P.bank()
                    P.mm(bk, bk[A, :], lora[H1, cw], swa_[H1, :], reads=[lora, swa_])
                    a = z[1]
                    P.act(a[A, :], bk[A, :], AF.Sigmoid, bias=pcc(PC_A0), reads=[bk, pc], writes=[a])
                    bk = P.bank()
                    P.mm(bk, bk[A, :], lora[A, 256 + hp * 128:256 + (hp + 1) * 128], sg[A, :], reads=[lora, sg])
                    g = z[2]
                    copy(g[A, :], bk[A, :], [bk], [g])
                    kk = P.alloc()
                    tmp = P.alloc()
                    P.ts(kk[A, :], sk[A, :], pcc(PC_KK), ALU.mult, reads=[sk, pc], writes=[kk])
                    P.tt(tmp[A, :], kk[A, :], kk[A, :], ALU.mult, reads=[kk], writes=[tmp])
                    bk = P.bank()
                    P.mm(bk, bk[A, :], C(C_BLK1, 128), tmp[A, :], reads=[cst, tmp])
                    P.act(tmp[A, :], bk[A, :], AF.Sqrt, reads=[bk], writes=[tmp])
                    P.ts(tmp[A, :], tmp[A, :], 1e-12, ALU.max, reads=[tmp], writes=[tmp])
                    P.recip(tmp[A, :], tmp[A, :], reads=[tmp], writes=[tmp])
                    P.tt(kk[A, :], kk[A, :], tmp[A, :], ALU.mult, reads=[kk, tmp], writes=[kk])
                    kmod = P.alloc()
                    P.ts(kmod[A, :], a[A, :], pcc(PC_KA), ALU.mult, dv[A, DV_OMKA + hp:DV_OMKA + hp + 1], ALU.add,
                         reads=[a, pc, dv], writes=[kmod])
                    P.tt(kmod[A, :], kmod[A, :], sk[A, :], ALU.mult, reads=[kmod, sk], writes=[kmod])
                    bonus = sk
                    P.stt(tmp[A, :], sr[A, :], pcc(PC_RK), kmod[A, :], ALU.mult, ALU.mult, reads=[sr, pc, kmod], writes=[tmp])
                    bk = P.bank()
                    P.mm(bk, bk[A, :], C(C_BLK1, 128), tmp[A, :], reads=[cst, tmp])
                    P.tt(bonus[A, :], bk[A, :], sv[A, :], ALU.mult, reads=[bk, sv], writes=[bonus])
                    bb = a
                    P.tt(bb[A, :], kk[A, :], a[A, :], ALU.mult, reads=[kk, a], writes=[bb])
                    cum = P.alloc()
                    ones = C(C_ALLONE, 64)
                    for c in range(8):
                        cs = slice(c * 64, (c + 1) * 64)
                        P.scan(cum[A, cs], ones, lw[A, cs], 0.0, ALU.mult, ALU.add, reads=[cst, lw], writes=[cum])
                    cumx = lw
                    P.tt(cumx[A, :], cum[A, :], lw[A, :], ALU.subtract, reads=[cum, lw], writes=[cumx])
                    Ep = P.alloc()
                    Em = P.alloc()
                    Ee = P.alloc()
                    P.act(Ep[A, :], cum[A, :], AF.Exp, reads=[cum], writes=[Ep])
                    P.act(Em[A, :], cum[A, :], AF.Exp, scale=-1.0, reads=[cum], writes=[Em])
                    P.act(cumx[A, :], cumx[A, :], AF.Exp, reads=[cumx], writes=[cumx])
                    for c in range(8):
                        cs = slice(c * 64, (c + 1) * 64)
                        P.act(Ee[A, cs], cum[A, cs], AF.Exp, bias=cum[A, c * 64 + 63:c * 64 + 64], scale=-1.0,
                              reads=[cum], writes=[Ee])
                    AR = P.alloc(1024)
                    AR3 = AR.v(A, "p (a t) -> p a t", a=2)
                    P.stt(AR3[:, 0, :], kk[A, :], -1.0, cumx[A, :], ALU.mult, ALU.mult, reads=[kk, cumx], writes=[AR])
                    P.tt(AR3[:, 1, :], sr[A, :], Ep[A, :], ALU.mult, reads=[sr, Ep], writes=[AR])
                    Bt, Kt, Bte, Kte = cum, tmp, kk, kmod
                    P.tt(Bt[A, :], bb[A, :], Em[A, :], ALU.mult, reads=[bb, Em], writes=[Bt])
                    P.tt(Kt[A, :], kmod[A, :], Em[A, :], ALU.mult, reads=[kmod, Em], writes=[Kt])
                    P.tt(Bte[A, :], bb[A, :], Ee[A, :], ALU.mult, reads=[bb, Ee], writes=[Bte])
                    P.tt(Kte[A, :], kmod[A, :], Ee[A, :], ALU.mult, reads=[kmod, Ee], writes=[Kte])
                    yr = Em
                    AR1 = Buf(z[0].t, z[0].off, 1024, z[0].k + z[1].k)
                    AR13 = AR1.v(A, "p (a t) -> p a t", a=2)
                    Bt1, Kt1 = sr, P.alloc()
                    idn1 = cst[H1, C_IDENT + 64:C_IDENT + 128]
                    for src, dst, wr in ((AR3[H1, 0, :], AR13[H0, 0, :], AR1), (AR3[H1, 1, :], AR13[H0, 1, :], AR1),
                                         (Bt[H1, :], Bt1[H0, :], Bt1), (Kt[H1, :], Kt1[H0, :], Kt1)):
                        bk = P.bank()
                        P.mm(bk, bk[H0, :], idn1, src, reads=[cst, AR, Bt, Kt])
                        copy(dst, bk[H0, :], [bk], [wr])
                    bk = P.bank()
                    P.mm(bk, bk[H0, 0:8], idn1, Ep.v(H1, "p (c t) -> p c t", c=8)[:, :, 63], reads=[cst, Ep])
                    copy(pc1[H0, 0:8], bk[H0, 0:8], [bk], [pc1])
                    ARh = (AR3, AR13)
                    Bth = (Bt, Bt1)
                    Kth = (Kt, Kt1)
                    ARb = (AR, AR1)
                    if RW_STOP == 2:
                        continue
                    NA = [P.alloc() for _ in range(2)]
                    nmslot = P.alloc()
                    NMp = [nmslot.sub(0, 256), nmslot.sub(256, 256)]
                    gslot = P.alloc()
                    Gp = [gslot.sub(0, 128), gslot.sub(128, 128)]
                    RU = [gslot.sub(256, 256), Ee.sub(0, 256)]
                    TM = [P.alloc() for _ in range(2)]
                    for c in range(8):
                        if RW_STOP == 6:
                            continue
                        cs = slice(c * 64, (c + 1) * 64)
                        na = NA[c % 2]
                        na5 = na.v(H0, "p (h b a t) -> p h b a t", h=2, b=2, a=2)
                        bk = P.bank()
                        bk5 = bk.v(H0, "p (h b a t) -> p h b a t", h=2, b=2, a=2)
                        bkm = P.bank()
                        bkm3 = bkm.v(H0, "p (x h t) -> p x h t", x=4, h=2)
                        for h2 in range(1 if RW_STOP == 7 else 2):
                            ph = slice(h2 * 64, h2 * 64 + 64)
                            for a_ in range(2):
                                P.mm(bk, bk5[:, h2, 0, a_, :], Bth[h2][H0, cs], ARh[h2][H0, a_, cs], reads=[Bth[h2], ARb[h2]])
                                P.mm(bk, bk5[:, h2, 1, a_, :], Kth[h2][H0, cs], ARh[h2][H0, a_, cs], reads=[Kth[h2], ARb[h2]])
                            P.mm(bkm, bkm3[:, 0, h2, :], ARh[h2][H0, 0, cs], Bth[h2][H0, cs], reads=[Bth[h2], ARb[h2]])
                        P.tt(na[H0, :], bk[H0, :], C(C_MA, 512, H0), ALU.mult, reads=[bk, cst], writes=[na])
                        if RW_STOP in (3, 7):
                            continue
                        nm = NMp[0]
                        nm4 = nm.v(H0, "p (x h t) -> p x h t", x=2, h=2)
                        P.cp(nm4[:, 0, :, :], na5[:, :, 0, 0, :], reads=[na], writes=[nm], eng="dve")
                        P.tt(nm4[:, 1, :, :], bkm3[:, 0, :, :], C(C_MB, 128, H0).rearrange("p (h t) -> p h t", h=2),
                             ALU.mult, reads=[bkm, cst], writes=[nm])
                        G3_0 = Gp[0].v(H0, "p (h t) -> p h t", h=2)
                        P.tt(G3_0, na5[:, :, 0, 0, :], C(C_IDG, 128, H0).rearrange("p (h t) -> p h t", h=2), ALU.add,
                             reads=[na, cst], writes=[Gp[0]])
                        cur = 0
                        for lev in range(5):
                            nmc = NMp[cur]
                            nmc4 = nmc.v(H0, "p (x h t) -> p x h t", x=2, h=2)
                            nmn = NMp[1 - cur]
                            nmn4 = nmn.v(H0, "p (x h t) -> p x h t", x=2, h=2)
                            Gc = Gp[cur]
                            Gc3 = Gc.v(H0, "p (h t) -> p h t", h=2)
                            Gn = Gp[1 - cur]
                            bkp = P.bank()
                            bkp4 = bkp.v(H0, "p (x h t) -> p x h t", x=4, h=2)
                            for h2 in range(2):
                                P.mm(bkp, bkp4[:, 0, h2, :], nmc4[:, 1, h2, :], nmc4[:, 0, h2, :], reads=[nmc])
                                P.mm(bkp, bkp4[:, 1, h2, :], nmc4[:, 0, h2, :], nmc4[:, 1, h2, :], reads=[nmc])
                            copy(nmn[H0, :], bkp[H0, 0:256], [bkp], [nmn])
                            bkg = P.bank()
                            for h2 in range(2):
                                P.mm(bkg, bkg[H0, h2 * 64:(h2 + 1) * 64], nmn4[:, 1, h2, :], Gc3[:, h2, :], reads=[nmn, Gc])
                            P.tt(Gn[H0, :], bkg[H0, 0:128], Gc[H0, :], ALU.add, reads=[bkg, Gc], writes=[Gn])
                            cur = 1 - cur
                        Gf = Gp[cur]
                        Gf3 = Gf.v(H0, "p (h t) -> p h t", h=2)
                        if RW_STOP == 4:
                            continue
                        tm = TM[c % 2]
                        bkt = P.bank()
                        P.tr(bkt, bkt[H0, 0:128], Bte[A, cs], ident, reads=[Bte, cst])
                        P.tr(bkt, bkt[H0, 128:256], Kte[A, cs], ident, reads=[Kte, cst])
                        P.tr(bkt, bkt[H0, 256:384], sv[A, cs], ident, reads=[sv, cst])
                        copy(tm[H0, 0:384], bkt[H0, 0:384], [bkt], [tm])
                        if RW_STOP == 5:
                            continue
                        ru = RU[c % 2]
                        sb_old = c % 2
                        sb_new = 1 - sb_old
                        bkr = P.bank()
                        for h2 in range(2):
                            ph = slice(h2 * 64, h2 * 64 + 64)
                            o = bkr[H0, h2 * 64:(h2 + 1) * 64]
                            P.mm(bkr, o, ARh[h2][H0, 0, cs], S5[:, hp, h2, sb_old, :], reads=[ARb[h2], Srw])
                            P.mm(bkr, o, na5[:, h2, 1, 0, :], tm[H0, 256 + h2 * 64:256 + (h2 + 1) * 64], reads=[na, tm])
                        copy(ru[H0, 0:128], bkr[H0, 0:128], [bkr], [ru], eng="act")
                        bku = P.bank()
                        for h2 in range(2):
                            P.mm(bku, bku[H0, h2 * 64:(h2 + 1) * 64], Gf3[:, h2, :], ru[H0, h2 * 64:(h2 + 1) * 64], reads=[Gf, ru])
                        copy(ru[H0, 128:256], bku[H0, 0:128], [bku], [ru], eng="dve")
                        bky = P.bank()
                        for h2 in range(2):
                            ph = slice(h2 * 64, h2 * 64 + 64)
                            o = bky[ph, 0:64]
                            P.mm(bky, o, S5[:, hp, h2, sb_old, :], ARh[h2][H0, 1, cs], reads=[ARb[h2], Srw])
                            P.mm(bky, o, ru[H0, 128 + h2 * 64:128 + (h2 + 1) * 64], na5[:, h2, 0, 1, :], reads=[ru, na])
                            P.mm(bky, o, tm[H0, 256 + h2 * 64:256 + (h2 + 1) * 64], na5[:, h2, 1, 1, :], reads=[tm, na])
                        copy(yr[A, cs], bky[A, 0:64], [bky], [yr], eng="act")
                        bks = P.bank()
                        for h2 in range(2):
                            o = bks[H0, h2 * 64:(h2 + 1) * 64]
                            P.mm(bks, o, tm[H0, h2 * 64:(h2 + 1) * 64], ru[H0, 128 + h2 * 64:128 + (h2 + 1) * 64], reads=[tm, ru])
                            P.mm(bks, o, tm[H0, 128 + h2 * 64:128 + (h2 + 1) * 64], tm[H0, 256 + h2 * 64:256 + (h2 + 1) * 64], reads=[tm])
                        for h2 in range(2):
                            pcs = Ep[H0, c * 64 + 63:c * 64 + 64] if h2 == 0 else pc1[H0, c:c + 1]
                            P.stt(S5[:, hp, h2, sb_new, :], S5[:, hp, h2, sb_old, :], pcs, bks[H0, h2 * 64:(h2 + 1) * 64],
                                  ALU.mult, ALU.add, reads=[Srw, Ep, pc1, bks], writes=[Srw])
                    t1 = Ep
                    bk = P.bank()
                    P.mm(bk, bk[A, :], C(C_BLKM, 128), yr[A, :], reads=[cst, yr])
                    P.tt(yr[A, :], yr[A, :], bk[A, :], ALU.subtract, reads=[yr, bk], writes=[yr])
                    P.tt(t1[A, :], yr[A, :], yr[A, :], ALU.mult, reads=[yr], writes=[t1])
                    bk = P.bank()
                    P.mm(bk, bk[A, :], C(C_BLKM, 128), t1[A, :], reads=[cst, t1])
                    P.ts(t1[A, :], bk[A, :], 64e-5, ALU.add, reads=[bk], writes=[t1])
                    P.recip(t1[A, :], t1[A, :], reads=[t1], writes=[t1])
                    P.act(t1[A, :], t1[A, :], AF.Sqrt, reads=[t1], writes=[t1])
                    P.tt(yr[A, :], yr[A, :], t1[A, :], ALU.mult, reads=[yr, t1], writes=[yr])
                    P.ts(yr[A, :], yr[A, :], pcc(PC_LNW), ALU.mult, pcc(PC_LNB), ALU.add, reads=[yr, pc], writes=[yr])
                    P.tt(yr[A, :], yr[A, :], bonus[A, :], ALU.add, reads=[yr, bonus], writes=[yr])
                    P.tt(RR(yT3[:, hp, :]), yr[A, :], g[A, :], ALU.mult, reads=[yr, g], writes=[yT.sub(hp * 512, 512)])

            P.ar_top = MIX_BASE
            if "r" in phases:
                rwkv()

            def mlstm():
                qk = [P.alloc() for _ in range(4)]
                vones = P.alloc()
                vo3 = vones.v(H0, "p (h c) -> p h c", h=4)
                P.memset(vones[H0, :], 1.0, writes=[vones])
                vT = [P.alloc(), P.alloc()]
                oT = [P.alloc(), P.alloc()]
                gi, gf, bcum, ge = (P.alloc() for _ in range(4))
                top = P.ar_top
                for i in range(4):
                    P.ar_top = top
                    uext = P.alloc(515)
                    acc = P.alloc()
                    proj_fm(1024 + i * 64, 64, uext[H0, 3:515], rows=uext)
                    P.cp(uext[H0, 0:3], cml[H0, i * 3:(i + 1) * 3], reads=[cml], writes=[uext], eng="dve")
                    wc = lambda j: pc[H0, PC_CW + i * 4 + j:PC_CW + i * 4 + j + 1]
                    P.ts(acc[H0, :], uext[H0, 0:512], wc(0), ALU.mult, reads=[uext, pc], writes=[acc])
                    for j in range(1, 4):
                        P.stt(acc[H0, :], uext[H0, j:j + 512], wc(j), acc[H0, :], ALU.mult, ALU.add, reads=[uext, pc, acc], writes=[acc])
                    P.cp(cml[H0, i * 3:(i + 1) * 3], uext[H0, 512:515], reads=[uext], writes=[cml], eng="dve")
                    P.act(qk[i][H0, :], acc[H0, :], AF.Silu, bias=pc[H0, PC_CB + i:PC_CB + i + 1], reads=[acc, pc], writes=[qk[i]])
                    if i < 2:
                        P.ts(qk[i][H0, :], qk[i][H0, :], 32.0 ** -0.5, ALU.mult, reads=[qk[i]], writes=[qk[i]])
                P.ar_top = top
                for hp in range(2):
                    proj_fm(1280 + hp * 128, 128, vT[hp][A, :], rows=vT[hp])
                    proj_fm(1536 + hp * 128, 128, oT[hp][A, :], rows=oT[hp])
                    P.act(oT[hp][A, :], oT[hp][A, :], AF.Sigmoid, reads=[oT[hp]], writes=[oT[hp]])
                proj_fm(1792, 4, gi[R4, :], rows=gi)
                proj_fm(1796, 4, gf[R4, :], rows=gf)
                P.act(gi[R4, :], gi[R4, :], AF.Tanh, bias=dv[R4, DV_BI15:DV_BI15 + 1], scale=1.0 / 15.0, reads=[gi, dv], writes=[gi])
                P.act(gf[R4, :], gf[R4, :], AF.Tanh, bias=dv[R4, DV_BF15:DV_BF15 + 1], scale=1.0 / 15.0, reads=[gf, dv], writes=[gf])
                P.ts(gi[R4, :], gi[R4, :], 15.0, ALU.mult, reads=[gi], writes=[gi])
                P.act(gf[R4, :], gf[R4, :], AF.Exp, scale=-15.0, reads=[gf], writes=[gf])
                P.act(gf[R4, :], gf[R4, :], AF.Ln, bias=1.0, reads=[gf], writes=[gf])
                ones4 = C(C_ALLONE, 64, R4)
                for c in range(8):
                    cs = slice(c * 64, (c + 1) * 64)
                    P.scan(bcum[R4, cs], ones4, gf[R4, cs], 0.0, ALU.mult, ALU.subtract, reads=[cst, gf], writes=[bcum])
                P.tt(gi[R4, :], gi[R4, :], bcum[R4, :], ALU.subtract, reads=[gi, bcum], writes=[gi])
                P.act(gi[R4, :], gi[R4, :], AF.Exp, reads=[gi], writes=[gi])
                P.act(bcum[R4, :], bcum[R4, :], AF.Exp, reads=[bcum], writes=[bcum])
                for c in range(8):
                    cs = slice(c * 64, (c + 1) * 64)
                    P.ts(ge[R4, cs], gi[R4, cs], bcum[R4, c * 64 + 63:c * 64 + 64], ALU.mult, reads=[gi, bcum], writes=[ge])
                kp = [P.alloc(), P.alloc()]
                kpe = [P.alloc(), P.alloc()]
                ebbc = [P.alloc(), P.alloc()]
                for j in range(2):
                    selk = C(C_SELK + j * 64, 64, R4)
                    bk = P.bank()
                    P.mm(bk, bk[H0, :], selk, gi[R4, :], reads=[cst, gi])
                    P.tt(kp[j][H0, :], qk[2 + j][H0, :], bk[H0, :], ALU.mult, reads=[qk[2 + j], bk], writes=[kp[j]])
                    bk = P.bank()
                    P.mm(bk, bk[H0, :], selk, ge[R4, :], reads=[cst, ge])
                    P.tt(kpe[j][H0, :], qk[2 + j][H0, :], bk[H0, :], ALU.mult, reads=[qk[2 + j], bk], writes=[kpe[j]])
                    bk = P.bank()
                    P.mm(bk, bk[H0, :], selk, bcum[R4, :], reads=[cst, bcum])
                    P.cp(ebks[H0, j * 8:(j + 1) * 8], bk.v(H0, "p (c t) -> p c t", c=8)[:, :, 63], reads=[bk], writes=[ebks])
                    bk = P.bank()
                    P.mm(bk, bk[A, :], C(C_SELV + j * 128, 128, R4), bcum[R4, :], reads=[cst, bcum])
                    copy(ebbc[j][A, :], bk[A, :], [bk], [ebbc[j]])
                NTs = P.alloc(1024)
                DNs = P.alloc(1024)
                NT3 = NTs.v(A, "p (j t) -> p j t", j=2)
                DN3 = DNs.v(A, "p (j t) -> p j t", j=2)
                sm = P.alloc()
                qmc = P.alloc(256)
                for c in range(8):
                    cs = slice(c * 64, (c + 1) * 64)
                    bkt = P.bank()
                    P.tr(bkt, bkt[H0, 0:128], vT[0][A, cs], ident, reads=[vT[0], cst])
                    P.tr(bkt, bkt[H0, 128:256], vT[1][A, cs], ident, reads=[vT[1], cst])
                    copy(vo3[:, :, 0:64], bkt.v(H0, "p (h c) -> p h c", h=8)[:, 0:4, :], [bkt], [vones])
                    bkt2 = P.bank()
                    P.tr(bkt2, bkt2[H0, 0:64], kpe[0][H0, cs], C(C_IDENT, 64, H0), reads=[kpe[0], cst])
                    P.tr(bkt2, bkt2[H0, 64:128], kpe[1][H0, cs], C(C_IDENT, 64, H0), reads=[kpe[1], cst])
                    copy(sm[H0, 256:384], bkt2[H0, 0:128], [bkt2], [sm])
                    for h in range(4):
                        P.ts(qmc[H0, h * 64:(h + 1) * 64], qk[h // 2][H0, cs], C(C_HM + h % 2, 1, H0), ALU.mult,
                             reads=[qk[h // 2], cst], writes=[qmc])
                    bka = P.bank()
                    for h in range(4):
                        j = h // 2
                        P.mm(bka, bka[H0, h * 64:(h + 1) * 64], kp[j][H0, cs], qmc[H0, h * 64:(h + 1) * 64], reads=[kp[j], qmc])
                    P.tt(sm[H0, 0:256], bka[H0, 0:256], C(C_MM, 256, H0), ALU.mult, reads=[bka, cst], writes=[sm])
                    bn = P.bank()
                    bd = P.bank()
                    for h in range(4):
                        j = h // 2
                        pq = slice((h % 2) * 32, (h % 2) * 32 + 32)
                        ph = slice((h % 2) * 64, (h % 2) * 64 + 64)
                        at = sm[H0, h * 64:(h + 1) * 64]
                        qm = qmc[H0, h * 64:(h + 1) * 64]
                        P.mm(bn, bn[ph, j * 64:(j + 1) * 64], vo3[:, h, 0:64], at, reads=[vones, sm])
                        P.mm(bn, bn[ph, j * 64:(j + 1) * 64], CN3[:, j, 0:64], qm, reads=[CN, qmc])
                        P.mm(bd, bd[ph, j * 64:(j + 1) * 64], vo3[:, h, 64:128], at, reads=[vones, sm])
                        P.mm(bd, bd[ph, j * 64:(j + 1) * 64], CN3[:, j, 64:128], qm, reads=[CN, qmc])
                    copy(NT3[:, :, cs], bn.v(A, "p (j t) -> p j t", j=8)[:, 0:2, :], [bn], [NTs], eng="act")
                    copy(DN3[:, :, cs], bd.v(A, "p (j t) -> p j t", j=8)[:, 0:2, :], [bd], [DNs], eng="act")
                    bs_ = P.bank()
                    for h in range(4):
                        j = h // 2
                        P.mm(bs_, bs_[H0, h * 128:(h + 1) * 128], sm[H0, 256 + j * 64:256 + (j + 1) * 64], vo3[:, h, :], reads=[sm, vones])
                    for j in range(2):
                        P.ts(CN3[:, j, :], CN3[:, j, :], ebks[H0, j * 8 + c:j * 8 + c + 1], ALU.mult, reads=[CN, ebks], writes=[CN])
                        for h2 in range(2):
                            h = 2 * j + h2
                            P.stt(CN3[:, j, :], bs_[H0, h * 128:(h + 1) * 128], C(C_HM + h2, 1, H0), CN3[:, j, :],
                                  ALU.mult, ALU.add, reads=[CN, bs_, cst], writes=[CN])
                for j in range(2):
                    d1 = DNs.sub(j * 512, 512)
                    n1 = NTs.sub(j * 512, 512)
                    P.tt(d1[A, :], d1[A, :], ebbc[j][A, :], ALU.mult, reads=[d1, ebbc[j]], writes=[d1])
                    P.stt(d1[A, :], d1[A, :], -1.0, d1[A, :], ALU.mult, ALU.max, reads=[d1], writes=[d1])
                    P.ts(d1[A, :], d1[A, :], 1.0, ALU.max, reads=[d1], writes=[d1])
                    P.recip(d1[A, :], d1[A, :], reads=[d1], writes=[d1])
                    P.tt(n1[A, :], n1[A, :], ebbc[j][A, :], ALU.mult, reads=[n1, ebbc[j]], writes=[n1])
                    P.tt(n1[A, :], n1[A, :], d1[A, :], ALU.mult, reads=[n1, d1], writes=[n1])
                    P.tt(d1[A, :], n1[A, :], n1[A, :], ALU.mult, reads=[n1], writes=[d1])
                    bk = P.bank()
                    P.mm(bk, bk[A, :], C(C_BLKM, 128), d1[A, :], reads=[cst, d1])
                    P.ts(d1[A, :], bk[A, :], 1e-6, ALU.add, reads=[bk], writes=[d1])
                    P.recip(d1[A, :], d1[A, :], reads=[d1], writes=[d1])
                    P.act(d1[A, :], d1[A, :], AF.Sqrt, reads=[d1], writes=[d1])
                    P.tt(n1[A, :], n1[A, :], d1[A, :], ALU.mult, reads=[n1, d1], writes=[n1])
                    P.stt(RR(yT3[:, 2 + j, :]), n1[A, :], pc[A, PC_MNORM + j:PC_MNORM + j + 1], oT[j][A, :], ALU.mult, ALU.mult,
                          reads=[n1, pc, oT[j]], writes=[yT.sub((2 + j) * 512, 512)])

            P.ar_top = MIX_BASE
            if "m" in phases:
                mlstm()

            def swa():
                swaB = P.alloc(1024)
                mtmp = P.alloc(1024)
                P.dma(swaB[A, :], swab_d, writes=[swaB])
                P.dma(mtmp[A, :], cst_d[:, C_SWAM:C_SWAM + 1024], writes=[mtmp])
                P.tt(swaB[A, :], swaB[A, :], mtmp[A, :], ALU.add, reads=[swaB, mtmp], writes=[swaB])
                P.ar_top -= 2
                swaB4 = swaB.v(A, "p (c h q) -> p c h q", c=2, h=4)
                swaK = P.alloc(1280)
                swaV = P.alloc(640)
                swaK3 = swaK.v(A, "p (j t) -> p j t", j=2)
                swaV3 = swaV.v(A, "p (n c) -> p n c", n=5)
                P.cp(swaK3[:, :, 0:128], swaKc3[:, :, :], reads=[swaKc], writes=[swaK], eng="dve")
                P.cp(swaV3[:, 0, :], swaVc[A, :], reads=[swaVc], writes=[swaV], eng="dve")
                qS = [P.alloc(), P.alloc()]
                for j in range(2):
                    proj_fm(1800 + j * 128, 128, qS[j][A, :], scale=0.125, rows=qS[j])
                    proj_fm(2056 + j * 64, 64, swaK3[:, j, 128:640], dup=True, rows=swaK)
                proj_tm(2184, 128, lambda tt_: swaV3[:, 1 + tt_, :], swaV)
                ETs = [P.alloc(1024), P.alloc(1024)]
                tmp = P.alloc()
                for i in range(4):
                    gi_ = blk * 4 + i
                    qc = slice(i * 128, (i + 1) * 128)
                    prev = slice(i * 128, (i + 1) * 128)
                    cur_ = slice((i + 1) * 128, (i + 2) * 128)
                    ET = ETs[i % 2]
                    bp = P.bank() if gi_ > 0 else None
                    bc = P.bank()
                    for hq in range(4):
                        j = hq // 2
                        ph = slice((hq % 2) * 64, (hq % 2) * 64 + 64)
                        o = slice(hq * 128, (hq + 1) * 128)
                        if gi_ > 0:
                            P.mm(bp, bp[A, o], swaK3[ph, j, prev], qS[j][ph, qc], reads=[swaK, qS[j]])
                            P.mm(bp, bp[A, o], ident, swaB4[:, 0, hq, :], reads=[cst, swaB])
                        P.mm(bc, bc[A, o], swaK3[ph, j, cur_], qS[j][ph, qc], reads=[swaK, qS[j]])
                        P.mm(bc, bc[A, o], ident, swaB4[:, 1, hq, :], reads=[cst, swaB])
                    if gi_ > 0:
                        P.act(ET[A, 0:512], bp[A, :], AF.Exp, reads=[bp], writes=[ET])
                    P.act(ET[A, 512:1024], bc[A, :], AF.Exp, reads=[bc], writes=[ET])
                    bo = P.bank()
                    bd = P.bank()
                    for hq in range(4):
                        j = hq // 2
                        ph = slice((hq % 2) * 64, (hq % 2) * 64 + 64)
                        o = slice(j * 128, (j + 1) * 128)
                        e = slice(hq * 128, (hq + 1) * 128)
                        if gi_ > 0:
                            P.mm(bo, bo[ph, o], swaV3[:, i, j * 64:(j + 1) * 64], ET[A, e.start:e.stop], reads=[swaV, ET])
                            P.mm(bd, bd[ph, o], C(C_ALLONE, 64), ET[A, e.start:e.stop], reads=[cst, ET])
                        P.mm(bo, bo[ph, o], swaV3[:, i + 1, j * 64:(j + 1) * 64], ET[A, 512 + e.start:512 + e.stop], reads=[swaV, ET])
                        P.mm(bd, bd[ph, o], C(C_ALLONE, 64), ET[A, 512 + e.start:512 + e.stop], reads=[cst, ET])
                    for j in range(2):
                        P.ts(tmp[A, j * 128:(j + 1) * 128], bd[A, j * 128:(j + 1) * 128], dv[A, DV_ESINK + j:DV_ESINK + j + 1], ALU.add,
                             reads=[bd, dv], writes=[tmp])
                    P.recip(tmp[A, 0:256], tmp[A, 0:256], reads=[tmp], writes=[tmp])
                    P.tt(RR(yT3[:, 4:6, qc]), bo.v(A, "p (j q) -> p j q", j=4)[:, 0:2, :], tmp.v(A, "p (j q) -> p j q", j=4)[:, 0:2, :],
                         ALU.mult, reads=[bo, tmp], writes=[yT.sub(4 * 512, 1024)])
                P.cp(swaKc3[:, :, :], swaK3[:, :, 512:640], reads=[swaK], writes=[swaKc], eng="dve")
                P.cp(swaVc[A, :], swaV3[:, 4, :], reads=[swaV], writes=[swaVc], eng="dve")

            P.ar_top = MIX_BASE
            if "s" in phases:
                swa()

            def fox():
                qtmp = [P.alloc(), P.alloc()]
                for j in range(2):
                    proj_fm(2312 + j * 128, 128, qtmp[j][A, :], scale=0.125, rows=qtmp[j])
                    proj_fm(2568 + j * 128, 128, RR(kTh3[:, j, bs]), rows=kTh)
                for half in range(2):
                    proj_tm(2824 + half * 128, 128, lambda tt_: RR(Vh3[:, blk * 4 + tt_, half * 128:(half + 1) * 128]), Vh)
                fr = P.alloc()
                crow = P.alloc()
                vtmp = P.alloc()
                chi = foxR.sub(2 * 512, 512)
                clo = foxR.sub(3 * 512, 512)
                qF = [foxR.sub(0, 512), foxR.sub(512, 512)]
                for R_ in (slice(0, 4), slice(64, 68)):
                    proj_fm(3080, 4, fr[R_, :], rows=fr, pbase=R_.start)
                for R_ in (slice(0, 4), slice(64, 68)):
                    P.act(fr[R_, :], fr[R_, :], AF.Exp, bias=dv[R_, DV_NFOXB:DV_NFOXB + 1], scale=-1.0, reads=[fr, dv], writes=[fr])
                    P.act(fr[R_, :], fr[R_, :], AF.Ln, bias=1.0, reads=[fr], writes=[fr])
                    for sg_ in range(4):
                        seg = slice(sg_ * 128, (sg_ + 1) * 128)
                        init = ccar[R_, 0:1] if sg_ == 0 else crow[R_, sg_ * 128 - 1:sg_ * 128]
                        P.scan(crow[R_, seg], C(C_ALLONE, 128, R_), fr[R_, seg], init, ALU.mult, ALU.subtract,
                               reads=[fr, ccar, cst, crow], writes=[crow])
                    P.cp(ccar[R_, 0:1], crow[R_, 511:512], reads=[crow], writes=[ccar], eng="dve")
                    P.ts(fr[R_, :], crow[R_, :], 4097.0, ALU.mult, reads=[crow], writes=[fr])
                    P.tt(vtmp[R_, :], fr[R_, :], crow[R_, :], ALU.subtract, reads=[fr, crow], writes=[vtmp])
                    P.tt(RR(chi[R_, :]), fr[R_, :], vtmp[R_, :], ALU.subtract, reads=[fr, vtmp], writes=[chi])
                    P.tt(RR(clo[R_, :]), crow[R_, :], chi[R_, :], ALU.subtract, reads=[crow, chi], writes=[clo])
                bk = P.bank()
                for tt_ in range(4):
                    P.mm(bk, bk[A, tt_ * 4:(tt_ + 1) * 4], crow[R4, tt_ * 128:(tt_ + 1) * 128], C(C_IDENT, 4, R4), reads=[crow, cst])
                P.ts(negc3[:, blk * 4:(blk + 1) * 4, :], bk.v(A, "p (n h) -> p n h", h=4)[:, 0:4, :], -1.0, ALU.mult,
                     reads=[bk], writes=[negc])
                for j in range(2):
                    P.cp(RR(qF[j][A, :]), qtmp[j][A, :], reads=[qtmp[j]], writes=[qF[j]], eng=("act", "dve")[j])
                ETs = [foxR.sub((4 + i_) * 512, 512) for i_ in range(3)]
                rec = P.alloc()
                ei = 0
                for j in range(2):
                    for h2 in range(2):
                        h = 2 * j + h2
                        ph = slice(h2 * 64, h2 * 64 + 64)
                        bo = P.bank(pin=True)
                        bd = P.bank(pin=True)
                        for kb in range(blk * 4 + 4):
                            q0 = max(0, kb - blk * 4) * 128
                            nq = 512 - q0
                            bs_ = P.bank()
                            P.mm(bs_, bs_[A, 0:nq], kTh3[ph, j, kb * 128:(kb + 1) * 128], qF[j][ph, q0:512], reads=[kTh, qF[j]], r=True)
                            Rh = slice(h2 * 64, h2 * 64 + 4)
                            P.mm(bs_, bs_[A, 0:nq], C(C_SELF + h * 128, 128, Rh), chi[Rh, q0:512], reads=[cst, chi], r=True)
                            P.mm(bs_, bs_[A, 0:nq], C(C_SELF + h * 128, 128, Rh), clo[Rh, q0:512], reads=[cst, clo], r=True)
                            et = ETs[ei % 3]
                            ei += 1
                            P.act(RR(et[A, 0:nq]), bs_[A, 0:nq], AF.Exp, bias=negc3[:, kb, h:h + 1], reads=[bs_, negc], writes=[et])
                            if kb >= blk * 4:
                                P.tt(RR(et[A, 0:128]), et[A, 0:128], C(C_TRI, 128), ALU.mult, reads=[et, cst], writes=[et], eng="dve")
                            P.mm(bo, bo[A, q0:512], Vh3[:, kb, j * 128:(j + 1) * 128], et[A, 0:nq], reads=[Vh, et], r=True)
                            P.mm(bd, bd[A, q0:512], C(C_ALLONE, 128), et[A, 0:nq], reads=[cst, et], r=True)
                        P.recip(rec[ph, :], bd[ph, :], reads=[bd], writes=[rec])
                        P.tt(RR(yT3[ph, 6 + j, :]), bo[ph, :], rec[ph, :], ALU.mult, reads=[bo, rec], writes=[yT.sub((6 + j) * 512, 512)])
                        bo.pinned = False
                        bd.pinned = False

            P.ar_top = MIX_BASE
            if "f" in phases:
                fox()

            if dbg == ("yT", l):
                for c in range(8):
                    P.dma(dbg_d[:, c, bs], yT3[:, c, :], reads=[yT], eng="pool")

            def post_residual(oT3, oT, gcol):
                rs = rms_rstd(lambda c: oT3[:, c, :], [oT])
                tmps = [P.alloc(), P.alloc()]
                for m in range(8):
                    t_ = tmps[m % 2]
                    P.stt(t_[A, :], oT3[:, m, :], pc[A, gcol + m:gcol + m + 1], rs[A, :], ALU.mult, ALU.mult,
                          reads=[oT, pc, rs], writes=[t_])
                    P.tt(xT3[:, m, bs], xT3[:, m, bs], t_[A, :], ALU.add, reads=[xT, t_], writes=[xT])

            if "o" not in phases:
                continue
            P.ar_top = 0
            oT = P.alloc(4096)
            oT3 = oT.v(A, "p (c t) -> p c t", c=8)
            for m in range(8):
                w = wchunk()
                w3 = w.v(A, "p (c m) -> p c m", c=8)
                P.dma(RR(w[A, :]), w_out_d[l, m], writes=[w], eng="pool")
                bk = P.bank()
                for j in range(8):
                    P.mm(bk, bk[A, :], w3[:, j, :], yT3[:, j, :], reads=[w, yT], r=True)
                copy(oT3[:, m, :], bk[A, :], [bk], [oT.sub(m * 512, 512)])
            post_residual(oT3, oT, PC_GPOST)

            if dbg == ("x1", l):
                for c in range(8):
                    P.dma(dbg_d[:, c, bs], xT3[:, c, bs], reads=[xT])

            if "n" not in phases:
                continue
            P.ar_top = 0
            rs3 = rms_rstd(lambda c: xT3[:, c, bs], [xT])
            fill_hTr(PC_FPRE)
            fT = yT
            fT3 = yT3
            oT = P.alloc(4096)
            oT3 = oT.v(A, "p (c t) -> p c t", c=8)
            gbuf = P.alloc(514)
            acc = P.alloc()
            gfp = pc[A, PC_FPRE:PC_FPRE + 8]
            for (c_lo, c_hi) in ((0, 8), (8, 16), (16, 22)):
                ng = c_hi - c_lo
                for cc in range(ng):
                    c = c_lo + cc
                    ws = []
                    for c2 in (c, NFC + c):
                        w = wchunk()
                        w3 = w.v(A, "p (c m) -> p c m", c=8)
                        P.dma(RR(w[A, :]), w_up_d[l, c2], writes=[w], eng="pool")
                        ws.append((w, w3))
                    bg = P.bank()
                    for k in range(8):
                        P.mm(bg, bg[A, :], ws[0][1][:, k, :], hTr3[:, k, :], reads=[ws[0][0], hTr], r=True)
                    bu = P.bank()
                    for k in range(8):
                        P.mm(bu, bu[A, :], ws[1][1][:, k, :], hTr3[:, k, :], reads=[ws[1][0], hTr], r=True)
                    P.tt(gbuf[A, 2:514], bg[A, :], rs3[A, :], ALU.mult, reads=[bg, rs3], writes=[gbuf])
                    P.cp(gbuf[A, 0:2], cffn[A, c * 2:c * 2 + 2], reads=[cffn], writes=[gbuf], eng="dve")
                    fw = lambda j: pc[A, PC_FCW + c * 3 + j:PC_FCW + c * 3 + j + 1]
                    P.ts(acc[A, :], gbuf[A, 0:512], fw(0), ALU.mult, reads=[gbuf, pc], writes=[acc])
                    P.stt(acc[A, :], gbuf[A, 1:513], fw(1), acc[A, :], ALU.mult, ALU.add, reads=[gbuf, pc, acc], writes=[acc])
                    P.stt(acc[A, :], gbuf[A, 2:514], fw(2), acc[A, :], ALU.mult, ALU.add, reads=[gbuf, pc, acc], writes=[acc])
                    P.cp(cffn[A, c * 2:c * 2 + 2], gbuf[A, 512:514], reads=[gbuf], writes=[cffn], eng="dve")
                    P.act(acc[A, :], acc[A, :], AF.Gelu_apprx_tanh, bias=pc[A, PC_FCB + c:PC_FCB + c + 1], reads=[acc, pc], writes=[acc])
                    P.tt(acc[A, :], acc[A, :], rs3[A, :], ALU.mult, reads=[acc, rs3], writes=[acc])
                    P.tt(RR(fT3[:, cc, :]), acc[A, :], bu[A, :], ALU.mult, reads=[acc, bu], writes=[fT.sub(cc * 512, 512)])
                for m in range(8):
                    wa = wchunk()
                    wa3 = wa.v(A, "p (c m) -> p c m", c=8)
                    P.dma(RR(wa[A, 0:ng * 128]), w_dn_d[l, m, :, c_lo * 128:c_hi * 128], writes=[wa], eng="pool")
                    bk = P.bank()
                    for cc in range(ng):
                        P.mm(bk, bk[A, :], wa3[:, cc, :], fT3[:, cc, :], reads=[wa, fT], r=True)
                    om = oT.sub(m * 512, 512)
                    if c_lo == 0:
                        copy(oT3[:, m, :], bk[A, :], [bk], [om])
                    else:
                        P.tt(oT3[:, m, :], oT3[:, m, :], bk[A, :], ALU.add, reads=[om, bk], writes=[om])
            post_residual(oT3, oT, PC_FPOST)

    outops = []
    for c in range(8):
        outops.append(P.dma(out_d[:, c, 0:T], xT3[:, c, :], reads=[xT]))
    P.emit(outops + [op for op in P.dma_ops if False])
    P.close()
    return nc


_NC_CACHE = {}


def kernel(**inputs):
    maps = prep_inputs(inputs)
    if "nc" not in _NC_CACHE:
        _NC_CACHE["nc"] = build()
    nc = _NC_CACHE["nc"]
    res = run_bass_kernel_spmd(nc, maps, core_ids=list(range(len(maps))))
    x = np.asarray(inputs["x"])
    out = np.empty(x.shape, np.float32)
    for b in range(x.shape[0]):
        oT = np.asarray(res.results[b]["outT"])
        out[b] = oT.transpose(1, 0, 2).reshape(D, SEQ).T
    return out
```

```python
import contextlib
import math
import numpy as np
import concourse.bass as bass
import concourse.mybir as mybir
from concourse.bass_utils import run_bass_kernel_spmd

F32 = mybir.dt.float32
F32R = mybir.dt.float32r
BF16 = mybir.dt.bfloat16
ALU = mybir.AluOpType
AF = mybir.ActivationFunctionType

ENGS = ("pe", "act", "dve", "pool", "sp")


def _flat(ks, out):
    for k in ks:
        if isinstance(k, (str, int)):
            out.append(k)
        elif isinstance(k, tuple) and (len(k) == 0 or isinstance(k[0], (str, int))):
            out.append(k)
        elif hasattr(k, "k"):
            _flat(k.k, out)
        else:
            _flat(k, out)
    return out


class Op:
    __slots__ = ("eng", "fn", "deps", "signals", "count", "pos", "dma", "dsem", "dval", "prewait")

    def __init__(self, eng, fn, dma=False):
        self.eng = eng
        self.fn = fn
        self.deps = []
        self.signals = False
        self.count = None
        self.pos = None
        self.dma = dma
        self.dsem = None
        self.dval = None
        self.prewait = None


class Buf:
    def __init__(self, t, off, ncols, keys):
        self.t, self.off, self.n, self.k = t, off, ncols, keys

    def __getitem__(self, idx):
        p, c = idx
        if isinstance(c, int):
            c = slice(c, c + 1)
        a = 0 if c.start is None else c.start
        b = self.n if c.stop is None else c.stop
        assert 0 <= a <= b <= self.n, (a, b, self.n)
        return self.t[p, self.off + a:self.off + b]

    def v(self, p, pat, **kw):
        return self.t[p, self.off:self.off + self.n].rearrange(pat, **kw)

    def sub(self, c0, n):
        ks = self.k
        if len(ks) > 1 and len(ks) * 512 >= self.n:
            ks = ks[c0 // 512:(c0 + n + 511) // 512]
        return Buf(self.t, self.off + c0, n, ks)


class Bank(Buf):
    def __init__(self, t, name):
        super().__init__(t, 0, 512, [name])
        self.opened = set()
        self.pinned = False


class Prog:
    NDMA = 32

    def __init__(self, nc):
        self.nc = nc
        self.ops = {e: [] for e in ENGS}
        self.lastw = {}
        self.readers = {}
        self.waited = {e: {} for e in ENGS}
        self.waited_dma = {e: set() for e in ENGS}
        self.dma_ops = []
        self.stack = contextlib.ExitStack()
        self.banks = []
        self.bank_i = 0
        self.ar = None
        self.ar_top = 0

    def sb(self, name, ncols, parts=128, dtype=F32):
        t = self.stack.enter_context(self.nc.sbuf_tensor("sb_" + name, [parts, ncols], dtype))
        return Buf(t, 0, ncols, [name])

    def sbs(self, name, nslots, dtype=F32):
        t = self.stack.enter_context(self.nc.sbuf_tensor("sb_" + name, [128, nslots * 512], dtype))
        return Buf(t, 0, nslots * 512, [(name, i) for i in range(nslots)])

    def make_banks(self):
        for i in range(8):
            t = self.stack.enter_context(self.nc.psum_tensor(f"bank{i}", [128, 512], F32))
            self.banks.append(Bank(t, f"bank{i}"))

    def bank(self, pin=False):
        for _ in range(16):
            b = self.banks[self.bank_i % 8]
            self.bank_i += 1
            if not b.pinned:
                b.opened = set()
                b.pinned = pin
                return b
        raise RuntimeError("no free psum bank")

    def make_arena(self, nslots):
        self.ar = self.stack.enter_context(self.nc.sbuf_tensor("arena", [128, nslots * 512], F32))
        self.ar_n = nslots

    def alloc(self, ncols=512):
        ns = (ncols + 511) // 512
        assert self.ar_top + ns <= self.ar_n, ("arena overflow", self.ar_top, ns, self.ar_n)
        b = Buf(self.ar, self.ar_top * 512, ncols, [("ar", s) for s in range(self.ar_top, self.ar_top + ns)])
        self.ar_top += ns
        return b

    def add(self, eng, fn, reads=(), writes=(), dma=False):
        op = Op(eng, fn, dma)
        op.pos = len(self.ops[eng])
        reads = _flat(reads, [])
        writes = _flat(writes, [])
        deps = []
        for k in reads:
            w = self.lastw.get(k)
            if w is not None:
                deps.append(w)
        for k in writes:
            w = self.lastw.get(k)
            if w is not None:
                deps.append(w)
            deps.extend(self.readers.get(k, ()))
        wt = self.waited[eng]
        wd = self.waited_dma[eng]
        best = {}
        dm = []
        for d in deps:
            if d is op:
                continue
            if d.dma:
                if id(d) not in wd:
                    wd.add(id(d))
                    dm.append(d)
                continue
            if d.eng == eng and eng in ("pe", "sp"):
                continue
            if wt.get(d.eng, -1) >= d.pos:
                continue
            if d.eng not in best or best[d.eng].pos < d.pos:
                best[d.eng] = d
        op.deps = list(best.values()) + dm
        for d in best.values():
            d.signals = True
            wt[d.eng] = max(wt.get(d.eng, -1), d.pos)
        for k in reads:
            self.readers.setdefault(k, []).append(op)
        for k in writes:
            self.lastw[k] = op
            self.readers[k] = []
        self.ops[eng].append(op)
        if dma:
            self.dma_ops.append(op)
        return op

    def emit(self, out_dma_ops=()):
        nc = self.nc
        nd = self.NDMA
        sems = {e: self.stack.enter_context(nc.semaphore(f"s_{e}")) for e in ENGS if e != "sp"}
        dsems = [self.stack.enter_context(nc.semaphore(f"s_dma{i}")) for i in range(nd)]
        for e in ENGS:
            c = 0
            for op in self.ops[e]:
                if not op.dma and op.signals:
                    c += 1
                    op.count = c
        per_eng = {}
        for op in self.dma_ops:
            per_eng.setdefault(op.eng, []).append(op)
        engs_with_dma = list(per_eng.keys())
        share = nd // max(1, len(engs_with_dma))
        for ei, e in enumerate(engs_with_dma):
            mysems = dsems[ei * share:(ei + 1) * share]
            for i, op in enumerate(per_eng[e]):
                op.dsem = mysems[i % share]
                op.dval = 16 * (i // share + 1)
                if i >= share:
                    op.prewait = (op.dsem, 16 * (i // share))
        final_waits = [(op.dsem, op.dval) for op in out_dma_ops]

        def run(e, eng):
            for op in self.ops[e]:
                if op.prewait is not None:
                    eng.wait_ge(op.prewait[0], op.prewait[1])
                for d in op.deps:
                    if d.dma:
                        eng.wait_ge(d.dsem, d.dval)
                    else:
                        eng.wait_ge(sems[d.eng], d.count)
                ins = op.fn(eng)
                if op.dma:
                    ins.then_inc(op.dsem, 16)
                elif op.signals:
                    ins.then_inc(sems[e], 1)
            if e == "sp":
                for s, v in final_waits:
                    eng.wait_ge(s, v)

        with nc.Block() as block:
            @block.tensor
            def _(eng):
                run("pe", eng)

            @block.scalar
            def _(eng):
                run("act", eng)

            @block.vector
            def _(eng):
                run("dve", eng)

            @block.gpsimd
            def _(eng):
                run("pool", eng)

            @block.sync
            def _(eng):
                run("sp", eng)

    def close(self):
        self.stack.close()

    def dma(self, out, in_, reads=(), writes=(), eng="sp"):
        return self.add(eng, lambda e: e.dma_start(out=out, in_=in_), reads, writes, dma=True)

    def mm(self, bank, out, lhsT, rhs, reads=(), r=False):
        p0 = out.start_partition()
        q = frozenset(range(p0 // 32, (p0 + out.partition_size() - 1) // 32 + 1))
        c0 = out.offset % 512 if hasattr(out, "offset") else 0
        c0 = self._ap_col0(out)
        c1 = c0 + self._ap_ncols(out)
        start = True
        for (qq, a, b) in bank.opened:
            if (qq & q) and a < c1 and c0 < b:
                assert q <= qq and a <= c0 and c1 <= b, ("partial psum overlap", q, qq, c0, c1, a, b)
                start = False
        if start:
            bank.opened.add((q, c0, c1))
        if r and FAST_MM and lhsT.dtype == F32:
            lhsT = lhsT.bitcast(F32R)
            rhs = rhs.bitcast(F32R)
        return self.add("pe", lambda e: e.matmul(out, lhsT, rhs, start=start, stop=True, skip_group_check=True),
                        reads, [bank])

    @staticmethod
    def _ap_col0(ap):
        pstride = ap.ap[0][0]
        return ap.offset % pstride

    @staticmethod
    def _ap_ncols(ap):
        span = 0
        for st, n in list(ap.ap)[1:]:
            span += st * (n - 1)
        return span + 1

    def tr(self, bank, out, in_, ident, reads=()):
        return self.add("pe", lambda e: e.transpose(out, in_, ident), reads, [bank])

    def act(self, out, in_, func, bias=None, scale=1.0, reads=(), writes=()):
        if bias is None:
            return self.add("act", lambda e: e.activation(out, in_, func, scale=scale), reads, writes)
        return self.add("act", lambda e: e.activation(out, in_, func, bias=bias, scale=scale), reads, writes)

    def tt(self, out, a, b, op, reads=(), writes=(), eng="dve"):
        return self.add(eng, lambda e: e.tensor_tensor(out, a, b, op), reads, writes)

    def ts(self, out, a, s1, op0, s2=None, op1=None, reads=(), writes=(), eng="dve"):
        if op1 is None:
            return self.add(eng, lambda e: e.tensor_scalar(out, a, s1, None, op0), reads, writes)
        return self.add(eng, lambda e: e.tensor_scalar(out, a, s1, s2, op0, op1), reads, writes)

    def stt(self, out, a, s, b, op0, op1, reads=(), writes=()):
        return self.add("dve", lambda e: e.scalar_tensor_tensor(out, a, s, b, op0, op1), reads, writes)

    def cp(self, out, a, reads=(), writes=(), eng="dve"):
        if eng == "act":
            return self.add(eng, lambda e: e.copy(out, a), reads, writes)
        return self.add(eng, lambda e: e.tensor_copy(out, a), reads, writes)

    def recip(self, out, a, reads=(), writes=()):
        return self.add("dve", lambda e: e.reciprocal(out, a), reads, writes)

    def scan(self, out, d0, d1, init, op0, op1, reads=(), writes=()):
        return self.add("dve", lambda e: e.tensor_tensor_scan(out, d0, d1, init, op0, op1), reads, writes)

    def memset(self, ap, v, writes=(), eng="dve"):
        return self.add(eng, lambda e: e.memset(ap, v), (), writes)


D = 1024
SEQ = 2048
DEPTH = 2
TB = 512
RW_STOP = 0
FAST_MM = True
N_IN = 3084
DFF = 2816
NFC = DFF // 128

PC_GPRE, PC_GPOST, PC_FPRE, PC_FPOST, PC_MU = 0, 8, 16, 24, 32
PC_W0, PC_A0, PC_KK, PC_KA, PC_RK, PC_LNW, PC_LNB, PC_MNORM, PC_SINK = 40, 42, 44, 46, 48, 50, 52, 54, 56
PC_CW, PC_CB, PC_BI, PC_BF, PC_FOXB, PC_FCW, PC_FCB, NPC = 58, 74, 78, 79, 80, 81, 147, 169
DV_OMKA, DV_BI15, DV_BF15, DV_NFOXB, DV_ESINK, NDV = 0, 2, 3, 4, 5, 8

C_IDENT, C_ONESD, C_ALLONE, C_BLK1, C_BLKM, C_TRI = 0, 128, 256, 384, 512, 640
C_MA, C_MB, C_IDG, C_MM, C_SELF, C_SELK, C_SELV, C_HM, C_SWAM, NCST = 768, 1280, 1408, 1536, 1792, 2304, 2432, 2688, 2696, 3720


W_IN_CHUNKS = ([(768, 128, False), (896, 128, False)]
               + [(i * 256 + hp * 128, 128, False) for hp in range(2) for i in range(3)]
               + [(1024 + i * 64, 64, False) for i in range(4)]
               + [(c0 + hp * 128, 128, False) for hp in range(2) for c0 in (1280, 1536)]
               + [(1792, 4, False), (1796, 4, False)]
               + [(1800, 128, False), (1928, 128, False), (2056, 64, True), (2120, 64, True), (2184, 128, False)]
               + [(2312, 128, False), (2440, 128, False), (2568, 128, False), (2696, 128, False)]
               + [(2824, 128, False), (2952, 128, False), (3080, 4, False)])
W_IN_OFF = {}
_o = 0
for (_c0, _m, _d) in W_IN_CHUNKS:
    W_IN_OFF[(_c0, _m)] = _o
    _o += 8 * (128 if _d else _m)
W_IN_TOT = _o


def _t5_bucket(dist):
    max_exact = 16
    d = np.maximum(dist, 1).astype(np.float32)
    large = max_exact + (np.log(d / max_exact) / math.log(128 / max_exact) * (32 - max_exact)).astype(np.int32)
    large = np.minimum(large, 31)
    return np.where(dist < max_exact, dist, large).astype(np.int32)


def _swa_dist():
    s = np.arange(128)[:, None, None]
    pcx = np.arange(2)[None, :, None]
    tq = np.arange(128)[None, None, :]
    sk = s + 128 * pcx
    dist = tq + 128 - sk
    vis = (dist >= 0) & (dist < 128)
    return dist, vis


def make_consts():
    c = np.zeros((128, NCST), np.float32)
    c[:, C_IDENT:C_IDENT + 128] = np.eye(128)
    c[:, C_ONESD:C_ONESD + 128] = 1.0 / 1024
    c[:, C_ALLONE:C_ALLONE + 128] = 1.0
    blk = np.zeros((128, 128), np.float32)
    blk[:64, :64] = 1
    blk[64:, 64:] = 1
    c[:, C_BLK1:C_BLK1 + 128] = blk
    c[:, C_BLKM:C_BLKM + 128] = blk / 64.0
    k = np.arange(128)
    c[:, C_TRI:C_TRI + 128] = (k[:, None] <= k[None, :])
    i = np.arange(64)[:, None]
    t = np.arange(64)[None, :]
    ma = np.zeros((64, 2, 2, 2, 64), np.float32)
    ma[:, :, :, 0, :] = (i < t)[:, None, None, :]
    ma[:, :, :, 1, :] = (i <= t)[:, None, None, :]
    c[:64, C_MA:C_MA + 512] = ma.reshape(64, 512)
    mb = np.zeros((64, 2, 64), np.float32)
    mb[:] = (i > t)[:, None, :]
    c[:64, C_MB:C_MB + 128] = mb.reshape(64, 128)
    idg = np.zeros((64, 2, 64), np.float32)
    idg[:] = np.eye(64)[:, None, :]
    c[:64, C_IDG:C_IDG + 128] = idg.reshape(64, 128)
    mm = np.zeros((64, 4, 64), np.float32)
    mm[:] = (i <= t)[:, None, :]
    c[:64, C_MM:C_MM + 256] = mm.reshape(64, 256)
    sf = np.zeros((4, 4, 128), np.float32)
    for h in range(4):
        sf[h, h, :] = 1
    c[:4, C_SELF:C_SELF + 512] = sf.reshape(4, 512)
    c[64:68, C_SELF:C_SELF + 512] = sf.reshape(4, 512)
    sk = np.zeros((4, 2, 64), np.float32)
    sv = np.zeros((4, 2, 128), np.float32)
    for h in range(4):
        for j in range(2):
            sk[h, j, :] = (h == 2 * j + np.arange(64) // 32)
            sv[h, j, :] = (h == 2 * j + np.arange(128) // 64)
    c[:4, C_SELK:C_SELK + 128] = sk.reshape(4, 128)
    c[:4, C_SELV:C_SELV + 256] = sv.reshape(4, 256)
    c[0:32, C_HM] = 1.0
    c[32:64, C_HM + 1] = 1.0
    _, vis = _swa_dist()
    m = np.where(vis, 0.0, -30000.0).astype(np.float32)
    m4 = np.broadcast_to(m[:, :, None, :], (128, 2, 4, 128))
    c[:, C_SWAM:C_SWAM + 1024] = m4.reshape(128, 1024)
    return c


def _cols(v, n):
    return np.ascontiguousarray(np.asarray(v, np.float32).reshape(n, 128).T)


def prep_inputs(inp):
    L = DEPTH
    f = lambda k: np.asarray(inp[k], np.float32)
    w_in4 = f("w_in").reshape(L, 8, 128, N_IN).transpose(0, 2, 1, 3)
    w_in_r = np.zeros((L, 128, W_IN_TOT), np.float32)
    for (c0, m, d) in W_IN_CHUNKS:
        blk = w_in4[:, :, :, c0:c0 + m]
        if d:
            blk = np.concatenate([blk, blk], axis=3)
        w_in_r[:, :, W_IN_OFF[(c0, m)]:W_IN_OFF[(c0, m)] + 8 * blk.shape[3]] = blk.reshape(L, 128, -1)
    w_out_r = np.ascontiguousarray(f("w_out").reshape(L, 8, 128, 8, 128).transpose(0, 3, 2, 1, 4)).reshape(L, 8, 128, 1024)
    w_up_r = np.ascontiguousarray(f("ffn_w_up").reshape(L, 8, 128, 2 * NFC, 128).transpose(0, 3, 2, 1, 4)).reshape(L, 2 * NFC, 128, 1024)
    w_dn_r = np.ascontiguousarray(f("ffn_w_down").reshape(L, NFC, 128, 8, 128).transpose(0, 3, 2, 1, 4)).reshape(L, 8, 128, NFC * 128)
    lora = np.zeros((L, 128, 512), np.float32)
    lora[:, 0:64, 0:256] = f("rwkv_w_up")
    lora[:, 64:128, 0:256] = f("rwkv_a_up")
    lora[:, :, 256:512] = f("rwkv_g_up")
    pc = np.zeros((L, 128, NPC), np.float32)
    for l in range(L):
        pc[l, :, PC_GPRE:PC_GPRE + 8] = _cols(f("norm_mix_pre")[l], 8)
        pc[l, :, PC_GPOST:PC_GPOST + 8] = _cols(f("norm_mix_post")[l], 8)
        pc[l, :, PC_FPRE:PC_FPRE + 8] = _cols(f("norm_ffn_pre")[l], 8)
        pc[l, :, PC_FPOST:PC_FPOST + 8] = _cols(f("norm_ffn_post")[l], 8)
        pc[l, :, PC_MU:PC_MU + 8] = _cols(f("rwkv_mu")[l], 8)
        for col, key in ((PC_W0, "rwkv_w0"), (PC_A0, "rwkv_a0"), (PC_KK, "rwkv_k_k"), (PC_KA, "rwkv_k_a"),
                         (PC_LNW, "rwkv_ln_w"), (PC_LNB, "rwkv_ln_b"), (PC_MNORM, "mlstm_norm")):
            pc[l, :, col:col + 2] = _cols(f(key)[l], 2)
        pc[l, :, PC_RK:PC_RK + 2] = _cols(f("rwkv_r_k")[l].reshape(256), 2)
        pc[l, :, PC_SINK:PC_SINK + 2] = _cols(np.repeat(f("swa_sinks")[l], 64), 2)
        cw = f("mlstm_conv_w")[l]
        for i in range(4):
            for j in range(4):
                pc[l, 0:64, PC_CW + i * 4 + j] = cw[j, i * 64:(i + 1) * 64]
            pc[l, 0:64, PC_CB + i] = f("mlstm_conv_b")[l][i * 64:(i + 1) * 64]
        pc[l, 0:4, PC_BI] = f("mlstm_b_i")[l]
        pc[l, 0:4, PC_BF] = f("mlstm_b_f")[l]
        pc[l, 0:4, PC_FOXB] = f("fox_b_f")[l]
        pc[l, 64:68, PC_FOXB] = f("fox_b_f")[l]
        fw = f("ffn_conv_w")[l]
        for j in range(3):
            pc[l, :, PC_FCW + j:PC_FCW + 66:3] = _cols(fw[j], NFC)
        pc[l, :, PC_FCB:PC_FCB + NFC] = _cols(f("ffn_conv_b")[l], NFC)
    dist, _ = _swa_dist()
    bk = _t5_bucket(np.clip(dist, 0, 127))
    swab = f("rel_bias")[bk]
    swab = np.ascontiguousarray(swab.transpose(0, 1, 3, 2)).reshape(128, 1024)
    cst = make_consts()
    x = f("x")
    maps = []
    for b in range(x.shape[0]):
        xT = np.ascontiguousarray(x[b].T.reshape(8, 128, x.shape[1]).transpose(1, 0, 2))
        maps.append({"xT": xT, "w_in_r": w_in_r, "w_out_r": w_out_r, "w_up_r": w_up_r, "w_dn_r": w_dn_r,
                     "lora": lora, "pc": pc, "cst": cst, "swab": swab})
    return maps


ARENA_SLOTS = 26


def build(nlayer=DEPTH, nblk=SEQ // TB, dbg=None, phases="rmsfon"):
    nc = bass.Bass("TRN2", target_bir_lowering=False)
    T = nblk * TB
    NT = T // 128
    L = DEPTH
    dr = lambda n, s, kind="ExternalInput": nc.dram_tensor(n, list(s), F32, kind=kind).ap()
    xT_d = dr("xT", [128, 8, SEQ])
    w_in_d = dr("w_in_r", [L, 128, W_IN_TOT])
    w_out_d = dr("w_out_r", [L, 8, 128, 1024])
    w_up_d = dr("w_up_r", [L, 2 * NFC, 128, 1024])
    w_dn_d = dr("w_dn_r", [L, 8, 128, NFC * 128])
    lora_d = dr("lora", [L, 128, 512])
    pc_d = dr("pc", [L, 128, NPC])
    cst_d = dr("cst", [128, NCST])
    swab_d = dr("swab", [128, 1024])
    out_d = dr("outT", [128, 8, SEQ], "ExternalOutput")
    dbg_d = dr("dbg", [128, 8, SEQ], "ExternalOutput") if dbg else None

    P = Prog(nc)
    RR = lambda ap: ap.bitcast(F32R) if (FAST_MM and ap.dtype == F32) else ap
    RR0 = RR
    P.make_banks()
    xT = P.sb("xT", 8 * T)
    kTh = P.sb("kTh", 2 * T)
    Vh = P.sb("Vh", NT * 256)
    negc = P.sb("negc", NT * 4)
    yT = P.sbs("yT", 8, dtype=BF16)
    foxR = P.sbs("foxR", 7)
    cst = P.sb("cst", C_SWAM)
    pc = P.sb("pc", NPC)
    dv = P.sb("dv", NDV)
    rstm = P.sb("rstm", 4)
    Srw = P.sb("Srw", 2 * 2 * 2 * 64)
    pc1 = P.sb("pc1", 8)
    CN = P.sb("CN", 2 * 128)
    crw = P.sb("crw", 8)
    cml = P.sb("cml", 12)
    cffn = P.sb("cffn", NFC * 2)
    ccar = P.sb("ccar", 2)
    swaKc = P.sb("swaKc", 2 * 128)
    swaVc = P.sb("swaVc", 128)
    hTr = P.sbs("hTr", 8, dtype=BF16)
    ebks = P.sb("ebks", 16)
    wbufs = [P.sb(f"wch{i}", 8 * 128, dtype=BF16) for i in range(6)]
    P.make_arena(ARENA_SLOTS)

    A = slice(0, 128)
    H0 = slice(0, 64)
    H1 = slice(64, 128)
    R4 = slice(0, 4)
    xT3 = xT.v(A, "p (c t) -> p c t", c=8)
    yT3 = yT.v(A, "p (c t) -> p c t", c=8)
    kTh3 = kTh.v(A, "p (j t) -> p j t", j=2)
    Vh3 = Vh.v(A, "p (n c) -> p n c", c=256)
    negc3 = negc.v(A, "p (n h) -> p n h", h=4)
    swaKc3 = swaKc.v(A, "p (j t) -> p j t", j=2)
    hTr3 = hTr.v(A, "p (c t) -> p c t", c=8)
    CN3 = CN.v(H0, "p (j c) -> p j c", j=2)
    S5 = Srw.v(H0, "p (j h b v) -> p j h b v", j=2, h=2, b=2)

    def C(off, n, p=A):
        return cst[p, off:off + n]

    ident = C(C_IDENT, 128)
    evac_i = [0]

    def copy(out, in_, reads, writes, eng=None):
        if eng is None:
            evac_i[0] += 1
            eng = "act" if evac_i[0] % 2 else "dve"
        return P.cp(out, in_, reads, writes, eng=eng)

    wb_i = [0]

    def wchunk():
        b = wbufs[wb_i[0] % 6]
        wb_i[0] += 1
        return b

    P.dma(RR0(cst[A, :]), cst_d[:, 0:C_SWAM], writes=[cst], eng="pool")
    for c in range(8):
        P.dma(xT3[:, c, :], xT_d[:, c, 0:T], writes=[xT])
    if phases != "rmsfon":
        P.ts(RR(yT[A, :]), cst[A, 0:4096 if C_SWAM >= 4096 else 2048].to_broadcast([128, 4096]) if False else xT[A, 0:4096], 0.0, ALU.mult, reads=[xT], writes=[yT])
    P.ar_top = 0

    def rms_rstd(src_fn, reads, eps=1e-6):
        ssb = P.bank(pin=True)
        rstd = P.alloc()
        sq = [P.alloc(), P.alloc()]
        for c in range(8):
            s = sq[c % 2]
            P.act(s[A, :], src_fn(c), AF.Square, reads=reads, writes=[s])
            P.mm(ssb, ssb[A, :], C(C_ONESD, 128), s[A, :], reads=[cst, s])
        P.ts(rstd[A, :], ssb[A, :], eps, ALU.add, reads=[ssb], writes=[rstd])
        ssb.pinned = False
        P.recip(rstd[A, :], rstd[A, :], reads=[rstd], writes=[rstd])
        P.act(rstd[A, :], rstd[A, :], AF.Sqrt, reads=[rstd], writes=[rstd])
        P.ar_top -= 2
        return rstd

    for l in range(nlayer):
        P.dma(pc[A, :], pc_d[l], writes=[pc])
        P.ts(dv[A, DV_OMKA:DV_OMKA + 2], pc[A, PC_KA:PC_KA + 2], -1.0, ALU.mult, 1.0, ALU.add, reads=[pc], writes=[dv])
        P.ts(dv[R4, DV_BI15:DV_BI15 + 2], pc[R4, PC_BI:PC_BI + 2], 1.0 / 15.0, ALU.mult, reads=[pc], writes=[dv])
        P.ts(dv[A, DV_NFOXB:DV_NFOXB + 1], pc[A, PC_FOXB:PC_FOXB + 1], -1.0, ALU.mult, reads=[pc], writes=[dv])
        P.act(dv[A, DV_ESINK:DV_ESINK + 2], pc[A, PC_SINK:PC_SINK + 2], AF.Exp, reads=[pc], writes=[dv])
        for b_ in (Srw, CN, crw, cml, cffn, ccar, swaKc, swaVc):
            P.memset(b_[A, :], 0.0, writes=[b_])

        for blk in range(nblk):
            t0 = blk * TB
            bs = slice(t0, t0 + TB)
            P.ar_top = 0
            rstd = rms_rstd(lambda c: xT3[:, c, bs], [xT])
            bk = P.bank()
            for tt_ in range(4):
                P.tr(bk, bk[A, tt_ * 128:(tt_ + 1) * 128], rstd[A, tt_ * 128:(tt_ + 1) * 128], ident, reads=[rstd, cst])
            P.cp(rstm[A, 0:4], bk.v(A, "p (n c) -> p n c", n=4)[:, :, 0], reads=[bk], writes=[rstm])
            MIX_BASE = P.ar_top

            def fill_hTr(gcol):
                for c in range(8):
                    gs = pc[A, gcol + c:gcol + c + 1]
                    if c % 2:
                        P.act(hTr3[:, c, :], xT3[:, c, bs], AF.Copy, scale=gs, reads=[xT, pc], writes=[hTr.sub(c * 512, 512)])
                    else:
                        P.ts(hTr3[:, c, :], xT3[:, c, bs], gs, ALU.mult, reads=[xT, pc], writes=[hTr.sub(c * 512, 512)], eng="dve")

            fill_hTr(PC_GPRE)
            gpre = pc[A, PC_GPRE:PC_GPRE + 8]

            def load_w(col0, M, dup=False):
                w = wchunk()
                w3 = w.v(A, "p (c m) -> p c m", c=8)
                off = W_IN_OFF[(col0, M)]
                if dup:
                    M = 128
                if M == 128:
                    P.dma(RR(w[A, :]), w_in_d[l, :, off:off + 1024], writes=[w], eng="pool")
                else:
                    P.dma(RR(w3[:, :, 0:M]), w_in_d[l, :, off:off + 8 * M].rearrange("p (c m) -> p c m", c=8), writes=[w], eng="pool")
                return w, w3, M

            def proj_fm(col0, M, dst, dup=False, scale=None, rows=None, pbase=0):
                w, w3, M = load_w(col0, M, dup)
                bk = P.bank()
                r = slice(pbase, pbase + M)
                for c in range(8):
                    P.mm(bk, bk[r, :], w3[:, c, 0:M], hTr3[:, c, :], reads=[w, hTr], r=(pbase == 0))
                if scale is None:
                    P.tt(dst, bk[r, :], rstd[r, :], ALU.mult, reads=[bk, rstd], writes=[rows])
                else:
                    P.stt(dst, bk[r, :], scale, rstd[r, :], ALU.mult, ALU.mult, reads=[bk, rstd], writes=[rows])

            def proj_tm(col0, ncols, dst_fn, wr):
                w, w3, _ = load_w(col0, ncols)
                for tt_ in range(4):
                    bk = P.bank()
                    for c in range(8):
                        P.mm(bk, bk[A, 0:ncols], hTr3[:, c, tt_ * 128:(tt_ + 1) * 128], w3[:, c, 0:ncols], reads=[w, hTr], r=True)
                    P.ts(dst_fn(tt_), bk[A, 0:ncols], rstm[A, tt_:tt_ + 1], ALU.mult, reads=[bk, rstm], writes=[wr])

            def shift_mix(z, ci, dst):
                d = P.alloc()
                P.tt(d[A, 1:512], z[A, 0:511], z[A, 1:512], ALU.subtract, reads=[z], writes=[d])
                P.tt(d[A, 0:1], crw[A, ci:ci + 1], z[A, 0:1], ALU.subtract, reads=[z, crw], writes=[d])
                P.cp(crw[A, ci:ci + 1], z[A, 511:512], reads=[z], writes=[crw], eng="dve")
                P.stt(dst[A, :], d[A, :], pc[A, PC_MU + ci:PC_MU + ci + 1], z[A, :], ALU.mult, ALU.add,
                      reads=[d, pc, z], writes=[dst])
                P.ar_top -= 1

            def rwkv():
                lora = P.alloc()
                P.dma(lora[A, :], lora_d[l], writes=[lora])
                swa_ = P.alloc()
                sg = P.alloc()
                ztmp = P.alloc()
                proj_fm(768, 128, ztmp[A, :], rows=ztmp)
                shift_mix(ztmp, 6, swa_)
                proj_fm(896, 128, ztmp[A, :], rows=ztmp)
                shift_mix(ztmp, 7, sg)
                P.ar_top -= 1
                P.act(swa_[H0, :], swa_[H0, :], AF.Tanh, reads=[swa_], writes=[swa_])
                P.act(sg[A, :], sg[A, :], AF.Sigmoid, reads=[sg], writes=[sg])
                if RW_STOP == 1:
                    return
                base_top = P.ar_top
                for hp in range(2):
                    P.ar_top = base_top
                    z = [P.alloc() for _ in range(3)]
                    s = [P.alloc() for _ in range(3)]
                    for i in range(3):
                        proj_fm(i * 256 + hp * 128, 128, z[i][A, :], rows=z[i])
                    for i in range(3):
                        shift_mix(z[i], i * 2 + hp, s[i])
                    sr, sk, sv = s
                    cw = slice(hp * 128, (hp + 1) * 128)
                    pcc = lambda c0: pc[A, c0 + hp:c0 + hp + 1]
                    bk = P.bank()
                    P.mm(bk, bk[A, :], lora[H0, cw], swa_[H0, :], reads=[lora, swa_])
                    lw = z[0]
                    P.act(lw[A, :], bk[A, :], AF.Sigmoid, bias=pcc(PC_W0), reads=[bk, pc], writes=[lw])
                    P.ts(lw[A, :], lw[A, :], -math.exp(-0.5), ALU.mult, reads=[lw], writes=[lw])
                    bk = P.bank()
                    P.mm(bk, bk[A, :], lora[H1, cw], swa_[H1, :], reads=[lora, swa_])
                    a = z[1]
                    P.act(a[A, :], bk[A, :], AF.Sigmoid, bias=pcc(PC_A0), reads=[bk, pc], writes=[a])
                    bk = P.bank()
                    P.mm(bk, bk[A, :], lora[A, 256 + hp * 128:256 + (hp + 1) * 128], sg[A, :], reads=[lora, sg])
                    g = z[2]
                    copy(g[A, :], bk[A, :], [bk], [g])
                    kk = P.alloc()
                    tmp = P.alloc()
                    P.ts(kk[A, :], sk[A, :], pcc(PC_KK), ALU.mult, reads=[sk, pc], writes=[kk])
                    P.tt(tmp[A, :], kk[A, :], kk[A, :], ALU.mult, reads=[kk], writes=[tmp])
                    bk = P.bank()
                    P.mm(bk, bk[A, :], C(C_BLK1, 128), tmp[A, :], reads=[cst, tmp])
                    P.act(tmp[A, :], bk[A, :], AF.Sqrt, reads=[bk], writes=[tmp])
                    P.ts(tmp[A, :], tmp[A, :], 1e-12, ALU.max, reads=[tmp], writes=[tmp])
                    P.recip(tmp[A, :], tmp[A, :], reads=[tmp], writes=[tmp])
                    P.tt(kk[A, :], kk[A, :], tmp[A, :], ALU.mult, reads=[kk, tmp], writes=[kk])
                    kmod = P.alloc()
                    P.ts(kmod[A, :], a[A, :], pcc(PC_KA), ALU.mult, dv[A, DV_OMKA + hp:DV_OMKA + hp + 1], ALU.add,
                         reads=[a, pc, dv], writes=[kmod])
                    P.tt(kmod[A, :], kmod[A, :], sk[A, :], ALU.mult, reads=[kmod, sk], writes=[kmod])
                    bonus = sk
                    P.stt(tmp[A, :], sr[A, :], pcc(PC_RK), kmod[A, :], ALU.mult, ALU.mult, reads=[sr, pc, kmod], writes=[tmp])
                    bk = P.bank()
                    P.mm(bk, bk[A, :], C(C_BLK1, 128), tmp[A, :], reads=[cst, tmp])
                    P.tt(bonus[A, :], bk[A, :], sv[A, :], ALU.mult, reads=[bk, sv], writes=[bonus])
                    bb = a
                    P.tt(bb[A, :], kk[A, :], a[A, :], ALU.mult, reads=[kk, a], writes=[bb])
                    cum = P.alloc()
                    ones = C(C_ALLONE, 64)
                    for c in range(8):
                        cs = slice(c * 64, (c + 1) * 64)
                        P.scan(cum[A, cs], ones, lw[A, cs], 0.0, ALU.mult, ALU.add, reads=[cst, lw], writes=[cum])
                    cumx = lw
                    P.tt(cumx[A, :], cum[A, :], lw[A, :], ALU.subtract, reads=[cum, lw], writes=[cumx])
                    Ep = P.alloc()
                    Em = P.alloc()
                    Ee = P.alloc()
                    P.act(Ep[A, :], cum[A, :], AF.Exp, reads=[cum], writes=[Ep])
                    P.act(Em[A, :], cum[A, :], AF.Exp, scale=-1.0, reads=[cum], writes=[Em])
                    P.act(cumx[A, :], cumx[A, :], AF.Exp, reads=[cumx], writes=[cumx])
                    for c in range(8):
                        cs = slice(c * 64, (c + 1) * 64)
                        P.act(Ee[A, cs], cum[A, cs], AF.Exp, bias=cum[A, c * 64 + 63:c * 64 + 64], scale=-1.0,
                              reads=[cum], writes=[Ee])
                    AR = P.alloc(1024)
                    AR3 = AR.v(A, "p (a t) -> p a t", a=2)
                    P.stt(AR3[:, 0, :], kk[A, :], -1.0, cumx[A, :], ALU.mult, ALU.mult, reads=[kk, cumx], writes=[AR])
                    P.tt(AR3[:, 1, :], sr[A, :], Ep[A, :], ALU.mult, reads=[sr, Ep], writes=[AR])
                    Bt, Kt, Bte, Kte = cum, tmp, kk, kmod
                    P.tt(Bt[A, :], bb[A, :], Em[A, :], ALU.mult, reads=[bb, Em], writes=[Bt])
                    P.tt(Kt[A, :], kmod[A, :], Em[A, :], ALU.mult, reads=[kmod, Em], writes=[Kt])
                    P.tt(Bte[A, :], bb[A, :], Ee[A, :], ALU.mult, reads=[bb, Ee], writes=[Bte])
                    P.tt(Kte[A, :], kmod[A, :], Ee[A, :], ALU.mult, reads=[kmod, Ee], writes=[Kte])
                    yr = Em
                    AR1 = Buf(z[0].t, z[0].off, 1024, z[0].k + z[1].k)
                    AR13 = AR1.v(A, "p (a t) -> p a t", a=2)
                    Bt1, Kt1 = sr, P.alloc()
                    idn1 = cst[H1, C_IDENT + 64:C_IDENT + 128]
                    for src, dst, wr in ((AR3[H1, 0, :], AR13[H0, 0, :], AR1), (AR3[H1, 1, :], AR13[H0, 1, :], AR1),
                                         (Bt[H1, :], Bt1[H0, :], Bt1), (Kt[H1, :], Kt1[H0, :], Kt1)):
                        bk = P.bank()
                        P.mm(bk, bk[H0, :], idn1, src, reads=[cst, AR, Bt, Kt])
                        copy(dst, bk[H0, :], [bk], [wr])
                    bk = P.bank()
                    P.mm(bk, bk[H0, 0:8], idn1, Ep.v(H1, "p (c t) -> p c t", c=8)[:, :, 63], reads=[cst, Ep])
                    copy(pc1[H0, 0:8], bk[H0, 0:8], [bk], [pc1])
                    ARh = (AR3, AR13)
                    Bth = (Bt, Bt1)
                    Kth = (Kt, Kt1)
                    ARb = (AR, AR1)
                    if RW_STOP == 2:
                        continue
                    NA = [P.alloc() for _ in range(2)]
                    nmslot = P.alloc()
                    NMp = [nmslot.sub(0, 256), nmslot.sub(256, 256)]
                    gslot = P.alloc()
                    Gp = [gslot.sub(0, 128), gslot.sub(128, 128)]
                    RU = [gslot.sub(256, 256), Ee.sub(0, 256)]
                    TM = [P.alloc() for _ in range(2)]
                    for c in range(8):
                        if RW_STOP == 6:
                            continue
                        cs = slice(c * 64, (c + 1) * 64)
                        na = NA[c % 2]
                        na5 = na.v(H0, "p (h b a t) -> p h b a t", h=2, b=2, a=2)
                        bk = P.bank()
                        bk5 = bk.v(H0, "p (h b a t) -> p h b a t", h=2, b=2, a=2)
                        bkm = P.bank()
                        bkm3 = bkm.v(H0, "p (x h t) -> p x h t", x=4, h=2)
                        for h2 in range(1 if RW_STOP == 7 else 2):
                            ph = slice(h2 * 64, h2 * 64 + 64)
                            for a_ in range(2):
                                P.mm(bk, bk5[:, h2, 0, a_, :], Bth[h2][H0, cs], ARh[h2][H0, a_, cs], reads=[Bth[h2], ARb[h2]])
                                P.mm(bk, bk5[:, h2, 1, a_, :], Kth[h2][H0, cs], ARh[h2][H0, a_, cs], reads=[Kth[h2], ARb[h2]])
                            P.mm(bkm, bkm3[:, 0, h2, :], ARh[h2][H0, 0, cs], Bth[h2][H0, cs], reads=[Bth[h2], ARb[h2]])
                        P.tt(na[H0, :], bk[H0, :], C(C_MA, 512, H0), ALU.mult, reads=[bk, cst], writes=[na])
                        if RW_STOP in (3, 7):
                            continue
                        nm = NMp[0]
                        nm4 = nm.v(H0, "p (x h t) -> p x h t", x=2, h=2)
                        P.cp(nm4[:, 0, :, :], na5[:, :, 0, 0, :], reads=[na], writes=[nm], eng="dve")
                        P.tt(nm4[:, 1, :, :], bkm3[:, 0, :, :], C(C_MB, 128, H0).rearrange("p (h t) -> p h t", h=2),
                             ALU.mult, reads=[bkm, cst], writes=[nm])
                        G3_0 = Gp[0].v(H0, "p (h t) -> p h t", h=2)
                        P.tt(G3_0, na5[:, :, 0, 0, :], C(C_IDG, 128, H0).rearrange("p (h t) -> p h t", h=2), ALU.add,
                             reads=[na, cst], writes=[Gp[0]])
                        cur = 0
                        for lev in range(5):
                            nmc = NMp[cur]
                            nmc4 = nmc.v(H0, "p (x h t) -> p x h t", x=2, h=2)
                            nmn = NMp[1 - cur]
                            nmn4 = nmn.v(H0, "p (x h t) -> p x h t", x=2, h=2)
                            Gc = Gp[cur]
                            Gc3 = Gc.v(H0, "p (h t) -> p h t", h=2)
                            Gn = Gp[1 - cur]
                            bkp = P.bank()
                            bkp4 = bkp.v(H0, "p (x h t) -> p x h t", x=4, h=2)
                            for h2 in range(2):
                                P.mm(bkp, bkp4[:, 0, h2, :], nmc4[:, 1, h2, :], nmc4[:, 0, h2, :], reads=[nmc])
                                P.mm(bkp, bkp4[:, 1, h2, :], nmc4[:, 0, h2, :], nmc4[:, 1, h2, :], reads=[nmc])
                            copy(nmn[H0, :], bkp[H0, 0:256], [bkp], [nmn])
                            bkg = P.bank()
                            for h2 in range(2):
                                P.mm(bkg, bkg[H0, h2 * 64:(h2 + 1) * 64], nmn4[:, 1, h2, :], Gc3[:, h2, :], reads=[nmn, Gc])
                            P.tt(Gn[H0, :], bkg[H0, 0:128], Gc[H0, :], ALU.add, reads=[bkg, Gc], writes=[Gn])
                            cur = 1 - cur
                        Gf = Gp[cur]
                        Gf3 = Gf.v(H0, "p (h t) -> p h t", h=2)
                        if RW_STOP == 4:
                            continue
                        tm = TM[c % 2]
                        bkt = P.bank()
                        P.tr(bkt, bkt[H0, 0:128], Bte[A, cs], ident, reads=[Bte, cst])
                        P.tr(bkt, bkt[H0, 128:256], Kte[A, cs], ident, reads=[Kte, cst])
                        P.tr(bkt, bkt[H0, 256:384], sv[A, cs], ident, reads=[sv, cst])
                        copy(tm[H0, 0:384], bkt[H0, 0:384], [bkt], [tm])
                        if RW_STOP == 5:
                            continue
                        ru = RU[c % 2]
                        sb_old = c % 2
                        sb_new = 1 - sb_old
                        bkr = P.bank()
                        for h2 in range(2):
                            ph = slice(h2 * 64, h2 * 64 + 64)
                            o = bkr[H0, h2 * 64:(h2 + 1) * 64]
                            P.mm(bkr, o, ARh[h2][H0, 0, cs], S5[:, hp, h2, sb_old, :], reads=[ARb[h2], Srw])
                            P.mm(bkr, o, na5[:, h2, 1, 0, :], tm[H0, 256 + h2 * 64:256 + (h2 + 1) * 64], reads=[na, tm])
                        copy(ru[H0, 0:128], bkr[H0, 0:128], [bkr], [ru], eng="act")
                        bku = P.bank()
                        for h2 in range(2):
                            P.mm(bku, bku[H0, h2 * 64:(h2 + 1) * 64], Gf3[:, h2, :], ru[H0, h2 * 64:(h2 + 1) * 64], reads=[Gf, ru])
                        copy(ru[H0, 128:256], bku[H0, 0:128], [bku], [ru], eng="dve")
                        bky = P.bank()
                        for h2 in range(2):
                            ph = slice(h2 * 64, h2 * 64 + 64)
                            o = bky[ph, 0:64]
                            P.mm(bky, o, S5[:, hp, h2, sb_old, :], ARh[h2][H0, 1, cs], reads=[ARb[h2], Srw])
                            P.mm(bky, o, ru[H0, 128 + h2 * 64:128 + (h2 + 1) * 64], na5[:, h2, 0, 1, :], reads=[ru, na])
                            P.mm(bky, o, tm[H0, 256 + h2 * 64:256 + (h2 + 1) * 64], na5[:, h2, 1, 1, :], reads=[tm, na])
                        copy(yr[A, cs], bky[A, 0:64], [bky], [yr], eng="act")
                        bks = P.bank()
                        for h2 in range(2):
                            o = bks[H0, h2 * 64:(h2 + 1) * 64]
                            P.mm(bks, o, tm[H0, h2 * 64:(h2 + 1) * 64], ru[H0, 128 + h2 * 64:128 + (h2 + 1) * 64], reads=[tm, ru])
                            P.mm(bks, o, tm[H0, 128 + h2 * 64:128 + (h2 + 1) * 64], tm[H0, 256 + h2 * 64:256 + (h2 + 1) * 64], reads=[tm])
                        for h2 in range(2):
                            pcs = Ep[H0, c * 64 + 63:c * 64 + 64] if h2 == 0 else pc1[H0, c:c + 1]
                            P.stt(S5[:, hp, h2, sb_new, :], S5[:, hp, h2, sb_old, :], pcs, bks[H0, h2 * 64:(h2 + 1) * 64],
                                  ALU.mult, ALU.add, reads=[Srw, Ep, pc1, bks], writes=[Srw])
                    t1 = Ep
                    bk = P.bank()
                    P.mm(bk, bk[A, :], C(C_BLKM, 128), yr[A, :], reads=[cst, yr])
                    P.tt(yr[A, :], yr[A, :], bk[A, :], ALU.subtract, reads=[yr, bk], writes=[yr])
                    P.tt(t1[A, :], yr[A, :], yr[A, :], ALU.mult, reads=[yr], writes=[t1])
                    bk = P.bank()
                    P.mm(bk, bk[A, :], C(C_BLKM, 128), t1[A, :], reads=[cst, t1])
                    P.ts(t1[A, :], bk[A, :], 64e-5, ALU.add, reads=[bk], writes=[t1])
                    P.recip(t1[A, :], t1[A, :], reads=[t1], writes=[t1])
                    P.act(t1[A, :], t1[A, :], AF.Sqrt, reads=[t1], writes=[t1])
                    P.tt(yr[A, :], yr[A, :], t1[A, :], ALU.mult, reads=[yr, t1], writes=[yr])
                    P.ts(yr[A, :], yr[A, :], pcc(PC_LNW), ALU.mult, pcc(PC_LNB), ALU.add, reads=[yr, pc], writes=[yr])
                    P.tt(yr[A, :], yr[A, :], bonus[A, :], ALU.add, reads=[yr, bonus], writes=[yr])
                    P.tt(RR(yT3[:, hp, :]), yr[A, :], g[A, :], ALU.mult, reads=[yr, g], writes=[yT.sub(hp * 512, 512)])

            P.ar_top = MIX_BASE
            if "r" in phases:
                rwkv()

            def mlstm():
                qk = [P.alloc() for _ in range(4)]
                vones = P.alloc()
                vo3 = vones.v(H0, "p (h c) -> p h c", h=4)
                P.memset(vones[H0, :], 1.0, writes=[vones])
                vT = [P.alloc(), P.alloc()]
                oT = [P.alloc(), P.alloc()]
                gi, gf, bcum, ge = (P.alloc() for _ in range(4))
                top = P.ar_top
                for i in range(4):
                    P.ar_top = top
                    uext = P.alloc(515)
                    acc = P.alloc()
                    proj_fm(1024 + i * 64, 64, uext[H0, 3:515], rows=uext)
                    P.cp(uext[H0, 0:3], cml[H0, i * 3:(i + 1) * 3], reads=[cml], writes=[uext], eng="dve")
                    wc = lambda j: pc[H0, PC_CW + i * 4 + j:PC_CW + i * 4 + j + 1]
                    P.ts(acc[H0, :], uext[H0, 0:512], wc(0), ALU.mult, reads=[uext, pc], writes=[acc])
                    for j in range(1, 4):
                        P.stt(acc[H0, :], uext[H0, j:j + 512], wc(j), acc[H0, :], ALU.mult, ALU.add, reads=[uext, pc, acc], writes=[acc])
                    P.cp(cml[H0, i * 3:(i + 1) * 3], uext[H0, 512:515], reads=[uext], writes=[cml], eng="dve")
                    P.act(qk[i][H0, :], acc[H0, :], AF.Silu, bias=pc[H0, PC_CB + i:PC_CB + i + 1], reads=[acc, pc], writes=[qk[i]])
                    if i < 2:
                        P.ts(qk[i][H0, :], qk[i][H0, :], 32.0 ** -0.5, ALU.mult, reads=[qk[i]], writes=[qk[i]])
                P.ar_top = top
                for hp in range(2):
                    proj_fm(1280 + hp * 128, 128, vT[hp][A, :], rows=vT[hp])
                    proj_fm(1536 + hp * 128, 128, oT[hp][A, :], rows=oT[hp])
                    P.act(oT[hp][A, :], oT[hp][A, :], AF.Sigmoid, reads=[oT[hp]], writes=[oT[hp]])
                proj_fm(1792, 4, gi[R4, :], rows=gi)
                proj_fm(1796, 4, gf[R4, :], rows=gf)
                P.act(gi[R4, :], gi[R4, :], AF.Tanh, bias=dv[R4, DV_BI15:DV_BI15 + 1], scale=1.0 / 15.0, reads=[gi, dv], writes=[gi])
                P.act(gf[R4, :], gf[R4, :], AF.Tanh, bias=dv[R4, DV_BF15:DV_BF15 + 1], scale=1.0 / 15.0, reads=[gf, dv], writes=[gf])
                P.ts(gi[R4, :], gi[R4, :], 15.0, ALU.mult, reads=[gi], writes=[gi])
                P.act(gf[R4, :], gf[R4, :], AF.Exp, scale=-15.0, reads=[gf], writes=[gf])
                P.act(gf[R4, :], gf[R4, :], AF.Ln, bias=1.0, reads=[gf], writes=[gf])
                ones4 = C(C_ALLONE, 64, R4)
                for c in range(8):
                    cs = slice(c * 64, (c + 1) * 64)
                    P.scan(bcum[R4, cs], ones4, gf[R4, cs], 0.0, ALU.mult, ALU.subtract, reads=[cst, gf], writes=[bcum])
                P.tt(gi[R4, :], gi[R4, :], bcum[R4, :], ALU.subtract, reads=[gi, bcum], writes=[gi])
                P.act(gi[R4, :], gi[R4, :], AF.Exp, reads=[gi], writes=[gi])
                P.act(bcum[R4, :], bcum[R4, :], AF.Exp, reads=[bcum], writes=[bcum])
                for c in range(8):
                    cs = slice(c * 64, (c + 1) * 64)
                    P.ts(ge[R4, cs], gi[R4, cs], bcum[R4, c * 64 + 63:c * 64 + 64], ALU.mult, reads=[gi, bcum], writes=[ge])
                kp = [P.alloc(), P.alloc()]
                kpe = [P.alloc(), P.alloc()]
                ebbc = [P.alloc(), P.alloc()]
                for j in range(2):
                    selk = C(C_SELK + j * 64, 64, R4)
                    bk = P.bank()
                    P.mm(bk, bk[H0, :], selk, gi[R4, :], reads=[cst, gi])
                    P.tt(kp[j][H0, :], qk[2 + j][H0, :], bk[H0, :], ALU.mult, reads=[qk[2 + j], bk], writes=[kp[j]])
                    bk = P.bank()
                    P.mm(bk, bk[H0, :], selk, ge[R4, :], reads=[cst, ge])
                    P.tt(kpe[j][H0, :], qk[2 + j][H0, :], bk[H0, :], ALU.mult, reads=[qk[2 + j], bk], writes=[kpe[j]])
                    bk = P.bank()
                    P.mm(bk, bk[H0, :], selk, bcum[R4, :], reads=[cst, bcum])
                    P.cp(ebks[H0, j * 8:(j + 1) * 8], bk.v(H0, "p (c t) -> p c t", c=8)[:, :, 63], reads=[bk], writes=[ebks])
                    bk = P.bank()
                    P.mm(bk, bk[A, :], C(C_SELV + j * 128, 128, R4), bcum[R4, :], reads=[cst, bcum])
                    copy(ebbc[j][A, :], bk[A, :], [bk], [ebbc[j]])
                NTs = P.alloc(1024)
                DNs = P.alloc(1024)
                NT3 = NTs.v(A, "p (j t) -> p j t", j=2)
                DN3 = DNs.v(A, "p (j t) -> p j t", j=2)
                sm = P.alloc()
                qmc = P.alloc(256)
                for c in range(8):
                    cs = slice(c * 64, (c + 1) * 64)
                    bkt = P.bank()
                    P.tr(bkt, bkt[H0, 0:128], vT[0][A, cs], ident, reads=[vT[0], cst])
                    P.tr(bkt, bkt[H0, 128:256], vT[1][A, cs], ident, reads=[vT[1], cst])
                    copy(vo3[:, :, 0:64], bkt.v(H0, "p (h c) -> p h c", h=8)[:, 0:4, :], [bkt], [vones])
                    bkt2 = P.bank()
                    P.tr(bkt2, bkt2[H0, 0:64], kpe[0][H0, cs], C(C_IDENT, 64, H0), reads=[kpe[0], cst])
                    P.tr(bkt2, bkt2[H0, 64:128], kpe[1][H0, cs], C(C_IDENT, 64, H0), reads=[kpe[1], cst])
                    copy(sm[H0, 256:384], bkt2[H0, 0:128], [bkt2], [sm])
                    for h in range(4):
                        P.ts(qmc[H0, h * 64:(h + 1) * 64], qk[h // 2][H0, cs], C(C_HM + h % 2, 1, H0), ALU.mult,
                             reads=[qk[h // 2], cst], writes=[qmc])
                    bka = P.bank()
                    for h in range(4):
                        j = h // 2
                        P.mm(bka, bka[H0, h * 64:(h + 1) * 64], kp[j][H0, cs], qmc[H0, h * 64:(h + 1) * 64], reads=[kp[j], qmc])
                    P.tt(sm[H0, 0:256], bka[H0, 0:256], C(C_MM, 256, H0), ALU.mult, reads=[bka, cst], writes=[sm])
                    bn = P.bank()
                    bd = P.bank()
                    for h in range(4):
                        j = h // 2
                        pq = slice((h % 2) * 32, (h % 2) * 32 + 32)
                        ph = slice((h % 2) * 64, (h % 2) * 64 + 64)
                        at = sm[H0, h * 64:(h + 1) * 64]
                        qm = qmc[H0, h * 64:(h + 1) * 64]
                        P.mm(bn, bn[ph, j * 64:(j + 1) * 64], vo3[:, h, 0:64], at, reads=[vones, sm])
                        P.mm(bn, bn[ph, j * 64:(j + 1) * 64], CN3[:, j, 0:64], qm, reads=[CN, qmc])
                        P.mm(bd, bd[ph, j * 64:(j + 1) * 64], vo3[:, h, 64:128], at, reads=[vones, sm])
                        P.mm(bd, bd[ph, j * 64:(j + 1) * 64], CN3[:, j, 64:128], qm, reads=[CN, qmc])
                    copy(NT3[:, :, cs], bn.v(A, "p (j t) -> p j t", j=8)[:, 0:2, :], [bn], [NTs], eng="act")
                    copy(DN3[:, :, cs], bd.v(A, "p (j t) -> p j t", j=8)[:, 0:2, :], [bd], [DNs], eng="act")
                    bs_ = P.bank()
                    for h in range(4):
                        j = h // 2
                        P.mm(bs_, bs_[H0, h * 128:(h + 1) * 128], sm[H0, 256 + j * 64:256 + (j + 1) * 64], vo3[:, h, :], reads=[sm, vones])
                    for j in range(2):
                        P.ts(CN3[:, j, :], CN3[:, j, :], ebks[H0, j * 8 + c:j * 8 + c + 1], ALU.mult, reads=[CN, ebks], writes=[CN])
                        for h2 in range(2):
                            h = 2 * j + h2
                            P.stt(CN3[:, j, :], bs_[H0, h * 128:(h + 1) * 128], C(C_HM + h2, 1, H0), CN3[:, j, :],
                                  ALU.mult, ALU.add, reads=[CN, bs_, cst], writes=[CN])
                for j in range(2):
                    d1 = DNs.sub(j * 512, 512)
                    n1 = NTs.sub(j * 512, 512)
                    P.tt(d1[A, :], d1[A, :], ebbc[j][A, :], ALU.mult, reads=[d1, ebbc[j]], writes=[d1])
                    P.stt(d1[A, :], d1[A, :], -1.0, d1[A, :], ALU.mult, ALU.max, reads=[d1], writes=[d1])
                    P.ts(d1[A, :], d1[A, :], 1.0, ALU.max, reads=[d1], writes=[d1])
                    P.recip(d1[A, :], d1[A, :], reads=[d1], writes=[d1])
                    P.tt(n1[A, :], n1[A, :], ebbc[j][A, :], ALU.mult, reads=[n1, ebbc[j]], writes=[n1])
                    P.tt(n1[A, :], n1[A, :], d1[A, :], ALU.mult, reads=[n1, d1], writes=[n1])
                    P.tt(d1[A, :], n1[A, :], n1[A, :], ALU.mult, reads=[n1], writes=[d1])
                    bk = P.bank()
                    P.mm(bk, bk[A, :], C(C_BLKM, 128), d1[A, :], reads=[cst, d1])
                    P.ts(d1[A, :], bk[A, :], 1e-6, ALU.add, reads=[bk], writes=[d1])
                    P.recip(d1[A, :], d1[A, :], reads=[d1], writes=[d1])
                    P.act(d1[A, :], d1[A, :], AF.Sqrt, reads=[d1], writes=[d1])
                    P.tt(n1[A, :], n1[A, :], d1[A, :], ALU.mult, reads=[n1, d1], writes=[n1])
                    P.stt(RR(yT3[:, 2 + j, :]), n1[A, :], pc[A, PC_MNORM + j:PC_MNORM + j + 1], oT[j][A, :], ALU.mult, ALU.mult,
                          reads=[n1, pc, oT[j]], writes=[yT.sub((2 + j) * 512, 512)])

            P.ar_top = MIX_BASE
            if "m" in phases:
                mlstm()

            def swa():
                swaB = P.alloc(1024)
                mtmp = P.alloc(1024)
                P.dma(swaB[A, :], swab_d, writes=[swaB])
                P.dma(mtmp[A, :], cst_d[:, C_SWAM:C_SWAM + 1024], writes=[mtmp])
                P.tt(swaB[A, :], swaB[A, :], mtmp[A, :], ALU.add, reads=[swaB, mtmp], writes=[swaB])
                P.ar_top -= 2
                swaB4 = swaB.v(A, "p (c h q) -> p c h q", c=2, h=4)
                swaK = P.alloc(1280)
                swaV = P.alloc(640)
                swaK3 = swaK.v(A, "p (j t) -> p j t", j=2)
                swaV3 = swaV.v(A, "p (n c) -> p n c", n=5)
                P.cp(swaK3[:, :, 0:128], swaKc3[:, :, :], reads=[swaKc], writes=[swaK], eng="dve")
                P.cp(swaV3[:, 0, :], swaVc[A, :], reads=[swaVc], writes=[swaV], eng="dve")
                qS = [P.alloc(), P.alloc()]
                for j in range(2):
                    proj_fm(1800 + j * 128, 128, qS[j][A, :], scale=0.125, rows=qS[j])
                    proj_fm(2056 + j * 64, 64, swaK3[:, j, 128:640], dup=True, rows=swaK)
                proj_tm(2184, 128, lambda tt_: swaV3[:, 1 + tt_, :], swaV)
                ETs = [P.alloc(1024), P.alloc(1024)]
                tmp = P.alloc()
                for i in range(4):
                    gi_ = blk * 4 + i
                    qc = slice(i * 128, (i + 1) * 128)
                    prev = slice(i * 128, (i + 1) * 128)
                    cur_ = slice((i + 1) * 128, (i + 2) * 128)
                    ET = ETs[i % 2]
                    bp = P.bank() if gi_ > 0 else None
                    bc = P.bank()
                    for hq in range(4):
                        j = hq // 2
                        ph = slice((hq % 2) * 64, (hq % 2) * 64 + 64)
                        o = slice(hq * 128, (hq + 1) * 128)
                        if gi_ > 0:
                            P.mm(bp, bp[A, o], swaK3[ph, j, prev], qS[j][ph, qc], reads=[swaK, qS[j]])
                            P.mm(bp, bp[A, o], ident, swaB4[:, 0, hq, :], reads=[cst, swaB])
                        P.mm(bc, bc[A, o], swaK3[ph, j, cur_], qS[j][ph, qc], reads=[swaK, qS[j]])
                        P.mm(bc, bc[A, o], ident, swaB4[:, 1, hq, :], reads=[cst, swaB])
                    if gi_ > 0:
                        P.act(ET[A, 0:512], bp[A, :], AF.Exp, reads=[bp], writes=[ET])
                    P.act(ET[A, 512:1024], bc[A, :], AF.Exp, reads=[bc], writes=[ET])
                    bo = P.bank()
                    bd = P.bank()
                    for hq in range(4):
                        j = hq // 2
                        ph = slice((hq % 2) * 64, (hq % 2) * 64 + 64)
                        o = slice(j * 128, (j + 1) * 128)
                        e = slice(hq * 128, (hq + 1) * 128)
                        if gi_ > 0:
                            P.mm(bo, bo[ph, o], swaV3[:, i, j * 64:(j + 1) * 64], ET[A, e.start:e.stop], reads=[swaV, ET])
                            P.mm(bd, bd[ph, o], C(C_ALLONE, 64), ET[A, e.start:e.stop], reads=[cst, ET])
                        P.mm(bo, bo[ph, o], swaV3[:, i + 1, j * 64:(j + 1) * 64], ET[A, 512 + e.start:512 + e.stop], reads=[swaV, ET])
                        P.mm(bd, bd[ph, o], C(C_ALLONE, 64), ET[A, 512 + e.start:512 + e.stop], reads=[cst, ET])
                    for j in range(2):
                        P.ts(tmp[A, j * 128:(j + 1) * 128], bd[A, j * 128:(j + 1) * 128], dv[A, DV_ESINK + j:DV_ESINK + j + 1], ALU.add,
                             reads=[bd, dv], writes=[tmp])
                    P.recip(tmp[A, 0:256], tmp[A, 0:256], reads=[tmp], writes=[tmp])
                    P.tt(RR(yT3[:, 4:6, qc]), bo.v(A, "p (j q) -> p j q", j=4)[:, 0:2, :], tmp.v(A, "p (j q) -> p j q", j=4)[:, 0:2, :],
                         ALU.mult, reads=[bo, tmp], writes=[yT.sub(4 * 512, 1024)])
                P.cp(swaKc3[:, :, :], swaK3[:, :, 512:640], reads=[swaK], writes=[swaKc], eng="dve")
                P.cp(swaVc[A, :], swaV3[:, 4, :], reads=[swaV], writes=[swaVc], eng="dve")

            P.ar_top = MIX_BASE
            if "s" in phases:
                swa()

            def fox():
                qtmp = [P.alloc(), P.alloc()]
                for j in range(2):
                    proj_fm(2312 + j * 128, 128, qtmp[j][A, :], scale=0.125, rows=qtmp[j])
                    proj_fm(2568 + j * 128, 128, RR(kTh3[:, j, bs]), rows=kTh)
                for half in range(2):
                    proj_tm(2824 + half * 128, 128, lambda tt_: RR(Vh3[:, blk * 4 + tt_, half * 128:(half + 1) * 128]), Vh)
                fr = P.alloc()
                crow = P.alloc()
                vtmp = P.alloc()
                chi = foxR.sub(2 * 512, 512)
                clo = foxR.sub(3 * 512, 512)
                qF = [foxR.sub(0, 512), foxR.sub(512, 512)]
                for R_ in (slice(0, 4), slice(64, 68)):
                    proj_fm(3080, 4, fr[R_, :], rows=fr, pbase=R_.start)
                for R_ in (slice(0, 4), slice(64, 68)):
                    P.act(fr[R_, :], fr[R_, :], AF.Exp, bias=dv[R_, DV_NFOXB:DV_NFOXB + 1], scale=-1.0, reads=[fr, dv], writes=[fr])
                    P.act(fr[R_, :], fr[R_, :], AF.Ln, bias=1.0, reads=[fr], writes=[fr])
                    for sg_ in range(4):
                        seg = slice(sg_ * 128, (sg_ + 1) * 128)
                        init = ccar[R_, 0:1] if sg_ == 0 else crow[R_, sg_ * 128 - 1:sg_ * 128]
                        P.scan(crow[R_, seg], C(C_ALLONE, 128, R_), fr[R_, seg], init, ALU.mult, ALU.subtract,
                               reads=[fr, ccar, cst, crow], writes=[crow])
                    P.cp(ccar[R_, 0:1], crow[R_, 511:512], reads=[crow], writes=[ccar], eng="dve")
                    P.ts(fr[R_, :], crow[R_, :], 4097.0, ALU.mult, reads=[crow], writes=[fr])
                    P.tt(vtmp[R_, :], fr[R_, :], crow[R_, :], ALU.subtract, reads=[fr, crow], writes=[vtmp])
                    P.tt(RR(chi[R_, :]), fr[R_, :], vtmp[R_, :], ALU.subtract, reads=[fr, vtmp], writes=[chi])
                    P.tt(RR(clo[R_, :]), crow[R_, :], chi[R_, :], ALU.subtract, reads=[crow, chi], writes=[clo])
                bk = P.bank()
                for tt_ in range(4):
                    P.mm(bk, bk[A, tt_ * 4:(tt_ + 1) * 4], crow[R4, tt_ * 128:(tt_ + 1) * 128], C(C_IDENT, 4, R4), reads=[crow, cst])
                P.ts(negc3[:, blk * 4:(blk + 1) * 4, :], bk.v(A, "p (n h) -> p n h", h=4)[:, 0:4, :], -1.0, ALU.mult,
                     reads=[bk], writes=[negc])
                for j in range(2):
                    P.cp(RR(qF[j][A, :]), qtmp[j][A, :], reads=[qtmp[j]], writes=[qF[j]], eng=("act", "dve")[j])
                ETs = [foxR.sub((4 + i_) * 512, 512) for i_ in range(3)]
                rec = P.alloc()
                ei = 0
                for j in range(2):
                    for h2 in range(2):
                        h = 2 * j + h2
                        ph = slice(h2 * 64, h2 * 64 + 64)
                        bo = P.bank(pin=True)
                        bd = P.bank(pin=True)
                        for kb in range(blk * 4 + 4):
                            q0 = max(0, kb - blk * 4) * 128
                            nq = 512 - q0
                            bs_ = P.bank()
                            P.mm(bs_, bs_[A, 0:nq], kTh3[ph, j, kb * 128:(kb + 1) * 128], qF[j][ph, q0:512], reads=[kTh, qF[j]], r=True)
                            Rh = slice(h2 * 64, h2 * 64 + 4)
                            P.mm(bs_, bs_[A, 0:nq], C(C_SELF + h * 128, 128, Rh), chi[Rh, q0:512], reads=[cst, chi], r=True)
                            P.mm(bs_, bs_[A, 0:nq], C(C_SELF + h * 128, 128, Rh), clo[Rh, q0:512], reads=[cst, clo], r=True)
                            et = ETs[ei % 3]
                            ei += 1
                            P.act(RR(et[A, 0:nq]), bs_[A, 0:nq], AF.Exp, bias=negc3[:, kb, h:h + 1], reads=[bs_, negc], writes=[et])
                            if kb >= blk * 4:
                                P.tt(RR(et[A, 0:128]), et[A, 0:128], C(C_TRI, 128), ALU.mult, reads=[et, cst], writes=[et], eng="dve")
                            P.mm(bo, bo[A, q0:512], Vh3[:, kb, j * 128:(j + 1) * 128], et[A, 0:nq], reads=[Vh, et], r=True)
                            P.mm(bd, bd[A, q0:512], C(C_ALLONE, 128), et[A, 0:nq], reads=[cst, et], r=True)
                        P.recip(rec[ph, :], bd[ph, :], reads=[bd], writes=[rec])
                        P.tt(RR(yT3[ph, 6 + j, :]), bo[ph, :], rec[ph, :], ALU.mult, reads=[bo, rec], writes=[yT.sub((6 + j) * 512, 512)])
                        bo.pinned = False
                        bd.pinned = False

            P.ar_top = MIX_BASE
            if "f" in phases:
                fox()

            if dbg == ("yT", l):
                for c in range(8):
                    P.dma(dbg_d[:, c, bs], yT3[:, c, :], reads=[yT], eng="pool")

            def post_residual(oT3, oT, gcol):
                rs = rms_rstd(lambda c: oT3[:, c, :], [oT])
                tmps = [P.alloc(), P.alloc()]
                for m in range(8):
                    t_ = tmps[m % 2]
                    P.stt(t_[A, :], oT3[:, m, :], pc[A, gcol + m:gcol + m + 1], rs[A, :], ALU.mult, ALU.mult,
                          reads=[oT, pc, rs], writes=[t_])
                    P.tt(xT3[:, m, bs], xT3[:, m, bs], t_[A, :], ALU.add, reads=[xT, t_], writes=[xT])

            if "o" not in phases:
                continue
            P.ar_top = 0
            oT = P.alloc(4096)
            oT3 = oT.v(A, "p (c t) -> p c t", c=8)
            for m in range(8):
                w = wchunk()
                w3 = w.v(A, "p (c m) -> p c m", c=8)
                P.dma(RR(w[A, :]), w_out_d[l, m], writes=[w], eng="pool")
                bk = P.bank()
                for j in range(8):
                    P.mm(bk, bk[A, :], w3[:, j, :], yT3[:, j, :], reads=[w, yT], r=True)
                copy(oT3[:, m, :], bk[A, :], [bk], [oT.sub(m * 512, 512)])
            post_residual(oT3, oT, PC_GPOST)

            if dbg == ("x1", l):
                for c in range(8):
                    P.dma(dbg_d[:, c, bs], xT3[:, c, bs], reads=[xT])

            if "n" not in phases:
                continue
            P.ar_top = 0
            rs3 = rms_rstd(lambda c: xT3[:, c, bs], [xT])
            fill_hTr(PC_FPRE)
            fT = yT
            fT3 = yT3
            oT = P.alloc(4096)
            oT3 = oT.v(A, "p (c t) -> p c t", c=8)
            gbuf = P.alloc(514)
            acc = P.alloc()
            gfp = pc[A, PC_FPRE:PC_FPRE + 8]
            for (c_lo, c_hi) in ((0, 8), (8, 16), (16, 22)):
                ng = c_hi - c_lo
                for cc in range(ng):
                    c = c_lo + cc
                    ws = []
                    for c2 in (c, NFC + c):
                        w = wchunk()
                        w3 = w.v(A, "p (c m) -> p c m", c=8)
                        P.dma(RR(w[A, :]), w_up_d[l, c2], writes=[w], eng="pool")
                        ws.append((w, w3))
                    bg = P.bank()
                    for k in range(8):
                        P.mm(bg, bg[A, :], ws[0][1][:, k, :], hTr3[:, k, :], reads=[ws[0][0], hTr], r=True)
                    bu = P.bank()
                    for k in range(8):
                        P.mm(bu, bu[A, :], ws[1][1][:, k, :], hTr3[:, k, :], reads=[ws[1][0], hTr], r=True)
                    P.tt(gbuf[A, 2:514], bg[A, :], rs3[A, :], ALU.mult, reads=[bg, rs3], writes=[gbuf])
                    P.cp(gbuf[A, 0:2], cffn[A, c * 2:c * 2 + 2], reads=[cffn], writes=[gbuf], eng="dve")
                    fw = lambda j: pc[A, PC_FCW + c * 3 + j:PC_FCW + c * 3 + j + 1]
                    P.ts(acc[A, :], gbuf[A, 0:512], fw(0), ALU.mult, reads=[gbuf, pc], writes=[acc])
                    P.stt(acc[A, :], gbuf[A, 1:513], fw(1), acc[A, :], ALU.mult, ALU.add, reads=[gbuf, pc, acc], writes=[acc])
                    P.stt(acc[A, :], gbuf[A, 2:514], fw(2), acc[A, :], ALU.mult, ALU.add, reads=[gbuf, pc, acc], writes=[acc])
                    P.cp(cffn[A, c * 2:c * 2 + 2], gbuf[A, 512:514], reads=[gbuf], writes=[cffn], eng="dve")
                    P.act(acc[A, :], acc[A, :], AF.Gelu_apprx_tanh, bias=pc[A, PC_FCB + c:PC_FCB + c + 1], reads=[acc, pc], writes=[acc])
                    P.tt(RR(fT3[:, cc, :]), acc[A, :], bu[A, :], ALU.mult, reads=[acc, bu], writes=[fT.sub(cc * 512, 512)])
                for m in range(8):
                    wa = wchunk()
                    wa3 = wa.v(A, "p (c m) -> p c m", c=8)
                    P.dma(RR(wa[A, 0:ng * 128]), w_dn_d[l, m, :, c_lo * 128:c_hi * 128], writes=[wa], eng="pool")
                    bk = P.bank()
                    for cc in range(ng):
                        P.mm(bk, bk[A, :], wa3[:, cc, :], fT3[:, cc, :], reads=[wa, fT], r=True)
                    om = oT.sub(m * 512, 512)
                    if c_lo == 0:
                        copy(oT3[:, m, :], bk[A, :], [bk], [om])
                    else:
                        P.tt(oT3[:, m, :], oT3[:, m, :], bk[A, :], ALU.add, reads=[om, bk], writes=[om])
            for m in range(8):
                om = oT.sub(m * 512, 512)
                P.tt(oT3[:, m, :], oT3[:, m, :], rs3[A, :], ALU.mult, reads=[om, rs3], writes=[om])
            post_residual(oT3, oT, PC_FPOST)

    outops = []
    for c in range(8):
        outops.append(P.dma(out_d[:, c, 0:T], xT3[:, c, :], reads=[xT]))
    P.emit(outops + [op for op in P.dma_ops if False])
    P.close()
    return nc


_NC_CACHE = {}


def kernel(**inputs):
    maps = prep_inputs(inputs)
    if "nc" not in _NC_CACHE:
        _NC_CACHE["nc"] = build()
    nc = _NC_CACHE["nc"]
    res = run_bass_kernel_spmd(nc, maps, core_ids=list(range(len(maps))))
    x = np.asarray(inputs["x"])
    out = np.empty(x.shape, np.float32)
    for b in range(x.shape[0]):
        oT = np.asarray(res.results[b]["outT"])
        out[b] = oT.transpose(1, 0, 2).reshape(D, SEQ).T
    return out
```
